# Optimizing a Trainium2 kernel written in Bass

```python
import math
import jax, jax.numpy as jnp
from jax import lax
import numpy as np

D_MODEL = 1024
BATCH = 2
SEQ = 8192
DEPTH = 4

DEEPNORM_ALPHA = (2 * DEPTH) ** 0.25
DEEPNORM_BETA = (8 * DEPTH) ** -0.25
LN_EPS = 1e-5
N_BRANCHES = 3

SSD_INNER = 2 * D_MODEL
SSD_HEAD_DIM = 64
SSD_HEADS = SSD_INNER // SSD_HEAD_DIM
SSD_GROUPS = 8
SSD_HPG = SSD_HEADS // SSD_GROUPS
SSD_STATE = 128
SSD_CONV = 5
SSD_CHUNK = 128
SSD_CONV_CH = SSD_INNER + 2 * SSD_GROUPS * SSD_STATE

ATT_HEAD_DIM = 64
ATT_PATTERNS = ((128, 1), (512, 4), (2048, 16))
ATT_GROUPS = len(ATT_PATTERNS)
ATT_HPG = 6
ATT_HEADS = ATT_GROUPS * ATT_HPG
ATT_WIDTH = ATT_HEADS * ATT_HEAD_DIM
ATT_OUT = ATT_HPG * ATT_HEAD_DIM
ATT_NEG = -1e30

S5_CH = 16
S5_STATE = 64
S5_WIDTH = 9 * D_MODEL // 8
S5_GROUPS = S5_WIDTH // S5_CH

IN_SIZES = (N_BRANCHES * D_MODEL, SSD_INNER, SSD_CONV_CH, 2 * SSD_HEADS, 3 * ATT_WIDTH, S5_WIDTH)
N_IN = sum(IN_SIZES)

MOE_EXPERTS = 32
MOE_TOP_K = 4
MOE_FF = D_MODEL
SWIGLU_LIMIT = 7.0
SWIGLU_ALPHA = 1.702
MOE_BLOCK = 128

kernel_name = "hybrid_ssd_dilattn_s5_moe_deepnorm"


def layer_norm(x, g, b):
    xf = x.astype(jnp.float32)
    mu = jnp.mean(xf, axis=-1, keepdims=True)
    var = jnp.mean(jnp.square(xf - mu), axis=-1, keepdims=True)
    return ((xf - mu) * lax.rsqrt(var + LN_EPS)).astype(x.dtype) * g + b


def rms_norm(x, w):
    xf = x.astype(jnp.float32)
    return (xf * lax.rsqrt(jnp.mean(xf * xf, axis=-1, keepdims=True) + LN_EPS)).astype(x.dtype) * w


def split_columns(proj):
    idx, acc = [], 0
    for s in IN_SIZES[:-1]:
        acc += s
        idx.append(acc)
    return jnp.split(proj, idx, axis=-1)


def centred_dwconv(x, w, b):
    k_taps = w.shape[0]
    half = k_taps // 2
    seqlen = x.shape[1]
    xp = jnp.pad(x, ((0, 0), (half, half), (0, 0)))
    out = b + w[0] * xp[:, 0:seqlen]
    for k in range(1, k_taps):
        out = out + w[k] * xp[:, k:k + seqlen]
    return out


def ssd_chunked(xin, a_dt, bm, cm):
    bsz, seqlen, ng, nr, hd = xin.shape
    nc = seqlen // SSD_CHUNK
    xc = xin.reshape(bsz, nc, SSD_CHUNK, ng, nr, hd)
    bc = bm.reshape(bsz, nc, SSD_CHUNK, ng, SSD_STATE)
    cc = cm.reshape(bsz, nc, SSD_CHUNK, ng, SSD_STATE)
    acs = jnp.cumsum(a_dt.astype(jnp.float32).reshape(bsz, nc, SSD_CHUNK, ng, nr).transpose(0, 3, 4, 1, 2), axis=-1)
    tri = jnp.tril(jnp.ones((SSD_CHUNK, SSD_CHUNK), bool))
    seg = acs[..., :, None] - acs[..., None, :]
    decay_in = jnp.where(tri, jnp.exp(jnp.where(tri, seg, 0.0)), 0.0)
    cb = jnp.einsum('bclgn,bcsgn->bgcls', cc, bc)
    y_diag = jnp.einsum('bgcls,bgrcls,bcsgrp->bclgrp', cb, decay_in, xc)
    decay_to_end = jnp.exp(acs[..., -1:] - acs)
    states = jnp.einsum('bclgn,bgrcl,bclgrp->bcgrpn', bc, decay_to_end, xc)
    totals = jnp.pad(acs[..., -1], ((0, 0), (0, 0), (0, 0), (1, 0)))
    tcs = jnp.cumsum(totals, axis=-1)
    tri_c = jnp.tril(jnp.ones((nc + 1, nc + 1), bool))
    seg_c = tcs[..., :, None] - tcs[..., None, :]
    decay_chunks = jnp.where(tri_c, jnp.exp(jnp.where(tri_c, seg_c, 0.0)), 0.0)
    states = jnp.pad(states, ((0, 0), (1, 0), (0, 0), (0, 0), (0, 0), (0, 0)))
    carried = jnp.einsum('bgrzc,bcgrpn->bzgrpn', decay_chunks, states)[:, :-1]
    y_off = jnp.einsum('bclgn,bcgrpn,bgrcl->bclgrp', cc, carried, jnp.exp(acs))
    return (y_diag + y_off).reshape(bsz, seqlen, ng, nr, hd)


def ssd_branch(z, xbc, dt, conv_w, conv_b, a_log, dt_bias, d_skip, norm_w):
    bsz, seqlen, _ = z.shape
    xbc = jax.nn.silu(centred_dwconv(xbc, conv_w, conv_b))
    xs, bm, cm = jnp.split(xbc, [SSD_INNER, SSD_INNER + SSD_GROUPS * SSD_STATE], axis=-1)
    xs = xs.reshape(bsz, seqlen, SSD_GROUPS, SSD_HPG, SSD_HEAD_DIM)
    bm = bm.reshape(bsz, seqlen, SSD_GROUPS, SSD_STATE)
    cm = cm.reshape(bsz, seqlen, SSD_GROUPS, SSD_STATE)
    dt = dt.astype(jnp.float32).reshape(bsz, seqlen, 2, SSD_GROUPS, SSD_HPG)
    y = d_skip.reshape(SSD_GROUPS, SSD_HPG)[:, :, None] * xs
    for direction in range(2):
        delta = jax.nn.softplus(dt[:, :, direction] + dt_bias[direction].astype(jnp.float32).reshape(SSD_GROUPS, SSD_HPG))
        a_dt = -jnp.exp(a_log[direction].astype(jnp.float32).reshape(SSD_GROUPS, SSD_HPG)) * delta
        xin = xs * delta[..., None]
        if direction == 1:
            yd = jnp.flip(ssd_chunked(jnp.flip(xin, 1), jnp.flip(a_dt, 1), jnp.flip(bm, 1), jnp.flip(cm, 1)), 1)
        else:
            yd = ssd_chunked(xin, a_dt, bm, cm)
        y = y + yd
    y = y.reshape(bsz, seqlen, SSD_INNER).astype(z.dtype) * jax.nn.silu(z)
    return rms_norm(y, norm_w)


def dilated_window_attention(q, k, v, dil, radius, slopes):
    bsz, seqlen, nh, hd = q.shape
    ls = seqlen // dil
    blk = radius
    nb = -(-ls // blk)
    lp = nb * blk

    def by_class(a):
        return a.reshape(bsz, ls, dil, nh, hd).transpose(0, 2, 3, 1, 4)

    qc = jnp.pad(by_class(q), ((0, 0), (0, 0), (0, 0), (0, lp - ls), (0, 0)))
    kc = jnp.pad(by_class(k), ((0, 0), (0, 0), (0, 0), (blk, lp - ls + blk), (0, 0)))
    vc = jnp.pad(by_class(v), ((0, 0), (0, 0), (0, 0), (blk, lp - ls + blk), (0, 0)))
    qb = qc.reshape(bsz, dil, nh, nb, blk, hd)

    def windows(a):
        return jnp.concatenate([a[:, :, :, o * blk:o * blk + lp].reshape(bsz, dil, nh, nb, blk, hd) for o in range(3)], axis=4)

    kb, vb = windows(kc), windows(vc)
    qpos = jnp.arange(lp).reshape(nb, blk)
    kpos = (jnp.arange(nb)[:, None] - 1) * blk + jnp.arange(3 * blk)[None, :]
    dist = jnp.abs(qpos[:, :, None] - kpos[:, None, :])
    valid = (dist <= radius) & (kpos[:, None, :] >= 0) & (kpos[:, None, :] < ls)
    alibi = -slopes.astype(jnp.float32)[:, None, None, None] * (dil * dist).astype(jnp.float32)
    s = jnp.einsum('bdhnqe,bdhnke->bdhnqk', qb, kb, preferred_element_type=jnp.float32) * (ATT_HEAD_DIM ** -0.5) + alibi[None, None]
    s = jnp.where(valid, s, ATT_NEG)
    m = jnp.max(s, axis=-1, keepdims=True)
    p = jnp.exp(s - m)
    zsum = jnp.sum(p, axis=-1, keepdims=True)
    o = jnp.einsum('bdhnqk,bdhnke->bdhnqe', p, vb.astype(jnp.float32)) / zsum
    lse = (m + jnp.log(zsum))[..., 0]
    o = o.reshape(bsz, dil, nh, lp, hd)[:, :, :, :ls].transpose(0, 3, 1, 2, 4).reshape(bsz, seqlen, nh, hd)
    lse = lse.reshape(bsz, dil, nh, lp)[..., :ls].transpose(0, 3, 1, 2).reshape(bsz, seqlen, nh)
    return o, lse


def attention_branch(qkv):
    bsz, seqlen, _ = qkv.shape
    q, k, v = [a.reshape(bsz, seqlen, ATT_GROUPS, ATT_HPG, ATT_HEAD_DIM) for a in jnp.split(qkv, 3, axis=-1)]
    slopes = (2.0 ** (-8.0 * jnp.arange(1, ATT_HEADS + 1, dtype=jnp.float32) / ATT_HEADS)).reshape(ATT_HPG, ATT_GROUPS)
    outs, lses = [], []
    for g, (window, dil) in enumerate(ATT_PATTERNS):
        o, lse = dilated_window_attention(q[:, :, g], k[:, :, g], v[:, :, g], dil, (window // 2) // dil, slopes[:, g])
        outs.append(o)
        lses.append(lse)
    wts = jax.nn.softmax(jnp.stack(lses, axis=0), axis=0)
    out = wts[0][..., None] * outs[0]
    for g in range(1, ATT_GROUPS):
        out = out + wts[g][..., None] * outs[g]
    return out.reshape(bsz, seqlen, ATT_OUT).astype(qkv.dtype)


def complex_affine_combine(left, right):
    ar_l, ai_l, br_l, bi_l = left
    ar_r, ai_r, br_r, bi_r = right
    return (ar_r * ar_l - ai_r * ai_l,
            ar_r * ai_l + ai_r * ar_l,
            ar_r * br_l - ai_r * bi_l + br_r,
            ar_r * bi_l + ai_r * br_l + bi_r)


def s5_branch(u, a_re, a_im, log_step, b_re, b_im, c_re, c_im, d_skip, glu_w1, glu_w2):
    bsz, seqlen, _ = u.shape
    f32 = jnp.float32
    uf = u.astype(f32)
    ug = uf.reshape(bsz, seqlen, S5_GROUPS, S5_CH)
    y = (uf * d_skip.astype(f32)).reshape(bsz, seqlen, S5_GROUPS, S5_CH)
    br, bi = b_re.astype(f32), b_im.astype(f32)
    for direction in range(2):
        ar, ai = a_re[direction].astype(f32), a_im[direction].astype(f32)
        step = jnp.exp(log_step[direction].astype(f32))[:, None]
        mag = jnp.exp(step * ar)
        abr, abi = mag * jnp.cos(step * ai), mag * jnp.sin(step * ai)
        den = ar * ar + ai * ai
        fr = ((abr - 1.0) * ar + abi * ai) / den
        fi = (abi * ar - (abr - 1.0) * ai) / den
        bbr = fr[..., None] * br - fi[..., None] * bi
        bbi = fr[..., None] * bi + fi[..., None] * br
        u_dir = ug if direction == 0 else jnp.flip(ug, axis=1)
        bu_r = jnp.einsum('blgc,gnc->lbgn', u_dir, bbr)
        bu_i = jnp.einsum('blgc,gnc->lbgn', u_dir, bbi)
        a_r = jnp.broadcast_to(abr[None, None], (seqlen, 1, S5_GROUPS, S5_STATE))
        a_i = jnp.broadcast_to(abi[None, None], (seqlen, 1, S5_GROUPS, S5_STATE))
        _, _, st_r, st_i = lax.associative_scan(complex_affine_combine, (a_r, a_i, bu_r, bu_i), axis=0)
        if direction == 1:
            st_r, st_i = jnp.flip(st_r, axis=0), jnp.flip(st_i, axis=0)
        y = y + jnp.einsum('lbgn,gcn->blgc', st_r, c_re[direction].astype(f32)) \
              - jnp.einsum('lbgn,gcn->blgc', st_i, c_im[direction].astype(f32))
    h = jax.nn.gelu(y.reshape(bsz, seqlen, S5_WIDTH)).astype(u.dtype)
    return (h @ glu_w1) * jax.nn.sigmoid(h @ glu_w2)


def hybrid_mixer(x, w_in, b_gate, ssd_conv_w, ssd_conv_b, ssd_a_log, ssd_dt_bias, ssd_d, ssd_norm_w,
                 s5_a_re, s5_a_im, s5_log_step, s5_b_re, s5_b_im, s5_c_re, s5_c_im, s5_d, s5_glu_w1, s5_glu_w2,
                 w_br_ssd, w_br_attn, w_br_s5, w_out):
    bsz, seqlen, _ = x.shape
    proj = x @ w_in
    gates, z, xbc, dt, qkv, u = split_columns(proj)
    y_ssd = ssd_branch(z, xbc, dt, ssd_conv_w, ssd_conv_b, ssd_a_log, ssd_dt_bias, ssd_d, ssd_norm_w)
    y_att = attention_branch(qkv)
    y_s5 = s5_branch(u, s5_a_re, s5_a_im, s5_log_step, s5_b_re, s5_b_im, s5_c_re, s5_c_im, s5_d, s5_glu_w1, s5_glu_w2)
    g = jax.nn.sigmoid(gates.reshape(bsz, seqlen, N_BRANCHES, D_MODEL) + b_gate)
    merged = g[:, :, 0] * (y_ssd @ w_br_ssd) + g[:, :, 1] * (y_att @ w_br_attn) + g[:, :, 2] * (y_s5 @ w_br_s5)
    return merged @ w_out


def moe_ffn(x, router_w, router_b, w_gu, b_gu, w_dn, b_dn):
    bsz, seqlen, d = x.shape
    ntok = bsz * seqlen
    h = x.reshape(ntok, d)
    logits = (h @ router_w + router_b).astype(jnp.float32)
    top_val, top_idx = lax.top_k(logits, MOE_TOP_K)
    gate = jax.nn.softmax(top_val, axis=-1)
    n_assign = ntok * MOE_TOP_K
    flat_e = top_idx.reshape(-1)
    flat_tok = jnp.repeat(jnp.arange(ntok, dtype=jnp.int32), MOE_TOP_K)
    order = jnp.argsort(flat_e)
    e_sorted = flat_e[order]
    tok_sorted = flat_tok[order]
    gate_sorted = gate.reshape(-1)[order]
    counts = jnp.bincount(flat_e, length=MOE_EXPERTS)
    padded = (counts + MOE_BLOCK - 1) // MOE_BLOCK * MOE_BLOCK
    start = jnp.cumsum(counts) - counts
    pend = jnp.cumsum(padded)
    pstart = pend - padded
    dest = pstart[e_sorted] + jnp.arange(n_assign, dtype=jnp.int32) - start[e_sorted]
    n_blocks = n_assign // MOE_BLOCK + MOE_EXPERTS
    slot_tok = jnp.full((n_blocks * MOE_BLOCK,), ntok, jnp.int32).at[dest].set(tok_sorted)
    h_pad = jnp.concatenate([h, jnp.zeros((1, d), h.dtype)], axis=0)
    xs = h_pad[slot_tok].reshape(n_blocks, MOE_BLOCK, d)
    block_expert = jnp.minimum(jnp.searchsorted(pend, jnp.arange(n_blocks, dtype=pend.dtype) * MOE_BLOCK, side='right'), MOE_EXPERTS - 1)

    def expert_block(args):
        xb, e = args
        hu = xb @ w_gu[e] + b_gu[e]
        x_glu, x_lin = hu[:, :MOE_FF], hu[:, MOE_FF:]
        x_glu = jnp.minimum(x_glu, SWIGLU_LIMIT)
        x_lin = jnp.clip(x_lin, -SWIGLU_LIMIT, SWIGLU_LIMIT)
        act = x_glu * jax.nn.sigmoid(SWIGLU_ALPHA * x_glu) * (x_lin + 1.0)
        return act @ w_dn[e] + b_dn[e]

    ys = lax.map(expert_block, (xs, block_expert)).reshape(-1, d)
    y = ys[dest] * gate_sorted[:, None].astype(ys.dtype)
    return jax.ops.segment_sum(y, tok_sorted, num_segments=ntok).reshape(bsz, seqlen, d)


def setup_inputs(seed: int = 0) -> dict:
    key = jax.random.key(seed)
    ks = iter(jax.random.split(key, 40))
    L = DEPTH

    def nrm(shape, std):
        return jax.random.normal(next(ks), shape, jnp.float32) * std

    def unif(shape, lo, hi):
        return jax.random.uniform(next(ks), shape, jnp.float32, lo, hi)

    dt0 = jnp.exp(unif((L, 2, SSD_HEADS), math.log(1e-3), math.log(1e-1)))
    n_idx = jnp.arange(S5_STATE, dtype=jnp.float32)
    return {
        "x": nrm((BATCH, SEQ, D_MODEL), 1.0),
        "w_in": nrm((L, D_MODEL, N_IN), D_MODEL ** -0.5),
        "b_gate": nrm((L, N_BRANCHES, D_MODEL), 0.1),
        "ssd_conv_w": nrm((L, SSD_CONV, SSD_CONV_CH), SSD_CONV ** -0.5),
        "ssd_conv_b": nrm((L, SSD_CONV_CH), 0.02),
        "ssd_a_log": jnp.log(unif((L, 2, SSD_HEADS), 1.0, 16.0)),
        "ssd_dt_bias": dt0 + jnp.log(-jnp.expm1(-dt0)),
        "ssd_d": 1.0 + nrm((L, SSD_HEADS), 0.1),
        "ssd_norm_w": 1.0 + nrm((L, SSD_INNER), 0.02),
        "s5_a_re": -0.5 + nrm((L, 2, S5_GROUPS, S5_STATE), 0.02),
        "s5_a_im": math.pi * n_idx + nrm((L, 2, S5_GROUPS, S5_STATE), 0.02),
        "s5_log_step": unif((L, 2, S5_GROUPS), math.log(1e-3), math.log(1e-1)),
        "s5_b_re": nrm((L, S5_GROUPS, S5_STATE, S5_CH), (2 * S5_CH) ** -0.5),
        "s5_b_im": nrm((L, S5_GROUPS, S5_STATE, S5_CH), (2 * S5_CH) ** -0.5),
        "s5_c_re": nrm((L, 2, S5_GROUPS, S5_CH, S5_STATE), (2 * S5_STATE) ** -0.5),
        "s5_c_im": nrm((L, 2, S5_GROUPS, S5_CH, S5_STATE), (2 * S5_STATE) ** -0.5),
        "s5_d": nrm((L, S5_WIDTH), 1.0),
        "s5_glu_w1": nrm((L, S5_WIDTH, S5_WIDTH), S5_WIDTH ** -0.5),
        "s5_glu_w2": nrm((L, S5_WIDTH, S5_WIDTH), S5_WIDTH ** -0.5),
        "w_br_ssd": nrm((L, SSD_INNER, D_MODEL), SSD_INNER ** -0.5),
        "w_br_attn": nrm((L, ATT_OUT, D_MODEL), ATT_OUT ** -0.5),
        "w_br_s5": nrm((L, S5_WIDTH, D_MODEL), S5_WIDTH ** -0.5),
        "w_out": nrm((L, D_MODEL, D_MODEL), DEEPNORM_BETA * D_MODEL ** -0.5),
        "ln1_g": 1.0 + nrm((L, D_MODEL), 0.02),
        "ln1_b": nrm((L, D_MODEL), 0.02),
        "router_w": nrm((L, D_MODEL, MOE_EXPERTS), D_MODEL ** -0.5),
        "router_b": nrm((L, MOE_EXPERTS), 0.01),
        "exp_w_gate_up": nrm((L, MOE_EXPERTS, D_MODEL, 2 * MOE_FF), D_MODEL ** -0.5),
        "exp_b_gate_up": nrm((L, MOE_EXPERTS, 2 * MOE_FF), 0.02),
        "exp_w_down": nrm((L, MOE_EXPERTS, MOE_FF, D_MODEL), DEEPNORM_BETA * MOE_FF ** -0.5),
        "exp_b_down": nrm((L, MOE_EXPERTS, D_MODEL), 0.02),
        "ln2_g": 1.0 + nrm((L, D_MODEL), 0.02),
        "ln2_b": nrm((L, D_MODEL), 0.02),
    }


def reference(x, w_in, b_gate, ssd_conv_w, ssd_conv_b, ssd_a_log, ssd_dt_bias, ssd_d, ssd_norm_w,
              s5_a_re, s5_a_im, s5_log_step, s5_b_re, s5_b_im, s5_c_re, s5_c_im, s5_d, s5_glu_w1, s5_glu_w2,
              w_br_ssd, w_br_attn, w_br_s5, w_out, ln1_g, ln1_b,
              router_w, router_b, exp_w_gate_up, exp_b_gate_up, exp_w_down, exp_b_down, ln2_g, ln2_b):
    for layer in range(DEPTH):
        mix = hybrid_mixer(x, w_in[layer], b_gate[layer], ssd_conv_w[layer], ssd_conv_b[layer], ssd_a_log[layer],
                           ssd_dt_bias[layer], ssd_d[layer], ssd_norm_w[layer],
                           s5_a_re[layer], s5_a_im[layer], s5_log_step[layer], s5_b_re[layer], s5_b_im[layer],
                           s5_c_re[layer], s5_c_im[layer], s5_d[layer], s5_glu_w1[layer], s5_glu_w2[layer],
                           w_br_ssd[layer], w_br_attn[layer], w_br_s5[layer], w_out[layer])
        x = layer_norm(DEEPNORM_ALPHA * x + mix, ln1_g[layer], ln1_b[layer])
        ffn = moe_ffn(x, router_w[layer], router_b[layer], exp_w_gate_up[layer], exp_b_gate_up[layer],
                      exp_w_down[layer], exp_b_down[layer])
        x = layer_norm(DEEPNORM_ALPHA * x + ffn, ln2_g[layer], ln2_b[layer])
    return x
```

```python
import math
from contextlib import ExitStack
import numpy as np
import concourse.bass as bass
import concourse.mybir as mybir
from concourse.bass_utils import run_bass_kernel_spmd

F32 = mybir.dt.float32
BF16 = mybir.dt.bfloat16
I32 = mybir.dt.int32
AF = mybir.ActivationFunctionType
ALU = mybir.AluOpType
AX = mybir.AxisListType


class Cfg:
    def __init__(self, L=8192, depth=4, n_exp=32):
        self.L = L
        self.depth = depth
        self.E = n_exp
        self.D = 1024
        self.alpha = (2 * 4) ** 0.25


class Buf:
    __slots__ = ("name", "writers", "readers", "dsem", "multi")

    def __init__(self, name, multi=False):
        self.name = name
        self.writers = {}
        self.readers = {}
        self.dsem = None
        self.multi = multi


class Sched:
    def __init__(self, nc):
        self.nc = nc
        self.eng = {"pe": nc.tensor, "dve": nc.vector, "act": nc.scalar,
                    "pool": nc.gpsimd, "sp": nc.sync}
        self.sems = []
        self.esem = {}
        self.cnt = {}
        self.latest = {}
        for k in self.eng:
            self.esem[k] = self._newsem("e_" + k)
            self.cnt[k] = 0
        self.known = {k: {} for k in self.eng}
        self.n_inst = 0
        self.n_wait = 0
        self.dsem_pool = {}
        self.free_dsems = []
        self.epoch_owners = []

    def _newsem(self, name):
        h = self.nc.alloc_semaphore(name=name)
        self.sems.append(h)
        return len(self.sems) - 1

    def _wait(self, e, deps):
        kn = self.known[e]
        for s, c in deps.items():
            if s == self.esem[e] and c > self.cnt[e]:
                continue
            if kn.get(s, 0) < c:
                self.eng[e].wait_ge(self.sems[s], c)
                kn[s] = c
                self.n_wait += 1

    @staticmethod
    def _merge(d, src):
        for s, c in src.items():
            if d.get(s, 0) < c:
                d[s] = c

    def _deps(self, reads, writes):
        deps = {}
        for b in reads:
            self._merge(deps, b.writers)
        for b in writes:
            if b.multi:
                continue
            self._merge(deps, b.writers)
            self._merge(deps, b.readers)
        return deps

    def _track(self, s, c, reads, writes):
        for b in writes:
            if b.multi:
                if b.writers.get(s, 0) < c:
                    b.writers[s] = c
                continue
            b.writers = {s: c}
            b.readers = {}
        for b in reads:
            if b.readers.get(s, 0) < c:
                b.readers[s] = c

    def op(self, e, fn, reads=(), writes=(), inc=True):
        self._wait(e, self._deps(reads, writes))
        ins = fn(self.eng[e])
        self.n_inst += 1
        s = self.esem[e]
        if inc:
            self.cnt[e] += 1
            ins.then_inc(self.sems[s], 1)
            c = self.cnt[e]
            self.latest[s] = c
        else:
            c = self.cnt[e] + 1
        self._track(s, c, reads, writes)
        return ins

    def dma(self, q, out, in_, reads=(), writes=(), owner=None, **kw):
        self._wait(q, self._deps(reads, writes))
        if owner.dsem is None:
            if owner.name not in self.dsem_pool:
                if self.free_dsems:
                    self.dsem_pool[owner.name] = self.free_dsems.pop()
                else:
                    self.dsem_pool[owner.name] = [self._newsem("d%d" % len(self.sems)), 0]
            owner.dsem = self.dsem_pool[owner.name]
            self.epoch_owners.append(owner)
        ins = self.eng[q].dma_start(out=out, in_=in_, **kw)
        owner.dsem[1] += 16
        s, c = owner.dsem[0], owner.dsem[1]
        ins.then_inc(self.sems[s], 16)
        self.latest[s] = c
        self.n_inst += 1
        self._track(s, c, reads, writes)
        return ins

    def barrier(self):
        for e in self.eng:
            self._wait(e, dict(self.latest))
        keep = {k: v for k, v in self.dsem_pool.items() if k in ("inj", "dbg")}
        self.free_dsems.extend(v for k, v in self.dsem_pool.items() if k not in keep)
        self.dsem_pool = keep
        for b in self.epoch_owners:
            b.dsem = None
        self.epoch_owners = []


def dram_copy(S, dst, src, b):
    n = dst.shape[0]
    step = 128 if n >= 128 else n
    for r in range(0, n, step):
        S.dma("sp", dst[r:r + step, :], src[r:r + step, :], writes=[b], owner=b)


_UNIQ = [0]


def uniq(name):
    _UNIQ[0] += 1
    return "%s_%d" % (name, _UNIQ[0])


def bufs(prefix, n):
    return [Buf(f"{prefix}{i}") for i in range(n)]


D = 1024
SSD_INNER = 2048
SSD_HEADS = 32
SSD_GROUPS = 8
SSD_STATE = 128
CONV_CH = 4096
ATT_W = 1152
S5_W = 1152
S5_G = 72
N_IN = 13888
C_GATE, C_Z, C_XBC, C_DT, C_Q, C_K, C_V, C_U = 0, 3072, 5120, 9216, 9280, 10432, 11584, 12736
LN_EPS = 1e-5


class Ctx:
    pass


def make_consts(S, nc, cx):
    cx.identf = nc.alloc_sbuf_tensor("identf", [128, 128], F32)
    cx.identb = nc.alloc_sbuf_tensor("identb", [128, 128], BF16)
    cx.b_const = Buf("const")
    b = cx.b_const
    S.op("pool", lambda e: e.memset(cx.identf[:], 0.0), writes=[b])
    S.op("pool", lambda e: e.affine_select(out=cx.identf[:], in_=cx.identf[:], pattern=[[-1, 128]], base=0,
                                           channel_multiplier=1, compare_op=ALU.not_equal, fill=1.0),
         reads=[b], writes=[b])
    S.op("dve", lambda e: e.tensor_copy(out=cx.identb[:], in_=cx.identf[:]), reads=[b], writes=[b])


def load_xT(S, nc, cx, x_ap, t0, ntok, xT, b_xT, xs, b_xs, xb, b_xb, pt, b_pt, ctr):
    for t in range(ntok // 128):
        i = ctr[0] % 2
        ctr[0] += 1
        S.dma("sp", xs[i][:], x_ap[t0 + t * 128:t0 + (t + 1) * 128, :], writes=[b_xs[i]], owner=b_xs[i])
        S.op("dve", lambda e: e.tensor_copy(out=xb[i][:], in_=xs[i][:]), reads=[b_xs[i]], writes=[b_xb[i]])
        for k4 in range(2):
            j = ctr[1] % 2
            ctr[1] += 1
            for kk in range(4):
                k = k4 * 4 + kk
                S.op("pe", lambda e: e.transpose(out=pt[j][:, kk, :], in_=xb[i][:, k * 128:(k + 1) * 128],
                                                 identity=cx.identb[:]),
                     reads=[b_xb[i], cx.b_const], writes=[b_pt[j]], inc=(kk == 3))
            S.op("act", lambda e: e.copy(out=xT[:, k4 * 4:(k4 + 1) * 4, t * 128:(t + 1) * 128], in_=pt[j][:]),
                 reads=[b_pt[j]], writes=[b_xT])


def phase_inproj(S, nc, cx, cfg, w_in_l, x_ap, scr):
    L = cfg.L
    TB = min(2048, L)
    secs = [
        (C_GATE, 3072, "tok", scr.gates, F32, 512),
        (C_Z, 2048, "tok", scr.z, F32, 512),
        (C_XBC, 4096, "feat", scr.xbcT, F32, 512),
        (C_DT, 64, "tok", scr.dt, F32, 64),
        (C_Q, 1152, "feat", scr.qT, BF16, 384),
        (C_K, 1152, "feat", scr.kT, BF16, 384),
        (C_V, 1152, "tok", scr.v, BF16, 384),
        (C_U, 1152, "featpad", scr.uT, BF16, 128),
    ]
    with ExitStack() as es:
        sb = lambda n, s, d: es.enter_context(nc.sbuf_tensor(uniq(n), s, d))
        ps = lambda n, s, d: es.enter_context(nc.psum_tensor(uniq(n), s, d))
        xT = sb("ip_xT", [128, 8, TB], BF16)
        b_xT = Buf("ip_xT")
        xs = [sb(f"ip_xs{i}", [128, D], F32) for i in range(2)]
        b_xs = bufs("ip_xs", 2)
        xb = [sb(f"ip_xb{i}", [128, D], BF16) for i in range(2)]
        b_xb = bufs("ip_xb", 2)
        pt = [ps(f"ip_pt{i}", [128, 4, 128], BF16) for i in range(2)]
        b_pt = bufs("ip_pt", 2)
        wst = [sb(f"ip_wst{i}", [128, 8, 512], F32) for i in range(2)]
        b_wst = bufs("ip_wst", 2)
        wb = [sb(f"ip_wb{i}", [128, 8, 512], BF16) for i in range(2)]
        b_wb = bufs("ip_wb", 2)
        po = [ps(f"ip_po{i}", [128, 512], F32) for i in range(4)]
        b_po = bufs("ip_po", 4)
        ot = [sb(f"ip_ot{i}", [128, 512], F32) for i in range(3)]
        otb = [sb(f"ip_otb{i}", [128, 512], BF16) for i in range(3)]
        b_ot = bufs("ip_ot", 3)
        ctr = [0, 0]
        nw = 0
        npo = 0
        no = 0
        for t0 in range(0, L, TB):
            load_xT(S, nc, cx, x_ap, t0, TB, xT, b_xT, xs, b_xs, xb, b_xb, pt, b_pt, ctr)
            for (c0, ncols, kind, dest, dt, cb) in secs:
                for cc in range(0, ncols, cb):
                    wi = nw % 2
                    nw += 1
                    wsrc = w_in_l[:, c0 + cc:c0 + cc + cb].rearrange("(k p) c -> p k c", p=128)
                    S.dma("sp", wst[wi][:, :, 0:cb], wsrc, writes=[b_wst[wi]], owner=b_wst[wi])
                    if kind == "featpad":
                        ng = cb // 16
                        S.op("pool", lambda e: e.memset(wb[wi][:], 0.0), writes=[b_wb[wi]])
                        S.op("pool", lambda e: e.tensor_copy(
                            out=wb[wi][:, :, 0:ng * 32].rearrange("p k (g c) -> p k g c", c=32)[:, :, :, 0:16],
                            in_=wst[wi][:, :, 0:cb].rearrange("p k (g c) -> p k g c", c=16)),
                            reads=[b_wst[wi]], writes=[b_wb[wi]])
                        wcols = ng * 32
                    else:
                        S.op("pool", lambda e: e.tensor_copy(out=wb[wi][:, :, 0:cb], in_=wst[wi][:, :, 0:cb]),
                             reads=[b_wst[wi]], writes=[b_wb[wi]])
                        wcols = cb
                    if kind == "tok":
                        for t in range(TB // 128):
                            pj = npo % 4
                            npo += 1
                            for k in range(8):
                                S.op("pe", lambda e: e.matmul(po[pj][:, 0:cb], lhsT=xT[:, k, t * 128:(t + 1) * 128],
                                                              rhs=wb[wi][:, k, 0:cb], start=(k == 0), stop=(k == 7)),
                                     reads=[b_xT, b_wb[wi]], writes=[b_po[pj]], inc=(k == 7))
                            oi = no % 3
                            no += 1
                            o_t = ot[oi] if dt == F32 else otb[oi]
                            ev = "act" if no % 2 else "dve"
                            if ev == "act":
                                S.op("act", lambda e: e.copy(out=o_t[:, 0:cb], in_=po[pj][:, 0:cb]),
                                     reads=[b_po[pj]], writes=[b_ot[oi]])
                            else:
                                S.op("dve", lambda e: e.tensor_copy(out=o_t[:, 0:cb], in_=po[pj][:, 0:cb]),
                                     reads=[b_po[pj]], writes=[b_ot[oi]])
                            S.dma("sp", dest[t0 + t * 128:t0 + (t + 1) * 128, cc:cc + cb], o_t[:, 0:cb],
                                  reads=[b_ot[oi]], writes=[scr.b_proj], owner=b_ot[oi])
                    else:
                        if kind == "featpad":
                            r0 = (cc // 16) * 32
                        else:
                            r0 = cc
                        for ts in range(TB // 512):
                            for ch in range(wcols // 128):
                                pj = npo % 4
                                npo += 1
                                for k in range(8):
                                    S.op("pe", lambda e: e.matmul(po[pj][:], lhsT=wb[wi][:, k, ch * 128:(ch + 1) * 128],
                                                                  rhs=xT[:, k, ts * 512:(ts + 1) * 512],
                                                                  start=(k == 0), stop=(k == 7)),
                                         reads=[b_xT, b_wb[wi]], writes=[b_po[pj]], inc=(k == 7))
                                oi = no % 3
                                no += 1
                                o_t = ot[oi] if dt == F32 else otb[oi]
                                if no % 2:
                                    S.op("act", lambda e: e.copy(out=o_t[:], in_=po[pj][:]),
                                         reads=[b_po[pj]], writes=[b_ot[oi]])
                                else:
                                    S.op("dve", lambda e: e.tensor_copy(out=o_t[:], in_=po[pj][:]),
                                         reads=[b_po[pj]], writes=[b_ot[oi]])
                                S.dma("sp", dest[r0 + ch * 128:r0 + (ch + 1) * 128, t0 + ts * 512:t0 + (ts + 1) * 512],
                                      o_t[:], reads=[b_ot[oi]], writes=[scr.b_proj], owner=b_ot[oi])
        S.barrier()


def alloc_scratch(nc, cfg):
    L = cfg.L
    scr = Ctx()
    dr = lambda n, s, d: nc.dram_tensor(n, s, d, kind="Internal").ap()
    scr.gates = dr("s_gates", [L, 3072], F32)
    scr.z = dr("s_z", [L, 2048], F32)
    scr.xbcT = dr("s_xbcT", [4096, L], F32)
    scr.dt = dr("s_dt", [L, 64], F32)
    scr.qT = dr("s_qT", [1152, L], BF16)
    scr.kT = dr("s_kT", [1152, L], BF16)
    scr.v = dr("s_v", [L, 1152], BF16)
    scr.uT = dr("s_uT", [2304, L], BF16)
    scr.b_proj = Buf("proj", multi=True)
    scr.BT = dr("s_BT", [1024, L], BF16)
    scr.CT = dr("s_CT", [1024, L], BF16)
    scr.xsB = dr("s_xsB", [L, 3072], BF16)
    scr.b_conv = Buf("conv", multi=True)
    scr.Sb = dr("s_Sb", [L // 128, 8, 128, 256], F32)
    scr.ypart = dr("s_ypart", [L, 2048], F32)
    scr.b_ssd = Buf("ssd", multi=True)
    scr.yssd = dr("s_yssd", [L, 2048], BF16)
    scr.b_mix = Buf("mix", multi=True)
    scr.hs5T = dr("s_hs5T", [2304, L], BF16)
    scr.b_s5 = Buf("s5", multi=True)
    scr.ys5T = dr("s_ys5T", [1152, L], BF16)
    scr.b_glu = Buf("glu", multi=True)
    scr.x1 = dr("s_x1", [L, 1024], F32)
    scr.b_x1 = Buf("x1", multi=True)
    scr.xcur = dr("s_xcur", [L, 1024], F32)
    scr.b_xcur = Buf("xcur", multi=True)
    scr.wgu16 = dr("s_wgu16", [cfg.E, 1024, 2048], BF16)
    scr.wdn16 = dr("s_wdn16", [cfg.E, 1024, 1024], BF16)
    scr.b_w16 = Buf("w16", multi=True)
    scr.x1T = dr("s_x1T", [1024, L], BF16)
    scr.rgate = dr("s_rgate", [L, cfg.E], F32)
    scr.b_x1T = Buf("x1T", multi=True)
    scr.attO = dr("s_attO", [3, L, 390], F32)
    scr.b_att = Buf("att", multi=True)
    scr.yatt = dr("s_yatt", [L, 384], BF16)
    return scr


def phase_conv(S, nc, cx, cfg, conv_w_l, conv_b_l, scr):
    L = cfg.L
    TS = min(2048, L)
    with ExitStack() as es:
        sb = lambda n, s, d: es.enter_context(nc.sbuf_tensor(uniq(n), s, d))
        ps = lambda n, s, d: es.enter_context(nc.psum_tensor(uniq(n), s, d))
        cw = sb("cv_w", [128, 32, 5], F32)
        cb = sb("cv_b", [128, 32], F32)
        b_cw = Buf("cv_w")
        with nc.allow_non_contiguous_dma(reason="tiny param load"):
            for k in range(5):
                S.dma("sp", cw[:, :, k], conv_w_l[k].rearrange("(t p) -> p t", p=128), writes=[b_cw], owner=b_cw)
            S.dma("sp", cb[:], conv_b_l.rearrange("(t p) -> p t", p=128), writes=[b_cw], owner=b_cw)
        xin = [sb(f"cv_x{i}", [128, TS + 4], F32) for i in range(2)]
        b_xin = bufs("cv_x", 2)
        acc = [sb(f"cv_a{i}", [128, TS], F32) for i in range(2)]
        b_acc = bufs("cv_a", 2)
        yb = [sb(f"cv_y{i}", [128, TS], BF16) for i in range(8)]
        b_yb = bufs("cv_y", 8)
        pt = [ps(f"cv_pt{i}", [128, 4, 128], BF16) for i in range(2)]
        b_pt = bufs("cv_pt", 2)
        ot = [sb(f"cv_o{i}", [128, 512], BF16) for i in range(3)]
        b_ot = bufs("cv_o", 3)
        nx = 0
        ny = 0
        npt = 0
        no = 0
        for cg in range(8):
            for t0 in range(0, L, TS):
                ys = []
                for j in range(4):
                    ct = cg * 4 + j
                    i = nx % 2
                    nx += 1
                    eng = "dve"
                    lo = max(0, t0 - 2)
                    hi = min(L, t0 + TS + 2)
                    d0 = lo - (t0 - 2)
                    if d0 > 0:
                        S.op("pool", lambda e: e.memset(xin[i][:, 0:d0], 0.0), writes=[b_xin[i]])
                    if d0 + (hi - lo) < TS + 4:
                        S.op("pool", lambda e: e.memset(xin[i][:, d0 + hi - lo:TS + 4], 0.0), writes=[b_xin[i]])
                    S.dma("sp", xin[i][:, d0:d0 + hi - lo], scr.xbcT[ct * 128:(ct + 1) * 128, lo:hi],
                          reads=[scr.b_proj], writes=[b_xin[i]], owner=b_xin[i])
                    a = acc[i]
                    S.op("act", lambda e: e.activation(out=a[:], in_=xin[i][:, 0:TS], func=AF.Copy, scale=cw[:, ct, 0:1]),
                         reads=[b_xin[i], b_cw], writes=[b_acc[i]])
                    for k in range(1, 5):
                        S.op(eng, lambda e: e.scalar_tensor_tensor(out=a[:], in0=xin[i][:, k:k + TS], scalar=cw[:, ct, k:k + 1],
                                                                   in1=a[:], op0=ALU.mult, op1=ALU.add),
                             reads=[b_xin[i], b_cw, b_acc[i]], writes=[b_acc[i]])
                    yi = ny % 8
                    ny += 1
                    S.op("act", lambda e: e.activation(out=yb[yi][:], in_=a[:], func=AF.Silu, bias=cb[:, ct:ct + 1], scale=1.0),
                         reads=[b_acc[i], b_cw], writes=[b_yb[yi]])
                    ys.append(yi)
                    if ct >= 16:
                        dst = scr.BT if ct < 24 else scr.CT
                        r0 = (ct - 16) * 128 if ct < 24 else (ct - 24) * 128
                        S.dma("sp", dst[r0:r0 + 128, t0:t0 + TS], yb[yi][:], reads=[b_yb[yi]], writes=[scr.b_conv],
                              owner=b_yb[yi])
                if cg < 6:
                    for tt in range(TS // 128):
                        pj = npt % 2
                        npt += 1
                        for j in range(4):
                            S.op("pe", lambda e: e.transpose(out=pt[pj][:, j, :], in_=yb[ys[j]][:, tt * 128:(tt + 1) * 128],
                                                             identity=cx.identb[:]),
                                 reads=[b_yb[ys[j]], cx.b_const], writes=[b_pt[pj]], inc=(j == 3))
                        oi = no % 3
                        no += 1
                        if no % 2:
                            S.op("act", lambda e: e.copy(out=ot[oi][:], in_=pt[pj][:].rearrange("p a b -> p (a b)")),
                                 reads=[b_pt[pj]], writes=[b_ot[oi]])
                        else:
                            S.op("dve", lambda e: e.tensor_copy(out=ot[oi][:], in_=pt[pj][:].rearrange("p a b -> p (a b)")),
                                 reads=[b_pt[pj]], writes=[b_ot[oi]])
                        S.dma("sp", scr.xsB[t0 + tt * 128:t0 + (tt + 1) * 128, cg * 512:(cg + 1) * 512], ot[oi][:],
                              reads=[b_ot[oi]], writes=[scr.b_conv], owner=b_ot[oi])
        S.barrier()


def make_masks(S, nc, cx):
    cx.maskLE = nc.alloc_sbuf_tensor("maskLE", [128, 128], F32)
    cx.maskGE = nc.alloc_sbuf_tensor("maskGE", [128, 128], F32)
    cx.U0 = nc.alloc_sbuf_tensor("U0", [128, 128], F32)
    cx.U1 = nc.alloc_sbuf_tensor("U1", [128, 128], F32)
    cx.ones = nc.alloc_sbuf_tensor("ones", [128, 128], F32)
    b = cx.b_const
    for t, pat, cm, op in ((cx.maskLE, 1, -1, ALU.is_ge), (cx.maskGE, -1, 1, ALU.is_ge),
                           (cx.U0, -1, 1, ALU.is_gt), (cx.U1, 1, -1, ALU.is_gt)):
        S.op("pool", lambda e: e.memset(t[:], 1.0), writes=[b])
        S.op("pool", lambda e: e.affine_select(out=t[:], in_=t[:], pattern=[[pat, 128]], base=0, channel_multiplier=cm,
                                               compare_op=op, fill=0.0), reads=[b], writes=[b])
    S.op("pool", lambda e: e.memset(cx.ones[:], 1.0), writes=[b])


def bc_r(ap, n):
    return ap.unsqueeze(1).to_broadcast([ap.shape[0], n, ap.shape[1]])


def bc_l(ap, n):
    return ap.unsqueeze(2).to_broadcast([ap.shape[0], ap.shape[1], n])


def phase_ssd(S, nc, cx, cfg, prm, l, scr):
    L = cfg.L
    NC = L // 128
    with ExitStack() as es:
        sb = lambda n, s, d: es.enter_context(nc.sbuf_tensor(uniq(n), s, d))
        ps = lambda n, s, d: es.enter_context(nc.psum_tensor(uniq(n), s, d))
        b_prm = Buf("sd_prm")
        biasb = sb("sd_bias", [128, 64], F32)
        negA = sb("sd_negA", [128, 64], F32)
        dsk = sb("sd_dsk", [128, 32], F32)
        nw = sb("sd_nw", [128, 2048], F32)
        S.dma("sp", biasb[:], prm["ssd_dt_bias"][l].rearrange("a b -> (a b)").partition_broadcast(128), writes=[b_prm], owner=b_prm)
        S.dma("sp", negA[:], prm["ssd_a_log"][l].rearrange("a b -> (a b)").partition_broadcast(128), writes=[b_prm], owner=b_prm)
        S.dma("sp", dsk[:], prm["ssd_d"][l].partition_broadcast(128), writes=[b_prm], owner=b_prm)
        S.dma("sp", nw[:], prm["ssd_norm_w"][l].partition_broadcast(128), writes=[b_prm], owner=b_prm)
        S.op("act", lambda e: e.activation(out=negA[:], in_=negA[:], func=AF.Exp), reads=[b_prm], writes=[b_prm])
        S.op("dve", lambda e: e.tensor_scalar(out=negA[:], in0=negA[:], scalar1=-1.0, scalar2=None, op0=ALU.mult),
             reads=[b_prm], writes=[b_prm])
        Hf = sb("sd_H", [128, 8, 256], F32)
        Hb = sb("sd_Hb", [128, 8, 256], BF16)
        b_H = bufs("sd_Hg", 8)
        tmpH = sb("sd_tH", [128, 256], F32)
        b_tmpH = Buf("sd_tH")
        dtt = [sb(f"sd_dt{i}", [128, 64], F32) for i in range(2)]
        b_dtt = bufs("sd_dt", 2)
        xB = [sb(f"sd_xB{i}", [128, 3072], BF16) for i in range(2)]
        b_xB = bufs("sd_xB", 2)
        BTc = [sb(f"sd_BT{i}", [128, 8, 128], BF16) for i in range(2)]
        b_BTc = bufs("sd_BT", 2)
        CTc = [sb(f"sd_CT{i}", [128, 8, 128], BF16) for i in range(2)]
        b_CTc = bufs("sd_CT", 2)
        names = ["ex", "delta", "a", "I", "Q", "G1", "G2", "G1d", "G2d", "dec", "tq"]
        sm = {n: sb("sd_" + n, [128, 64], F32) for n in names}
        b_sm = {n: Buf("sd_" + n) for n in names}
        ps_I = ps("sd_psI", [128, 64], F32)
        b_psI = Buf("sd_psI")
        ps_T = ps("sd_psT", [128, 64], F32)
        b_psT = Buf("sd_psT")
        ps_CB = ps("sd_psCB", [128, 128], F32)
        b_psCB = Buf("sd_psCB")
        ps_seg = [ps(f"sd_psS{i}", [128, 512], F32) for i in range(2)]
        b_psseg = bufs("sd_psS", 2)
        ps_yd = ps("sd_psyd", [128, 4, 64], F32)
        b_psyd = Buf("sd_psyd")
        ps_st = ps("sd_psst", [128, 2, 256], F32)
        b_psst = bufs("sd_psst", 2)
        ps_yo = ps("sd_psyo", [128, 256], F32)
        b_psyo = Buf("sd_psyo")
        CBm = [sb(f"sd_CBm{i}", [128, 128], F32) for i in range(2)]
        b_CBm = bufs("sd_CBm", 2)
        Rt = [sb(f"sd_R{i}", [128, 4, 128], F32) for i in range(2)]
        b_Rt = bufs("sd_R", 2)
        Dx = [sb(f"sd_Dx{i}", [128, 4, 128], F32) for i in range(2)]
        b_Dx = bufs("sd_Dx", 2)
        MT = [sb(f"sd_MT{i}", [128, 4, 128], BF16) for i in range(2)]
        b_MT = bufs("sd_MT", 2)
        xw = [sb(f"sd_xw{i}", [128, 4, 64], BF16) for i in range(2)]
        b_xw = bufs("sd_xw", 2)
        yp = [sb(f"sd_yp{i}", [128, 2048], F32) for i in range(2)]
        b_yp = bufs("sd_yp", 2)
        ytmp = sb("sd_ytmp", [128, 256], F32)
        b_ytmp = Buf("sd_ytmp")
        sbst = [sb(f"sd_sb{i}", [128, 256], F32) for i in range(2)]
        b_sbst = bufs("sd_sb", 2)
        zt = [sb(f"sd_z{i}", [128, 2048], F32) for i in range(2)]
        b_zt = bufs("sd_z", 2)
        yo16 = [sb(f"sd_y16{i}", [128, 2048], BF16) for i in range(2)]
        b_yo16 = bufs("sd_y16", 2)
        ssq = sb("sd_ssq", [128, 2], F32)
        b_ssq = Buf("sd_ssq")

        for g in range(8):
            S.op("pool", lambda e: e.memset(Hf[:, g, :], 0.0), writes=[b_H[g]])
            S.op("pool", lambda e: e.memset(Hb[:, g, :], 0.0), writes=[b_H[g]])

        def chunk_scalars(c, i):
            S.dma("sp", dtt[i][:], scr.dt[c * 128:(c + 1) * 128, :], reads=[scr.b_proj], writes=[b_dtt[i]], owner=b_dtt[i])
            S.op("dve", lambda e: e.tensor_tensor(out=sm["ex"][:], in0=dtt[i][:], in1=biasb[:], op=ALU.add),
                 reads=[b_dtt[i], b_prm], writes=[b_sm["ex"]])
            S.op("act", lambda e: e.activation(out=sm["ex"][:], in_=sm["ex"][:], func=AF.Exp), reads=[b_sm["ex"]], writes=[b_sm["ex"]])
            S.op("act", lambda e: e.activation(out=sm["delta"][:], in_=sm["ex"][:], func=AF.Ln, bias=1.0, scale=1.0),
                 reads=[b_sm["ex"]], writes=[b_sm["delta"]])
            S.op("dve", lambda e: e.tensor_tensor(out=sm["a"][:], in0=sm["delta"][:], in1=negA[:], op=ALU.mult),
                 reads=[b_sm["delta"], b_prm], writes=[b_sm["a"]])
            S.op("pe", lambda e: e.matmul(ps_I[:], lhsT=cx.maskLE[:], rhs=sm["a"][:], start=True, stop=True),
                 reads=[b_sm["a"], cx.b_const], writes=[b_psI])
            S.op("pe", lambda e: e.matmul(ps_T[:], lhsT=cx.ones[:], rhs=sm["a"][:], start=True, stop=True),
                 reads=[b_sm["a"], cx.b_const], writes=[b_psT])
            S.op("dve", lambda e: e.tensor_copy(out=sm["Q"][:, 0:32], in_=ps_I[:, 0:32]), reads=[b_psI], writes=[b_sm["Q"]])
            S.op("dve", lambda e: e.tensor_tensor(out=sm["Q"][:, 32:64], in0=ps_I[:, 32:64], in1=sm["a"][:, 32:64], op=ALU.subtract),
                 reads=[b_psI, b_sm["a"], b_sm["Q"]], writes=[b_sm["Q"]])
            S.op("dve", lambda e: e.tensor_tensor(out=sm["tq"][:], in0=ps_T[:], in1=sm["Q"][:], op=ALU.subtract),
                 reads=[b_psT, b_sm["Q"]], writes=[b_sm["tq"]])
            S.op("act", lambda e: e.activation(out=sm["G1"][:], in_=sm["Q"][:], func=AF.Exp), reads=[b_sm["Q"]], writes=[b_sm["G1"]])
            S.op("act", lambda e: e.activation(out=sm["G2"][:], in_=sm["tq"][:], func=AF.Exp), reads=[b_sm["tq"]], writes=[b_sm["G2"]])
            S.op("act", lambda e: e.activation(out=sm["dec"][:], in_=ps_T[:], func=AF.Exp), reads=[b_psT], writes=[b_sm["dec"]])
            S.op("dve", lambda e: e.tensor_tensor(out=sm["G1d"][:], in0=sm["G1"][:], in1=sm["delta"][:], op=ALU.mult),
                 reads=[b_sm["G1"], b_sm["delta"]], writes=[b_sm["G1d"]])
            S.op("dve", lambda e: e.tensor_tensor(out=sm["G2d"][:], in0=sm["G2"][:], in1=sm["delta"][:], op=ALU.mult),
                 reads=[b_sm["G2"], b_sm["delta"]], writes=[b_sm["G2d"]])

        nseg = 0
        nst = 0
        for c in range(NC):
            i = c % 2
            chunk_scalars(c, i)
            S.dma("sp", xB[i][:], scr.xsB[c * 128:(c + 1) * 128, :], reads=[scr.b_conv], writes=[b_xB[i]], owner=b_xB[i])
            S.dma("sp", BTc[i][:], scr.BT[:, c * 128:(c + 1) * 128].rearrange("(g n) l -> n g l", n=128),
                  reads=[scr.b_conv], writes=[b_BTc[i]], owner=b_BTc[i])
            S.dma("sp", CTc[i][:], scr.CT[:, c * 128:(c + 1) * 128].rearrange("(g n) l -> n g l", n=128),
                  reads=[scr.b_conv], writes=[b_CTc[i]], owner=b_CTc[i])
            xv = xB[i][:, 0:2048].rearrange("p (g r q) -> p g r q", g=8, r=4)
            Btok = xB[i][:, 2048:3072].rearrange("p (g n) -> p g n", g=8)
            for g in range(8):
                S.op("pe", lambda e: e.matmul(ps_CB[:], lhsT=BTc[i][:, g, :], rhs=CTc[i][:, g, :], start=True, stop=True),
                     reads=[b_BTc[i], b_CTc[i]], writes=[b_psCB])
                S.op("dve", lambda e: e.tensor_tensor(out=CBm[0][:], in0=ps_CB[:], in1=cx.maskLE[:], op=ALU.mult),
                     reads=[b_psCB, cx.b_const], writes=[b_CBm[0]])
                S.op("dve", lambda e: e.tensor_tensor(out=CBm[1][:], in0=ps_CB[:], in1=cx.maskGE[:], op=ALU.mult),
                     reads=[b_psCB, cx.b_const], writes=[b_CBm[1]])
                for d in range(2):
                    cols = slice(d * 32 + g * 4, d * 32 + g * 4 + 4)
                    msk = cx.maskLE if d == 0 else cx.maskGE
                    U = cx.U0 if d == 0 else cx.U1
                    wd = sm["G2d"] if d == 0 else sm["G1d"]
                    b_wd = b_sm["G2d"] if d == 0 else b_sm["G1d"]
                    sj = nseg % 2
                    nseg += 1
                    S.op("pool", lambda e: e.tensor_tensor(out=Rt[d][:], in0=bc_r(msk[:], 4), in1=bc_l(sm["a"][:, cols], 128), op=ALU.mult),
                         reads=[cx.b_const, b_sm["a"]], writes=[b_Rt[d]])
                    S.op("pe", lambda e: e.matmul(ps_seg[sj][:], lhsT=U[:], rhs=Rt[d][:].rearrange("p r l -> p (r l)"), start=True, stop=True),
                         reads=[cx.b_const, b_Rt[d]], writes=[b_psseg[sj]])
                    S.op("act", lambda e: e.activation(out=Dx[d][:].rearrange("p r l -> p (r l)"), in_=ps_seg[sj][:], func=AF.Exp),
                         reads=[b_psseg[sj]], writes=[b_Dx[d]])
                    S.op("dve", lambda e: e.tensor_tensor(out=Dx[d][:], in0=Dx[d][:], in1=bc_l(sm["delta"][:, cols], 128), op=ALU.mult),
                         reads=[b_Dx[d], b_sm["delta"]], writes=[b_Dx[d]])
                    S.op("pool", lambda e: e.tensor_tensor(out=MT[d][:], in0=Dx[d][:], in1=bc_r(CBm[d][:], 4), op=ALU.mult),
                         reads=[b_Dx[d], b_CBm[d]], writes=[b_MT[d]])
                    S.op("dve", lambda e: e.tensor_tensor(out=xw[d][:], in0=xv[:, g, :, :], in1=bc_l(wd[:, cols], 64), op=ALU.mult),
                         reads=[b_xB[i], b_wd], writes=[b_xw[d]])
                    S.op("pe", lambda e: e.matmul(ps_st[:, d, :], lhsT=Btok[:, g, :], rhs=xw[d][:].rearrange("p r q -> p (r q)"),
                                                  start=True, stop=True),
                         reads=[b_xB[i], b_xw[d]], writes=[b_psst[d]])
                for r in range(4):
                    for d in range(2):
                        S.op("pe", lambda e: e.matmul(ps_yd[:, r, :], lhsT=MT[d][:, r, :], rhs=xv[:, g, r, :], start=(d == 0), stop=(d == 1)),
                             reads=[b_MT[d], b_xB[i]], writes=[b_psyd], inc=(r == 3 and d == 1))
                colsf = slice(g * 4, g * 4 + 4)
                S.op("pe", lambda e: e.matmul(ps_yo[:], lhsT=CTc[i][:, g, :], rhs=Hb[:, g, :], start=True, stop=True),
                     reads=[b_CTc[i], b_H[g]], writes=[b_psyo])
                S.op("dve", lambda e: e.tensor_tensor(out=ytmp[:].rearrange("p (r q) -> p r q", r=4), in0=ps_yo[:].rearrange("p (r q) -> p r q", r=4),
                                                      in1=bc_l(sm["G1"][:, colsf], 64), op=ALU.mult),
                     reads=[b_psyo, b_sm["G1"]], writes=[b_ytmp])
                S.op("dve", lambda e: e.tensor_tensor(out=yp[i][:, g * 256:(g + 1) * 256], in0=ps_yd[:].rearrange("p r q -> p (r q)"),
                                                      in1=ytmp[:], op=ALU.add),
                     reads=[b_psyd, b_ytmp], writes=[b_yp[i]])
                S.op("pool", lambda e: e.tensor_tensor(out=tmpH[:].rearrange("p (r q) -> p r q", r=4), in0=Hf[:, g, :].rearrange("p (r q) -> p r q", r=4),
                                                       in1=bc_l(sm["dec"][:, colsf], 64), op=ALU.mult),
                     reads=[b_H[g], b_sm["dec"]], writes=[b_tmpH])
                S.op("dve", lambda e: e.tensor_tensor(out=Hf[:, g, :], in0=tmpH[:], in1=ps_st[:, 0, :], op=ALU.add),
                     reads=[b_tmpH, b_psst[0]], writes=[b_H[g]])
                S.op("act", lambda e: e.copy(out=Hb[:, g, :], in_=Hf[:, g, :]), reads=[b_H[g]], writes=[b_H[g]])
                si = nst % 2
                nst += 1
                S.op("act", lambda e: e.copy(out=sbst[si][:], in_=ps_st[:, 1, :]), reads=[b_psst[1]], writes=[b_sbst[si]])
                S.dma("sp", scr.Sb[c, g], sbst[si][:], reads=[b_sbst[si]], writes=[scr.b_ssd], owner=b_sbst[si])
            S.dma("sp", scr.ypart[c * 128:(c + 1) * 128, :], yp[i][:], reads=[b_yp[i]], writes=[scr.b_ssd], owner=b_yp[i])
        S.barrier()
        for g in range(8):
            S.op("pool", lambda e: e.memset(Hf[:, g, :], 0.0), writes=[b_H[g]])
            S.op("pool", lambda e: e.memset(Hb[:, g, :], 0.0), writes=[b_H[g]])
        for ci, c in enumerate(range(NC - 1, -1, -1)):
            i = ci % 2
            chunk_scalars(c, i)
            S.dma("sp", CTc[i][:], scr.CT[:, c * 128:(c + 1) * 128].rearrange("(g n) l -> n g l", n=128),
                  reads=[scr.b_conv], writes=[b_CTc[i]], owner=b_CTc[i])
            S.dma("sp", xB[i][:, 0:2048], scr.xsB[c * 128:(c + 1) * 128, 0:2048], reads=[scr.b_conv], writes=[b_xB[i]], owner=b_xB[i])
            S.dma("sp", yp[i][:], scr.ypart[c * 128:(c + 1) * 128, :], reads=[scr.b_ssd], writes=[b_yp[i]], owner=b_yp[i])
            S.dma("sp", zt[i][:], scr.z[c * 128:(c + 1) * 128, :], reads=[scr.b_proj], writes=[b_zt[i]], owner=b_zt[i])
            for g in range(8):
                colsb = slice(32 + g * 4, 32 + g * 4 + 4)
                si = nst % 2
                nst += 1
                S.dma("sp", sbst[si][:], scr.Sb[c, g], reads=[scr.b_ssd], writes=[b_sbst[si]], owner=b_sbst[si])
                S.op("pe", lambda e: e.matmul(ps_yo[:], lhsT=CTc[i][:, g, :], rhs=Hb[:, g, :], start=True, stop=True),
                     reads=[b_CTc[i], b_H[g]], writes=[b_psyo])
                S.op("dve", lambda e: e.tensor_tensor(out=ytmp[:].rearrange("p (r q) -> p r q", r=4), in0=ps_yo[:].rearrange("p (r q) -> p r q", r=4),
                                                      in1=bc_l(sm["G2"][:, colsb], 64), op=ALU.mult),
                     reads=[b_psyo, b_sm["G2"]], writes=[b_ytmp])
                S.op("dve", lambda e: e.tensor_tensor(out=yp[i][:, g * 256:(g + 1) * 256], in0=yp[i][:, g * 256:(g + 1) * 256],
                                                      in1=ytmp[:], op=ALU.add),
                     reads=[b_yp[i], b_ytmp], writes=[b_yp[i]])
                S.op("pool", lambda e: e.tensor_tensor(out=tmpH[:].rearrange("p (r q) -> p r q", r=4), in0=Hf[:, g, :].rearrange("p (r q) -> p r q", r=4),
                                                       in1=bc_l(sm["dec"][:, colsb], 64), op=ALU.mult),
                     reads=[b_H[g], b_sm["dec"]], writes=[b_tmpH])
                S.op("pool", lambda e: e.tensor_tensor(out=Hf[:, g, :], in0=tmpH[:], in1=sbst[si][:], op=ALU.add),
                     reads=[b_tmpH, b_sbst[si]], writes=[b_H[g]])
                S.op("act", lambda e: e.copy(out=Hb[:, g, :], in_=Hf[:, g, :]), reads=[b_H[g]], writes=[b_H[g]])
            S.op("act", lambda e: e.activation(out=zt[i][:], in_=zt[i][:], func=AF.Silu), reads=[b_zt[i]], writes=[b_zt[i]])
            S.op("dve", lambda e: e.tensor_tensor(out=yo16[i][:].rearrange("p (h q) -> p h q", h=32), in0=xB[i][:, 0:2048].rearrange("p (h q) -> p h q", h=32),
                                                  in1=bc_l(dsk[:], 64), op=ALU.mult),
                 reads=[b_xB[i], b_prm], writes=[b_yo16[i]])
            S.op("dve", lambda e: e.tensor_tensor(out=yp[i][:], in0=yp[i][:], in1=yo16[i][:], op=ALU.add),
                 reads=[b_yp[i], b_yo16[i]], writes=[b_yp[i]])
            S.op("dve", lambda e: e.tensor_tensor(out=yp[i][:], in0=yp[i][:], in1=zt[i][:], op=ALU.mult),
                 reads=[b_yp[i], b_zt[i]], writes=[b_yp[i]])
            S.op("act", lambda e: e.activation(out=zt[i][:], in_=yp[i][:], func=AF.Square, accum_out=ssq[:, 0:1]),
                 reads=[b_yp[i]], writes=[b_zt[i], b_ssq])
            S.op("dve", lambda e: e.tensor_scalar(out=ssq[:, 1:2], in0=ssq[:, 0:1], scalar1=1.0 / 2048, scalar2=LN_EPS, op0=ALU.mult, op1=ALU.add),
                 reads=[b_ssq], writes=[b_ssq])
            S.op("act", lambda e: e.activation(out=ssq[:, 1:2], in_=ssq[:, 1:2], func=AF.Sqrt), reads=[b_ssq], writes=[b_ssq])
            S.op("dve", lambda e: e.reciprocal(out=ssq[:, 1:2], in_=ssq[:, 1:2]), reads=[b_ssq], writes=[b_ssq])
            S.op("dve", lambda e: e.scalar_tensor_tensor(out=yo16[i][:], in0=yp[i][:], scalar=ssq[:, 1:2], in1=nw[:], op0=ALU.mult, op1=ALU.mult),
                 reads=[b_yp[i], b_ssq, b_prm, b_yo16[i]], writes=[b_yo16[i]])
            S.dma("sp", scr.yssd[c * 128:(c + 1) * 128, :], yo16[i][:], reads=[b_yo16[i]], writes=[scr.b_mix], owner=b_yo16[i])
        S.barrier()


PARAM_SHAPES = lambda cfg: {
    "w_in": [cfg.depth, D, N_IN], "b_gate": [cfg.depth, 3, D],
    "ssd_conv_w": [cfg.depth, 5, 4096], "ssd_conv_b": [cfg.depth, 4096], "ssd_a_log": [cfg.depth, 2, 32],
    "ssd_dt_bias": [cfg.depth, 2, 32], "ssd_d": [cfg.depth, 32], "ssd_norm_w": [cfg.depth, 2048],
    "s5_a_re": [cfg.depth, 2, 72, 64], "s5_a_im": [cfg.depth, 2, 72, 64], "s5_log_step": [cfg.depth, 2, 72],
    "s5_b_re": [cfg.depth, 72, 64, 16], "s5_b_im": [cfg.depth, 72, 64, 16],
    "s5_c_re": [cfg.depth, 2, 72, 16, 64], "s5_c_im": [cfg.depth, 2, 72, 16, 64], "s5_d": [cfg.depth, 1152],
    "s5_glu_w1": [cfg.depth, 1152, 1152], "s5_glu_w2": [cfg.depth, 1152, 1152],
    "w_br_ssd": [cfg.depth, 2048, D], "w_br_attn": [cfg.depth, 384, D], "w_br_s5": [cfg.depth, 1152, D],
    "w_out": [cfg.depth, D, D], "ln1_g": [cfg.depth, D], "ln1_b": [cfg.depth, D],
    "router_w": [cfg.depth, D, cfg.E], "router_b": [cfg.depth, cfg.E],
    "exp_w_gate_up": [cfg.depth, cfg.E, D, 2048], "exp_b_gate_up": [cfg.depth, cfg.E, 2048],
    "exp_w_down": [cfg.depth, cfg.E, D, D], "exp_b_down": [cfg.depth, cfg.E, D],
    "ln2_g": [cfg.depth, D], "ln2_b": [cfg.depth, D],
}


def build(cfg, debug=(), phases=("inproj", "conv", "ssd", "att", "s5", "mix", "moe"), inject=()):
    nc = bass.Bass("TRN2", target_bir_lowering=False)
    L = cfg.L
    prm = {}
    x = nc.dram_tensor("x", [L, D], F32, kind="ExternalInput").ap()
    for name, shape in PARAM_SHAPES(cfg).items():
        prm[name] = nc.dram_tensor(name, list(shape), F32, kind="ExternalInput").ap()
    out = nc.dram_tensor("out", [L, D], F32, kind="ExternalOutput").ap()
    S = Sched(nc)
    cx = Ctx()
    make_consts(S, nc, cx)
    make_masks(S, nc, cx)
    make_att_bias(S, nc, cx)
    scr = alloc_scratch(nc, cfg)
    dbg = {}
    for name in debug:
        src = getattr(scr, name)
        dbg[name] = nc.dram_tensor("dbg_" + name, list(src.shape), src.dtype, kind="ExternalOutput").ap()
    cur = x
    b_inj = Buf("inj", multi=True)
    for name in inject:
        dst = getattr(scr, name)
        src = nc.dram_tensor("inj_" + name, list(dst.shape), dst.dtype, kind="ExternalInput").ap()
        dram_copy(S, dst, src, b_inj)
    if inject:
        S.barrier()
    for l in range(cfg.depth):
        if "inproj" in phases:
            phase_inproj(S, nc, cx, cfg, prm["w_in"][l], cur, scr)
        if "conv" in phases:
            phase_conv(S, nc, cx, cfg, prm["ssd_conv_w"][l], prm["ssd_conv_b"][l], scr)
        if "ssd" in phases:
            phase_ssd(S, nc, cx, cfg, prm, l, scr)
        if "att" in phases:
            phase_att(S, nc, cx, cfg, scr)
        if "s5" in phases:
            phase_s5(S, nc, cx, cfg, prm, l, scr)
        if "mix" in phases:
            phase_mix(S, nc, cx, cfg, prm, l, cur, scr)
            cur = scr.x1
        if "moe" in phases:
            last = (l == cfg.depth - 1) and not debug
            phase_moe(S, nc, cx, cfg, prm, l, scr, out if last else scr.xcur)
            cur = None if last else scr.xcur
    b_dbg = Buf("dbg", multi=True)
    for name in debug:
        dram_copy(S, dbg[name], getattr(scr, name), b_dbg)
    if cur is not None:
        dram_copy(S, out, cur, b_dbg)
    S.barrier()
    return nc, S


def kernel(**inputs):
    cfg = Cfg(L=8192, depth=4, n_exp=32)
    nc, _ = build(cfg)
    x = np.asarray(inputs["x"], dtype=np.float32)
    names = list(PARAM_SHAPES(cfg).keys())
    params = {k: np.ascontiguousarray(np.asarray(inputs[k], dtype=np.float32)) for k in names}
    in_maps = []
    for b in range(x.shape[0]):
        m = {"x": np.ascontiguousarray(x[b])}
        m.update(params)
        in_maps.append(m)
    res = run_bass_kernel_spmd(nc, in_maps, core_ids=list(range(x.shape[0])))
    return np.stack([np.asarray(r["out"], dtype=np.float32) for r in res.results], axis=0)


ATT_PAT = ((128, 1), (512, 4), (2048, 16))
NEG_BIG = -30000.0


def make_att_bias(S, nc, cx):
    cx.abias = nc.alloc_sbuf_tensor("abias", [128, 18, 256], F32)
    cx.b_abias = Buf("abias")
    b = cx.b_abias
    with ExitStack() as es:
        di = es.enter_context(nc.sbuf_tensor("ab_di", [128, 256], I32))
        df = es.enter_context(nc.sbuf_tensor("ab_df", [128, 256], F32))
        mk = es.enter_context(nc.sbuf_tensor("ab_mk", [128, 256], F32))
        bt = Buf("ab_tmp")
        S.op("pool", lambda e: e.iota(di[:], pattern=[[-128, 2], [1, 128]], base=64, channel_multiplier=-1), writes=[bt])
        S.op("dve", lambda e: e.tensor_copy(out=df[:], in_=di[:]), reads=[bt], writes=[bt])
        S.op("act", lambda e: e.activation(out=df[:], in_=df[:], func=AF.Abs), reads=[bt], writes=[bt])
        S.op("dve", lambda e: e.tensor_scalar(out=mk[:], in0=df[:], scalar1=64.0, scalar2=NEG_BIG, op0=ALU.is_gt, op1=ALU.mult),
             reads=[bt], writes=[bt])
        for g, (window, dil) in enumerate(ATT_PAT):
            for h in range(6):
                slope = 2.0 ** (-8.0 * (h * 3 + g + 1) / 18.0)
                S.op("dve", lambda e: e.scalar_tensor_tensor(out=cx.abias[:, g * 6 + h, :], in0=df[:], scalar=-slope * dil, in1=mk[:],
                                                             op0=ALU.mult, op1=ALU.add), reads=[bt], writes=[b])
        S.barrier()


def phase_att(S, nc, cx, cfg, scr):
    L = cfg.L
    with ExitStack() as es:
        sb = lambda n, s, d: es.enter_context(nc.sbuf_tensor(uniq(n), s, d))
        ps = lambda n, s, d: es.enter_context(nc.psum_tensor(uniq(n), s, d))
        PADM = 64 * 16
        qT2 = [sb(f"at_q{i}", [128, L], BF16) for i in range(3)]
        kT2 = [sb(f"at_k{i}", [128, L + 2 * PADM], BF16) for i in range(3)]
        b_qk = bufs("at_qk", 3)
        raw = [sb(f"at_raw{i}", [128, L], BF16) for i in range(2)]
        b_raw = bufs("at_raw", 2)
        Vn = [sb(f"at_v{i}", [128, 6, 65], BF16) for i in range(4)]
        b_Vn = bufs("at_v", 4)
        Ve = [sb(f"at_ve{i}", [128, 6, 65], BF16) for i in range(2)]
        b_Ve = bufs("at_ve", 2)
        for i in range(4):
            S.op("pool", lambda e: e.memset(Vn[i][:, :, 64:65], 1.0), writes=[b_Vn[i]])
        S.op("pool", lambda e: e.memset(Ve[0][:], 0.0), writes=[b_Ve[0]])
        S.op("pool", lambda e: e.memset(Ve[0][64:128, :, 64:65], 1.0), writes=[b_Ve[0]])
        S.op("pool", lambda e: e.memset(Ve[1][:], 0.0), writes=[b_Ve[1]])
        S.op("pool", lambda e: e.memset(Ve[1][0:64, :, 64:65], 1.0), writes=[b_Ve[1]])
        S_ps = [ps(f"at_ps{i}", [128, 2, 128], F32) for i in range(2)]
        b_Sps = bufs("at_ps", 2)
        O_ps = [ps(f"at_po{i}", [128, 6, 65], F32) for i in range(2)]
        b_Ops = bufs("at_po", 2)
        sbt = [sb(f"at_sb{i}", [128, 256], F32) for i in range(2)]
        b_sbt = bufs("at_sb", 2)
        PT = [sb(f"at_pt{i}", [128, 2, 128], BF16) for i in range(2)]
        b_PT = bufs("at_pt", 2)
        Ot = [sb(f"at_o{i}", [128, 390], F32) for i in range(2)]
        b_Ot = bufs("at_o", 2)
        nv = 0
        nsp = 0
        nop = 0
        for g, (window, dil) in enumerate(ATT_PAT):
            ls = L // dil
            PAD = 64 * dil
            nt = ls // 128
            for hp in range(3):
                r0 = g * 384 + hp * 128
                S.dma("sp", raw[0][:], scr.qT[r0:r0 + 128, :], reads=[scr.b_proj], writes=[b_raw[0]], owner=b_raw[0])
                S.dma("sp", raw[1][:], scr.kT[r0:r0 + 128, :], reads=[scr.b_proj], writes=[b_raw[1]], owner=b_raw[1])
                qv = qT2[hp][:, 0:L].rearrange("p (r i) -> p r i", r=dil)
                kv = kT2[hp][:, 0:dil * (ls + 128)].rearrange("p (r i) -> p r i", r=dil)
                S.op("pool", lambda e: e.tensor_copy(out=qv, in_=raw[0][:].rearrange("p (i r) -> p r i", r=dil)),
                     reads=[b_raw[0]], writes=[b_qk[hp]])
                S.op("pool", lambda e: e.memset(kv[:, :, 0:64], 0.0), writes=[b_qk[hp]])
                S.op("pool", lambda e: e.memset(kv[:, :, 64 + ls:128 + ls], 0.0), writes=[b_qk[hp]])
                S.op("dve", lambda e: e.tensor_copy(out=kv[:, :, 64:64 + ls], in_=raw[1][:].rearrange("p (i r) -> p r i", r=dil)),
                     reads=[b_raw[1]], writes=[b_qk[hp]])
            for r in range(dil):
                for n in range(nt):
                    vch = []
                    for c in range(2):
                        kp0 = 128 * n - 64 + 128 * c
                        lo_bad = kp0 < 0
                        hi_bad = kp0 + 128 > ls
                        if lo_bad:
                            vt, bv, p0, p1 = Ve[0], b_Ve[0], 64, 128
                        elif hi_bad:
                            vt, bv, p0, p1 = Ve[1], b_Ve[1], 0, 64
                        else:
                            vi = nv % 4
                            nv += 1
                            vt, bv, p0, p1 = Vn[vi], b_Vn[vi], 0, 128
                        tok0 = (kp0 + p0) * dil + r
                        npart = p1 - p0
                        src = scr.v[tok0:tok0 + (npart - 1) * dil + 1:dil, g * 384:(g + 1) * 384].rearrange("p (h e) -> p h e", h=6)
                        S.dma("sp", vt[p0:p1, :, 0:64], src, reads=[scr.b_proj], writes=[bv], owner=bv)
                        vch.append((vt, bv))
                    oj = nop % 2
                    nop += 1
                    for h in range(6):
                        hp, hh = h // 2, h % 2
                        sj = nsp % 2
                        nsp += 1
                        q0 = r * ls + 128 * n
                        for c in range(2):
                            k0 = r * (ls + 128) + 128 * n + 128 * c
                            S.op("pe", lambda e: e.matmul(S_ps[sj][:, c, :],
                                                          lhsT=kT2[hp][hh * 64:(hh + 1) * 64, k0:k0 + 128],
                                                          rhs=qT2[hp][hh * 64:(hh + 1) * 64, q0:q0 + 128],
                                                          start=True, stop=True),
                                 reads=[b_qk[hp]], writes=[b_Sps[sj]], inc=(c == 1))
                        S.op("dve", lambda e: e.scalar_tensor_tensor(out=sbt[sj][:], in0=S_ps[sj][:].rearrange("p c q -> p (c q)"), scalar=0.125,
                                                                     in1=cx.abias[:, g * 6 + h, :], op0=ALU.mult, op1=ALU.add),
                             reads=[b_Sps[sj], cx.b_abias], writes=[b_sbt[sj]])
                        S.op("act", lambda e: e.activation(out=PT[sj][:].rearrange("p c q -> p (c q)"), in_=sbt[sj][:], func=AF.Exp),
                             reads=[b_sbt[sj]], writes=[b_PT[sj]])
                        for c in range(2):
                            vt, bv = vch[c]
                            S.op("pe", lambda e: e.matmul(O_ps[oj][:, h, :], lhsT=PT[sj][:, c, :], rhs=vt[:, h, :], start=(c == 0), stop=(c == 1)),
                                 reads=[b_PT[sj], bv], writes=[b_Ops[oj]], inc=(c == 1))
                    S.op("act", lambda e: e.copy(out=Ot[oj][:], in_=O_ps[oj][:].rearrange("p h e -> p (h e)")), reads=[b_Ops[oj]], writes=[b_Ot[oj]])
                    t0 = (128 * n) * dil + r
                    S.dma("sp", scr.attO[g, t0:t0 + 127 * dil + 1:dil, :], Ot[oj][:], reads=[b_Ot[oj]], writes=[scr.b_att], owner=b_Ot[oj])
        S.barrier()
        At = [sb(f"at_m{i}", [128, 3, 390], F32) for i in range(2)]
        b_At = bufs("at_m", 2)
        rc = sb("at_rc", [128, 6], F32)
        b_rc = Buf("at_rc")
        yo = [sb(f"at_y{i}", [128, 384], BF16) for i in range(2)]
        b_yo = bufs("at_y", 2)
        for t in range(L // 128):
            i = t % 2
            for g in range(3):
                S.dma("sp", At[i][:, g, :], scr.attO[g, t * 128:(t + 1) * 128, :], reads=[scr.b_att], writes=[b_At[i]], owner=b_At[i])
            S.op("dve", lambda e: e.tensor_tensor(out=At[i][:, 0, :], in0=At[i][:, 0, :], in1=At[i][:, 1, :], op=ALU.add), reads=[b_At[i]], writes=[b_At[i]])
            S.op("dve", lambda e: e.tensor_tensor(out=At[i][:, 0, :], in0=At[i][:, 0, :], in1=At[i][:, 2, :], op=ALU.add), reads=[b_At[i]], writes=[b_At[i]])
            a3 = At[i][:, 0, :].rearrange("p (h e) -> p h e", h=6)
            S.op("dve", lambda e: e.reciprocal(out=rc[:], in_=a3[:, :, 64]), reads=[b_At[i]], writes=[b_rc])
            S.op("dve", lambda e: e.tensor_tensor(out=yo[i][:].rearrange("p (h e) -> p h e", h=6), in0=a3[:, :, 0:64], in1=bc_l(rc[:], 64), op=ALU.mult),
                 reads=[b_At[i], b_rc], writes=[b_yo[i]])
            S.dma("sp", scr.yatt[t * 128:(t + 1) * 128, :], yo[i][:], reads=[b_yo[i]], writes=[scr.b_mix], owner=b_yo[i])
        S.barrier()


def layer_norm_tile(S, h, b_h, out, b_out, gam, bet, b_par, st, b_st):
    for j in range(2):
        S.op("dve", lambda e: e.bn_stats(out=st[:, j * 6:(j + 1) * 6], in_=h[:, j * 512:(j + 1) * 512]), reads=[b_h], writes=[b_st])
    S.op("dve", lambda e: e.bn_aggr(out=st[:, 12:14], in_=st[:, 0:12]), reads=[b_st], writes=[b_st])
    S.op("dve", lambda e: e.tensor_scalar(out=st[:, 14:15], in0=st[:, 13:14], scalar1=LN_EPS, scalar2=None, op0=ALU.add), reads=[b_st], writes=[b_st])
    S.op("act", lambda e: e.activation(out=st[:, 14:15], in_=st[:, 14:15], func=AF.Sqrt), reads=[b_st], writes=[b_st])
    S.op("dve", lambda e: e.reciprocal(out=st[:, 14:15], in_=st[:, 14:15]), reads=[b_st], writes=[b_st])
    S.op("dve", lambda e: e.tensor_scalar(out=out[:], in0=h[:], scalar1=st[:, 12:13], scalar2=st[:, 14:15], op0=ALU.subtract, op1=ALU.mult),
         reads=[b_h, b_st], writes=[b_out])
    S.op("dve", lambda e: e.tensor_tensor(out=out[:], in0=out[:], in1=gam[:], op=ALU.mult), reads=[b_out, b_par], writes=[b_out])
    S.op("dve", lambda e: e.tensor_tensor(out=out[:], in0=out[:], in1=bet[:], op=ALU.add), reads=[b_out, b_par], writes=[b_out])


def load_w_bf16(S, w_ap, dst, b_dst, stg, b_stg, ctr, ncols_max=512):
    K, N = w_ap.shape
    for k in range(K // 128):
        for c0 in range(0, N, ncols_max):
            c1 = min(N, c0 + ncols_max)
            i = ctr[0] % 2
            ctr[0] += 1
            S.dma("sp", stg[i][:, 0:c1 - c0], w_ap[k * 128:(k + 1) * 128, c0:c1], writes=[b_stg[i]], owner=b_stg[i])
            eng = "pool" if ctr[0] % 2 else "act"
            if eng == "pool":
                S.op("pool", lambda e: e.tensor_copy(out=dst[:, k, c0:c1], in_=stg[i][:, 0:c1 - c0]), reads=[b_stg[i]], writes=[b_dst])
            else:
                S.op("act", lambda e: e.copy(out=dst[:, k, c0:c1], in_=stg[i][:, 0:c1 - c0]), reads=[b_stg[i]], writes=[b_dst])


def phase_mix(S, nc, cx, cfg, prm, l, x_ap, scr):
    L = cfg.L
    with ExitStack() as es:
        sb = lambda n, s, d: es.enter_context(nc.sbuf_tensor(uniq(n), s, d))
        ps = lambda n, s, d: es.enter_context(nc.psum_tensor(uniq(n), s, d))
        W1 = sb("gl_w1", [128, 18, 1152], BF16)
        W2 = sb("gl_w2", [128, 18, 1152], BF16)
        b_W = Buf("gl_w")
        stg = [sb(f"gl_st{i}", [128, 1152], F32) for i in range(2)]
        b_stg = bufs("gl_st", 2)
        for i in range(2):
            S.op("pool", lambda e: e.memset(stg[i][:], 0.0), writes=[b_stg[i]])
        n = 0
        for W, wname in ((W1, "s5_glu_w1"), (W2, "s5_glu_w2")):
            for kt in range(18):
                i = n % 2
                n += 1
                for gl in range(4):
                    g = kt * 4 + gl
                    S.dma("sp", stg[i][gl * 32:gl * 32 + 16, :], prm[wname][l][g * 16:(g + 1) * 16, :], writes=[b_stg[i]], owner=b_stg[i])
                S.op("pool", lambda e: e.tensor_copy(out=W[:, kt, :], in_=stg[i][:]), reads=[b_stg[i]], writes=[b_W])
        hT = [sb(f"gl_h{i}", [128, 18, 512], BF16) for i in range(2)]
        b_hT = bufs("gl_h", 2)
        g_ps = [ps(f"gl_pg{i}", [128, 512], F32) for i in range(2)]
        b_gps = bufs("gl_pg", 2)
        l_ps = [ps(f"gl_pl{i}", [128, 512], F32) for i in range(2)]
        b_lps = bufs("gl_pl", 2)
        sg = [sb(f"gl_sg{i}", [128, 512], F32) for i in range(2)]
        b_sg = bufs("gl_sg", 2)
        yo = [sb(f"gl_y{i}", [128, 512], BF16) for i in range(2)]
        b_yo = bufs("gl_y", 2)
        np_ = 0
        for tb in range(L // 512):
            i = tb % 2
            S.dma("sp", hT[i][:], scr.hs5T[:, tb * 512:(tb + 1) * 512].rearrange("(k p) t -> p k t", p=128),
                  reads=[scr.b_s5], writes=[b_hT[i]], owner=b_hT[i])
            for co in range(9):
                j = np_ % 2
                np_ += 1
                for kt in range(18):
                    S.op("pe", lambda e: e.matmul(g_ps[j][:], lhsT=W1[:, kt, co * 128:(co + 1) * 128], rhs=hT[i][:, kt, :], start=(kt == 0), stop=(kt == 17)),
                         reads=[b_W, b_hT[i]], writes=[b_gps[j]], inc=(kt == 17))
                for kt in range(18):
                    S.op("pe", lambda e: e.matmul(l_ps[j][:], lhsT=W2[:, kt, co * 128:(co + 1) * 128], rhs=hT[i][:, kt, :], start=(kt == 0), stop=(kt == 17)),
                         reads=[b_W, b_hT[i]], writes=[b_lps[j]], inc=(kt == 17))
                S.op("act", lambda e: e.activation(out=sg[j][:], in_=l_ps[j][:], func=AF.Sigmoid), reads=[b_lps[j]], writes=[b_sg[j]])
                S.op("dve", lambda e: e.tensor_tensor(out=yo[j][:], in0=g_ps[j][:], in1=sg[j][:], op=ALU.mult), reads=[b_gps[j], b_sg[j]], writes=[b_yo[j]])
                S.dma("sp", scr.ys5T[co * 128:(co + 1) * 128, tb * 512:(tb + 1) * 512], yo[j][:], reads=[b_yo[j]], writes=[scr.b_glu], owner=b_yo[j])
        S.barrier()
    with ExitStack() as es:
        sb = lambda n, s, d: es.enter_context(nc.sbuf_tensor(uniq(n), s, d))
        ps = lambda n, s, d: es.enter_context(nc.psum_tensor(uniq(n), s, d))
        Wssd = sb("mx_wssd", [128, 16, 1024], BF16)
        Watt = sb("mx_watt", [128, 3, 1024], BF16)
        Ws5 = sb("mx_ws5", [128, 9, 1024], BF16)
        Wout = sb("mx_wout", [128, 8, 1024], BF16)
        b_W = Buf("mx_w")
        stg = [sb(f"mx_st{i}", [128, 512], F32) for i in range(2)]
        b_stg = bufs("mx_st", 2)
        ctr = [0]
        load_w_bf16(S, prm["w_br_ssd"][l], Wssd, b_W, stg, b_stg, ctr)
        load_w_bf16(S, prm["w_br_attn"][l], Watt, b_W, stg, b_stg, ctr)
        load_w_bf16(S, prm["w_br_s5"][l], Ws5, b_W, stg, b_stg, ctr)
        load_w_bf16(S, prm["w_out"][l], Wout, b_W, stg, b_stg, ctr)
        bg = sb("mx_bg", [128, 3072], F32)
        lng = sb("mx_lng", [128, 1024], F32)
        lnb = sb("mx_lnb", [128, 1024], F32)
        b_par = Buf("mx_par")
        S.dma("sp", bg[:], prm["b_gate"][l].rearrange("a b -> (a b)").partition_broadcast(128), writes=[b_par], owner=b_par)
        S.dma("sp", lng[:], prm["ln1_g"][l].partition_broadcast(128), writes=[b_par], owner=b_par)
        S.dma("sp", lnb[:], prm["ln1_b"][l].partition_broadcast(128), writes=[b_par], owner=b_par)
        ys = [sb(f"mx_ys{i}", [128, 2048], BF16) for i in range(2)]
        b_ys = bufs("mx_ys", 2)
        ya = [sb(f"mx_ya{i}", [128, 384], BF16) for i in range(2)]
        b_ya = bufs("mx_ya", 2)
        y5T = [sb(f"mx_y5{i}", [128, 9, 128], BF16) for i in range(2)]
        b_y5T = bufs("mx_y5", 2)
        gt = [sb(f"mx_g{i}", [128, 3072], F32) for i in range(2)]
        b_gt = bufs("mx_g", 2)
        xt = [sb(f"mx_x{i}", [128, 1024], F32) for i in range(2)]
        b_xt = bufs("mx_x", 2)
        ysT = sb("mx_ysT", [128, 16, 128], BF16)
        b_ysT = Buf("mx_ysT")
        yaT = sb("mx_yaT", [128, 3, 128], BF16)
        b_yaT = Buf("mx_yaT")
        mT = sb("mx_mT", [128, 8, 128], BF16)
        b_mT = Buf("mx_mT")
        pt = [ps(f"mx_pt{i}", [128, 4, 128], BF16) for i in range(2)]
        b_pt = bufs("mx_pt", 2)
        Pb = [ps(f"mx_pb{i}", [128, 1024], F32) for i in range(2)]
        b_Pb = bufs("mx_pb", 2)
        acc = sb("mx_acc", [128, 1024], F32)
        b_acc = Buf("mx_acc")
        tmp = sb("mx_tmp", [128, 1024], F32)
        b_tmp = Buf("mx_tmp")
        mb = sb("mx_mb", [128, 1024], BF16)
        b_mb = Buf("mx_mb")
        hh = sb("mx_h", [128, 1024], F32)
        b_hh = Buf("mx_h")
        xo = [sb(f"mx_xo{i}", [128, 1024], F32) for i in range(2)]
        b_xo = bufs("mx_xo", 2)
        st = sb("mx_stt", [128, 16], F32)
        b_st = Buf("mx_stt")
        npt = 0
        npb = 0

        def transposes(src, nk, dst, b_src, b_dst):
            nonlocal npt
            for k0 in range(0, nk, 4):
                kn = min(4, nk - k0)
                j = npt % 2
                npt += 1
                for kk in range(kn):
                    S.op("pe", lambda e: e.transpose(out=pt[j][:, kk, :], in_=src[:, (k0 + kk) * 128:(k0 + kk + 1) * 128], identity=cx.identb[:]),
                         reads=[b_src, cx.b_const], writes=[b_pt[j]], inc=(kk == kn - 1))
                S.op("act", lambda e: e.copy(out=dst[:, k0:k0 + kn, :], in_=pt[j][:, 0:kn, :]), reads=[b_pt[j]], writes=[b_dst])

        for t in range(L // 128):
            i = t % 2
            rows = slice(t * 128, (t + 1) * 128)
            S.dma("sp", ys[i][:], scr.yssd[rows, :], reads=[scr.b_mix], writes=[b_ys[i]], owner=b_ys[i])
            S.dma("sp", ya[i][:], scr.yatt[rows, :], reads=[scr.b_mix], writes=[b_ya[i]], owner=b_ya[i])
            S.dma("sp", y5T[i][:], scr.ys5T[:, rows].rearrange("(k p) t -> p k t", p=128), reads=[scr.b_glu], writes=[b_y5T[i]], owner=b_y5T[i])
            S.dma("sp", gt[i][:], scr.gates[rows, :], reads=[scr.b_proj], writes=[b_gt[i]], owner=b_gt[i])
            S.dma("sp", xt[i][:], x_ap[rows, :], writes=[b_xt[i]], owner=b_xt[i])
            S.op("pool", lambda e: e.tensor_tensor(out=gt[i][:], in0=gt[i][:], in1=bg[:], op=ALU.add), reads=[b_gt[i], b_par], writes=[b_gt[i]])
            S.op("act", lambda e: e.activation(out=gt[i][:], in_=gt[i][:], func=AF.Sigmoid), reads=[b_gt[i]], writes=[b_gt[i]])
            transposes(ys[i], 16, ysT, b_ys[i], b_ysT)
            transposes(ya[i], 3, yaT, b_ya[i], b_yaT)
            for bi, (srcT, b_srcT, nk, W) in enumerate(((ysT, b_ysT, 16, Wssd), (yaT, b_yaT, 3, Watt), (y5T[i], b_y5T[i], 9, Ws5))):
                j = npb % 2
                npb += 1
                for nh in range(2):
                    for k in range(nk):
                        S.op("pe", lambda e: e.matmul(Pb[j][:, nh * 512:(nh + 1) * 512], lhsT=srcT[:, k, :], rhs=W[:, k, nh * 512:(nh + 1) * 512],
                                                      start=(k == 0), stop=(k == nk - 1)),
                             reads=[b_srcT, b_W], writes=[b_Pb[j]], inc=(k == nk - 1 and nh == 1))
                if bi == 0:
                    S.op("dve", lambda e: e.tensor_tensor(out=acc[:], in0=Pb[j][:], in1=gt[i][:, 0:1024], op=ALU.mult), reads=[b_Pb[j], b_gt[i]], writes=[b_acc])
                else:
                    S.op("dve", lambda e: e.tensor_tensor(out=tmp[:], in0=Pb[j][:], in1=gt[i][:, bi * 1024:(bi + 1) * 1024], op=ALU.mult),
                         reads=[b_Pb[j], b_gt[i]], writes=[b_tmp])
                    if bi == 1:
                        S.op("pool", lambda e: e.tensor_tensor(out=acc[:], in0=acc[:], in1=tmp[:], op=ALU.add), reads=[b_acc, b_tmp], writes=[b_acc])
                    else:
                        S.op("pool", lambda e: e.tensor_tensor(out=mb[:], in0=acc[:], in1=tmp[:], op=ALU.add), reads=[b_acc, b_tmp], writes=[b_mb])
            transposes(mb, 8, mT, b_mb, b_mT)
            j = npb % 2
            npb += 1
            for nh in range(2):
                for k in range(8):
                    S.op("pe", lambda e: e.matmul(Pb[j][:, nh * 512:(nh + 1) * 512], lhsT=mT[:, k, :], rhs=Wout[:, k, nh * 512:(nh + 1) * 512],
                                                  start=(k == 0), stop=(k == 7)),
                         reads=[b_mT, b_W], writes=[b_Pb[j]], inc=(k == 7 and nh == 1))
            S.op("dve", lambda e: e.scalar_tensor_tensor(out=hh[:], in0=xt[i][:], scalar=cfg.alpha, in1=Pb[j][:], op0=ALU.mult, op1=ALU.add),
                 reads=[b_xt[i], b_Pb[j]], writes=[b_hh])
            layer_norm_tile(S, hh, b_hh, xo[i], b_xo[i], lng, lnb, b_par, st, b_st)
            S.dma("sp", scr.x1[rows, :], xo[i][:], reads=[b_xo[i]], writes=[scr.b_x1], owner=b_xo[i])
        S.barrier()


def cast_dram_bf16(S, nc, src, dst, b_dst, stg, stb, b_stg, b_stb, ctr):
    R, N = src.shape
    for r in range(0, R, 128):
        i = ctr[0] % 2
        ctr[0] += 1
        S.dma("sp", stg[i][:, 0:N], src[r:r + 128, :], writes=[b_stg[i]], owner=b_stg[i])
        if ctr[0] % 2:
            S.op("pool", lambda e: e.tensor_copy(out=stb[i][:, 0:N], in_=stg[i][:, 0:N]), reads=[b_stg[i]], writes=[b_stb[i]])
        else:
            S.op("act", lambda e: e.copy(out=stb[i][:, 0:N], in_=stg[i][:, 0:N]), reads=[b_stg[i]], writes=[b_stb[i]])
        S.dma("sp", dst[r:r + 128, :], stb[i][:, 0:N], reads=[b_stb[i]], writes=[b_dst], owner=b_stb[i])


def phase_moe(S, nc, cx, cfg, prm, l, scr, xdst):
    L = cfg.L
    E = cfg.E
    with ExitStack() as es:
        sb = lambda n, s, d: es.enter_context(nc.sbuf_tensor(uniq(n), s, d))
        ps = lambda n, s, d: es.enter_context(nc.psum_tensor(uniq(n), s, d))
        stg = [sb(f"mo_cs{i}", [128, 2048], F32) for i in range(2)]
        stb = [sb(f"mo_cb{i}", [128, 2048], BF16) for i in range(2)]
        b_stg = bufs("mo_cs", 2)
        b_stb = bufs("mo_cb", 2)
        ctr = [0]
        for e_ in range(E):
            cast_dram_bf16(S, nc, prm["exp_w_gate_up"][l][e_], scr.wgu16[e_], scr.b_w16, stg, stb, b_stg, b_stb, ctr)
            cast_dram_bf16(S, nc, prm["exp_w_down"][l][e_], scr.wdn16[e_], scr.b_w16, stg, stb, b_stg, b_stb, ctr)
        Wr = sb("mo_wr", [128, 8, E], F32)
        Wrh = sb("mo_wrh", [128, 8, E], BF16)
        Wrl = sb("mo_wrl", [128, 8, E], BF16)
        rb = sb("mo_rb", [128, E], F32)
        b_wr = Buf("mo_wr")
        S.dma("sp", Wr[:], prm["router_w"][l].rearrange("(k p) e -> p k e", p=128), writes=[b_wr], owner=b_wr)
        S.dma("sp", rb[:], prm["router_b"][l].partition_broadcast(128), writes=[b_wr], owner=b_wr)
        S.op("dve", lambda e: e.tensor_copy(out=Wrh[:], in_=Wr[:]), reads=[b_wr], writes=[b_wr])
        S.op("dve", lambda e: e.tensor_tensor(out=Wr[:], in0=Wr[:], in1=Wrh[:], op=ALU.subtract), reads=[b_wr], writes=[b_wr])
        S.op("dve", lambda e: e.tensor_copy(out=Wrl[:], in_=Wr[:]), reads=[b_wr], writes=[b_wr])
        xt = [sb(f"mo_x{i}", [128, 1024], F32) for i in range(2)]
        b_xt = bufs("mo_x", 2)
        xh = sb("mo_xh", [128, 1024], BF16)
        xl = sb("mo_xl", [128, 1024], BF16)
        b_xhl = Buf("mo_xhl")
        xTl = sb("mo_xTl", [128, 8, 128], BF16)
        b_xTl = Buf("mo_xTl")
        xTb = [sb(f"mo_xTb{i}", [128, 8, 128], BF16) for i in range(2)]
        b_xTb = bufs("mo_xTb", 2)
        ptf = [ps(f"mo_ptf{i}", [128, 4, 128], BF16) for i in range(2)]
        b_ptf = bufs("mo_ptf", 2)
        lg_ps = ps("mo_lg", [128, E], F32)
        b_lgps = Buf("mo_lg")
        lg = sb("mo_lgs", [128, E], F32)
        b_lg = Buf("mo_lgs")
        v8 = sb("mo_v8", [128, 8], F32)
        b_v8 = Buf("mo_v8")
        mk = sb("mo_mk", [128, E], F32)
        b_mk = Buf("mo_mk")
        sm = sb("mo_sm", [128, 4], F32)
        b_sm = Buf("mo_sm")
        gts = [sb(f"mo_gt{i}", [128, E], F32) for i in range(2)]
        b_gts = bufs("mo_gt", 2)
        npt = 0
        for t in range(L // 128):
            i = t % 2
            rows = slice(t * 128, (t + 1) * 128)
            S.dma("sp", xt[i][:], scr.x1[rows, :], reads=[scr.b_x1], writes=[b_xt[i]], owner=b_xt[i])
            S.op("dve", lambda e: e.tensor_copy(out=xh[:], in_=xt[i][:]), reads=[b_xt[i]], writes=[b_xhl])
            S.op("dve", lambda e: e.tensor_tensor(out=xl[:], in0=xt[i][:], in1=xh[:], op=ALU.subtract), reads=[b_xt[i], b_xhl], writes=[b_xhl])
            for src, dstT, b_dstT in ((xh, xTb[i], b_xTb[i]), (xl, xTl, b_xTl)):
                for k4 in range(2):
                    j = npt % 2
                    npt += 1
                    for kk in range(4):
                        k = k4 * 4 + kk
                        S.op("pe", lambda e: e.transpose(out=ptf[j][:, kk, :], in_=src[:, k * 128:(k + 1) * 128], identity=cx.identb[:]),
                             reads=[b_xhl, cx.b_const], writes=[b_ptf[j]], inc=(kk == 3))
                    S.op("act", lambda e: e.copy(out=dstT[:, k4 * 4:(k4 + 1) * 4, :], in_=ptf[j][:]), reads=[b_ptf[j]], writes=[b_dstT])
            S.dma("sp", scr.x1T[:, rows].rearrange("(k p) t -> p k t", p=128), xTb[i][:], reads=[b_xTb[i]], writes=[scr.b_x1T], owner=b_xTb[i])
            n_mm = 0
            for k in range(8):
                for (xa, b_xa, wa) in ((xTb[i], b_xTb[i], Wrh), (xTl, b_xTl, Wrh), (xTb[i], b_xTb[i], Wrl)):
                    S.op("pe", lambda e: e.matmul(lg_ps[:], lhsT=xa[:, k, :], rhs=wa[:, k, :], start=(n_mm == 0), stop=(n_mm == 23)),
                         reads=[b_xa, b_wr], writes=[b_lgps], inc=(n_mm == 23))
                    n_mm += 1
            S.op("dve", lambda e: e.tensor_tensor(out=lg[:], in0=lg_ps[:], in1=rb[:], op=ALU.add), reads=[b_lgps, b_wr], writes=[b_lg])
            S.op("dve", lambda e: e.max(out=v8[:], in_=lg[:]), reads=[b_lg], writes=[b_v8])
            S.op("dve", lambda e: e.tensor_scalar(out=mk[:], in0=lg[:], scalar1=v8[:, 3:4], scalar2=None, op0=ALU.is_ge), reads=[b_lg, b_v8], writes=[b_mk])
            S.op("dve", lambda e: e.tensor_scalar(out=sm[:, 0:1], in0=v8[:, 0:1], scalar1=-1.0, scalar2=None, op0=ALU.mult), reads=[b_v8], writes=[b_sm])
            S.op("act", lambda e: e.activation(out=lg[:], in_=lg[:], func=AF.Exp, bias=sm[:, 0:1], scale=1.0), reads=[b_lg, b_sm], writes=[b_lg])
            S.op("dve", lambda e: e.tensor_tensor(out=lg[:], in0=lg[:], in1=mk[:], op=ALU.mult), reads=[b_lg, b_mk], writes=[b_lg])
            S.op("dve", lambda e: e.reduce_sum(out=sm[:, 1:2], in_=lg[:], axis=AX.X), reads=[b_lg, b_sm], writes=[b_sm])
            S.op("dve", lambda e: e.reciprocal(out=sm[:, 2:3], in_=sm[:, 1:2]), reads=[b_sm], writes=[b_sm])
            S.op("dve", lambda e: e.tensor_scalar(out=gts[i][:], in0=lg[:], scalar1=sm[:, 2:3], scalar2=None, op0=ALU.mult), reads=[b_lg, b_sm], writes=[b_gts[i]])
            S.dma("sp", scr.rgate[rows, :], gts[i][:], reads=[b_gts[i]], writes=[scr.b_x1T], owner=b_gts[i])
        S.barrier()
    TBm = 512
    with ExitStack() as es:
        sb = lambda n, s, d: es.enter_context(nc.sbuf_tensor(uniq(n), s, d))
        ps = lambda n, s, d: es.enter_context(nc.psum_tensor(uniq(n), s, d))
        Wgu = [sb(f"me_wgu{i}", [128, 8, 2048], BF16) for i in range(2)]
        Wdn = [sb(f"me_wdn{i}", [128, 8, 1024], BF16) for i in range(2)]
        b_Wgu = bufs("me_wgu", 2)
        b_Wdn = bufs("me_wdn", 2)
        bgu = sb("me_bgu", [128, E, 16], F32)
        bdn = sb("me_bdn", [E, 1024], F32)
        bdn16 = sb("me_bdn16", [E, 1024], BF16)
        lng = sb("me_lng", [128, 1024], F32)
        lnb = sb("me_lnb", [128, 1024], F32)
        b_par = Buf("me_par")
        with nc.allow_non_contiguous_dma(reason="tiny param load"):
            for e_ in range(E):
                S.dma("sp", bgu[:, e_, :], prm["exp_b_gate_up"][l][e_].rearrange("(c p) -> p c", p=128), writes=[b_par], owner=b_par)
        S.dma("sp", bdn[:], prm["exp_b_down"][l], writes=[b_par], owner=b_par)
        S.op("dve", lambda e: e.tensor_copy(out=bdn16[:], in_=bdn[:]), reads=[b_par], writes=[b_par])
        S.dma("sp", lng[:], prm["ln2_g"][l].partition_broadcast(128), writes=[b_par], owner=b_par)
        S.dma("sp", lnb[:], prm["ln2_b"][l].partition_broadcast(128), writes=[b_par], owner=b_par)
        xT = [sb(f"me_xT{i}", [128, 8, TBm], BF16) for i in range(2)]
        b_xT = bufs("me_xT", 2)
        gt = [sb(f"me_gt{i}", [128, 4, E], F32) for i in range(2)]
        b_gt = bufs("me_gt", 2)
        gT = sb("me_gT", [E, 4, 128], BF16)
        gtb = sb("me_gtb", [128, 4, E], BF16)
        b_gtb = Buf("me_gtb")
        b_gT = Buf("me_gT")
        acc = sb("me_acc", [128, 4, 1024], F32)
        b_acc = bufs("me_acc", 4)
        act = [sb(f"me_act{i}", [128, 8, TBm], BF16) for i in range(2)]
        b_act = bufs("me_act", 2)
        t1 = [sb(f"me_t1{i}", [128, TBm], F32) for i in range(2)]
        b_t1 = bufs("me_t1", 2)
        t2 = [sb(f"me_t2{i}", [128, TBm], F32) for i in range(2)]
        b_t2 = bufs("me_t2", 2)
        sg = [sb(f"me_sg{i}", [128, TBm], F32) for i in range(2)]
        b_sg = bufs("me_sg", 2)
        g_ps = [ps(f"me_pg{i}", [128, 512], F32) for i in range(2)]
        b_gps = bufs("me_pg", 2)
        l_ps = [ps(f"me_pl{i}", [128, 512], F32) for i in range(2)]
        b_lps = bufs("me_pl", 2)
        y_ps = [ps(f"me_py{i}", [128, 512], F32) for i in range(2)]
        b_yps = bufs("me_py", 2)
        ptg = ps("me_ptg", [E, 4, 128], BF16)
        b_ptg = Buf("me_ptg")
        x1t = [sb(f"me_x1{i}", [128, 1024], F32) for i in range(1)] * 2
        b_x1t = bufs("me_x1", 1) * 2
        xo = [sb(f"me_xo{i}", [128, 1024], F32) for i in range(1)] * 2
        b_xo = bufs("me_xo", 1) * 2
        st = sb("me_stt", [128, 16], F32)
        b_st = Buf("me_stt")
        nw = 0
        nfp = 0
        nyp = 0
        nx1 = 0
        for tb in range(L // TBm):
            bi = tb % 2
            cols = slice(tb * TBm, (tb + 1) * TBm)
            S.dma("sp", xT[bi][:], scr.x1T[:, cols].rearrange("(k p) t -> p k t", p=128), reads=[scr.b_x1T], writes=[b_xT[bi]], owner=b_xT[bi])
            S.dma("sp", gt[bi][:], scr.rgate[cols, :].rearrange("(a p) e -> p a e", p=128), reads=[scr.b_x1T], writes=[b_gt[bi]], owner=b_gt[bi])
            S.op("dve", lambda e: e.tensor_copy(out=gtb[:], in_=gt[bi][:]), reads=[b_gt[bi]], writes=[b_gtb])
            for tt in range(4):
                S.op("pe", lambda e: e.transpose(out=ptg[:, tt, :], in_=gtb[:, tt, :], identity=cx.identb[:]),
                     reads=[b_gtb, cx.b_const], writes=[b_ptg], inc=(tt == 3))
            S.op("act", lambda e: e.copy(out=gT[:], in_=ptg[:]), reads=[b_ptg], writes=[b_gT])
            for tt in range(4):
                for nh in range(2):
                    j = nyp % 2
                    nyp += 1
                    S.op("pe", lambda e: e.matmul(y_ps[j][:], lhsT=gT[:, tt, :], rhs=bdn16[:, nh * 512:(nh + 1) * 512], start=True, stop=True),
                         reads=[b_gT, b_par], writes=[b_yps[j]])
                    S.op("act", lambda e: e.copy(out=acc[:, tt, nh * 512:(nh + 1) * 512], in_=y_ps[j][:]), reads=[b_yps[j]], writes=[b_acc[tt]])
            for e_ in range(E):
                wi = nw % 2
                nw += 1
                S.dma("sp", Wgu[wi][:], scr.wgu16[e_].rearrange("(k p) f -> p k f", p=128), reads=[scr.b_w16], writes=[b_Wgu[wi]], owner=b_Wgu[wi])
                S.dma("sp", Wdn[wi][:], scr.wdn16[e_].rearrange("(k p) f -> p k f", p=128), reads=[scr.b_w16], writes=[b_Wdn[wi]], owner=b_Wdn[wi])
                ai = nw % 2
                for fj in range(8):
                    j = nfp % 2
                    nfp += 1
                    for k in range(8):
                        S.op("pe", lambda e: e.matmul(g_ps[j][:], lhsT=Wgu[wi][:, k, fj * 128:(fj + 1) * 128], rhs=xT[bi][:, k, :], start=(k == 0), stop=(k == 7)),
                             reads=[b_Wgu[wi], b_xT[bi]], writes=[b_gps[j]], inc=(k == 7))
                    for k in range(8):
                        S.op("pe", lambda e: e.matmul(l_ps[j][:], lhsT=Wgu[wi][:, k, 1024 + fj * 128:1024 + (fj + 1) * 128], rhs=xT[bi][:, k, :], start=(k == 0), stop=(k == 7)),
                             reads=[b_Wgu[wi], b_xT[bi]], writes=[b_lps[j]], inc=(k == 7))
                    S.op("dve", lambda e: e.tensor_scalar(out=t1[j][:], in0=g_ps[j][:], scalar1=bgu[:, e_, fj:fj + 1], scalar2=7.0, op0=ALU.add, op1=ALU.min),
                         reads=[b_gps[j], b_par], writes=[b_t1[j]])
                    S.op("act", lambda e: e.activation(out=sg[j][:], in_=t1[j][:], func=AF.Sigmoid, scale=1.702), reads=[b_t1[j]], writes=[b_sg[j]])
                    S.op("dve", lambda e: e.tensor_scalar(out=t2[j][:], in0=l_ps[j][:], scalar1=bgu[:, e_, 8 + fj:9 + fj], scalar2=7.0, op0=ALU.add, op1=ALU.min),
                         reads=[b_lps[j], b_par], writes=[b_t2[j]])
                    S.op("pool", lambda e: e.tensor_scalar(out=t2[j][:], in0=t2[j][:], scalar1=-7.0, scalar2=1.0, op0=ALU.max, op1=ALU.add),
                         reads=[b_t2[j]], writes=[b_t2[j]])
                    S.op("pool", lambda e: e.tensor_tensor(out=t1[j][:], in0=t1[j][:], in1=sg[j][:], op=ALU.mult), reads=[b_t1[j], b_sg[j]], writes=[b_t1[j]])
                    S.op("pool", lambda e: e.tensor_tensor(out=act[ai][:, fj, :], in0=t1[j][:], in1=t2[j][:], op=ALU.mult), reads=[b_t1[j], b_t2[j]], writes=[b_act[ai]])
                for tt in range(4):
                    for nh in range(2):
                        j = nyp % 2
                        nyp += 1
                        for fk in range(8):
                            S.op("pe", lambda e: e.matmul(y_ps[j][:], lhsT=act[ai][:, fk, tt * 128:(tt + 1) * 128], rhs=Wdn[wi][:, fk, nh * 512:(nh + 1) * 512],
                                                          start=(fk == 0), stop=(fk == 7)),
                                 reads=[b_act[ai], b_Wdn[wi]], writes=[b_yps[j]], inc=(fk == 7))
                        S.op("dve", lambda e: e.scalar_tensor_tensor(out=acc[:, tt, nh * 512:(nh + 1) * 512], in0=y_ps[j][:], scalar=gt[bi][:, tt, e_:e_ + 1],
                                                                     in1=acc[:, tt, nh * 512:(nh + 1) * 512], op0=ALU.mult, op1=ALU.add),
                             reads=[b_yps[j], b_gt[bi], b_acc[tt]], writes=[b_acc[tt]])
            for tt in range(4):
                xi = nx1 % 2
                nx1 += 1
                rows = slice(tb * TBm + tt * 128, tb * TBm + (tt + 1) * 128)
                S.dma("sp", x1t[xi][:], scr.x1[rows, :], reads=[scr.b_x1], writes=[b_x1t[xi]], owner=b_x1t[xi])
                S.op("dve", lambda e: e.scalar_tensor_tensor(out=x1t[xi][:], in0=x1t[xi][:], scalar=cfg.alpha, in1=acc[:, tt, :], op0=ALU.mult, op1=ALU.add),
                     reads=[b_x1t[xi], b_acc[tt]], writes=[b_x1t[xi]])
                layer_norm_tile(S, x1t[xi], b_x1t[xi], xo[xi], b_xo[xi], lng, lnb, b_par, st, b_st)
                S.dma("sp", xdst[rows, :], xo[xi][:], reads=[b_xo[xi]], writes=[scr.b_xcur], owner=b_xo[xi])
        S.barrier()


TWO_PI = 2.0 * math.pi


def phase_s5(S, nc, cx, cfg, prm, l, scr):
    L = cfg.L
    NCk = L // 8
    J = int(math.log2(NCk))
    CB = min(512, NCk)
    with ExitStack() as es:
        sb = lambda n, s, d: es.enter_context(nc.sbuf_tensor(uniq(n), s, d))
        ps = lambda n, s, d: es.enter_context(nc.psum_tensor(uniq(n), s, d))
        bP = Buf("s5_par")
        NP = 72

        def T(name, cols, dt=F32):
            return sb("s5_" + name, [128, cols], dt)

        ar, ai, ls = T("ar", NP), T("ai", NP), T("ls", NP)
        with nc.allow_non_contiguous_dma(reason="tiny param load"):
            for d in range(2):
                S.dma("sp", ar[:, d * 36:(d + 1) * 36], prm["s5_a_re"][l][d].rearrange("(p g) n -> (g n) p", g=2), writes=[bP], owner=bP)
                S.dma("sp", ai[:, d * 36:(d + 1) * 36], prm["s5_a_im"][l][d].rearrange("(p g) n -> (g n) p", g=2), writes=[bP], owner=bP)
                for gl in range(2):
                    S.dma("sp", ls[64 * gl:64 * gl + 64, d * 36:(d + 1) * 36],
                          prm["s5_log_step"][l][d].rearrange("(p g) -> g p", g=2)[gl].partition_broadcast(64), writes=[bP], owner=bP)
        BR = sb("s5_BR", [128, 36, 16], F32)
        BI = sb("s5_BI", [128, 36, 16], F32)
        S.dma("sp", BR[:], prm["s5_b_re"][l].rearrange("(p g) n c -> (g n) p c", g=2), writes=[bP], owner=bP)
        S.dma("sp", BI[:], prm["s5_b_im"][l].rearrange("(p g) n c -> (g n) p c", g=2), writes=[bP], owner=bP)
        dpad = sb("s5_dpad", [128, 18], F32)
        S.op("pool", lambda e: e.memset(dpad[:], 0.0), writes=[bP])
        with nc.allow_non_contiguous_dma(reason="tiny param load"):
            for g4 in range(4):
                S.dma("sp", dpad[32 * g4:32 * g4 + 16, :], prm["s5_d"][l].rearrange("(t g c) -> g c t", g=4, c=16)[g4], writes=[bP], owner=bP)

        def dv(fn, eng="dve"):
            S.op(eng, fn, reads=[bP], writes=[bP])

        def tt(out, a, b, op, eng="dve"):
            dv(lambda e: e.tensor_tensor(out=out, in0=a, in1=b, op=op), eng)

        def ts(out, a, s1, op0, s2=None, op1=None):
            if op1 is None:
                dv(lambda e: e.tensor_scalar(out=out, in0=a, scalar1=s1, scalar2=None, op0=op0))
            else:
                dv(lambda e: e.tensor_scalar(out=out, in0=a, scalar1=s1, scalar2=s2, op0=op0, op1=op1))

        def act(out, in_, func, scale=1.0):
            S.op("act", lambda e: e.activation(out=out, in_=in_, func=func, scale=scale), reads=[bP], writes=[bP])

        step, sr, th, mag = T("step", NP), T("sr", NP), T("th", NP), T("mag", NP)
        t0, t1_, t2_, ki = T("t0", NP), T("t1", NP), T("t2", NP), sb("s5_ki", [128, NP], I32)
        sn, cs = T("sn", NP), T("cs", NP)
        act(step[:], ls[:], AF.Exp)
        tt(sr[:], step[:], ar[:], ALU.mult)
        tt(th[:], step[:], ai[:], ALU.mult)
        act(mag[:], sr[:], AF.Exp)

        def sin_of(out, ang):
            ts(t0[:], ang, 1.0 / TWO_PI, ALU.mult, 0.5, ALU.add)
            dv(lambda e: e.tensor_copy(out=ki[:], in_=t0[:]))
            dv(lambda e: e.tensor_copy(out=t1_[:], in_=ki[:]))
            dv(lambda e: e.scalar_tensor_tensor(out=t2_[:], in0=t1_[:], scalar=-TWO_PI, in1=ang, op0=ALU.mult, op1=ALU.add))
            ts(t0[:], t2_[:], -math.pi, ALU.is_lt, TWO_PI, ALU.mult)
            tt(t2_[:], t2_[:], t0[:], ALU.add)
            ts(t0[:], t2_[:], math.pi, ALU.is_gt, -TWO_PI, ALU.mult)
            tt(t2_[:], t2_[:], t0[:], ALU.add)
            act(out, t2_[:], AF.Sin)

        thc = T("thc", NP)
        sin_of(sn[:], th[:])
        ts(thc[:], th[:], math.pi / 2, ALU.add)
        sin_of(cs[:], thc[:])
        abr, abi = T("abr", NP), T("abi", NP)
        tt(abr[:], mag[:], cs[:], ALU.mult)
        tt(abi[:], mag[:], sn[:], ALU.mult)
        den, m1, fr, fi = T("den", NP), T("m1", NP), T("fr", NP), T("fi", NP)
        tt(den[:], ar[:], ar[:], ALU.mult)
        tt(t0[:], ai[:], ai[:], ALU.mult)
        tt(den[:], den[:], t0[:], ALU.add)
        dv(lambda e: e.reciprocal(out=den[:], in_=den[:]))
        ts(m1[:], abr[:], -1.0, ALU.add)
        tt(fr[:], m1[:], ar[:], ALU.mult)
        tt(t0[:], abi[:], ai[:], ALU.mult)
        tt(fr[:], fr[:], t0[:], ALU.add)
        tt(fr[:], fr[:], den[:], ALU.mult)
        tt(fi[:], abi[:], ar[:], ALU.mult)
        tt(t0[:], m1[:], ai[:], ALU.mult)
        tt(fi[:], fi[:], t0[:], ALU.subtract)
        tt(fi[:], fi[:], den[:], ALU.mult)
        ivr, ivi = T("ivr", NP), T("ivi", NP)
        act(t0[:], sr[:], AF.Exp, scale=-2.0)
        tt(ivr[:], abr[:], t0[:], ALU.mult)
        tt(ivi[:], abi[:], t0[:], ALU.mult)
        ts(ivi[:], ivi[:], -1.0, ALU.mult)
        PWr = sb("s5_PWr", [128, 9, NP], F32)
        PWi = sb("s5_PWi", [128, 9, NP], F32)
        NGr = sb("s5_NGr", [128, 9, NP], F32)
        NGi = sb("s5_NGi", [128, 9, NP], F32)
        PWrR = sb("s5_PWrR", [128, 9, NP], F32)
        PWiR = sb("s5_PWiR", [128, 9, NP], F32)
        NGrR = sb("s5_NGrR", [128, 9, NP], F32)
        NGiR = sb("s5_NGiR", [128, 9, NP], F32)

        def cmul(or_, oi_, ar_, ai_, br_, bi_):
            tt(t0[:], ar_, br_, ALU.mult)
            tt(t1_[:], ai_, bi_, ALU.mult)
            tt(t2_[:], ar_, bi_, ALU.mult)
            tt(thc[:], ai_, br_, ALU.mult)
            tt(or_, t0[:], t1_[:], ALU.subtract)
            tt(oi_, t2_[:], thc[:], ALU.add)

        for (Pr, Pi, br_, bi_) in ((PWr, PWi, abr, abi), (NGr, NGi, ivr, ivi)):
            dv(lambda e: e.memset(Pr[:, 0, :], 1.0), "pool")
            dv(lambda e: e.memset(Pi[:, 0, :], 0.0), "pool")
            for e_ in range(1, 9):
                cmul(Pr[:, e_, :], Pi[:, e_, :], Pr[:, e_ - 1, :], Pi[:, e_ - 1, :], br_[:], bi_[:])
        for (Pr, PrR) in ((PWr, PWrR), (PWi, PWiR), (NGr, NGrR), (NGi, NGiR)):
            for e_ in range(9):
                dv(lambda e: e.tensor_copy(out=PrR[:, e_, :], in_=Pr[:, 8 - e_, :]), "pool")
        SPr = sb("s5_SPr", [128, J + 1, NP], F32)
        SPi = sb("s5_SPi", [128, J + 1, NP], F32)
        SPn = sb("s5_SPn", [128, J + 1, NP], F32)
        dv(lambda e: e.tensor_copy(out=SPr[:, 0, :], in_=PWr[:, 8, :]))
        dv(lambda e: e.tensor_copy(out=SPi[:, 0, :], in_=PWi[:, 8, :]))
        for j in range(1, J + 1):
            cmul(SPr[:, j, :], SPi[:, j, :], SPr[:, j - 1, :], SPi[:, j - 1, :], SPr[:, j - 1, :], SPi[:, j - 1, :])
        ts(SPn[:], SPi[:], -1.0, ALU.mult)
        bbr = sb("s5_bbr", [128, 2, 36, 16], F32)
        bbi = sb("s5_bbi", [128, 2, 36, 16], F32)
        tb1 = sb("s5_tb1", [128, 2, 36, 16], F32)
        frv = fr[:].rearrange("p (d q) -> p d q", d=2).unsqueeze(3).to_broadcast([128, 2, 36, 16])
        fiv = fi[:].rearrange("p (d q) -> p d q", d=2).unsqueeze(3).to_broadcast([128, 2, 36, 16])
        BRv = BR[:].unsqueeze(1).to_broadcast([128, 2, 36, 16])
        BIv = BI[:].unsqueeze(1).to_broadcast([128, 2, 36, 16])
        tt(bbr[:], frv, BRv, ALU.mult)
        tt(tb1[:], fiv, BIv, ALU.mult)
        tt(bbr[:], bbr[:], tb1[:], ALU.subtract)
        tt(bbi[:], frv, BIv, ALU.mult)
        tt(tb1[:], fiv, BRv, ALU.mult)
        tt(bbi[:], bbi[:], tb1[:], ALU.add)
        CRT = sb("s5_CRT", [128, 2, 36, 16], F32)
        CIT = sb("s5_CIT", [128, 2, 36, 16], F32)
        cin = sb("s5_cin", [128, 128], F32)
        cinb = sb("s5_cinb", [128, 128], BF16)
        pc = ps("s5_pc", [128, 128], BF16)
        b_pc = Buf("s5_pc")
        for (cname, CT_) in (("s5_c_re", CRT), ("s5_c_im", CIT)):
            for d in range(2):
                for pb in range(0, 36, 8):
                    npair = min(8, 36 - pb)
                    for q in range(npair):
                        pr_ = pb + q
                        S.dma("sp", cin[16 * q:16 * q + 16, :].rearrange("c (g n) -> c g n", g=2),
                              prm[cname][l][d][2 * pr_:2 * pr_ + 2].rearrange("g c n -> c g n"), writes=[bP], owner=bP)
                    dv(lambda e: e.tensor_copy(out=cinb[:], in_=cin[:]))
                    S.op("pe", lambda e: e.transpose(out=pc[:], in_=cinb[:], identity=cx.identb[:]), reads=[bP, cx.b_const], writes=[b_pc])
                    S.op("act", lambda e: e.copy(out=CT_[:, d, pb:pb + npair, :], in_=pc[:, 0:npair * 16].rearrange("p (q c) -> p q c", c=16)),
                         reads=[b_pc], writes=[bP])

        Uraw = sb("s5_Uraw", [128, L], BF16)
        b_Ur = Buf("s5_Ur")
        Us = sb("s5_Us", [128, 8, NCk], BF16)
        b_U = Buf("s5_U")
        Wi = sb("s5_Wi", [128, 8, 8, 128], BF16)
        b_Wi = Buf("s5_Wi")
        WOre = [sb(f"s5_WOre{i}", [128, 8, 128], BF16) for i in range(4)]
        WOim = [sb(f"s5_WOim{i}", [128, 8, 128], BF16) for i in range(4)]
        b_WO = bufs("s5_WO", 4)
        Lre = sb("s5_Lre", [128, 8, 128], BF16)
        Lim = sb("s5_Lim", [128, 8, 128], BF16)
        b_L = Buf("s5_L")
        LTre = sb("s5_LTre", [128, 8, 128], BF16)
        LTim = sb("s5_LTim", [128, 8, 128], BF16)
        b_LT = Buf("s5_LT")
        Rr = sb("s5_Rr", [128, 8, 16], F32)
        Ri = sb("s5_Ri", [128, 8, 16], F32)
        Rt = sb("s5_Rt", [128, 8, 16], F32)
        b_R = Buf("s5_R")
        ZR = [sb(f"s5_ZR{i}", [128, NCk], F32) for i in range(2)]
        ZI = [sb(f"s5_ZI{i}", [128, NCk], F32) for i in range(2)]
        b_Z = bufs("s5_Z", 2)
        ztmp = sb("s5_ztmp", [128, NCk], F32)
        b_ztmp = Buf("s5_ztmp")
        HR = [sb(f"s5_HR{i}", [128, NCk], BF16) for i in range(4)]
        HI = [sb(f"s5_HI{i}", [128, NCk], BF16) for i in range(4)]
        b_H = bufs("s5_H", 4)
        pt4 = ps("s5_pt4", [128, 4, 128], BF16)
        b_pt4 = Buf("s5_pt4")
        pK = [ps(f"s5_pK{i}", [128, 4, 128], F32) for i in range(2)]
        b_pK = bufs("s5_pK", 2)
        pS = [ps(f"s5_pS{i}", [128, CB], F32) for i in range(2)]
        b_pS = bufs("s5_pS", 2)
        pY = [ps(f"s5_pY{i}", [128, CB], F32) for i in range(2)]
        b_pY = bufs("s5_pY", 2)
        Dd = sb("s5_Dd", [128, 128], F32)
        b_Dd = Buf("s5_Dd")
        g1 = [sb(f"s5_g1{i}", [128, CB], F32) for i in range(2)]
        g2 = [sb(f"s5_g2{i}", [128, CB], F32) for i in range(2)]
        b_g = bufs("s5_g", 2)
        Yo = Uraw[:].rearrange("p (c t) -> p c t", t=8)
        b_Yo = b_Ur
        npk = 0
        npy = 0
        for tl in range(18):
            S.dma("sp", Uraw[:], scr.uT[tl * 128:(tl + 1) * 128, :], reads=[scr.b_proj], writes=[b_Ur], owner=b_Ur)
            S.op("pool", lambda e: e.tensor_copy(out=Us[:], in_=Uraw[:].rearrange("p (c s) -> p s c", s=8)), reads=[b_Ur], writes=[b_U])
            S.op("pool", lambda e: e.memset(Wi[:].rearrange("p s t c -> p (s t c)"), 0.0), writes=[b_Wi])
            S.op("dve", lambda e: e.tensor_scalar(out=Dd[:], in0=cx.identf[:], scalar1=dpad[:, tl:tl + 1], scalar2=None, op0=ALU.mult),
                 reads=[cx.b_const, bP], writes=[b_Dd])
            for pp in range(2):
                pr_ = 2 * tl + pp
                c0 = 64 * pp
                for d in range(2):
                    k = pp * 2 + d
                    dp = d * 36 + pr_
                    PrT, PiT = (PWr, PWi) if d == 0 else (PWrR, PWiR)
                    NrT, NiT = (NGr, NGi) if d == 0 else (NGrR, NGiR)
                    esl = slice(1, 9) if d == 0 else slice(0, 8)
                    pwr = PrT[:, esl, dp].unsqueeze(2).to_broadcast([128, 8, 16])
                    pwi = PiT[:, esl, dp].unsqueeze(2).to_broadcast([128, 8, 16])
                    ngr = NrT[:, esl, dp].unsqueeze(2).to_broadcast([128, 8, 16])
                    ngi = NiT[:, esl, dp].unsqueeze(2).to_broadcast([128, 8, 16])
                    crt = CRT[:, d, pr_, :].unsqueeze(1).to_broadcast([128, 8, 16])
                    cit = CIT[:, d, pr_, :].unsqueeze(1).to_broadcast([128, 8, 16])
                    bbrv = bbr[:, d, pr_, :].unsqueeze(1).to_broadcast([128, 8, 16])
                    bbiv = bbi[:, d, pr_, :].unsqueeze(1).to_broadcast([128, 8, 16])

                    def cplx(ar_, ai_, br_, bi_):
                        S.op("dve", lambda e: e.tensor_tensor(out=Rr[:], in0=ar_, in1=br_, op=ALU.mult), reads=[bP], writes=[b_R])
                        S.op("dve", lambda e: e.tensor_tensor(out=Rt[:], in0=ai_, in1=bi_, op=ALU.mult), reads=[bP, b_R], writes=[b_R])
                        S.op("dve", lambda e: e.tensor_tensor(out=Rr[:], in0=Rr[:], in1=Rt[:], op=ALU.subtract), reads=[b_R], writes=[b_R])
                        S.op("dve", lambda e: e.tensor_tensor(out=Ri[:], in0=ar_, in1=bi_, op=ALU.mult), reads=[bP, b_R], writes=[b_R])
                        S.op("dve", lambda e: e.tensor_tensor(out=Rt[:], in0=ai_, in1=br_, op=ALU.mult), reads=[bP, b_R], writes=[b_R])
                        S.op("dve", lambda e: e.tensor_tensor(out=Ri[:], in0=Ri[:], in1=Rt[:], op=ALU.add), reads=[b_R], writes=[b_R])

                    cplx(crt, cit, pwr, pwi)
                    S.op("pool", lambda e: e.memset(WOre[k][:].rearrange("p t c -> p (t c)"), 0.0), writes=[b_WO[k]])
                    S.op("pool", lambda e: e.memset(WOim[k][:].rearrange("p t c -> p (t c)"), 0.0), writes=[b_WO[k]])
                    for gl in range(2):
                        prt = slice(64 * gl, 64 * gl + 64)
                        cl = slice(c0 + 32 * gl, c0 + 32 * gl + 16)
                        S.op("act", lambda e: e.copy(out=WOre[k][prt, :, cl], in_=Rr[prt, :, :]), reads=[b_R], writes=[b_WO[k]])
                        S.op("act", lambda e: e.activation(out=WOim[k][prt, :, cl], in_=Ri[prt, :, :], func=AF.Copy, scale=-1.0), reads=[b_R], writes=[b_WO[k]])
                    cplx(ngr, ngi, bbrv, bbiv)
                    S.op("pool", lambda e: e.memset(Lre[:].rearrange("p t c -> p (t c)"), 0.0), writes=[b_L])
                    S.op("pool", lambda e: e.memset(Lim[:].rearrange("p t c -> p (t c)"), 0.0), writes=[b_L])
                    for gl in range(2):
                        prt = slice(64 * gl, 64 * gl + 64)
                        cl = slice(c0 + 32 * gl, c0 + 32 * gl + 16)
                        S.op("act", lambda e: e.copy(out=Lre[prt, :, cl], in_=Rr[prt, :, :]), reads=[b_R], writes=[b_L])
                        S.op("act", lambda e: e.copy(out=Lim[prt, :, cl], in_=Ri[prt, :, :]), reads=[b_R], writes=[b_L])
                    for (Lx, LTx) in ((Lre, LTre), (Lim, LTim)):
                        for s4 in range(2):
                            for ss in range(4):
                                S.op("pe", lambda e: e.transpose(out=pt4[:, ss, :], in_=Lx[:, s4 * 4 + ss, :], identity=cx.identb[:]),
                                     reads=[b_L, cx.b_const], writes=[b_pt4], inc=(ss == 3))
                            S.op("act", lambda e: e.copy(out=LTx[:, s4 * 4:(s4 + 1) * 4, :], in_=pt4[:]), reads=[b_pt4], writes=[b_LT])
                    for s_ in range(8):
                        for th_ in range(2):
                            j = npk % 2
                            npk += 1
                            tsl = slice(th_ * 4, th_ * 4 + 4)
                            S.op("pe", lambda e: e.matmul(pK[j][:].rearrange("p t c -> p (t c)"), lhsT=Lre[:, s_, :],
                                                          rhs=WOre[k][:, tsl, :].rearrange("p t c -> p (t c)"), start=True, stop=False),
                                 reads=[b_L, b_WO[k]], writes=[b_pK[j]], inc=False)
                            S.op("pe", lambda e: e.matmul(pK[j][:].rearrange("p t c -> p (t c)"), lhsT=Lim[:, s_, :],
                                                          rhs=WOim[k][:, tsl, :].rearrange("p t c -> p (t c)"), start=False, stop=True),
                                 reads=[b_L, b_WO[k]], writes=[b_pK[j]])
                            for tq in range(4):
                                t_ = th_ * 4 + tq
                                use = (t_ >= s_) if d == 0 else (t_ <= s_)
                                if not use:
                                    continue
                                S.op("dve", lambda e: e.tensor_tensor(out=Wi[:, s_, t_, :], in0=pK[j][:, tq, :], in1=Wi[:, s_, t_, :], op=ALU.add),
                                     reads=[b_pK[j], b_Wi], writes=[b_Wi])
                    zi = 0
                    for cb in range(NCk // CB):
                        csl = slice(cb * CB, (cb + 1) * CB)
                        for s_ in range(8):
                            S.op("pe", lambda e: e.matmul(pS[0][:], lhsT=LTre[:, s_, :], rhs=Us[:, s_, csl], start=(s_ == 0), stop=(s_ == 7)),
                                 reads=[b_LT, b_U], writes=[b_pS[0]], inc=(s_ == 7))
                        for s_ in range(8):
                            S.op("pe", lambda e: e.matmul(pS[1][:], lhsT=LTim[:, s_, :], rhs=Us[:, s_, csl], start=(s_ == 0), stop=(s_ == 7)),
                                 reads=[b_LT, b_U], writes=[b_pS[1]], inc=(s_ == 7))
                        p8r, p8i, p8n = SPr[:, 0, dp:dp + 1], SPi[:, 0, dp:dp + 1], SPn[:, 0, dp:dp + 1]
                        S.op("dve", lambda e: e.tensor_scalar(out=ztmp[:, csl], in0=pS[1][:], scalar1=p8n, scalar2=None, op0=ALU.mult),
                             reads=[b_pS[1], bP], writes=[b_ztmp])
                        S.op("dve", lambda e: e.scalar_tensor_tensor(out=ZR[0][:, csl], in0=pS[0][:], scalar=p8r, in1=ztmp[:, csl], op0=ALU.mult, op1=ALU.add),
                             reads=[b_pS[0], b_ztmp, bP], writes=[b_Z[0]])
                        S.op("dve", lambda e: e.tensor_scalar(out=ztmp[:, csl], in0=pS[1][:], scalar1=p8r, scalar2=None, op0=ALU.mult),
                             reads=[b_pS[1], bP, b_Z[0]], writes=[b_ztmp])
                        S.op("dve", lambda e: e.scalar_tensor_tensor(out=ZI[0][:, csl], in0=pS[0][:], scalar=p8i, in1=ztmp[:, csl], op0=ALU.mult, op1=ALU.add),
                             reads=[b_pS[0], b_ztmp, bP], writes=[b_Z[0]])
                    cur = 0
                    for j in range(J):
                        sh = 1 << j
                        nx = 1 - cur
                        qr, qi, qn = SPr[:, j, dp:dp + 1], SPi[:, j, dp:dp + 1], SPn[:, j, dp:dp + 1]
                        if d == 0:
                            dst, src, keep = slice(sh, NCk), slice(0, NCk - sh), slice(0, sh)
                        else:
                            dst, src, keep = slice(0, NCk - sh), slice(sh, NCk), slice(NCk - sh, NCk)
                        S.op("act", lambda e: e.copy(out=ZR[nx][:, keep], in_=ZR[cur][:, keep]), reads=[b_Z[cur]], writes=[b_Z[nx]])
                        S.op("act", lambda e: e.copy(out=ZI[nx][:, keep], in_=ZI[cur][:, keep]), reads=[b_Z[cur]], writes=[b_Z[nx]])
                        S.op("dve", lambda e: e.scalar_tensor_tensor(out=ztmp[:, dst], in0=ZR[cur][:, src], scalar=qr, in1=ZR[cur][:, dst], op0=ALU.mult, op1=ALU.add),
                             reads=[b_Z[cur], bP], writes=[b_ztmp])
                        S.op("dve", lambda e: e.scalar_tensor_tensor(out=ZR[nx][:, dst], in0=ZI[cur][:, src], scalar=qn, in1=ztmp[:, dst], op0=ALU.mult, op1=ALU.add),
                             reads=[b_Z[cur], b_ztmp, bP], writes=[b_Z[nx]])
                        S.op("dve", lambda e: e.scalar_tensor_tensor(out=ztmp[:, dst], in0=ZI[cur][:, src], scalar=qr, in1=ZI[cur][:, dst], op0=ALU.mult, op1=ALU.add),
                             reads=[b_Z[cur], bP, b_Z[nx]], writes=[b_ztmp])
                        S.op("dve", lambda e: e.scalar_tensor_tensor(out=ZI[nx][:, dst], in0=ZR[cur][:, src], scalar=qi, in1=ztmp[:, dst], op0=ALU.mult, op1=ALU.add),
                             reads=[b_Z[cur], b_ztmp, bP], writes=[b_Z[nx]])
                        cur = nx
                    if d == 0:
                        S.op("pool", lambda e: e.memset(HR[k][:, 0:1], 0.0), writes=[b_H[k]])
                        S.op("pool", lambda e: e.memset(HI[k][:, 0:1], 0.0), writes=[b_H[k]])
                        S.op("act", lambda e: e.copy(out=HR[k][:, 1:NCk], in_=ZR[cur][:, 0:NCk - 1]), reads=[b_Z[cur]], writes=[b_H[k]])
                        S.op("act", lambda e: e.copy(out=HI[k][:, 1:NCk], in_=ZI[cur][:, 0:NCk - 1]), reads=[b_Z[cur]], writes=[b_H[k]])
                    else:
                        S.op("pool", lambda e: e.memset(HR[k][:, NCk - 1:NCk], 0.0), writes=[b_H[k]])
                        S.op("pool", lambda e: e.memset(HI[k][:, NCk - 1:NCk], 0.0), writes=[b_H[k]])
                        S.op("act", lambda e: e.copy(out=HR[k][:, 0:NCk - 1], in_=ZR[cur][:, 1:NCk]), reads=[b_Z[cur]], writes=[b_H[k]])
                        S.op("act", lambda e: e.copy(out=HI[k][:, 0:NCk - 1], in_=ZI[cur][:, 1:NCk]), reads=[b_Z[cur]], writes=[b_H[k]])
            for s_ in range(8):
                S.op("dve", lambda e: e.tensor_tensor(out=Wi[:, s_, s_, :], in0=Wi[:, s_, s_, :], in1=Dd[:], op=ALU.add), reads=[b_Wi, b_Dd], writes=[b_Wi])
            for cb in range(NCk // CB):
                csl = slice(cb * CB, (cb + 1) * CB)
                for t_ in range(8):
                    j = npy % 2
                    npy += 1
                    for s_ in range(8):
                        S.op("pe", lambda e: e.matmul(pY[j][:], lhsT=Wi[:, s_, t_, :], rhs=Us[:, s_, csl], start=(s_ == 0), stop=False),
                             reads=[b_Wi, b_U], writes=[b_pY[j]], inc=False)
                    for k in range(4):
                        S.op("pe", lambda e: e.matmul(pY[j][:], lhsT=WOre[k][:, t_, :], rhs=HR[k][:, csl], start=False, stop=False),
                             reads=[b_WO[k], b_H[k]], writes=[b_pY[j]], inc=False)
                        S.op("pe", lambda e: e.matmul(pY[j][:], lhsT=WOim[k][:, t_, :], rhs=HI[k][:, csl], start=False, stop=(k == 3)),
                             reads=[b_WO[k], b_H[k]], writes=[b_pY[j]], inc=(k == 3))
                    S.op("act", lambda e: e.activation(out=g1[j][:], in_=pY[j][:], func=AF.Square), reads=[b_pY[j]], writes=[b_g[j]])
                    S.op("dve", lambda e: e.tensor_scalar(out=g1[j][:], in0=g1[j][:], scalar1=0.044715, scalar2=1.0, op0=ALU.mult, op1=ALU.add),
                         reads=[b_g[j]], writes=[b_g[j]])
                    S.op("dve", lambda e: e.tensor_tensor(out=g1[j][:], in0=pY[j][:], in1=g1[j][:], op=ALU.mult), reads=[b_pY[j], b_g[j]], writes=[b_g[j]])
                    S.op("act", lambda e: e.activation(out=g2[j][:], in_=g1[j][:], func=AF.Sigmoid, scale=1.5957691216057308), reads=[b_g[j]], writes=[b_g[j]])
                    S.op("dve", lambda e: e.tensor_tensor(out=Yo[:, csl, t_], in0=pY[j][:], in1=g2[j][:], op=ALU.mult), reads=[b_pY[j], b_g[j], b_Yo], writes=[b_Yo])
            S.dma("sp", scr.hs5T[tl * 128:(tl + 1) * 128, :], Uraw[:], reads=[b_Yo], writes=[scr.b_s5], owner=b_Yo)
        S.barrier()
```

```python
import math
from contextlib import ExitStack
import numpy as np
import concourse.bass as bass
import concourse.mybir as mybir
from concourse.bass_utils import run_bass_kernel_spmd

F32 = mybir.dt.float32
BF16 = mybir.dt.bfloat16
I32 = mybir.dt.int32
AF = mybir.ActivationFunctionType
ALU = mybir.AluOpType
AX = mybir.AxisListType


class Cfg:
    def __init__(self, L=8192, depth=4, n_exp=32):
        self.L = L
        self.depth = depth
        self.E = n_exp
        self.D = 1024
        self.alpha = (2 * 4) ** 0.25


class Buf:
    __slots__ = ("name", "writers", "readers", "dsem", "multi")

    def __init__(self, name, multi=False):
        self.name = name
        self.writers = {}
        self.readers = {}
        self.dsem = None
        self.multi = multi


class Sched:
    def __init__(self, nc):
        self.nc = nc
        self.eng = {"pe": nc.tensor, "dve": nc.vector, "act": nc.scalar,
                    "pool": nc.gpsimd, "sp": nc.sync}
        self.sems = []
        self.esem = {}
        self.cnt = {}
        self.latest = {}
        for k in self.eng:
            self.esem[k] = self._newsem("e_" + k)
            self.cnt[k] = 0
        self.known = {k: {} for k in self.eng}
        self.n_inst = 0
        self.n_wait = 0
        self.dsem_pool = {}
        self.free_dsems = []
        self.epoch_owners = []

    def _newsem(self, name):
        h = self.nc.alloc_semaphore(name=name)
        self.sems.append(h)
        return len(self.sems) - 1

    def _wait(self, e, deps):
        kn = self.known[e]
        for s, c in deps.items():
            if s == self.esem[e] and c > self.cnt[e]:
                continue
            if kn.get(s, 0) < c:
                self.eng[e].wait_ge(self.sems[s], c)
                kn[s] = c
                self.n_wait += 1

    @staticmethod
    def _merge(d, src):
        for s, c in src.items():
            if d.get(s, 0) < c:
                d[s] = c

    def _deps(self, reads, writes):
        deps = {}
        for b in reads:
            self._merge(deps, b.writers)
        for b in writes:
            if b.multi:
                continue
            self._merge(deps, b.writers)
            self._merge(deps, b.readers)
        return deps

    def _track(self, s, c, reads, writes):
        for b in writes:
            if b.multi:
                if b.writers.get(s, 0) < c:
                    b.writers[s] = c
                continue
            b.writers = {s: c}
            b.readers = {}
        for b in reads:
            if b.readers.get(s, 0) < c:
                b.readers[s] = c

    def op(self, e, fn, reads=(), writes=(), inc=True):
        self._wait(e, self._deps(reads, writes))
        ins = fn(self.eng[e])
        self.n_inst += 1
        s = self.esem[e]
        if inc:
            self.cnt[e] += 1
            ins.then_inc(self.sems[s], 1)
            c = self.cnt[e]
            self.latest[s] = c
        else:
            c = self.cnt[e] + 1
        self._track(s, c, reads, writes)
        return ins

    def dma(self, q, out, in_, reads=(), writes=(), owner=None, **kw):
        self._wait(q, self._deps(reads, writes))
        if owner.dsem is None:
            if owner.name not in self.dsem_pool:
                if self.free_dsems:
                    self.dsem_pool[owner.name] = self.free_dsems.pop()
                else:
                    self.dsem_pool[owner.name] = [self._newsem("d%d" % len(self.sems)), 0]
            owner.dsem = self.dsem_pool[owner.name]
            self.epoch_owners.append(owner)
        ins = self.eng[q].dma_start(out=out, in_=in_, **kw)
        owner.dsem[1] += 16
        s, c = owner.dsem[0], owner.dsem[1]
        ins.then_inc(self.sems[s], 16)
        self.latest[s] = c
        self.n_inst += 1
        self._track(s, c, reads, writes)
        return ins

    def barrier(self):
        for e in self.eng:
            self._wait(e, dict(self.latest))
        keep = {k: v for k, v in self.dsem_pool.items() if k in ("inj", "dbg")}
        self.free_dsems.extend(v for k, v in self.dsem_pool.items() if k not in keep)
        self.dsem_pool = keep
        for b in self.epoch_owners:
            b.dsem = None
        self.epoch_owners = []


def dram_copy(S, dst, src, b):
    n = dst.shape[0]
    step = 128 if n >= 128 else n
    for r in range(0, n, step):
        S.dma("sp", dst[r:r + step, :], src[r:r + step, :], writes=[b], owner=b)


_UNIQ = [0]


def uniq(name):
    _UNIQ[0] += 1
    return "%s_%d" % (name, _UNIQ[0])


def bufs(prefix, n):
    return [Buf(f"{prefix}{i}") for i in range(n)]


D = 1024
SSD_INNER = 2048
SSD_HEADS = 32
SSD_GROUPS = 8
SSD_STATE = 128
CONV_CH = 4096
ATT_W = 1152
S5_W = 1152
S5_G = 72
N_IN = 13888
C_GATE, C_Z, C_XBC, C_DT, C_Q, C_K, C_V, C_U = 0, 3072, 5120, 9216, 9280, 10432, 11584, 12736
LN_EPS = 1e-5


class Ctx:
    pass


def make_consts(S, nc, cx):
    cx.identf = nc.alloc_sbuf_tensor("identf", [128, 128], F32)
    cx.identb = nc.alloc_sbuf_tensor("identb", [128, 128], BF16)
    cx.b_const = Buf("const")
    b = cx.b_const
    S.op("pool", lambda e: e.memset(cx.identf[:], 0.0), writes=[b])
    S.op("pool", lambda e: e.affine_select(out=cx.identf[:], in_=cx.identf[:], pattern=[[-1, 128]], base=0,
                                           channel_multiplier=1, compare_op=ALU.not_equal, fill=1.0),
         reads=[b], writes=[b])
    S.op("dve", lambda e: e.tensor_copy(out=cx.identb[:], in_=cx.identf[:]), reads=[b], writes=[b])


def load_xT(S, nc, cx, x_ap, t0, ntok, xT, b_xT, xs, b_xs, xb, b_xb, pt, b_pt, ctr):
    for t in range(ntok // 128):
        i = ctr[0] % 2
        ctr[0] += 1
        S.dma("sp", xs[i][:], x_ap[t0 + t * 128:t0 + (t + 1) * 128, :], writes=[b_xs[i]], owner=b_xs[i])
        S.op("dve", lambda e: e.tensor_copy(out=xb[i][:], in_=xs[i][:]), reads=[b_xs[i]], writes=[b_xb[i]])
        for k4 in range(2):
            j = ctr[1] % 2
            ctr[1] += 1
            for kk in range(4):
                k = k4 * 4 + kk
                S.op("pe", lambda e: e.transpose(out=pt[j][:, kk, :], in_=xb[i][:, k * 128:(k + 1) * 128],
                                                 identity=cx.identb[:]),
                     reads=[b_xb[i], cx.b_const], writes=[b_pt[j]], inc=(kk == 3))
            S.op("act", lambda e: e.copy(out=xT[:, k4 * 4:(k4 + 1) * 4, t * 128:(t + 1) * 128], in_=pt[j][:]),
                 reads=[b_pt[j]], writes=[b_xT])


def phase_inproj(S, nc, cx, cfg, w_in_l, x_ap, scr):
    L = cfg.L
    TB = min(2048, L)
    secs = [
        (C_GATE, 3072, "tok", scr.gates, F32, 512),
        (C_Z, 2048, "tok", scr.z, F32, 512),
        (C_XBC, 4096, "feat", scr.xbcT, F32, 512),
        (C_DT, 64, "tok", scr.dt, F32, 64),
        (C_Q, 1152, "feat", scr.qT, BF16, 384),
        (C_K, 1152, "feat", scr.kT, BF16, 384),
        (C_V, 1152, "tok", scr.v, BF16, 384),
        (C_U, 1152, "featpad", scr.uT, BF16, 128),
    ]
    with ExitStack() as es:
        sb = lambda n, s, d: es.enter_context(nc.sbuf_tensor(uniq(n), s, d))
        ps = lambda n, s, d: es.enter_context(nc.psum_tensor(uniq(n), s, d))
        xT = sb("ip_xT", [128, 8, TB], BF16)
        b_xT = Buf("ip_xT")
        xs = [sb(f"ip_xs{i}", [128, D], F32) for i in range(2)]
        b_xs = bufs("ip_xs", 2)
        xb = [sb(f"ip_xb{i}", [128, D], BF16) for i in range(2)]
        b_xb = bufs("ip_xb", 2)
        pt = [ps(f"ip_pt{i}", [128, 4, 128], BF16) for i in range(2)]
        b_pt = bufs("ip_pt", 2)
        wst = [sb(f"ip_wst{i}", [128, 8, 512], F32) for i in range(2)]
        b_wst = bufs("ip_wst", 2)
        wb = [sb(f"ip_wb{i}", [128, 8, 512], BF16) for i in range(2)]
        b_wb = bufs("ip_wb", 2)
        po = [ps(f"ip_po{i}", [128, 512], F32) for i in range(4)]
        b_po = bufs("ip_po", 4)
        ot = [sb(f"ip_ot{i}", [128, 512], F32) for i in range(3)]
        otb = [sb(f"ip_otb{i}", [128, 512], BF16) for i in range(3)]
        b_ot = bufs("ip_ot", 3)
        ctr = [0, 0]
        nw = 0
        npo = 0
        no = 0
        for t0 in range(0, L, TB):
            load_xT(S, nc, cx, x_ap, t0, TB, xT, b_xT, xs, b_xs, xb, b_xb, pt, b_pt, ctr)
            for (c0, ncols, kind, dest, dt, cb) in secs:
                for cc in range(0, ncols, cb):
                    wi = nw % 2
                    nw += 1
                    wsrc = w_in_l[:, c0 + cc:c0 + cc + cb].rearrange("(k p) c -> p k c", p=128)
                    S.dma("sp", wst[wi][:, :, 0:cb], wsrc, writes=[b_wst[wi]], owner=b_wst[wi])
                    if kind == "featpad":
                        ng = cb // 16
                        S.op("pool", lambda e: e.memset(wb[wi][:], 0.0), writes=[b_wb[wi]])
                        S.op("pool", lambda e: e.tensor_copy(
                            out=wb[wi][:, :, 0:ng * 32].rearrange("p k (g c) -> p k g c", c=32)[:, :, :, 0:16],
                            in_=wst[wi][:, :, 0:cb].rearrange("p k (g c) -> p k g c", c=16)),
                            reads=[b_wst[wi]], writes=[b_wb[wi]])
                        wcols = ng * 32
                    else:
                        S.op("pool", lambda e: e.tensor_copy(out=wb[wi][:, :, 0:cb], in_=wst[wi][:, :, 0:cb]),
                             reads=[b_wst[wi]], writes=[b_wb[wi]])
                        wcols = cb
                    if kind == "tok":
                        for t in range(TB // 128):
                            pj = npo % 4
                            npo += 1
                            for k in range(8):
                                S.op("pe", lambda e: e.matmul(po[pj][:, 0:cb], lhsT=xT[:, k, t * 128:(t + 1) * 128],
                                                              rhs=wb[wi][:, k, 0:cb], start=(k == 0), stop=(k == 7)),
                                     reads=[b_xT, b_wb[wi]], writes=[b_po[pj]], inc=(k == 7))
                            oi = no % 3
                            no += 1
                            o_t = ot[oi] if dt == F32 else otb[oi]
                            ev = "act" if no % 2 else "dve"
                            if ev == "act":
                                S.op("act", lambda e: e.copy(out=o_t[:, 0:cb], in_=po[pj][:, 0:cb]),
                                     reads=[b_po[pj]], writes=[b_ot[oi]])
                            else:
                                S.op("dve", lambda e: e.tensor_copy(out=o_t[:, 0:cb], in_=po[pj][:, 0:cb]),
                                     reads=[b_po[pj]], writes=[b_ot[oi]])
                            S.dma("sp", dest[t0 + t * 128:t0 + (t + 1) * 128, cc:cc + cb], o_t[:, 0:cb],
                                  reads=[b_ot[oi]], writes=[scr.b_proj], owner=b_ot[oi])
                    else:
                        if kind == "featpad":
                            r0 = (cc // 16) * 32
                        else:
                            r0 = cc
                        for ts in range(TB // 512):
                            for ch in range(wcols // 128):
                                pj = npo % 4
                                npo += 1
                                for k in range(8):
                                    S.op("pe", lambda e: e.matmul(po[pj][:], lhsT=wb[wi][:, k, ch * 128:(ch + 1) * 128],
                                                                  rhs=xT[:, k, ts * 512:(ts + 1) * 512],
                                                                  start=(k == 0), stop=(k == 7)),
                                         reads=[b_xT, b_wb[wi]], writes=[b_po[pj]], inc=(k == 7))
                                oi = no % 3
                                no += 1
                                o_t = ot[oi] if dt == F32 else otb[oi]
                                if no % 2:
                                    S.op("act", lambda e: e.copy(out=o_t[:], in_=po[pj][:]),
                                         reads=[b_po[pj]], writes=[b_ot[oi]])
                                else:
                                    S.op("dve", lambda e: e.tensor_copy(out=o_t[:], in_=po[pj][:]),
                                         reads=[b_po[pj]], writes=[b_ot[oi]])
                                S.dma("sp", dest[r0 + ch * 128:r0 + (ch + 1) * 128, t0 + ts * 512:t0 + (ts + 1) * 512],
                                      o_t[:], reads=[b_ot[oi]], writes=[scr.b_proj], owner=b_ot[oi])
        S.barrier()


def alloc_scratch(nc, cfg):
    L = cfg.L
    scr = Ctx()
    dr = lambda n, s, d: nc.dram_tensor(n, s, d, kind="Internal").ap()
    scr.gates = dr("s_gates", [L, 3072], F32)
    scr.z = dr("s_z", [L, 2048], F32)
    scr.xbcT = dr("s_xbcT", [4096, L], F32)
    scr.dt = dr("s_dt", [L, 64], F32)
    scr.qT = dr("s_qT", [1152, L], BF16)
    scr.kT = dr("s_kT", [1152, L], BF16)
    scr.v = dr("s_v", [L, 1152], BF16)
    scr.uT = dr("s_uT", [2304, L], BF16)
    scr.b_proj = Buf("proj", multi=True)
    scr.BT = dr("s_BT", [1024, L], BF16)
    scr.CT = dr("s_CT", [1024, L], BF16)
    scr.xsB = dr("s_xsB", [L, 3072], BF16)
    scr.b_conv = Buf("conv", multi=True)
    scr.Sb = dr("s_Sb", [L // 128, 8, 128, 256], F32)
    scr.ypart = dr("s_ypart", [L, 2048], F32)
    scr.b_ssd = Buf("ssd", multi=True)
    scr.yssd = dr("s_yssd", [L, 2048], BF16)
    scr.b_mix = Buf("mix", multi=True)
    scr.hs5T = dr("s_hs5T", [2304, L], BF16)
    scr.b_s5 = Buf("s5", multi=True)
    scr.ys5T = dr("s_ys5T", [1152, L], BF16)
    scr.b_glu = Buf("glu", multi=True)
    scr.x1 = dr("s_x1", [L, 1024], F32)
    scr.b_x1 = Buf("x1", multi=True)
    scr.xcur = dr("s_xcur", [L, 1024], F32)
    scr.b_xcur = Buf("xcur", multi=True)
    scr.wgu16 = dr("s_wgu16", [cfg.E, 1024, 2048], BF16)
    scr.wdn16 = dr("s_wdn16", [cfg.E, 1024, 1024], BF16)
    scr.b_w16 = Buf("w16", multi=True)
    scr.x1T = dr("s_x1T", [1024, L], BF16)
    scr.rgate = dr("s_rgate", [L, cfg.E], F32)
    scr.b_x1T = Buf("x1T", multi=True)
    scr.attO = dr("s_attO", [3, L, 390], F32)
    scr.b_att = Buf("att", multi=True)
    scr.yatt = dr("s_yatt", [L, 384], BF16)
    return scr


def phase_conv(S, nc, cx, cfg, conv_w_l, conv_b_l, scr):
    L = cfg.L
    TS = min(2048, L)
    with ExitStack() as es:
        sb = lambda n, s, d: es.enter_context(nc.sbuf_tensor(uniq(n), s, d))
        ps = lambda n, s, d: es.enter_context(nc.psum_tensor(uniq(n), s, d))
        cw = sb("cv_w", [128, 32, 5], F32)
        cb = sb("cv_b", [128, 32], F32)
        b_cw = Buf("cv_w")
        with nc.allow_non_contiguous_dma(reason="tiny param load"):
            for k in range(5):
                S.dma("sp", cw[:, :, k], conv_w_l[k].rearrange("(t p) -> p t", p=128), writes=[b_cw], owner=b_cw)
            S.dma("sp", cb[:], conv_b_l.rearrange("(t p) -> p t", p=128), writes=[b_cw], owner=b_cw)
        xin = [sb(f"cv_x{i}", [128, TS + 4], F32) for i in range(2)]
        b_xin = bufs("cv_x", 2)
        acc = [sb(f"cv_a{i}", [128, TS], F32) for i in range(2)]
        b_acc = bufs("cv_a", 2)
        yb = [sb(f"cv_y{i}", [128, TS], BF16) for i in range(8)]
        b_yb = bufs("cv_y", 8)
        pt = [ps(f"cv_pt{i}", [128, 4, 128], BF16) for i in range(2)]
        b_pt = bufs("cv_pt", 2)
        ot = [sb(f"cv_o{i}", [128, 512], BF16) for i in range(3)]
        b_ot = bufs("cv_o", 3)
        nx = 0
        ny = 0
        npt = 0
        no = 0
        for cg in range(8):
            for t0 in range(0, L, TS):
                ys = []
                for j in range(4):
                    ct = cg * 4 + j
                    i = nx % 2
                    nx += 1
                    eng = "dve"
                    lo = max(0, t0 - 2)
                    hi = min(L, t0 + TS + 2)
                    d0 = lo - (t0 - 2)
                    if d0 > 0:
                        S.op("pool", lambda e: e.memset(xin[i][:, 0:d0], 0.0), writes=[b_xin[i]])
                    if d0 + (hi - lo) < TS + 4:
                        S.op("pool", lambda e: e.memset(xin[i][:, d0 + hi - lo:TS + 4], 0.0), writes=[b_xin[i]])
                    S.dma("sp", xin[i][:, d0:d0 + hi - lo], scr.xbcT[ct * 128:(ct + 1) * 128, lo:hi],
                          reads=[scr.b_proj], writes=[b_xin[i]], owner=b_xin[i])
                    a = acc[i]
                    S.op("act", lambda e: e.activation(out=a[:], in_=xin[i][:, 0:TS], func=AF.Copy, scale=cw[:, ct, 0:1]),
                         reads=[b_xin[i], b_cw], writes=[b_acc[i]])
                    for k in range(1, 5):
                        S.op(eng, lambda e: e.scalar_tensor_tensor(out=a[:], in0=xin[i][:, k:k + TS], scalar=cw[:, ct, k:k + 1],
                                                                   in1=a[:], op0=ALU.mult, op1=ALU.add),
                             reads=[b_xin[i], b_cw, b_acc[i]], writes=[b_acc[i]])
                    yi = ny % 8
                    ny += 1
                    S.op("act", lambda e: e.activation(out=yb[yi][:], in_=a[:], func=AF.Silu, bias=cb[:, ct:ct + 1], scale=1.0),
                         reads=[b_acc[i], b_cw], writes=[b_yb[yi]])
                    ys.append(yi)
                    if ct >= 16:
                        dst = scr.BT if ct < 24 else scr.CT
                        r0 = (ct - 16) * 128 if ct < 24 else (ct - 24) * 128
                        S.dma("sp", dst[r0:r0 + 128, t0:t0 + TS], yb[yi][:], reads=[b_yb[yi]], writes=[scr.b_conv],
                              owner=b_yb[yi])
                if cg < 6:
                    for tt in range(TS // 128):
                        pj = npt % 2
                        npt += 1
                        for j in range(4):
                            S.op("pe", lambda e: e.transpose(out=pt[pj][:, j, :], in_=yb[ys[j]][:, tt * 128:(tt + 1) * 128],
                                                             identity=cx.identb[:]),
                                 reads=[b_yb[ys[j]], cx.b_const], writes=[b_pt[pj]], inc=(j == 3))
                        oi = no % 3
                        no += 1
                        if no % 2:
                            S.op("act", lambda e: e.copy(out=ot[oi][:], in_=pt[pj][:].rearrange("p a b -> p (a b)")),
                                 reads=[b_pt[pj]], writes=[b_ot[oi]])
                        else:
                            S.op("dve", lambda e: e.tensor_copy(out=ot[oi][:], in_=pt[pj][:].rearrange("p a b -> p (a b)")),
                                 reads=[b_pt[pj]], writes=[b_ot[oi]])
                        S.dma("sp", scr.xsB[t0 + tt * 128:t0 + (tt + 1) * 128, cg * 512:(cg + 1) * 512], ot[oi][:],
                              reads=[b_ot[oi]], writes=[scr.b_conv], owner=b_ot[oi])
        S.barrier()


def make_masks(S, nc, cx):
    cx.maskLE = nc.alloc_sbuf_tensor("maskLE", [128, 128], F32)
    cx.maskGE = nc.alloc_sbuf_tensor("maskGE", [128, 128], F32)
    cx.U0 = nc.alloc_sbuf_tensor("U0", [128, 128], F32)
    cx.U1 = nc.alloc_sbuf_tensor("U1", [128, 128], F32)
    cx.ones = nc.alloc_sbuf_tensor("ones", [128, 128], F32)
    b = cx.b_const
    for t, pat, cm, op in ((cx.maskLE, 1, -1, ALU.is_ge), (cx.maskGE, -1, 1, ALU.is_ge),
                           (cx.U0, -1, 1, ALU.is_gt), (cx.U1, 1, -1, ALU.is_gt)):
        S.op("pool", lambda e: e.memset(t[:], 1.0), writes=[b])
        S.op("pool", lambda e: e.affine_select(out=t[:], in_=t[:], pattern=[[pat, 128]], base=0, channel_multiplier=cm,
                                               compare_op=op, fill=0.0), reads=[b], writes=[b])
    S.op("pool", lambda e: e.memset(cx.ones[:], 1.0), writes=[b])


def bc_r(ap, n):
    return ap.unsqueeze(1).to_broadcast([ap.shape[0], n, ap.shape[1]])


def bc_l(ap, n):
    return ap.unsqueeze(2).to_broadcast([ap.shape[0], ap.shape[1], n])


def phase_ssd(S, nc, cx, cfg, prm, l, scr):
    L = cfg.L
    NC = L // 128
    with ExitStack() as es:
        sb = lambda n, s, d: es.enter_context(nc.sbuf_tensor(uniq(n), s, d))
        ps = lambda n, s, d: es.enter_context(nc.psum_tensor(uniq(n), s, d))
        b_prm = Buf("sd_prm")
        biasb = sb("sd_bias", [128, 64], F32)
        negA = sb("sd_negA", [128, 64], F32)
        dsk = sb("sd_dsk", [128, 32], F32)
        nw = sb("sd_nw", [128, 2048], F32)
        S.dma("sp", biasb[:], prm["ssd_dt_bias"][l].rearrange("a b -> (a b)").partition_broadcast(128), writes=[b_prm], owner=b_prm)
        S.dma("sp", negA[:], prm["ssd_a_log"][l].rearrange("a b -> (a b)").partition_broadcast(128), writes=[b_prm], owner=b_prm)
        S.dma("sp", dsk[:], prm["ssd_d"][l].partition_broadcast(128), writes=[b_prm], owner=b_prm)
        S.dma("sp", nw[:], prm["ssd_norm_w"][l].partition_broadcast(128), writes=[b_prm], owner=b_prm)
        S.op("act", lambda e: e.activation(out=negA[:], in_=negA[:], func=AF.Exp), reads=[b_prm], writes=[b_prm])
        S.op("dve", lambda e: e.tensor_scalar(out=negA[:], in0=negA[:], scalar1=-1.0, scalar2=None, op0=ALU.mult),
             reads=[b_prm], writes=[b_prm])
        Hf = sb("sd_H", [128, 8, 256], F32)
        Hb = sb("sd_Hb", [128, 8, 256], BF16)
        b_H = bufs("sd_Hg", 8)
        tmpH = sb("sd_tH", [128, 256], F32)
        b_tmpH = Buf("sd_tH")
        dtt = [sb(f"sd_dt{i}", [128, 64], F32) for i in range(2)]
        b_dtt = bufs("sd_dt", 2)
        xB = [sb(f"sd_xB{i}", [128, 3072], BF16) for i in range(2)]
        b_xB = bufs("sd_xB", 2)
        BTc = [sb(f"sd_BT{i}", [128, 8, 128], BF16) for i in range(2)]
        b_BTc = bufs("sd_BT", 2)
        CTc = [sb(f"sd_CT{i}", [128, 8, 128], BF16) for i in range(2)]
        b_CTc = bufs("sd_CT", 2)
        names = ["ex", "delta", "a", "I", "Q", "G1", "G2", "G1d", "G2d", "dec", "tq"]
        sm = {n: sb("sd_" + n, [128, 64], F32) for n in names}
        b_sm = {n: Buf("sd_" + n) for n in names}
        ps_I = ps("sd_psI", [128, 64], F32)
        b_psI = Buf("sd_psI")
        ps_T = ps("sd_psT", [128, 64], F32)
        b_psT = Buf("sd_psT")
        ps_CB = ps("sd_psCB", [128, 128], F32)
        b_psCB = Buf("sd_psCB")
        ps_seg = [ps(f"sd_psS{i}", [128, 512], F32) for i in range(2)]
        b_psseg = bufs("sd_psS", 2)
        ps_yd = ps("sd_psyd", [128, 4, 64], F32)
        b_psyd = Buf("sd_psyd")
        ps_st = ps("sd_psst", [128, 2, 256], F32)
        b_psst = bufs("sd_psst", 2)
        ps_yo = ps("sd_psyo", [128, 256], F32)
        b_psyo = Buf("sd_psyo")
        CBm = [sb(f"sd_CBm{i}", [128, 128], F32) for i in range(2)]
        b_CBm = bufs("sd_CBm", 2)
        Rt = [sb(f"sd_R{i}", [128, 4, 128], F32) for i in range(2)]
        b_Rt = bufs("sd_R", 2)
        Dx = [sb(f"sd_Dx{i}", [128, 4, 128], F32) for i in range(2)]
        b_Dx = bufs("sd_Dx", 2)
        MT = [sb(f"sd_MT{i}", [128, 4, 128], BF16) for i in range(2)]
        b_MT = bufs("sd_MT", 2)
        xw = [sb(f"sd_xw{i}", [128, 4, 64], BF16) for i in range(2)]
        b_xw = bufs("sd_xw", 2)
        yp = [sb(f"sd_yp{i}", [128, 2048], F32) for i in range(2)]
        b_yp = bufs("sd_yp", 2)
        ytmp = sb("sd_ytmp", [128, 256], F32)
        b_ytmp = Buf("sd_ytmp")
        sbst = [sb(f"sd_sb{i}", [128, 256], F32) for i in range(2)]
        b_sbst = bufs("sd_sb", 2)
        zt = [sb(f"sd_z{i}", [128, 2048], F32) for i in range(2)]
        b_zt = bufs("sd_z", 2)
        yo16 = [sb(f"sd_y16{i}", [128, 2048], BF16) for i in range(2)]
        b_yo16 = bufs("sd_y16", 2)
        ssq = sb("sd_ssq", [128, 2], F32)
        b_ssq = Buf("sd_ssq")

        for g in range(8):
            S.op("pool", lambda e: e.memset(Hf[:, g, :], 0.0), writes=[b_H[g]])
            S.op("pool", lambda e: e.memset(Hb[:, g, :], 0.0), writes=[b_H[g]])

        def chunk_scalars(c, i):
            S.dma("sp", dtt[i][:], scr.dt[c * 128:(c + 1) * 128, :], reads=[scr.b_proj], writes=[b_dtt[i]], owner=b_dtt[i])
            S.op("dve", lambda e: e.tensor_tensor(out=sm["ex"][:], in0=dtt[i][:], in1=biasb[:], op=ALU.add),
                 reads=[b_dtt[i], b_prm], writes=[b_sm["ex"]])
            S.op("act", lambda e: e.activation(out=sm["ex"][:], in_=sm["ex"][:], func=AF.Exp), reads=[b_sm["ex"]], writes=[b_sm["ex"]])
            S.op("act", lambda e: e.activation(out=sm["delta"][:], in_=sm["ex"][:], func=AF.Ln, bias=1.0, scale=1.0),
                 reads=[b_sm["ex"]], writes=[b_sm["delta"]])
            S.op("dve", lambda e: e.tensor_tensor(out=sm["a"][:], in0=sm["delta"][:], in1=negA[:], op=ALU.mult),
                 reads=[b_sm["delta"], b_prm], writes=[b_sm["a"]])
            S.op("pe", lambda e: e.matmul(ps_I[:], lhsT=cx.maskLE[:], rhs=sm["a"][:], start=True, stop=True),
                 reads=[b_sm["a"], cx.b_const], writes=[b_psI])
            S.op("pe", lambda e: e.matmul(ps_T[:], lhsT=cx.ones[:], rhs=sm["a"][:], start=True, stop=True),
                 reads=[b_sm["a"], cx.b_const], writes=[b_psT])
            S.op("dve", lambda e: e.tensor_copy(out=sm["Q"][:, 0:32], in_=ps_I[:, 0:32]), reads=[b_psI], writes=[b_sm["Q"]])
            S.op("dve", lambda e: e.tensor_tensor(out=sm["Q"][:, 32:64], in0=ps_I[:, 32:64], in1=sm["a"][:, 32:64], op=ALU.subtract),
                 reads=[b_psI, b_sm["a"], b_sm["Q"]], writes=[b_sm["Q"]])
            S.op("dve", lambda e: e.tensor_tensor(out=sm["tq"][:], in0=ps_T[:], in1=sm["Q"][:], op=ALU.subtract),
                 reads=[b_psT, b_sm["Q"]], writes=[b_sm["tq"]])
            S.op("act", lambda e: e.activation(out=sm["G1"][:], in_=sm["Q"][:], func=AF.Exp), reads=[b_sm["Q"]], writes=[b_sm["G1"]])
            S.op("act", lambda e: e.activation(out=sm["G2"][:], in_=sm["tq"][:], func=AF.Exp), reads=[b_sm["tq"]], writes=[b_sm["G2"]])
            S.op("act", lambda e: e.activation(out=sm["dec"][:], in_=ps_T[:], func=AF.Exp), reads=[b_psT], writes=[b_sm["dec"]])
            S.op("dve", lambda e: e.tensor_tensor(out=sm["G1d"][:], in0=sm["G1"][:], in1=sm["delta"][:], op=ALU.mult),
                 reads=[b_sm["G1"], b_sm["delta"]], writes=[b_sm["G1d"]])
            S.op("dve", lambda e: e.tensor_tensor(out=sm["G2d"][:], in0=sm["G2"][:], in1=sm["delta"][:], op=ALU.mult),
                 reads=[b_sm["G2"], b_sm["delta"]], writes=[b_sm["G2d"]])

        nseg = 0
        nst = 0
        for c in range(NC):
            i = c % 2
            chunk_scalars(c, i)
            S.dma("sp", xB[i][:], scr.xsB[c * 128:(c + 1) * 128, :], reads=[scr.b_conv], writes=[b_xB[i]], owner=b_xB[i])
            S.dma("sp", BTc[i][:], scr.BT[:, c * 128:(c + 1) * 128].rearrange("(g n) l -> n g l", n=128),
                  reads=[scr.b_conv], writes=[b_BTc[i]], owner=b_BTc[i])
            S.dma("sp", CTc[i][:], scr.CT[:, c * 128:(c + 1) * 128].rearrange("(g n) l -> n g l", n=128),
                  reads=[scr.b_conv], writes=[b_CTc[i]], owner=b_CTc[i])
            xv = xB[i][:, 0:2048].rearrange("p (g r q) -> p g r q", g=8, r=4)
            Btok = xB[i][:, 2048:3072].rearrange("p (g n) -> p g n", g=8)
            for g in range(8):
                S.op("pe", lambda e: e.matmul(ps_CB[:], lhsT=BTc[i][:, g, :], rhs=CTc[i][:, g, :], start=True, stop=True),
                     reads=[b_BTc[i], b_CTc[i]], writes=[b_psCB])
                S.op("dve", lambda e: e.tensor_tensor(out=CBm[0][:], in0=ps_CB[:], in1=cx.maskLE[:], op=ALU.mult),
                     reads=[b_psCB, cx.b_const], writes=[b_CBm[0]])
                S.op("dve", lambda e: e.tensor_tensor(out=CBm[1][:], in0=ps_CB[:], in1=cx.maskGE[:], op=ALU.mult),
                     reads=[b_psCB, cx.b_const], writes=[b_CBm[1]])
                for d in range(2):
                    cols = slice(d * 32 + g * 4, d * 32 + g * 4 + 4)
                    msk = cx.maskLE if d == 0 else cx.maskGE
                    U = cx.U0 if d == 0 else cx.U1
                    wd = sm["G2d"] if d == 0 else sm["G1d"]
                    b_wd = b_sm["G2d"] if d == 0 else b_sm["G1d"]
                    sj = nseg % 2
                    nseg += 1
                    S.op("pool", lambda e: e.tensor_tensor(out=Rt[d][:], in0=bc_r(msk[:], 4), in1=bc_l(sm["a"][:, cols], 128), op=ALU.mult),
                         reads=[cx.b_const, b_sm["a"]], writes=[b_Rt[d]])
                    S.op("pe", lambda e: e.matmul(ps_seg[sj][:], lhsT=U[:], rhs=Rt[d][:].rearrange("p r l -> p (r l)"), start=True, stop=True),
                         reads=[cx.b_const, b_Rt[d]], writes=[b_psseg[sj]])
                    S.op("act", lambda e: e.activation(out=Dx[d][:].rearrange("p r l -> p (r l)"), in_=ps_seg[sj][:], func=AF.Exp),
                         reads=[b_psseg[sj]], writes=[b_Dx[d]])
                    S.op("dve", lambda e: e.tensor_tensor(out=Dx[d][:], in0=Dx[d][:], in1=bc_l(sm["delta"][:, cols], 128), op=ALU.mult),
                         reads=[b_Dx[d], b_sm["delta"]], writes=[b_Dx[d]])
                    S.op("pool", lambda e: e.tensor_tensor(out=MT[d][:], in0=Dx[d][:], in1=bc_r(CBm[d][:], 4), op=ALU.mult),
                         reads=[b_Dx[d], b_CBm[d]], writes=[b_MT[d]])
                    S.op("dve", lambda e: e.tensor_tensor(out=xw[d][:], in0=xv[:, g, :, :], in1=bc_l(wd[:, cols], 64), op=ALU.mult),
                         reads=[b_xB[i], b_wd], writes=[b_xw[d]])
                    S.op("pe", lambda e: e.matmul(ps_st[:, d, :], lhsT=Btok[:, g, :], rhs=xw[d][:].rearrange("p r q -> p (r q)"),
                                                  start=True, stop=True),
                         reads=[b_xB[i], b_xw[d]], writes=[b_psst[d]])
                for r in range(4):
                    for d in range(2):
                        S.op("pe", lambda e: e.matmul(ps_yd[:, r, :], lhsT=MT[d][:, r, :], rhs=xv[:, g, r, :], start=(d == 0), stop=(d == 1)),
                             reads=[b_MT[d], b_xB[i]], writes=[b_psyd], inc=(r == 3 and d == 1))
                colsf = slice(g * 4, g * 4 + 4)
                S.op("pe", lambda e: e.matmul(ps_yo[:], lhsT=CTc[i][:, g, :], rhs=Hb[:, g, :], start=True, stop=True),
                     reads=[b_CTc[i], b_H[g]], writes=[b_psyo])
                S.op("dve", lambda e: e.tensor_tensor(out=ytmp[:].rearrange("p (r q) -> p r q", r=4), in0=ps_yo[:].rearrange("p (r q) -> p r q", r=4),
                                                      in1=bc_l(sm["G1"][:, colsf], 64), op=ALU.mult),
                     reads=[b_psyo, b_sm["G1"]], writes=[b_ytmp])
                S.op("dve", lambda e: e.tensor_tensor(out=yp[i][:, g * 256:(g + 1) * 256], in0=ps_yd[:].rearrange("p r q -> p (r q)"),
                                                      in1=ytmp[:], op=ALU.add),
                     reads=[b_psyd, b_ytmp], writes=[b_yp[i]])
                S.op("pool", lambda e: e.tensor_tensor(out=tmpH[:].rearrange("p (r q) -> p r q", r=4), in0=Hf[:, g, :].rearrange("p (r q) -> p r q", r=4),
                                                       in1=bc_l(sm["dec"][:, colsf], 64), op=ALU.mult),
                     reads=[b_H[g], b_sm["dec"]], writes=[b_tmpH])
                S.op("dve", lambda e: e.tensor_tensor(out=Hf[:, g, :], in0=tmpH[:], in1=ps_st[:, 0, :], op=ALU.add),
                     reads=[b_tmpH, b_psst[0]], writes=[b_H[g]])
                S.op("act", lambda e: e.copy(out=Hb[:, g, :], in_=Hf[:, g, :]), reads=[b_H[g]], writes=[b_H[g]])
                si = nst % 2
                nst += 1
                S.op("act", lambda e: e.copy(out=sbst[si][:], in_=ps_st[:, 1, :]), reads=[b_psst[1]], writes=[b_sbst[si]])
                S.dma("sp", scr.Sb[c, g], sbst[si][:], reads=[b_sbst[si]], writes=[scr.b_ssd], owner=b_sbst[si])
            S.dma("sp", scr.ypart[c * 128:(c + 1) * 128, :], yp[i][:], reads=[b_yp[i]], writes=[scr.b_ssd], owner=b_yp[i])
        S.barrier()
        for g in range(8):
            S.op("pool", lambda e: e.memset(Hf[:, g, :], 0.0), writes=[b_H[g]])
            S.op("pool", lambda e: e.memset(Hb[:, g, :], 0.0), writes=[b_H[g]])
        for ci, c in enumerate(range(NC - 1, -1, -1)):
            i = ci % 2
            chunk_scalars(c, i)
            S.dma("sp", CTc[i][:], scr.CT[:, c * 128:(c + 1) * 128].rearrange("(g n) l -> n g l", n=128),
                  reads=[scr.b_conv], writes=[b_CTc[i]], owner=b_CTc[i])
            S.dma("sp", xB[i][:, 0:2048], scr.xsB[c * 128:(c + 1) * 128, 0:2048], reads=[scr.b_conv], writes=[b_xB[i]], owner=b_xB[i])
            S.dma("sp", yp[i][:], scr.ypart[c * 128:(c + 1) * 128, :], reads=[scr.b_ssd], writes=[b_yp[i]], owner=b_yp[i])
            S.dma("sp", zt[i][:], scr.z[c * 128:(c + 1) * 128, :], reads=[scr.b_proj], writes=[b_zt[i]], owner=b_zt[i])
            for g in range(8):
                colsb = slice(32 + g * 4, 32 + g * 4 + 4)
                si = nst % 2
                nst += 1
                S.dma("sp", sbst[si][:], scr.Sb[c, g], reads=[scr.b_ssd], writes=[b_sbst[si]], owner=b_sbst[si])
                S.op("pe", lambda e: e.matmul(ps_yo[:], lhsT=CTc[i][:, g, :], rhs=Hb[:, g, :], start=True, stop=True),
                     reads=[b_CTc[i], b_H[g]], writes=[b_psyo])
                S.op("dve", lambda e: e.tensor_tensor(out=ytmp[:].rearrange("p (r q) -> p r q", r=4), in0=ps_yo[:].rearrange("p (r q) -> p r q", r=4),
                                                      in1=bc_l(sm["G2"][:, colsb], 64), op=ALU.mult),
                     reads=[b_psyo, b_sm["G2"]], writes=[b_ytmp])
                S.op("dve", lambda e: e.tensor_tensor(out=yp[i][:, g * 256:(g + 1) * 256], in0=yp[i][:, g * 256:(g + 1) * 256],
                                                      in1=ytmp[:], op=ALU.add),
                     reads=[b_yp[i], b_ytmp], writes=[b_yp[i]])
                S.op("pool", lambda e: e.tensor_tensor(out=tmpH[:].rearrange("p (r q) -> p r q", r=4), in0=Hf[:, g, :].rearrange("p (r q) -> p r q", r=4),
                                                       in1=bc_l(sm["dec"][:, colsb], 64), op=ALU.mult),
                     reads=[b_H[g], b_sm["dec"]], writes=[b_tmpH])
                S.op("pool", lambda e: e.tensor_tensor(out=Hf[:, g, :], in0=tmpH[:], in1=sbst[si][:], op=ALU.add),
                     reads=[b_tmpH, b_sbst[si]], writes=[b_H[g]])
                S.op("act", lambda e: e.copy(out=Hb[:, g, :], in_=Hf[:, g, :]), reads=[b_H[g]], writes=[b_H[g]])
            S.op("act", lambda e: e.activation(out=zt[i][:], in_=zt[i][:], func=AF.Silu), reads=[b_zt[i]], writes=[b_zt[i]])
            S.op("dve", lambda e: e.tensor_tensor(out=yo16[i][:].rearrange("p (h q) -> p h q", h=32), in0=xB[i][:, 0:2048].rearrange("p (h q) -> p h q", h=32),
                                                  in1=bc_l(dsk[:], 64), op=ALU.mult),
                 reads=[b_xB[i], b_prm], writes=[b_yo16[i]])
            S.op("dve", lambda e: e.tensor_tensor(out=yp[i][:], in0=yp[i][:], in1=yo16[i][:], op=ALU.add),
                 reads=[b_yp[i], b_yo16[i]], writes=[b_yp[i]])
            S.op("dve", lambda e: e.tensor_tensor(out=yp[i][:], in0=yp[i][:], in1=zt[i][:], op=ALU.mult),
                 reads=[b_yp[i], b_zt[i]], writes=[b_yp[i]])
            S.op("act", lambda e: e.activation(out=zt[i][:], in_=yp[i][:], func=AF.Square, accum_out=ssq[:, 0:1]),
                 reads=[b_yp[i]], writes=[b_zt[i], b_ssq])
            S.op("dve", lambda e: e.tensor_scalar(out=ssq[:, 1:2], in0=ssq[:, 0:1], scalar1=1.0 / 2048, scalar2=LN_EPS, op0=ALU.mult, op1=ALU.add),
                 reads=[b_ssq], writes=[b_ssq])
            S.op("act", lambda e: e.activation(out=ssq[:, 1:2], in_=ssq[:, 1:2], func=AF.Sqrt), reads=[b_ssq], writes=[b_ssq])
            S.op("dve", lambda e: e.reciprocal(out=ssq[:, 1:2], in_=ssq[:, 1:2]), reads=[b_ssq], writes=[b_ssq])
            S.op("dve", lambda e: e.scalar_tensor_tensor(out=yo16[i][:], in0=yp[i][:], scalar=ssq[:, 1:2], in1=nw[:], op0=ALU.mult, op1=ALU.mult),
                 reads=[b_yp[i], b_ssq, b_prm, b_yo16[i]], writes=[b_yo16[i]])
            S.dma("sp", scr.yssd[c * 128:(c + 1) * 128, :], yo16[i][:], reads=[b_yo16[i]], writes=[scr.b_mix], owner=b_yo16[i])
        S.barrier()


PARAM_SHAPES = lambda cfg: {
    "w_in": [cfg.depth, D, N_IN], "b_gate": [cfg.depth, 3, D],
    "ssd_conv_w": [cfg.depth, 5, 4096], "ssd_conv_b": [cfg.depth, 4096], "ssd_a_log": [cfg.depth, 2, 32],
    "ssd_dt_bias": [cfg.depth, 2, 32], "ssd_d": [cfg.depth, 32], "ssd_norm_w": [cfg.depth, 2048],
    "s5_a_re": [cfg.depth, 2, 72, 64], "s5_a_im": [cfg.depth, 2, 72, 64], "s5_log_step": [cfg.depth, 2, 72],
    "s5_b_re": [cfg.depth, 72, 64, 16], "s5_b_im": [cfg.depth, 72, 64, 16],
    "s5_c_re": [cfg.depth, 2, 72, 16, 64], "s5_c_im": [cfg.depth, 2, 72, 16, 64], "s5_d": [cfg.depth, 1152],
    "s5_glu_w1": [cfg.depth, 1152, 1152], "s5_glu_w2": [cfg.depth, 1152, 1152],
    "w_br_ssd": [cfg.depth, 2048, D], "w_br_attn": [cfg.depth, 384, D], "w_br_s5": [cfg.depth, 1152, D],
    "w_out": [cfg.depth, D, D], "ln1_g": [cfg.depth, D], "ln1_b": [cfg.depth, D],
    "router_w": [cfg.depth, D, cfg.E], "router_b": [cfg.depth, cfg.E],
    "exp_w_gate_up": [cfg.depth, cfg.E, D, 2048], "exp_b_gate_up": [cfg.depth, cfg.E, 2048],
    "exp_w_down": [cfg.depth, cfg.E, D, D], "exp_b_down": [cfg.depth, cfg.E, D],
    "ln2_g": [cfg.depth, D], "ln2_b": [cfg.depth, D],
}


def build(cfg, debug=(), phases=("inproj", "conv", "ssd", "att", "s5", "mix", "moe"), inject=()):
    nc = bass.Bass("TRN2", target_bir_lowering=False)
    L = cfg.L
    prm = {}
    x = nc.dram_tensor("x", [L, D], F32, kind="ExternalInput").ap()
    for name, shape in PARAM_SHAPES(cfg).items():
        prm[name] = nc.dram_tensor(name, list(shape), F32, kind="ExternalInput").ap()
    out = nc.dram_tensor("out", [L, D], F32, kind="ExternalOutput").ap()
    S = Sched(nc)
    cx = Ctx()
    make_consts(S, nc, cx)
    make_masks(S, nc, cx)
    make_att_bias(S, nc, cx)
    scr = alloc_scratch(nc, cfg)
    dbg = {}
    for name in debug:
        src = getattr(scr, name)
        dbg[name] = nc.dram_tensor("dbg_" + name, list(src.shape), src.dtype, kind="ExternalOutput").ap()
    cur = x
    b_inj = Buf("inj", multi=True)
    for name in inject:
        dst = getattr(scr, name)
        src = nc.dram_tensor("inj_" + name, list(dst.shape), dst.dtype, kind="ExternalInput").ap()
        dram_copy(S, dst, src, b_inj)
    if inject:
        S.barrier()
    for l in range(cfg.depth):
        if "inproj" in phases:
            phase_inproj(S, nc, cx, cfg, prm["w_in"][l], cur, scr)
        if "conv" in phases:
            phase_conv(S, nc, cx, cfg, prm["ssd_conv_w"][l], prm["ssd_conv_b"][l], scr)
        if "ssd" in phases:
            phase_ssd(S, nc, cx, cfg, prm, l, scr)
        if "att" in phases:
            phase_att(S, nc, cx, cfg, scr)
        if "s5" in phases:
            phase_s5(S, nc, cx, cfg, prm, l, scr)
        if "mix" in phases:
            phase_mix(S, nc, cx, cfg, prm, l, cur, scr)
            cur = scr.x1
        if "moe" in phases:
            last = (l == cfg.depth - 1) and not debug
            phase_moe(S, nc, cx, cfg, prm, l, scr, out if last else scr.xcur)
            cur = None if last else scr.xcur
    b_dbg = Buf("dbg", multi=True)
    for name in debug:
        dram_copy(S, dbg[name], getattr(scr, name), b_dbg)
    if cur is not None:
        dram_copy(S, out, cur, b_dbg)
    S.barrier()
    return nc, S


def kernel(**inputs):
    cfg = Cfg(L=8192, depth=4, n_exp=32)
    nc, _ = build(cfg)
    x = np.asarray(inputs["x"], dtype=np.float32)
    names = list(PARAM_SHAPES(cfg).keys())
    params = {k: np.ascontiguousarray(np.asarray(inputs[k], dtype=np.float32)) for k in names}
    in_maps = []
    for b in range(x.shape[0]):
        m = {"x": np.ascontiguousarray(x[b])}
        m.update(params)
        in_maps.append(m)
    res = run_bass_kernel_spmd(nc, in_maps, core_ids=list(range(x.shape[0])))
    return np.stack([np.asarray(r["out"], dtype=np.float32) for r in res.results], axis=0)


ATT_PAT = ((128, 1), (512, 4), (2048, 16))
NEG_BIG = -30000.0


def make_att_bias(S, nc, cx):
    cx.abias = nc.alloc_sbuf_tensor("abias", [128, 18, 256], F32)
    cx.b_abias = Buf("abias")
    b = cx.b_abias
    with ExitStack() as es:
        di = es.enter_context(nc.sbuf_tensor("ab_di", [128, 256], I32))
        df = es.enter_context(nc.sbuf_tensor("ab_df", [128, 256], F32))
        mk = es.enter_context(nc.sbuf_tensor("ab_mk", [128, 256], F32))
        bt = Buf("ab_tmp")
        S.op("pool", lambda e: e.iota(di[:], pattern=[[-128, 2], [1, 128]], base=64, channel_multiplier=-1), writes=[bt])
        S.op("dve", lambda e: e.tensor_copy(out=df[:], in_=di[:]), reads=[bt], writes=[bt])
        S.op("act", lambda e: e.activation(out=df[:], in_=df[:], func=AF.Abs), reads=[bt], writes=[bt])
        S.op("dve", lambda e: e.tensor_scalar(out=mk[:], in0=df[:], scalar1=64.0, scalar2=NEG_BIG, op0=ALU.is_gt, op1=ALU.mult),
             reads=[bt], writes=[bt])
        for g, (window, dil) in enumerate(ATT_PAT):
            for h in range(6):
                slope = 2.0 ** (-8.0 * (h * 3 + g + 1) / 18.0)
                S.op("dve", lambda e: e.scalar_tensor_tensor(out=cx.abias[:, g * 6 + h, :], in0=df[:], scalar=-slope * dil, in1=mk[:],
                                                             op0=ALU.mult, op1=ALU.add), reads=[bt], writes=[b])
        S.barrier()


def phase_att(S, nc, cx, cfg, scr):
    L = cfg.L
    with ExitStack() as es:
        sb = lambda n, s, d: es.enter_context(nc.sbuf_tensor(uniq(n), s, d))
        ps = lambda n, s, d: es.enter_context(nc.psum_tensor(uniq(n), s, d))
        PADM = 64 * 16
        qT2 = [sb(f"at_q{i}", [128, L], BF16) for i in range(3)]
        kT2 = [sb(f"at_k{i}", [128, L + 2 * PADM], BF16) for i in range(3)]
        b_qk = bufs("at_qk", 3)
        raw = [sb(f"at_raw{i}", [128, L], BF16) for i in range(2)]
        b_raw = bufs("at_raw", 2)
        Vn = [sb(f"at_v{i}", [128, 6, 65], BF16) for i in range(4)]
        b_Vn = bufs("at_v", 4)
        Ve = [sb(f"at_ve{i}", [128, 6, 65], BF16) for i in range(2)]
        b_Ve = bufs("at_ve", 2)
        for i in range(4):
            S.op("pool", lambda e: e.memset(Vn[i][:, :, 64:65], 1.0), writes=[b_Vn[i]])
        S.op("pool", lambda e: e.memset(Ve[0][:], 0.0), writes=[b_Ve[0]])
        S.op("pool", lambda e: e.memset(Ve[0][64:128, :, 64:65], 1.0), writes=[b_Ve[0]])
        S.op("pool", lambda e: e.memset(Ve[1][:], 0.0), writes=[b_Ve[1]])
        S.op("pool", lambda e: e.memset(Ve[1][0:64, :, 64:65], 1.0), writes=[b_Ve[1]])
        S_ps = [ps(f"at_ps{i}", [128, 2, 128], F32) for i in range(2)]
        b_Sps = bufs("at_ps", 2)
        O_ps = [ps(f"at_po{i}", [128, 6, 65], F32) for i in range(2)]
        b_Ops = bufs("at_po", 2)
        sbt = [sb(f"at_sb{i}", [128, 256], F32) for i in range(2)]
        b_sbt = bufs("at_sb", 2)
        PT = [sb(f"at_pt{i}", [128, 2, 128], BF16) for i in range(2)]
        b_PT = bufs("at_pt", 2)
        Ot = [sb(f"at_o{i}", [128, 390], F32) for i in range(2)]
        b_Ot = bufs("at_o", 2)
        nv = 0
        nsp = 0
        nop = 0
        for g, (window, dil) in enumerate(ATT_PAT):
            ls = L // dil
            PAD = 64 * dil
            nt = ls // 128
            for hp in range(3):
                r0 = g * 384 + hp * 128
                S.dma("sp", raw[0][:], scr.qT[r0:r0 + 128, :], reads=[scr.b_proj], writes=[b_raw[0]], owner=b_raw[0])
                S.dma("sp", raw[1][:], scr.kT[r0:r0 + 128, :], reads=[scr.b_proj], writes=[b_raw[1]], owner=b_raw[1])
                qv = qT2[hp][:, 0:L].rearrange("p (r i) -> p r i", r=dil)
                kv = kT2[hp][:, 0:dil * (ls + 128)].rearrange("p (r i) -> p r i", r=dil)
                S.op("pool", lambda e: e.tensor_copy(out=qv, in_=raw[0][:].rearrange("p (i r) -> p r i", r=dil)),
                     reads=[b_raw[0]], writes=[b_qk[hp]])
                S.op("pool", lambda e: e.memset(kv[:, :, 0:64], 0.0), writes=[b_qk[hp]])
                S.op("pool", lambda e: e.memset(kv[:, :, 64 + ls:128 + ls], 0.0), writes=[b_qk[hp]])
                S.op("dve", lambda e: e.tensor_copy(out=kv[:, :, 64:64 + ls], in_=raw[1][:].rearrange("p (i r) -> p r i", r=dil)),
                     reads=[b_raw[1]], writes=[b_qk[hp]])
            for r in range(dil):
                for n in range(nt):
                    vch = []
                    for c in range(2):
                        kp0 = 128 * n - 64 + 128 * c
                        lo_bad = kp0 < 0
                        hi_bad = kp0 + 128 > ls
                        if lo_bad:
                            vt, bv, p0, p1 = Ve[0], b_Ve[0], 64, 128
                        elif hi_bad:
                            vt, bv, p0, p1 = Ve[1], b_Ve[1], 0, 64
                        else:
                            vi = nv % 4
                            nv += 1
                            vt, bv, p0, p1 = Vn[vi], b_Vn[vi], 0, 128
                        tok0 = (kp0 + p0) * dil + r
                        npart = p1 - p0
                        src = scr.v[tok0:tok0 + (npart - 1) * dil + 1:dil, g * 384:(g + 1) * 384].rearrange("p (h e) -> p h e", h=6)
                        S.dma("sp", vt[p0:p1, :, 0:64], src, reads=[scr.b_proj], writes=[bv], owner=bv)
                        vch.append((vt, bv))
                    oj = nop % 2
                    nop += 1
                    for h in range(6):
                        hp, hh = h // 2, h % 2
                        sj = nsp % 2
                        nsp += 1
                        q0 = r * ls + 128 * n
                        for c in range(2):
                            k0 = r * (ls + 128) + 128 * n + 128 * c
                            S.op("pe", lambda e: e.matmul(S_ps[sj][:, c, :],
                                                          lhsT=kT2[hp][hh * 64:(hh + 1) * 64, k0:k0 + 128],
                                                          rhs=qT2[hp][hh * 64:(hh + 1) * 64, q0:q0 + 128],
                                                          start=True, stop=True),
                                 reads=[b_qk[hp]], writes=[b_Sps[sj]], inc=(c == 1))
                        S.op("dve", lambda e: e.scalar_tensor_tensor(out=sbt[sj][:], in0=S_ps[sj][:].rearrange("p c q -> p (c q)"), scalar=0.125,
                                                                     in1=cx.abias[:, g * 6 + h, :], op0=ALU.mult, op1=ALU.add),
                             reads=[b_Sps[sj], cx.b_abias], writes=[b_sbt[sj]])
                        S.op("act", lambda e: e.activation(out=PT[sj][:].rearrange("p c q -> p (c q)"), in_=sbt[sj][:], func=AF.Exp),
                             reads=[b_sbt[sj]], writes=[b_PT[sj]])
                        for c in range(2):
                            vt, bv = vch[c]
                            S.op("pe", lambda e: e.matmul(O_ps[oj][:, h, :], lhsT=PT[sj][:, c, :], rhs=vt[:, h, :], start=(c == 0), stop=(c == 1)),
                                 reads=[b_PT[sj], bv], writes=[b_Ops[oj]], inc=(c == 1))
                    S.op("act", lambda e: e.copy(out=Ot[oj][:], in_=O_ps[oj][:].rearrange("p h e -> p (h e)")), reads=[b_Ops[oj]], writes=[b_Ot[oj]])
                    t0 = (128 * n) * dil + r
                    S.dma("sp", scr.attO[g, t0:t0 + 127 * dil + 1:dil, :], Ot[oj][:], reads=[b_Ot[oj]], writes=[scr.b_att], owner=b_Ot[oj])
        S.barrier()
        At = [sb(f"at_m{i}", [128, 3, 390], F32) for i in range(2)]
        b_At = bufs("at_m", 2)
        rc = sb("at_rc", [128, 6], F32)
        b_rc = Buf("at_rc")
        yo = [sb(f"at_y{i}", [128, 384], BF16) for i in range(2)]
        b_yo = bufs("at_y", 2)
        for t in range(L // 128):
            i = t % 2
            for g in range(3):
                S.dma("sp", At[i][:, g, :], scr.attO[g, t * 128:(t + 1) * 128, :], reads=[scr.b_att], writes=[b_At[i]], owner=b_At[i])
            S.op("dve", lambda e: e.tensor_tensor(out=At[i][:, 0, :], in0=At[i][:, 0, :], in1=At[i][:, 1, :], op=ALU.add), reads=[b_At[i]], writes=[b_At[i]])
            S.op("dve", lambda e: e.tensor_tensor(out=At[i][:, 0, :], in0=At[i][:, 0, :], in1=At[i][:, 2, :], op=ALU.add), reads=[b_At[i]], writes=[b_At[i]])
            a3 = At[i][:, 0, :].rearrange("p (h e) -> p h e", h=6)
            S.op("dve", lambda e: e.reciprocal(out=rc[:], in_=a3[:, :, 64]), reads=[b_At[i]], writes=[b_rc])
            S.op("dve", lambda e: e.tensor_tensor(out=yo[i][:].rearrange("p (h e) -> p h e", h=6), in0=a3[:, :, 0:64], in1=bc_l(rc[:], 64), op=ALU.mult),
                 reads=[b_At[i], b_rc], writes=[b_yo[i]])
            S.dma("sp", scr.yatt[t * 128:(t + 1) * 128, :], yo[i][:], reads=[b_yo[i]], writes=[scr.b_mix], owner=b_yo[i])
        S.barrier()


def layer_norm_tile(S, h, b_h, out, b_out, gam, bet, b_par, st, b_st):
    for j in range(2):
        S.op("dve", lambda e: e.bn_stats(out=st[:, j * 6:(j + 1) * 6], in_=h[:, j * 512:(j + 1) * 512]), reads=[b_h], writes=[b_st])
    S.op("dve", lambda e: e.bn_aggr(out=st[:, 12:14], in_=st[:, 0:12]), reads=[b_st], writes=[b_st])
    S.op("dve", lambda e: e.tensor_scalar(out=st[:, 14:15], in0=st[:, 13:14], scalar1=LN_EPS, scalar2=None, op0=ALU.add), reads=[b_st], writes=[b_st])
    S.op("act", lambda e: e.activation(out=st[:, 14:15], in_=st[:, 14:15], func=AF.Sqrt), reads=[b_st], writes=[b_st])
    S.op("dve", lambda e: e.reciprocal(out=st[:, 14:15], in_=st[:, 14:15]), reads=[b_st], writes=[b_st])
    S.op("dve", lambda e: e.tensor_scalar(out=out[:], in0=h[:], scalar1=st[:, 12:13], scalar2=st[:, 14:15], op0=ALU.subtract, op1=ALU.mult),
         reads=[b_h, b_st], writes=[b_out])
    S.op("dve", lambda e: e.tensor_tensor(out=out[:], in0=out[:], in1=gam[:], op=ALU.mult), reads=[b_out, b_par], writes=[b_out])
    S.op("dve", lambda e: e.tensor_tensor(out=out[:], in0=out[:], in1=bet[:], op=ALU.add), reads=[b_out, b_par], writes=[b_out])


def load_w_bf16(S, w_ap, dst, b_dst, stg, b_stg, ctr, ncols_max=512):
    K, N = w_ap.shape
    for k in range(K // 128):
        for c0 in range(0, N, ncols_max):
            c1 = min(N, c0 + ncols_max)
            i = ctr[0] % 2
            ctr[0] += 1
            S.dma("sp", stg[i][:, 0:c1 - c0], w_ap[k * 128:(k + 1) * 128, c0:c1], writes=[b_stg[i]], owner=b_stg[i])
            eng = "pool" if ctr[0] % 2 else "act"
            if eng == "pool":
                S.op("pool", lambda e: e.tensor_copy(out=dst[:, k, c0:c1], in_=stg[i][:, 0:c1 - c0]), reads=[b_stg[i]], writes=[b_dst])
            else:
                S.op("act", lambda e: e.copy(out=dst[:, k, c0:c1], in_=stg[i][:, 0:c1 - c0]), reads=[b_stg[i]], writes=[b_dst])


def phase_mix(S, nc, cx, cfg, prm, l, x_ap, scr):
    L = cfg.L
    with ExitStack() as es:
        sb = lambda n, s, d: es.enter_context(nc.sbuf_tensor(uniq(n), s, d))
        ps = lambda n, s, d: es.enter_context(nc.psum_tensor(uniq(n), s, d))
        W1 = sb("gl_w1", [128, 18, 1152], BF16)
        W2 = sb("gl_w2", [128, 18, 1152], BF16)
        b_W = Buf("gl_w")
        stg = [sb(f"gl_st{i}", [128, 1152], F32) for i in range(2)]
        b_stg = bufs("gl_st", 2)
        for i in range(2):
            S.op("pool", lambda e: e.memset(stg[i][:], 0.0), writes=[b_stg[i]])
        n = 0
        for W, wname in ((W1, "s5_glu_w1"), (W2, "s5_glu_w2")):
            for kt in range(18):
                i = n % 2
                n += 1
                for gl in range(4):
                    g = kt * 4 + gl
                    S.dma("sp", stg[i][gl * 32:gl * 32 + 16, :], prm[wname][l][g * 16:(g + 1) * 16, :], writes=[b_stg[i]], owner=b_stg[i])
                S.op("pool", lambda e: e.tensor_copy(out=W[:, kt, :], in_=stg[i][:]), reads=[b_stg[i]], writes=[b_W])
        hT = [sb(f"gl_h{i}", [128, 18, 512], BF16) for i in range(2)]
        b_hT = bufs("gl_h", 2)
        g_ps = [ps(f"gl_pg{i}", [128, 512], F32) for i in range(2)]
        b_gps = bufs("gl_pg", 2)
        l_ps = [ps(f"gl_pl{i}", [128, 512], F32) for i in range(2)]
        b_lps = bufs("gl_pl", 2)
        sg = [sb(f"gl_sg{i}", [128, 512], F32) for i in range(2)]
        b_sg = bufs("gl_sg", 2)
        yo = [sb(f"gl_y{i}", [128, 512], BF16) for i in range(2)]
        b_yo = bufs("gl_y", 2)
        np_ = 0
        for tb in range(L // 512):
            i = tb % 2
            S.dma("sp", hT[i][:], scr.hs5T[:, tb * 512:(tb + 1) * 512].rearrange("(k p) t -> p k t", p=128),
                  reads=[scr.b_s5], writes=[b_hT[i]], owner=b_hT[i])
            for co in range(9):
                j = np_ % 2
                np_ += 1
                for kt in range(18):
                    S.op("pe", lambda e: e.matmul(g_ps[j][:], lhsT=W1[:, kt, co * 128:(co + 1) * 128], rhs=hT[i][:, kt, :], start=(kt == 0), stop=(kt == 17)),
                         reads=[b_W, b_hT[i]], writes=[b_gps[j]], inc=(kt == 17))
                for kt in range(18):
                    S.op("pe", lambda e: e.matmul(l_ps[j][:], lhsT=W2[:, kt, co * 128:(co + 1) * 128], rhs=hT[i][:, kt, :], start=(kt == 0), stop=(kt == 17)),
                         reads=[b_W, b_hT[i]], writes=[b_lps[j]], inc=(kt == 17))
                S.op("act", lambda e: e.activation(out=sg[j][:], in_=l_ps[j][:], func=AF.Sigmoid), reads=[b_lps[j]], writes=[b_sg[j]])
                S.op("dve", lambda e: e.tensor_tensor(out=yo[j][:], in0=g_ps[j][:], in1=sg[j][:], op=ALU.mult), reads=[b_gps[j], b_sg[j]], writes=[b_yo[j]])
                S.dma("sp", scr.ys5T[co * 128:(co + 1) * 128, tb * 512:(tb + 1) * 512], yo[j][:], reads=[b_yo[j]], writes=[scr.b_glu], owner=b_yo[j])
        S.barrier()
    with ExitStack() as es:
        sb = lambda n, s, d: es.enter_context(nc.sbuf_tensor(uniq(n), s, d))
        ps = lambda n, s, d: es.enter_context(nc.psum_tensor(uniq(n), s, d))
        Wssd = sb("mx_wssd", [128, 16, 1024], BF16)
        Watt = sb("mx_watt", [128, 3, 1024], BF16)
        Ws5 = sb("mx_ws5", [128, 9, 1024], BF16)
        Wout = sb("mx_wout", [128, 8, 1024], BF16)
        b_W = Buf("mx_w")
        stg = [sb(f"mx_st{i}", [128, 512], F32) for i in range(2)]
        b_stg = bufs("mx_st", 2)
        ctr = [0]
        load_w_bf16(S, prm["w_br_ssd"][l], Wssd, b_W, stg, b_stg, ctr)
        load_w_bf16(S, prm["w_br_attn"][l], Watt, b_W, stg, b_stg, ctr)
        load_w_bf16(S, prm["w_br_s5"][l], Ws5, b_W, stg, b_stg, ctr)
        load_w_bf16(S, prm["w_out"][l], Wout, b_W, stg, b_stg, ctr)
        bg = sb("mx_bg", [128, 3072], F32)
        lng = sb("mx_lng", [128, 1024], F32)
        lnb = sb("mx_lnb", [128, 1024], F32)
        b_par = Buf("mx_par")
        S.dma("sp", bg[:], prm["b_gate"][l].rearrange("a b -> (a b)").partition_broadcast(128), writes=[b_par], owner=b_par)
        S.dma("sp", lng[:], prm["ln1_g"][l].partition_broadcast(128), writes=[b_par], owner=b_par)
        S.dma("sp", lnb[:], prm["ln1_b"][l].partition_broadcast(128), writes=[b_par], owner=b_par)
        ys = [sb(f"mx_ys{i}", [128, 2048], BF16) for i in range(2)]
        b_ys = bufs("mx_ys", 2)
        ya = [sb(f"mx_ya{i}", [128, 384], BF16) for i in range(2)]
        b_ya = bufs("mx_ya", 2)
        y5T = [sb(f"mx_y5{i}", [128, 9, 128], BF16) for i in range(2)]
        b_y5T = bufs("mx_y5", 2)
        gt = [sb(f"mx_g{i}", [128, 3072], F32) for i in range(2)]
        b_gt = bufs("mx_g", 2)
        xt = [sb(f"mx_x{i}", [128, 1024], F32) for i in range(2)]
        b_xt = bufs("mx_x", 2)
        ysT = sb("mx_ysT", [128, 16, 128], BF16)
        b_ysT = Buf("mx_ysT")
        yaT = sb("mx_yaT", [128, 3, 128], BF16)
        b_yaT = Buf("mx_yaT")
        mT = sb("mx_mT", [128, 8, 128], BF16)
        b_mT = Buf("mx_mT")
        pt = [ps(f"mx_pt{i}", [128, 4, 128], BF16) for i in range(2)]
        b_pt = bufs("mx_pt", 2)
        Pb = [ps(f"mx_pb{i}", [128, 1024], F32) for i in range(2)]
        b_Pb = bufs("mx_pb", 2)
        acc = sb("mx_acc", [128, 1024], F32)
        b_acc = Buf("mx_acc")
        tmp = sb("mx_tmp", [128, 1024], F32)
        b_tmp = Buf("mx_tmp")
        mb = sb("mx_mb", [128, 1024], BF16)
        b_mb = Buf("mx_mb")
        hh = sb("mx_h", [128, 1024], F32)
        b_hh = Buf("mx_h")
        xo = [sb(f"mx_xo{i}", [128, 1024], F32) for i in range(2)]
        b_xo = bufs("mx_xo", 2)
        st = sb("mx_stt", [128, 16], F32)
        b_st = Buf("mx_stt")
        npt = 0
        npb = 0

        def transposes(src, nk, dst, b_src, b_dst):
            nonlocal npt
            for k0 in range(0, nk, 4):
                kn = min(4, nk - k0)
                j = npt % 2
                npt += 1
                for kk in range(kn):
                    S.op("pe", lambda e: e.transpose(out=pt[j][:, kk, :], in_=src[:, (k0 + kk) * 128:(k0 + kk + 1) * 128], identity=cx.identb[:]),
                         reads=[b_src, cx.b_const], writes=[b_pt[j]], inc=(kk == kn - 1))
                S.op("act", lambda e: e.copy(out=dst[:, k0:k0 + kn, :], in_=pt[j][:, 0:kn, :]), reads=[b_pt[j]], writes=[b_dst])

        for t in range(L // 128):
            i = t % 2
            rows = slice(t * 128, (t + 1) * 128)
            S.dma("sp", ys[i][:], scr.yssd[rows, :], reads=[scr.b_mix], writes=[b_ys[i]], owner=b_ys[i])
            S.dma("sp", ya[i][:], scr.yatt[rows, :], reads=[scr.b_mix], writes=[b_ya[i]], owner=b_ya[i])
            S.dma("sp", y5T[i][:], scr.ys5T[:, rows].rearrange("(k p) t -> p k t", p=128), reads=[scr.b_glu], writes=[b_y5T[i]], owner=b_y5T[i])
            S.dma("sp", gt[i][:], scr.gates[rows, :], reads=[scr.b_proj], writes=[b_gt[i]], owner=b_gt[i])
            S.dma("sp", xt[i][:], x_ap[rows, :], writes=[b_xt[i]], owner=b_xt[i])
            S.op("pool", lambda e: e.tensor_tensor(out=gt[i][:], in0=gt[i][:], in1=bg[:], op=ALU.add), reads=[b_gt[i], b_par], writes=[b_gt[i]])
            S.op("act", lambda e: e.activation(out=gt[i][:], in_=gt[i][:], func=AF.Sigmoid), reads=[b_gt[i]], writes=[b_gt[i]])
            transposes(ys[i], 16, ysT, b_ys[i], b_ysT)
            transposes(ya[i], 3, yaT, b_ya[i], b_yaT)
            for bi, (srcT, b_srcT, nk, W) in enumerate(((ysT, b_ysT, 16, Wssd), (yaT, b_yaT, 3, Watt), (y5T[i], b_y5T[i], 9, Ws5))):
                j = npb % 2
                npb += 1
                for nh in range(2):
                    for k in range(nk):
                        S.op("pe", lambda e: e.matmul(Pb[j][:, nh * 512:(nh + 1) * 512], lhsT=srcT[:, k, :], rhs=W[:, k, nh * 512:(nh + 1) * 512],
                                                      start=(k == 0), stop=(k == nk - 1)),
                             reads=[b_srcT, b_W], writes=[b_Pb[j]], inc=(k == nk - 1 and nh == 1))
                if bi == 0:
                    S.op("dve", lambda e: e.tensor_tensor(out=acc[:], in0=Pb[j][:], in1=gt[i][:, 0:1024], op=ALU.mult), reads=[b_Pb[j], b_gt[i]], writes=[b_acc])
                else:
                    S.op("dve", lambda e: e.tensor_tensor(out=tmp[:], in0=Pb[j][:], in1=gt[i][:, bi * 1024:(bi + 1) * 1024], op=ALU.mult),
                         reads=[b_Pb[j], b_gt[i]], writes=[b_tmp])
                    if bi == 1:
                        S.op("pool", lambda e: e.tensor_tensor(out=acc[:], in0=acc[:], in1=tmp[:], op=ALU.add), reads=[b_acc, b_tmp], writes=[b_acc])
                    else:
                        S.op("pool", lambda e: e.tensor_tensor(out=mb[:], in0=acc[:], in1=tmp[:], op=ALU.add), reads=[b_acc, b_tmp], writes=[b_mb])
            transposes(mb, 8, mT, b_mb, b_mT)
            j = npb % 2
            npb += 1
            for nh in range(2):
                for k in range(8):
                    S.op("pe", lambda e: e.matmul(Pb[j][:, nh * 512:(nh + 1) * 512], lhsT=mT[:, k, :], rhs=Wout[:, k, nh * 512:(nh + 1) * 512],
                                                  start=(k == 0), stop=(k == 7)),
                         reads=[b_mT, b_W], writes=[b_Pb[j]], inc=(k == 7 and nh == 1))
            S.op("dve", lambda e: e.scalar_tensor_tensor(out=hh[:], in0=xt[i][:], scalar=cfg.alpha, in1=Pb[j][:], op0=ALU.mult, op1=ALU.add),
                 reads=[b_xt[i], b_Pb[j]], writes=[b_hh])
            layer_norm_tile(S, hh, b_hh, xo[i], b_xo[i], lng, lnb, b_par, st, b_st)
            S.dma("sp", scr.x1[rows, :], xo[i][:], reads=[b_xo[i]], writes=[scr.b_x1], owner=b_xo[i])
        S.barrier()


def cast_dram_bf16(S, nc, src, dst, b_dst, stg, stb, b_stg, b_stb, ctr):
    R, N = src.shape
    for r in range(0, R, 128):
        i = ctr[0] % 2
        ctr[0] += 1
        S.dma("sp", stg[i][:, 0:N], src[r:r + 128, :], writes=[b_stg[i]], owner=b_stg[i])
        if ctr[0] % 2:
            S.op("pool", lambda e: e.tensor_copy(out=stb[i][:, 0:N], in_=stg[i][:, 0:N]), reads=[b_stg[i]], writes=[b_stb[i]])
        else:
            S.op("act", lambda e: e.copy(out=stb[i][:, 0:N], in_=stg[i][:, 0:N]), reads=[b_stg[i]], writes=[b_stb[i]])
        S.dma("sp", dst[r:r + 128, :], stb[i][:, 0:N], reads=[b_stb[i]], writes=[b_dst], owner=b_stb[i])


def phase_moe(S, nc, cx, cfg, prm, l, scr, xdst):
    L = cfg.L
    E = cfg.E
    with ExitStack() as es:
        sb = lambda n, s, d: es.enter_context(nc.sbuf_tensor(uniq(n), s, d))
        ps = lambda n, s, d: es.enter_context(nc.psum_tensor(uniq(n), s, d))
        stg = [sb(f"mo_cs{i}", [128, 2048], F32) for i in range(2)]
        stb = [sb(f"mo_cb{i}", [128, 2048], BF16) for i in range(2)]
        b_stg = bufs("mo_cs", 2)
        b_stb = bufs("mo_cb", 2)
        ctr = [0]
        for e_ in range(E):
            cast_dram_bf16(S, nc, prm["exp_w_gate_up"][l][e_], scr.wgu16[e_], scr.b_w16, stg, stb, b_stg, b_stb, ctr)
            cast_dram_bf16(S, nc, prm["exp_w_down"][l][e_], scr.wdn16[e_], scr.b_w16, stg, stb, b_stg, b_stb, ctr)
        Wr = sb("mo_wr", [128, 8, E], F32)
        Wrh = sb("mo_wrh", [128, 8, E], BF16)
        Wrl = sb("mo_wrl", [128, 8, E], BF16)
        rb = sb("mo_rb", [128, E], F32)
        b_wr = Buf("mo_wr")
        S.dma("sp", Wr[:], prm["router_w"][l].rearrange("(k p) e -> p k e", p=128), writes=[b_wr], owner=b_wr)
        S.dma("sp", rb[:], prm["router_b"][l].partition_broadcast(128), writes=[b_wr], owner=b_wr)
        S.op("dve", lambda e: e.tensor_copy(out=Wrh[:], in_=Wr[:]), reads=[b_wr], writes=[b_wr])
        S.op("dve", lambda e: e.tensor_tensor(out=Wr[:], in0=Wr[:], in1=Wrh[:], op=ALU.subtract), reads=[b_wr], writes=[b_wr])
        S.op("dve", lambda e: e.tensor_copy(out=Wrl[:], in_=Wr[:]), reads=[b_wr], writes=[b_wr])
        xt = [sb(f"mo_x{i}", [128, 1024], F32) for i in range(2)]
        b_xt = bufs("mo_x", 2)
        xh = sb("mo_xh", [128, 1024], BF16)
        xl = sb("mo_xl", [128, 1024], BF16)
        b_xhl = Buf("mo_xhl")
        xTl = sb("mo_xTl", [128, 8, 128], BF16)
        b_xTl = Buf("mo_xTl")
        xTb = [sb(f"mo_xTb{i}", [128, 8, 128], BF16) for i in range(2)]
        b_xTb = bufs("mo_xTb", 2)
        ptf = [ps(f"mo_ptf{i}", [128, 4, 128], BF16) for i in range(2)]
        b_ptf = bufs("mo_ptf", 2)
        lg_ps = ps("mo_lg", [128, E], F32)
        b_lgps = Buf("mo_lg")
        lg = sb("mo_lgs", [128, E], F32)
        b_lg = Buf("mo_lgs")
        v8 = sb("mo_v8", [128, 8], F32)
        b_v8 = Buf("mo_v8")
        mk = sb("mo_mk", [128, E], F32)
        b_mk = Buf("mo_mk")
        sm = sb("mo_sm", [128, 4], F32)
        b_sm = Buf("mo_sm")
        gts = [sb(f"mo_gt{i}", [128, E], F32) for i in range(2)]
        b_gts = bufs("mo_gt", 2)
        npt = 0
        for t in range(L // 128):
            i = t % 2
            rows = slice(t * 128, (t + 1) * 128)
            S.dma("sp", xt[i][:], scr.x1[rows, :], reads=[scr.b_x1], writes=[b_xt[i]], owner=b_xt[i])
            S.op("dve", lambda e: e.tensor_copy(out=xh[:], in_=xt[i][:]), reads=[b_xt[i]], writes=[b_xhl])
            S.op("dve", lambda e: e.tensor_tensor(out=xl[:], in0=xt[i][:], in1=xh[:], op=ALU.subtract), reads=[b_xt[i], b_xhl], writes=[b_xhl])
            for src, dstT, b_dstT in ((xh, xTb[i], b_xTb[i]), (xl, xTl, b_xTl)):
                for k4 in range(2):
                    j = npt % 2
                    npt += 1
                    for kk in range(4):
                        k = k4 * 4 + kk
                        S.op("pe", lambda e: e.transpose(out=ptf[j][:, kk, :], in_=src[:, k * 128:(k + 1) * 128], identity=cx.identb[:]),
                             reads=[b_xhl, cx.b_const], writes=[b_ptf[j]], inc=(kk == 3))
                    S.op("act", lambda e: e.copy(out=dstT[:, k4 * 4:(k4 + 1) * 4, :], in_=ptf[j][:]), reads=[b_ptf[j]], writes=[b_dstT])
            S.dma("sp", scr.x1T[:, rows].rearrange("(k p) t -> p k t", p=128), xTb[i][:], reads=[b_xTb[i]], writes=[scr.b_x1T], owner=b_xTb[i])
            n_mm = 0
            for k in range(8):
                for (xa, b_xa, wa) in ((xTb[i], b_xTb[i], Wrh), (xTl, b_xTl, Wrh), (xTb[i], b_xTb[i], Wrl)):
                    S.op("pe", lambda e: e.matmul(lg_ps[:], lhsT=xa[:, k, :], rhs=wa[:, k, :], start=(n_mm == 0), stop=(n_mm == 23)),
                         reads=[b_xa, b_wr], writes=[b_lgps], inc=(n_mm == 23))
                    n_mm += 1
            S.op("dve", lambda e: e.tensor_tensor(out=lg[:], in0=lg_ps[:], in1=rb[:], op=ALU.add), reads=[b_lgps, b_wr], writes=[b_lg])
            S.op("dve", lambda e: e.max(out=v8[:], in_=lg[:]), reads=[b_lg], writes=[b_v8])
            S.op("dve", lambda e: e.tensor_scalar(out=mk[:], in0=lg[:], scalar1=v8[:, 3:4], scalar2=None, op0=ALU.is_ge), reads=[b_lg, b_v8], writes=[b_mk])
            S.op("dve", lambda e: e.tensor_scalar(out=sm[:, 0:1], in0=v8[:, 0:1], scalar1=-1.0, scalar2=None, op0=ALU.mult), reads=[b_v8], writes=[b_sm])
            S.op("act", lambda e: e.activation(out=lg[:], in_=lg[:], func=AF.Exp, bias=sm[:, 0:1], scale=1.0), reads=[b_lg, b_sm], writes=[b_lg])
            S.op("dve", lambda e: e.tensor_tensor(out=lg[:], in0=lg[:], in1=mk[:], op=ALU.mult), reads=[b_lg, b_mk], writes=[b_lg])
            S.op("dve", lambda e: e.reduce_sum(out=sm[:, 1:2], in_=lg[:], axis=AX.X), reads=[b_lg, b_sm], writes=[b_sm])
            S.op("dve", lambda e: e.reciprocal(out=sm[:, 2:3], in_=sm[:, 1:2]), reads=[b_sm], writes=[b_sm])
            S.op("dve", lambda e: e.tensor_scalar(out=gts[i][:], in0=lg[:], scalar1=sm[:, 2:3], scalar2=None, op0=ALU.mult), reads=[b_lg, b_sm], writes=[b_gts[i]])
            S.dma("sp", scr.rgate[rows, :], gts[i][:], reads=[b_gts[i]], writes=[scr.b_x1T], owner=b_gts[i])
        S.barrier()
    TBm = 512
    with ExitStack() as es:
        sb = lambda n, s, d: es.enter_context(nc.sbuf_tensor(uniq(n), s, d))
        ps = lambda n, s, d: es.enter_context(nc.psum_tensor(uniq(n), s, d))
        Wgu = [sb(f"me_wgu{i}", [128, 8, 2048], BF16) for i in range(2)]
        Wdn = [sb(f"me_wdn{i}", [128, 8, 1024], BF16) for i in range(2)]
        b_Wgu = bufs("me_wgu", 2)
        b_Wdn = bufs("me_wdn", 2)
        bgu = sb("me_bgu", [128, E, 16], F32)
        bdn = sb("me_bdn", [E, 1024], F32)
        bdn16 = sb("me_bdn16", [E, 1024], BF16)
        lng = sb("me_lng", [128, 1024], F32)
        lnb = sb("me_lnb", [128, 1024], F32)
        b_par = Buf("me_par")
        with nc.allow_non_contiguous_dma(reason="tiny param load"):
            for e_ in range(E):
                S.dma("sp", bgu[:, e_, :], prm["exp_b_gate_up"][l][e_].rearrange("(c p) -> p c", p=128), writes=[b_par], owner=b_par)
        S.op("dve", lambda e: e.tensor_scalar(out=bgu[:, :, 8:16], in0=bgu[:, :, 8:16], scalar1=1.0, scalar2=None, op0=ALU.add), reads=[b_par], writes=[b_par])
        S.dma("sp", bdn[:], prm["exp_b_down"][l], writes=[b_par], owner=b_par)
        S.op("dve", lambda e: e.tensor_copy(out=bdn16[:], in_=bdn[:]), reads=[b_par], writes=[b_par])
        S.dma("sp", lng[:], prm["ln2_g"][l].partition_broadcast(128), writes=[b_par], owner=b_par)
        S.dma("sp", lnb[:], prm["ln2_b"][l].partition_broadcast(128), writes=[b_par], owner=b_par)
        xT = [sb(f"me_xT{i}", [128, 8, TBm], BF16) for i in range(2)]
        b_xT = bufs("me_xT", 2)
        gt = [sb(f"me_gt{i}", [128, 4, E], F32) for i in range(2)]
        b_gt = bufs("me_gt", 2)
        gT = sb("me_gT", [E, 4, 128], BF16)
        gtb = sb("me_gtb", [128, 4, E], BF16)
        b_gtb = Buf("me_gtb")
        b_gT = Buf("me_gT")
        acc = sb("me_acc", [128, 4, 1024], F32)
        b_acc = bufs("me_acc", 4)
        act = [sb(f"me_act{i}", [128, 8, TBm], BF16) for i in range(2)]
        b_act = bufs("me_act", 2)
        t1 = [sb(f"me_t1{i}", [128, TBm], F32) for i in range(2)]
        b_t1 = bufs("me_t1", 2)
        t2 = [sb(f"me_t2{i}", [128, TBm], F32) for i in range(2)]
        b_t2 = bufs("me_t2", 2)
        sg = [sb(f"me_sg{i}", [128, TBm], F32) for i in range(2)]
        b_sg = bufs("me_sg", 2)
        g_ps = [ps(f"me_pg{i}", [128, 512], F32) for i in range(2)]
        b_gps = bufs("me_pg", 2)
        l_ps = [ps(f"me_pl{i}", [128, 512], F32) for i in range(2)]
        b_lps = bufs("me_pl", 2)
        y_ps = [ps(f"me_py{i}", [128, 512], F32) for i in range(2)]
        b_yps = bufs("me_py", 2)
        ptg = ps("me_ptg", [E, 4, 128], BF16)
        b_ptg = Buf("me_ptg")
        x1t = [sb(f"me_x1{i}", [128, 1024], F32) for i in range(1)] * 2
        b_x1t = bufs("me_x1", 1) * 2
        xo = [sb(f"me_xo{i}", [128, 1024], F32) for i in range(1)] * 2
        b_xo = bufs("me_xo", 1) * 2
        st = sb("me_stt", [128, 16], F32)
        b_st = Buf("me_stt")
        nw = 0
        nfp = 0
        nyp = 0
        nx1 = 0
        for tb in range(L // TBm):
            bi = tb % 2
            cols = slice(tb * TBm, (tb + 1) * TBm)
            S.dma("sp", xT[bi][:], scr.x1T[:, cols].rearrange("(k p) t -> p k t", p=128), reads=[scr.b_x1T], writes=[b_xT[bi]], owner=b_xT[bi])
            S.dma("sp", gt[bi][:], scr.rgate[cols, :].rearrange("(a p) e -> p a e", p=128), reads=[scr.b_x1T], writes=[b_gt[bi]], owner=b_gt[bi])
            S.op("dve", lambda e: e.tensor_copy(out=gtb[:], in_=gt[bi][:]), reads=[b_gt[bi]], writes=[b_gtb])
            for tt in range(4):
                S.op("pe", lambda e: e.transpose(out=ptg[:, tt, :], in_=gtb[:, tt, :], identity=cx.identb[:]),
                     reads=[b_gtb, cx.b_const], writes=[b_ptg], inc=(tt == 3))
            S.op("act", lambda e: e.copy(out=gT[:], in_=ptg[:]), reads=[b_ptg], writes=[b_gT])
            for tt in range(4):
                for nh in range(2):
                    j = nyp % 2
                    nyp += 1
                    S.op("pe", lambda e: e.matmul(y_ps[j][:], lhsT=gT[:, tt, :], rhs=bdn16[:, nh * 512:(nh + 1) * 512], start=True, stop=True),
                         reads=[b_gT, b_par], writes=[b_yps[j]])
                    S.op("act", lambda e: e.copy(out=acc[:, tt, nh * 512:(nh + 1) * 512], in_=y_ps[j][:]), reads=[b_yps[j]], writes=[b_acc[tt]])
            for e_ in range(E):
                wi = nw % 2
                nw += 1
                S.dma("sp", Wgu[wi][:], scr.wgu16[e_].rearrange("(k p) f -> p k f", p=128), reads=[scr.b_w16], writes=[b_Wgu[wi]], owner=b_Wgu[wi])
                S.dma("sp", Wdn[wi][:], scr.wdn16[e_].rearrange("(k p) f -> p k f", p=128), reads=[scr.b_w16], writes=[b_Wdn[wi]], owner=b_Wdn[wi])
                ai = nw % 2
                for fj in range(8):
                    j = nfp % 2
                    nfp += 1
                    for k in range(8):
                        S.op("pe", lambda e: e.matmul(g_ps[j][:], lhsT=Wgu[wi][:, k, fj * 128:(fj + 1) * 128], rhs=xT[bi][:, k, :], start=(k == 0), stop=(k == 7)),
                             reads=[b_Wgu[wi], b_xT[bi]], writes=[b_gps[j]], inc=(k == 7))
                    for k in range(8):
                        S.op("pe", lambda e: e.matmul(l_ps[j][:], lhsT=Wgu[wi][:, k, 1024 + fj * 128:1024 + (fj + 1) * 128], rhs=xT[bi][:, k, :], start=(k == 0), stop=(k == 7)),
                             reads=[b_Wgu[wi], b_xT[bi]], writes=[b_lps[j]], inc=(k == 7))
                    S.op("dve", lambda e: e.tensor_scalar(out=t1[j][:], in0=g_ps[j][:], scalar1=bgu[:, e_, fj:fj + 1], scalar2=7.0, op0=ALU.add, op1=ALU.min),
                         reads=[b_gps[j], b_par], writes=[b_t1[j]])
                    S.op("act", lambda e: e.activation(out=sg[j][:], in_=t1[j][:], func=AF.Sigmoid, scale=1.702), reads=[b_t1[j]], writes=[b_sg[j]])
                    S.op("dve", lambda e: e.tensor_scalar(out=t2[j][:], in0=l_ps[j][:], scalar1=bgu[:, e_, 8 + fj:9 + fj], scalar2=8.0, op0=ALU.add, op1=ALU.min),
                         reads=[b_lps[j], b_par], writes=[b_t2[j]])
                    S.op("pool", lambda e: e.tensor_tensor(out=t1[j][:], in0=t1[j][:], in1=sg[j][:], op=ALU.mult), reads=[b_t1[j], b_sg[j]], writes=[b_t1[j]])
                    S.op("dve", lambda e: e.scalar_tensor_tensor(out=act[ai][:, fj, :], in0=t2[j][:], scalar=-6.0, in1=t1[j][:], op0=ALU.max, op1=ALU.mult),
                         reads=[b_t1[j], b_t2[j]], writes=[b_act[ai]])
                for tt in range(4):
                    for nh in range(2):
                        j = nyp % 2
                        nyp += 1
                        for fk in range(8):
                            S.op("pe", lambda e: e.matmul(y_ps[j][:], lhsT=act[ai][:, fk, tt * 128:(tt + 1) * 128], rhs=Wdn[wi][:, fk, nh * 512:(nh + 1) * 512],
                                                          start=(fk == 0), stop=(fk == 7)),
                                 reads=[b_act[ai], b_Wdn[wi]], writes=[b_yps[j]], inc=(fk == 7))
                        S.op("dve", lambda e: e.scalar_tensor_tensor(out=acc[:, tt, nh * 512:(nh + 1) * 512], in0=y_ps[j][:], scalar=gt[bi][:, tt, e_:e_ + 1],
                                                                     in1=acc[:, tt, nh * 512:(nh + 1) * 512], op0=ALU.mult, op1=ALU.add),
                             reads=[b_yps[j], b_gt[bi], b_acc[tt]], writes=[b_acc[tt]])
            for tt in range(4):
                xi = nx1 % 2
                nx1 += 1
                rows = slice(tb * TBm + tt * 128, tb * TBm + (tt + 1) * 128)
                S.dma("sp", x1t[xi][:], scr.x1[rows, :], reads=[scr.b_x1], writes=[b_x1t[xi]], owner=b_x1t[xi])
                S.op("dve", lambda e: e.scalar_tensor_tensor(out=x1t[xi][:], in0=x1t[xi][:], scalar=cfg.alpha, in1=acc[:, tt, :], op0=ALU.mult, op1=ALU.add),
                     reads=[b_x1t[xi], b_acc[tt]], writes=[b_x1t[xi]])
                layer_norm_tile(S, x1t[xi], b_x1t[xi], xo[xi], b_xo[xi], lng, lnb, b_par, st, b_st)
                S.dma("sp", xdst[rows, :], xo[xi][:], reads=[b_xo[xi]], writes=[scr.b_xcur], owner=b_xo[xi])
        S.barrier()


TWO_PI = 2.0 * math.pi


def phase_s5(S, nc, cx, cfg, prm, l, scr):
    L = cfg.L
    NCk = L // 8
    J = int(math.log2(NCk))
    CB = min(512, NCk)
    with ExitStack() as es:
        sb = lambda n, s, d: es.enter_context(nc.sbuf_tensor(uniq(n), s, d))
        ps = lambda n, s, d: es.enter_context(nc.psum_tensor(uniq(n), s, d))
        bP = Buf("s5_par")
        NP = 72

        def T(name, cols, dt=F32):
            return sb("s5_" + name, [128, cols], dt)

        ar, ai, ls = T("ar", NP), T("ai", NP), T("ls", NP)
        with nc.allow_non_contiguous_dma(reason="tiny param load"):
            for d in range(2):
                S.dma("sp", ar[:, d * 36:(d + 1) * 36], prm["s5_a_re"][l][d].rearrange("(p g) n -> (g n) p", g=2), writes=[bP], owner=bP)
                S.dma("sp", ai[:, d * 36:(d + 1) * 36], prm["s5_a_im"][l][d].rearrange("(p g) n -> (g n) p", g=2), writes=[bP], owner=bP)
                for gl in range(2):
                    S.dma("sp", ls[64 * gl:64 * gl + 64, d * 36:(d + 1) * 36],
                          prm["s5_log_step"][l][d].rearrange("(p g) -> g p", g=2)[gl].partition_broadcast(64), writes=[bP], owner=bP)
        BR = sb("s5_BR", [128, 36, 16], F32)
        BI = sb("s5_BI", [128, 36, 16], F32)
        S.dma("sp", BR[:], prm["s5_b_re"][l].rearrange("(p g) n c -> (g n) p c", g=2), writes=[bP], owner=bP)
        S.dma("sp", BI[:], prm["s5_b_im"][l].rearrange("(p g) n c -> (g n) p c", g=2), writes=[bP], owner=bP)
        dpad = sb("s5_dpad", [128, 18], F32)
        S.op("pool", lambda e: e.memset(dpad[:], 0.0), writes=[bP])
        with nc.allow_non_contiguous_dma(reason="tiny param load"):
            for g4 in range(4):
                S.dma("sp", dpad[32 * g4:32 * g4 + 16, :], prm["s5_d"][l].rearrange("(t g c) -> g c t", g=4, c=16)[g4], writes=[bP], owner=bP)

        def dv(fn, eng="dve"):
            S.op(eng, fn, reads=[bP], writes=[bP])

        def tt(out, a, b, op, eng="dve"):
            dv(lambda e: e.tensor_tensor(out=out, in0=a, in1=b, op=op), eng)

        def ts(out, a, s1, op0, s2=None, op1=None):
            if op1 is None:
                dv(lambda e: e.tensor_scalar(out=out, in0=a, scalar1=s1, scalar2=None, op0=op0))
            else:
                dv(lambda e: e.tensor_scalar(out=out, in0=a, scalar1=s1, scalar2=s2, op0=op0, op1=op1))

        def act(out, in_, func, scale=1.0):
            S.op("act", lambda e: e.activation(out=out, in_=in_, func=func, scale=scale), reads=[bP], writes=[bP])

        step, sr, th, mag = T("step", NP), T("sr", NP), T("th", NP), T("mag", NP)
        t0, t1_, t2_, ki = T("t0", NP), T("t1", NP), T("t2", NP), sb("s5_ki", [128, NP], I32)
        sn, cs = T("sn", NP), T("cs", NP)
        act(step[:], ls[:], AF.Exp)
        tt(sr[:], step[:], ar[:], ALU.mult)
        tt(th[:], step[:], ai[:], ALU.mult)
        act(mag[:], sr[:], AF.Exp)

        def sin_of(out, ang):
            ts(t0[:], ang, 1.0 / TWO_PI, ALU.mult, 0.5, ALU.add)
            dv(lambda e: e.tensor_copy(out=ki[:], in_=t0[:]))
            dv(lambda e: e.tensor_copy(out=t1_[:], in_=ki[:]))
            dv(lambda e: e.scalar_tensor_tensor(out=t2_[:], in0=t1_[:], scalar=-TWO_PI, in1=ang, op0=ALU.mult, op1=ALU.add))
            ts(t0[:], t2_[:], -math.pi, ALU.is_lt, TWO_PI, ALU.mult)
            tt(t2_[:], t2_[:], t0[:], ALU.add)
            ts(t0[:], t2_[:], math.pi, ALU.is_gt, -TWO_PI, ALU.mult)
            tt(t2_[:], t2_[:], t0[:], ALU.add)
            act(out, t2_[:], AF.Sin)

        thc = T("thc", NP)
        sin_of(sn[:], th[:])
        ts(thc[:], th[:], math.pi / 2, ALU.add)
        sin_of(cs[:], thc[:])
        abr, abi = T("abr", NP), T("abi", NP)
        tt(abr[:], mag[:], cs[:], ALU.mult)
        tt(abi[:], mag[:], sn[:], ALU.mult)
        den, m1, fr, fi = T("den", NP), T("m1", NP), T("fr", NP), T("fi", NP)
        tt(den[:], ar[:], ar[:], ALU.mult)
        tt(t0[:], ai[:], ai[:], ALU.mult)
        tt(den[:], den[:], t0[:], ALU.add)
        dv(lambda e: e.reciprocal(out=den[:], in_=den[:]))
        ts(m1[:], abr[:], -1.0, ALU.add)
        tt(fr[:], m1[:], ar[:], ALU.mult)
        tt(t0[:], abi[:], ai[:], ALU.mult)
        tt(fr[:], fr[:], t0[:], ALU.add)
        tt(fr[:], fr[:], den[:], ALU.mult)
        tt(fi[:], abi[:], ar[:], ALU.mult)
        tt(t0[:], m1[:], ai[:], ALU.mult)
        tt(fi[:], fi[:], t0[:], ALU.subtract)
        tt(fi[:], fi[:], den[:], ALU.mult)
        ivr, ivi = T("ivr", NP), T("ivi", NP)
        act(t0[:], sr[:], AF.Exp, scale=-2.0)
        tt(ivr[:], abr[:], t0[:], ALU.mult)
        tt(ivi[:], abi[:], t0[:], ALU.mult)
        ts(ivi[:], ivi[:], -1.0, ALU.mult)
        PWr = sb("s5_PWr", [128, 9, NP], F32)
        PWi = sb("s5_PWi", [128, 9, NP], F32)
        NGr = sb("s5_NGr", [128, 9, NP], F32)
        NGi = sb("s5_NGi", [128, 9, NP], F32)
        PWrR = sb("s5_PWrR", [128, 9, NP], F32)
        PWiR = sb("s5_PWiR", [128, 9, NP], F32)
        NGrR = sb("s5_NGrR", [128, 9, NP], F32)
        NGiR = sb("s5_NGiR", [128, 9, NP], F32)

        def cmul(or_, oi_, ar_, ai_, br_, bi_):
            tt(t0[:], ar_, br_, ALU.mult)
            tt(t1_[:], ai_, bi_, ALU.mult)
            tt(t2_[:], ar_, bi_, ALU.mult)
            tt(thc[:], ai_, br_, ALU.mult)
            tt(or_, t0[:], t1_[:], ALU.subtract)
            tt(oi_, t2_[:], thc[:], ALU.add)

        for (Pr, Pi, br_, bi_) in ((PWr, PWi, abr, abi), (NGr, NGi, ivr, ivi)):
            dv(lambda e: e.memset(Pr[:, 0, :], 1.0), "pool")
            dv(lambda e: e.memset(Pi[:, 0, :], 0.0), "pool")
            for e_ in range(1, 9):
                cmul(Pr[:, e_, :], Pi[:, e_, :], Pr[:, e_ - 1, :], Pi[:, e_ - 1, :], br_[:], bi_[:])
        for (Pr, PrR) in ((PWr, PWrR), (PWi, PWiR), (NGr, NGrR), (NGi, NGiR)):
            for e_ in range(9):
                dv(lambda e: e.tensor_copy(out=PrR[:, e_, :], in_=Pr[:, 8 - e_, :]), "pool")
        SPr = sb("s5_SPr", [128, J + 1, NP], F32)
        SPi = sb("s5_SPi", [128, J + 1, NP], F32)
        SPn = sb("s5_SPn", [128, J + 1, NP], F32)
        dv(lambda e: e.tensor_copy(out=SPr[:, 0, :], in_=PWr[:, 8, :]))
        dv(lambda e: e.tensor_copy(out=SPi[:, 0, :], in_=PWi[:, 8, :]))
        for j in range(1, J + 1):
            cmul(SPr[:, j, :], SPi[:, j, :], SPr[:, j - 1, :], SPi[:, j - 1, :], SPr[:, j - 1, :], SPi[:, j - 1, :])
        ts(SPn[:], SPi[:], -1.0, ALU.mult)
        bbr = sb("s5_bbr", [128, 2, 36, 16], F32)
        bbi = sb("s5_bbi", [128, 2, 36, 16], F32)
        tb1 = sb("s5_tb1", [128, 2, 36, 16], F32)
        frv = fr[:].rearrange("p (d q) -> p d q", d=2).unsqueeze(3).to_broadcast([128, 2, 36, 16])
        fiv = fi[:].rearrange("p (d q) -> p d q", d=2).unsqueeze(3).to_broadcast([128, 2, 36, 16])
        BRv = BR[:].unsqueeze(1).to_broadcast([128, 2, 36, 16])
        BIv = BI[:].unsqueeze(1).to_broadcast([128, 2, 36, 16])
        tt(bbr[:], frv, BRv, ALU.mult)
        tt(tb1[:], fiv, BIv, ALU.mult)
        tt(bbr[:], bbr[:], tb1[:], ALU.subtract)
        tt(bbi[:], frv, BIv, ALU.mult)
        tt(tb1[:], fiv, BRv, ALU.mult)
        tt(bbi[:], bbi[:], tb1[:], ALU.add)
        CRT = sb("s5_CRT", [128, 2, 36, 16], F32)
        CIT = sb("s5_CIT", [128, 2, 36, 16], F32)
        cin = sb("s5_cin", [128, 128], F32)
        cinb = sb("s5_cinb", [128, 128], BF16)
        pc = ps("s5_pc", [128, 128], BF16)
        b_pc = Buf("s5_pc")
        for (cname, CT_) in (("s5_c_re", CRT), ("s5_c_im", CIT)):
            for d in range(2):
                for pb in range(0, 36, 8):
                    npair = min(8, 36 - pb)
                    for q in range(npair):
                        pr_ = pb + q
                        S.dma("sp", cin[16 * q:16 * q + 16, :].rearrange("c (g n) -> c g n", g=2),
                              prm[cname][l][d][2 * pr_:2 * pr_ + 2].rearrange("g c n -> c g n"), writes=[bP], owner=bP)
                    dv(lambda e: e.tensor_copy(out=cinb[:], in_=cin[:]))
                    S.op("pe", lambda e: e.transpose(out=pc[:], in_=cinb[:], identity=cx.identb[:]), reads=[bP, cx.b_const], writes=[b_pc])
                    S.op("act", lambda e: e.copy(out=CT_[:, d, pb:pb + npair, :], in_=pc[:, 0:npair * 16].rearrange("p (q c) -> p q c", c=16)),
                         reads=[b_pc], writes=[bP])

        Uraw = sb("s5_Uraw", [128, L], BF16)
        b_Ur = Buf("s5_Ur")
        Us = sb("s5_Us", [128, 8, NCk], BF16)
        b_U = Buf("s5_U")
        Wi = sb("s5_Wi", [128, 8, 8, 128], BF16)
        b_Wi = Buf("s5_Wi")
        WOre = [sb(f"s5_WOre{i}", [128, 8, 128], BF16) for i in range(4)]
        WOim = [sb(f"s5_WOim{i}", [128, 8, 128], BF16) for i in range(4)]
        b_WO = bufs("s5_WO", 4)
        Lre = sb("s5_Lre", [128, 8, 128], BF16)
        Lim = sb("s5_Lim", [128, 8, 128], BF16)
        b_L = Buf("s5_L")
        LTre = sb("s5_LTre", [128, 8, 128], BF16)
        LTim = sb("s5_LTim", [128, 8, 128], BF16)
        b_LT = Buf("s5_LT")
        Rr = sb("s5_Rr", [128, 8, 16], F32)
        Ri = sb("s5_Ri", [128, 8, 16], F32)
        Rt = sb("s5_Rt", [128, 8, 16], F32)
        b_R = Buf("s5_R")
        ZR = [sb(f"s5_ZR{i}", [128, NCk], F32) for i in range(2)]
        ZI = [sb(f"s5_ZI{i}", [128, NCk], F32) for i in range(2)]
        b_Z = bufs("s5_Z", 2)
        ztmp = sb("s5_ztmp", [128, NCk], F32)
        b_ztmp = Buf("s5_ztmp")
        HR = [sb(f"s5_HR{i}", [128, NCk], BF16) for i in range(4)]
        HI = [sb(f"s5_HI{i}", [128, NCk], BF16) for i in range(4)]
        b_H = bufs("s5_H", 4)
        pt4 = ps("s5_pt4", [128, 4, 128], BF16)
        b_pt4 = Buf("s5_pt4")
        pK = [ps(f"s5_pK{i}", [128, 4, 128], F32) for i in range(2)]
        b_pK = bufs("s5_pK", 2)
        pS = [ps(f"s5_pS{i}", [128, CB], F32) for i in range(2)]
        b_pS = bufs("s5_pS", 2)
        pY = [ps(f"s5_pY{i}", [128, CB], F32) for i in range(2)]
        b_pY = bufs("s5_pY", 2)
        Dd = sb("s5_Dd", [128, 128], F32)
        b_Dd = Buf("s5_Dd")
        g1 = [sb(f"s5_g1{i}", [128, CB], F32) for i in range(2)]
        g2 = [sb(f"s5_g2{i}", [128, CB], F32) for i in range(2)]
        b_g = bufs("s5_g", 2)
        Yo = Uraw[:].rearrange("p (c t) -> p c t", t=8)
        b_Yo = b_Ur
        npk = 0
        npy = 0
        for tl in range(18):
            S.dma("sp", Uraw[:], scr.uT[tl * 128:(tl + 1) * 128, :], reads=[scr.b_proj], writes=[b_Ur], owner=b_Ur)
            S.op("pool", lambda e: e.tensor_copy(out=Us[:], in_=Uraw[:].rearrange("p (c s) -> p s c", s=8)), reads=[b_Ur], writes=[b_U])
            S.op("pool", lambda e: e.memset(Wi[:].rearrange("p s t c -> p (s t c)"), 0.0), writes=[b_Wi])
            S.op("dve", lambda e: e.tensor_scalar(out=Dd[:], in0=cx.identf[:], scalar1=dpad[:, tl:tl + 1], scalar2=None, op0=ALU.mult),
                 reads=[cx.b_const, bP], writes=[b_Dd])
            for pp in range(2):
                pr_ = 2 * tl + pp
                c0 = 64 * pp
                for d in range(2):
                    k = pp * 2 + d
                    dp = d * 36 + pr_
                    PrT, PiT = (PWr, PWi) if d == 0 else (PWrR, PWiR)
                    NrT, NiT = (NGr, NGi) if d == 0 else (NGrR, NGiR)
                    esl = slice(1, 9) if d == 0 else slice(0, 8)
                    pwr = PrT[:, esl, dp].unsqueeze(2).to_broadcast([128, 8, 16])
                    pwi = PiT[:, esl, dp].unsqueeze(2).to_broadcast([128, 8, 16])
                    ngr = NrT[:, esl, dp].unsqueeze(2).to_broadcast([128, 8, 16])
                    ngi = NiT[:, esl, dp].unsqueeze(2).to_broadcast([128, 8, 16])
                    crt = CRT[:, d, pr_, :].unsqueeze(1).to_broadcast([128, 8, 16])
                    cit = CIT[:, d, pr_, :].unsqueeze(1).to_broadcast([128, 8, 16])
                    bbrv = bbr[:, d, pr_, :].unsqueeze(1).to_broadcast([128, 8, 16])
                    bbiv = bbi[:, d, pr_, :].unsqueeze(1).to_broadcast([128, 8, 16])

                    def cplx(ar_, ai_, br_, bi_):
                        S.op("dve", lambda e: e.tensor_tensor(out=Rr[:], in0=ar_, in1=br_, op=ALU.mult), reads=[bP], writes=[b_R])
                        S.op("dve", lambda e: e.tensor_tensor(out=Rt[:], in0=ai_, in1=bi_, op=ALU.mult), reads=[bP, b_R], writes=[b_R])
                        S.op("dve", lambda e: e.tensor_tensor(out=Rr[:], in0=Rr[:], in1=Rt[:], op=ALU.subtract), reads=[b_R], writes=[b_R])
                        S.op("dve", lambda e: e.tensor_tensor(out=Ri[:], in0=ar_, in1=bi_, op=ALU.mult), reads=[bP, b_R], writes=[b_R])
                        S.op("dve", lambda e: e.tensor_tensor(out=Rt[:], in0=ai_, in1=br_, op=ALU.mult), reads=[bP, b_R], writes=[b_R])
                        S.op("dve", lambda e: e.tensor_tensor(out=Ri[:], in0=Ri[:], in1=Rt[:], op=ALU.add), reads=[b_R], writes=[b_R])

                    cplx(crt, cit, pwr, pwi)
                    S.op("pool", lambda e: e.memset(WOre[k][:].rearrange("p t c -> p (t c)"), 0.0), writes=[b_WO[k]])
                    S.op("pool", lambda e: e.memset(WOim[k][:].rearrange("p t c -> p (t c)"), 0.0), writes=[b_WO[k]])
                    for gl in range(2):
                        prt = slice(64 * gl, 64 * gl + 64)
                        cl = slice(c0 + 32 * gl, c0 + 32 * gl + 16)
                        S.op("act", lambda e: e.copy(out=WOre[k][prt, :, cl], in_=Rr[prt, :, :]), reads=[b_R], writes=[b_WO[k]])
                        S.op("act", lambda e: e.activation(out=WOim[k][prt, :, cl], in_=Ri[prt, :, :], func=AF.Copy, scale=-1.0), reads=[b_R], writes=[b_WO[k]])
                    cplx(ngr, ngi, bbrv, bbiv)
                    S.op("pool", lambda e: e.memset(Lre[:].rearrange("p t c -> p (t c)"), 0.0), writes=[b_L])
                    S.op("pool", lambda e: e.memset(Lim[:].rearrange("p t c -> p (t c)"), 0.0), writes=[b_L])
                    for gl in range(2):
                        prt = slice(64 * gl, 64 * gl + 64)
                        cl = slice(c0 + 32 * gl, c0 + 32 * gl + 16)
                        S.op("act", lambda e: e.copy(out=Lre[prt, :, cl], in_=Rr[prt, :, :]), reads=[b_R], writes=[b_L])
                        S.op("act", lambda e: e.copy(out=Lim[prt, :, cl], in_=Ri[prt, :, :]), reads=[b_R], writes=[b_L])
                    for (Lx, LTx) in ((Lre, LTre), (Lim, LTim)):
                        for s4 in range(2):
                            for ss in range(4):
                                S.op("pe", lambda e: e.transpose(out=pt4[:, ss, :], in_=Lx[:, s4 * 4 + ss, :], identity=cx.identb[:]),
                                     reads=[b_L, cx.b_const], writes=[b_pt4], inc=(ss == 3))
                            S.op("act", lambda e: e.copy(out=LTx[:, s4 * 4:(s4 + 1) * 4, :], in_=pt4[:]), reads=[b_pt4], writes=[b_LT])
                    for s_ in range(8):
                        for th_ in range(2):
                            j = npk % 2
                            npk += 1
                            tsl = slice(th_ * 4, th_ * 4 + 4)
                            S.op("pe", lambda e: e.matmul(pK[j][:].rearrange("p t c -> p (t c)"), lhsT=Lre[:, s_, :],
                                                          rhs=WOre[k][:, tsl, :].rearrange("p t c -> p (t c)"), start=True, stop=False),
                                 reads=[b_L, b_WO[k]], writes=[b_pK[j]], inc=False)
                            S.op("pe", lambda e: e.matmul(pK[j][:].rearrange("p t c -> p (t c)"), lhsT=Lim[:, s_, :],
                                                          rhs=WOim[k][:, tsl, :].rearrange("p t c -> p (t c)"), start=False, stop=True),
                                 reads=[b_L, b_WO[k]], writes=[b_pK[j]])
                            for tq in range(4):
                                t_ = th_ * 4 + tq
                                use = (t_ >= s_) if d == 0 else (t_ <= s_)
                                if not use:
                                    continue
                                S.op("dve", lambda e: e.tensor_tensor(out=Wi[:, s_, t_, :], in0=pK[j][:, tq, :], in1=Wi[:, s_, t_, :], op=ALU.add),
                                     reads=[b_pK[j], b_Wi], writes=[b_Wi])
                    zi = 0
                    for cb in range(NCk // CB):
                        csl = slice(cb * CB, (cb + 1) * CB)
                        for s_ in range(8):
                            S.op("pe", lambda e: e.matmul(pS[0][:], lhsT=LTre[:, s_, :], rhs=Us[:, s_, csl], start=(s_ == 0), stop=(s_ == 7)),
                                 reads=[b_LT, b_U], writes=[b_pS[0]], inc=(s_ == 7))
                        for s_ in range(8):
                            S.op("pe", lambda e: e.matmul(pS[1][:], lhsT=LTim[:, s_, :], rhs=Us[:, s_, csl], start=(s_ == 0), stop=(s_ == 7)),
                                 reads=[b_LT, b_U], writes=[b_pS[1]], inc=(s_ == 7))
                        p8r, p8i, p8n = SPr[:, 0, dp:dp + 1], SPi[:, 0, dp:dp + 1], SPn[:, 0, dp:dp + 1]
                        S.op("dve", lambda e: e.tensor_scalar(out=ztmp[:, csl], in0=pS[1][:], scalar1=p8n, scalar2=None, op0=ALU.mult),
                             reads=[b_pS[1], bP], writes=[b_ztmp])
                        S.op("dve", lambda e: e.scalar_tensor_tensor(out=ZR[0][:, csl], in0=pS[0][:], scalar=p8r, in1=ztmp[:, csl], op0=ALU.mult, op1=ALU.add),
                             reads=[b_pS[0], b_ztmp, bP], writes=[b_Z[0]])
                        S.op("dve", lambda e: e.tensor_scalar(out=ztmp[:, csl], in0=pS[1][:], scalar1=p8r, scalar2=None, op0=ALU.mult),
                             reads=[b_pS[1], bP, b_Z[0]], writes=[b_ztmp])
                        S.op("dve", lambda e: e.scalar_tensor_tensor(out=ZI[0][:, csl], in0=pS[0][:], scalar=p8i, in1=ztmp[:, csl], op0=ALU.mult, op1=ALU.add),
                             reads=[b_pS[0], b_ztmp, bP], writes=[b_Z[0]])
                    cur = 0
                    for j in range(J):
                        sh = 1 << j
                        nx = 1 - cur
                        qr, qi, qn = SPr[:, j, dp:dp + 1], SPi[:, j, dp:dp + 1], SPn[:, j, dp:dp + 1]
                        if d == 0:
                            dst, src, keep = slice(sh, NCk), slice(0, NCk - sh), slice(0, sh)
                        else:
                            dst, src, keep = slice(0, NCk - sh), slice(sh, NCk), slice(NCk - sh, NCk)
                        S.op("act", lambda e: e.copy(out=ZR[nx][:, keep], in_=ZR[cur][:, keep]), reads=[b_Z[cur]], writes=[b_Z[nx]])
                        S.op("act", lambda e: e.copy(out=ZI[nx][:, keep], in_=ZI[cur][:, keep]), reads=[b_Z[cur]], writes=[b_Z[nx]])
                        S.op("dve", lambda e: e.scalar_tensor_tensor(out=ztmp[:, dst], in0=ZR[cur][:, src], scalar=qr, in1=ZR[cur][:, dst], op0=ALU.mult, op1=ALU.add),
                             reads=[b_Z[cur], bP], writes=[b_ztmp])
                        S.op("dve", lambda e: e.scalar_tensor_tensor(out=ZR[nx][:, dst], in0=ZI[cur][:, src], scalar=qn, in1=ztmp[:, dst], op0=ALU.mult, op1=ALU.add),
                             reads=[b_Z[cur], b_ztmp, bP], writes=[b_Z[nx]])
                        S.op("dve", lambda e: e.scalar_tensor_tensor(out=ztmp[:, dst], in0=ZI[cur][:, src], scalar=qr, in1=ZI[cur][:, dst], op0=ALU.mult, op1=ALU.add),
                             reads=[b_Z[cur], bP, b_Z[nx]], writes=[b_ztmp])
                        S.op("dve", lambda e: e.scalar_tensor_tensor(out=ZI[nx][:, dst], in0=ZR[cur][:, src], scalar=qi, in1=ztmp[:, dst], op0=ALU.mult, op1=ALU.add),
                             reads=[b_Z[cur], b_ztmp, bP], writes=[b_Z[nx]])
                        cur = nx
                    if d == 0:
                        S.op("pool", lambda e: e.memset(HR[k][:, 0:1], 0.0), writes=[b_H[k]])
                        S.op("pool", lambda e: e.memset(HI[k][:, 0:1], 0.0), writes=[b_H[k]])
                        S.op("act", lambda e: e.copy(out=HR[k][:, 1:NCk], in_=ZR[cur][:, 0:NCk - 1]), reads=[b_Z[cur]], writes=[b_H[k]])
                        S.op("act", lambda e: e.copy(out=HI[k][:, 1:NCk], in_=ZI[cur][:, 0:NCk - 1]), reads=[b_Z[cur]], writes=[b_H[k]])
                    else:
                        S.op("pool", lambda e: e.memset(HR[k][:, NCk - 1:NCk], 0.0), writes=[b_H[k]])
                        S.op("pool", lambda e: e.memset(HI[k][:, NCk - 1:NCk], 0.0), writes=[b_H[k]])
                        S.op("act", lambda e: e.copy(out=HR[k][:, 0:NCk - 1], in_=ZR[cur][:, 1:NCk]), reads=[b_Z[cur]], writes=[b_H[k]])
                        S.op("act", lambda e: e.copy(out=HI[k][:, 0:NCk - 1], in_=ZI[cur][:, 1:NCk]), reads=[b_Z[cur]], writes=[b_H[k]])
            for s_ in range(8):
                S.op("dve", lambda e: e.tensor_tensor(out=Wi[:, s_, s_, :], in0=Wi[:, s_, s_, :], in1=Dd[:], op=ALU.add), reads=[b_Wi, b_Dd], writes=[b_Wi])
            for cb in range(NCk // CB):
                csl = slice(cb * CB, (cb + 1) * CB)
                for t_ in range(8):
                    j = npy % 2
                    npy += 1
                    for s_ in range(8):
                        S.op("pe", lambda e: e.matmul(pY[j][:], lhsT=Wi[:, s_, t_, :], rhs=Us[:, s_, csl], start=(s_ == 0), stop=False),
                             reads=[b_Wi, b_U], writes=[b_pY[j]], inc=False)
                    for k in range(4):
                        S.op("pe", lambda e: e.matmul(pY[j][:], lhsT=WOre[k][:, t_, :], rhs=HR[k][:, csl], start=False, stop=False),
                             reads=[b_WO[k], b_H[k]], writes=[b_pY[j]], inc=False)
                        S.op("pe", lambda e: e.matmul(pY[j][:], lhsT=WOim[k][:, t_, :], rhs=HI[k][:, csl], start=False, stop=(k == 3)),
                             reads=[b_WO[k], b_H[k]], writes=[b_pY[j]], inc=(k == 3))
                    S.op("act", lambda e: e.activation(out=g1[j][:], in_=pY[j][:], func=AF.Square), reads=[b_pY[j]], writes=[b_g[j]])
                    S.op("dve", lambda e: e.tensor_scalar(out=g1[j][:], in0=g1[j][:], scalar1=0.044715, scalar2=1.0, op0=ALU.mult, op1=ALU.add),
                         reads=[b_g[j]], writes=[b_g[j]])
                    S.op("dve", lambda e: e.tensor_tensor(out=g1[j][:], in0=pY[j][:], in1=g1[j][:], op=ALU.mult), reads=[b_pY[j], b_g[j]], writes=[b_g[j]])
                    S.op("act", lambda e: e.activation(out=g2[j][:], in_=g1[j][:], func=AF.Sigmoid, scale=1.5957691216057308), reads=[b_g[j]], writes=[b_g[j]])
                    S.op("dve", lambda e: e.tensor_tensor(out=Yo[:, csl, t_], in0=pY[j][:], in1=g2[j][:], op=ALU.mult), reads=[b_pY[j], b_g[j], b_Yo], writes=[b_Yo])
            S.dma("sp", scr.hs5T[tl * 128:(tl + 1) * 128, :], Uraw[:], reads=[b_Yo], writes=[scr.b_s5], owner=b_Yo)
        S.barrier()
```

```python
import math
from contextlib import ExitStack
import numpy as np
import concourse.bass as bass
import concourse.mybir as mybir
from concourse.bass_utils import run_bass_kernel_spmd

F32 = mybir.dt.float32
BF16 = mybir.dt.bfloat16
I32 = mybir.dt.int32
AF = mybir.ActivationFunctionType
ALU = mybir.AluOpType
AX = mybir.AxisListType


class Cfg:
    def __init__(self, L=8192, depth=4, n_exp=32):
        self.L = L
        self.depth = depth
        self.E = n_exp
        self.D = 1024
        self.alpha = (2 * 4) ** 0.25


class Buf:
    __slots__ = ("name", "writers", "readers", "dsem", "multi")

    def __init__(self, name, multi=False):
        self.name = name
        self.writers = {}
        self.readers = {}
        self.dsem = None
        self.multi = multi


class Sched:
    def __init__(self, nc):
        self.nc = nc
        self.eng = {"pe": nc.tensor, "dve": nc.vector, "act": nc.scalar,
                    "pool": nc.gpsimd, "sp": nc.sync}
        self.sems = []
        self.esem = {}
        self.cnt = {}
        self.latest = {}
        for k in self.eng:
            self.esem[k] = self._newsem("e_" + k)
            self.cnt[k] = 0
        self.known = {k: {} for k in self.eng}
        self.n_inst = 0
        self.n_wait = 0
        self.dsem_pool = {}
        self.free_dsems = []
        self.epoch_owners = []

    def _newsem(self, name):
        h = self.nc.alloc_semaphore(name=name)
        self.sems.append(h)
        return len(self.sems) - 1

    def _wait(self, e, deps):
        kn = self.known[e]
        for s, c in deps.items():
            if s == self.esem[e] and c > self.cnt[e]:
                continue
            if kn.get(s, 0) < c:
                self.eng[e].wait_ge(self.sems[s], c)
                kn[s] = c
                self.n_wait += 1

    @staticmethod
    def _merge(d, src):
        for s, c in src.items():
            if d.get(s, 0) < c:
                d[s] = c

    def _deps(self, reads, writes):
        deps = {}
        for b in reads:
            self._merge(deps, b.writers)
        for b in writes:
            if b.multi:
                continue
            self._merge(deps, b.writers)
            self._merge(deps, b.readers)
        return deps

    def _track(self, s, c, reads, writes):
        for b in writes:
            if b.multi:
                if b.writers.get(s, 0) < c:
                    b.writers[s] = c
                continue
            b.writers = {s: c}
            b.readers = {}
        for b in reads:
            if b.readers.get(s, 0) < c:
                b.readers[s] = c

    def op(self, e, fn, reads=(), writes=(), inc=True):
        self._wait(e, self._deps(reads, writes))
        ins = fn(self.eng[e])
        self.n_inst += 1
        s = self.esem[e]
        if inc:
            self.cnt[e] += 1
            ins.then_inc(self.sems[s], 1)
            c = self.cnt[e]
            self.latest[s] = c
        else:
            c = self.cnt[e] + 1
        self._track(s, c, reads, writes)
        return ins

    def dma(self, q, out, in_, reads=(), writes=(), owner=None, **kw):
        self._wait(q, self._deps(reads, writes))
        if owner.dsem is None:
            if owner.name not in self.dsem_pool:
                if self.free_dsems:
                    self.dsem_pool[owner.name] = self.free_dsems.pop()
                else:
                    self.dsem_pool[owner.name] = [self._newsem("d%d" % len(self.sems)), 0]
            owner.dsem = self.dsem_pool[owner.name]
            self.epoch_owners.append(owner)
        ins = self.eng[q].dma_start(out=out, in_=in_, **kw)
        owner.dsem[1] += 16
        s, c = owner.dsem[0], owner.dsem[1]
        ins.then_inc(self.sems[s], 16)
        self.latest[s] = c
        self.n_inst += 1
        self._track(s, c, reads, writes)
        return ins

    def barrier(self):
        for e in self.eng:
            self._wait(e, dict(self.latest))
        keep = {k: v for k, v in self.dsem_pool.items() if k in ("inj", "dbg")}
        self.free_dsems.extend(v for k, v in self.dsem_pool.items() if k not in keep)
        self.dsem_pool = keep
        for b in self.epoch_owners:
            b.dsem = None
        self.epoch_owners = []


def dram_copy(S, dst, src, b):
    n = dst.shape[0]
    step = 128 if n >= 128 else n
    for r in range(0, n, step):
        S.dma("sp", dst[r:r + step, :], src[r:r + step, :], writes=[b], owner=b)


_UNIQ = [0]


def uniq(name):
    _UNIQ[0] += 1
    return "%s_%d" % (name, _UNIQ[0])


def bufs(prefix, n):
    return [Buf(f"{prefix}{i}") for i in range(n)]


D = 1024
SSD_INNER = 2048
SSD_HEADS = 32
SSD_GROUPS = 8
SSD_STATE = 128
CONV_CH = 4096
ATT_W = 1152
S5_W = 1152
S5_G = 72
N_IN = 13888
C_GATE, C_Z, C_XBC, C_DT, C_Q, C_K, C_V, C_U = 0, 3072, 5120, 9216, 9280, 10432, 11584, 12736
LN_EPS = 1e-5


class Ctx:
    pass


def make_consts(S, nc, cx):
    cx.identf = nc.alloc_sbuf_tensor("identf", [128, 128], F32)
    cx.identb = nc.alloc_sbuf_tensor("identb", [128, 128], BF16)
    cx.b_const = Buf("const")
    b = cx.b_const
    S.op("pool", lambda e: e.memset(cx.identf[:], 0.0), writes=[b])
    S.op("pool", lambda e: e.affine_select(out=cx.identf[:], in_=cx.identf[:], pattern=[[-1, 128]], base=0,
                                           channel_multiplier=1, compare_op=ALU.not_equal, fill=1.0),
         reads=[b], writes=[b])
    S.op("dve", lambda e: e.tensor_copy(out=cx.identb[:], in_=cx.identf[:]), reads=[b], writes=[b])


def load_xT(S, nc, cx, x_ap, t0, ntok, xT, b_xT, xs, b_xs, xb, b_xb, pt, b_pt, ctr):
    for t in range(ntok // 128):
        i = ctr[0] % 2
        ctr[0] += 1
        S.dma("sp", xs[i][:], x_ap[t0 + t * 128:t0 + (t + 1) * 128, :], writes=[b_xs[i]], owner=b_xs[i])
        S.op("dve", lambda e: e.tensor_copy(out=xb[i][:], in_=xs[i][:]), reads=[b_xs[i]], writes=[b_xb[i]])
        for k4 in range(2):
            j = ctr[1] % 2
            ctr[1] += 1
            for kk in range(4):
                k = k4 * 4 + kk
                S.op("pe", lambda e: e.transpose(out=pt[j][:, kk, :], in_=xb[i][:, k * 128:(k + 1) * 128],
                                                 identity=cx.identb[:]),
                     reads=[b_xb[i], cx.b_const], writes=[b_pt[j]], inc=(kk == 3))
            S.op("act", lambda e: e.copy(out=xT[:, k4 * 4:(k4 + 1) * 4, t * 128:(t + 1) * 128], in_=pt[j][:]),
                 reads=[b_pt[j]], writes=[b_xT])


def phase_inproj(S, nc, cx, cfg, w_in_l, x_ap, scr):
    L = cfg.L
    TB = min(2048, L)
    secs = [
        (C_GATE, 3072, "tok", scr.gates, F32, 512),
        (C_Z, 2048, "tok", scr.z, F32, 512),
        (C_XBC, 4096, "feat", scr.xbcT, F32, 512),
        (C_DT, 64, "tok", scr.dt, F32, 64),
        (C_Q, 1152, "feat", scr.qT, BF16, 384),
        (C_K, 1152, "feat", scr.kT, BF16, 384),
        (C_V, 1152, "tok", scr.v, BF16, 384),
        (C_U, 1152, "featpad", scr.uT, BF16, 128),
    ]
    with ExitStack() as es:
        sb = lambda n, s, d: es.enter_context(nc.sbuf_tensor(uniq(n), s, d))
        ps = lambda n, s, d: es.enter_context(nc.psum_tensor(uniq(n), s, d))
        xT = sb("ip_xT", [128, 8, TB], BF16)
        b_xT = Buf("ip_xT")
        xs = [sb(f"ip_xs{i}", [128, D], F32) for i in range(2)]
        b_xs = bufs("ip_xs", 2)
        xb = [sb(f"ip_xb{i}", [128, D], BF16) for i in range(2)]
        b_xb = bufs("ip_xb", 2)
        pt = [ps(f"ip_pt{i}", [128, 4, 128], BF16) for i in range(2)]
        b_pt = bufs("ip_pt", 2)
        wst = [sb(f"ip_wst{i}", [128, 8, 512], F32) for i in range(2)]
        b_wst = bufs("ip_wst", 2)
        wb = [sb(f"ip_wb{i}", [128, 8, 512], BF16) for i in range(2)]
        b_wb = bufs("ip_wb", 2)
        po = [ps(f"ip_po{i}", [128, 512], F32) for i in range(4)]
        b_po = bufs("ip_po", 4)
        ot = [sb(f"ip_ot{i}", [128, 512], F32) for i in range(3)]
        otb = [sb(f"ip_otb{i}", [128, 512], BF16) for i in range(3)]
        b_ot = bufs("ip_ot", 3)
        ctr = [0, 0]
        nw = 0
        npo = 0
        no = 0
        for t0 in range(0, L, TB):
            load_xT(S, nc, cx, x_ap, t0, TB, xT, b_xT, xs, b_xs, xb, b_xb, pt, b_pt, ctr)
            for (c0, ncols, kind, dest, dt, cb) in secs:
                for cc in range(0, ncols, cb):
                    wi = nw % 2
                    nw += 1
                    wsrc = w_in_l[:, c0 + cc:c0 + cc + cb].rearrange("(k p) c -> p k c", p=128)
                    S.dma("sp", wst[wi][:, :, 0:cb], wsrc, writes=[b_wst[wi]], owner=b_wst[wi])
                    if kind == "featpad":
                        ng = cb // 16
                        S.op("pool", lambda e: e.memset(wb[wi][:], 0.0), writes=[b_wb[wi]])
                        S.op("pool", lambda e: e.tensor_copy(
                            out=wb[wi][:, :, 0:ng * 32].rearrange("p k (g c) -> p k g c", c=32)[:, :, :, 0:16],
                            in_=wst[wi][:, :, 0:cb].rearrange("p k (g c) -> p k g c", c=16)),
                            reads=[b_wst[wi]], writes=[b_wb[wi]])
                        wcols = ng * 32
                    else:
                        S.op("pool", lambda e: e.tensor_copy(out=wb[wi][:, :, 0:cb], in_=wst[wi][:, :, 0:cb]),
                             reads=[b_wst[wi]], writes=[b_wb[wi]])
                        wcols = cb
                    if kind == "tok":
                        for t in range(TB // 128):
                            pj = npo % 4
                            npo += 1
                            for k in range(8):
                                S.op("pe", lambda e: e.matmul(po[pj][:, 0:cb], lhsT=xT[:, k, t * 128:(t + 1) * 128],
                                                              rhs=wb[wi][:, k, 0:cb], start=(k == 0), stop=(k == 7)),
                                     reads=[b_xT, b_wb[wi]], writes=[b_po[pj]], inc=(k == 7))
                            oi = no % 3
                            no += 1
                            o_t = ot[oi] if dt == F32 else otb[oi]
                            ev = "act" if no % 2 else "dve"
                            if ev == "act":
                                S.op("act", lambda e: e.copy(out=o_t[:, 0:cb], in_=po[pj][:, 0:cb]),
                                     reads=[b_po[pj]], writes=[b_ot[oi]])
                            else:
                                S.op("dve", lambda e: e.tensor_copy(out=o_t[:, 0:cb], in_=po[pj][:, 0:cb]),
                                     reads=[b_po[pj]], writes=[b_ot[oi]])
                            S.dma("sp", dest[t0 + t * 128:t0 + (t + 1) * 128, cc:cc + cb], o_t[:, 0:cb],
                                  reads=[b_ot[oi]], writes=[scr.b_proj], owner=b_ot[oi])
                    else:
                        if kind == "featpad":
                            r0 = (cc // 16) * 32
                        else:
                            r0 = cc
                        for ts in range(TB // 512):
                            for ch in range(wcols // 128):
                                pj = npo % 4
                                npo += 1
                                for k in range(8):
                                    S.op("pe", lambda e: e.matmul(po[pj][:], lhsT=wb[wi][:, k, ch * 128:(ch + 1) * 128],
                                                                  rhs=xT[:, k, ts * 512:(ts + 1) * 512],
                                                                  start=(k == 0), stop=(k == 7)),
                                         reads=[b_xT, b_wb[wi]], writes=[b_po[pj]], inc=(k == 7))
                                oi = no % 3
                                no += 1
                                o_t = ot[oi] if dt == F32 else otb[oi]
                                if no % 2:
                                    S.op("act", lambda e: e.copy(out=o_t[:], in_=po[pj][:]),
                                         reads=[b_po[pj]], writes=[b_ot[oi]])
                                else:
                                    S.op("dve", lambda e: e.tensor_copy(out=o_t[:], in_=po[pj][:]),
                                         reads=[b_po[pj]], writes=[b_ot[oi]])
                                S.dma("sp", dest[r0 + ch * 128:r0 + (ch + 1) * 128, t0 + ts * 512:t0 + (ts + 1) * 512],
                                      o_t[:], reads=[b_ot[oi]], writes=[scr.b_proj], owner=b_ot[oi])
        S.barrier()


def alloc_scratch(nc, cfg):
    L = cfg.L
    scr = Ctx()
    dr = lambda n, s, d: nc.dram_tensor(n, s, d, kind="Internal").ap()
    scr.gates = dr("s_gates", [L, 3072], F32)
    scr.z = dr("s_z", [L, 2048], F32)
    scr.xbcT = dr("s_xbcT", [4096, L], F32)
    scr.dt = dr("s_dt", [L, 64], F32)
    scr.qT = dr("s_qT", [1152, L], BF16)
    scr.kT = dr("s_kT", [1152, L], BF16)
    scr.v = dr("s_v", [L, 1152], BF16)
    scr.uT = dr("s_uT", [2304, L], BF16)
    scr.b_proj = Buf("proj", multi=True)
    scr.BT = dr("s_BT", [1024, L], BF16)
    scr.CT = dr("s_CT", [1024, L], BF16)
    scr.xsB = dr("s_xsB", [L, 3072], BF16)
    scr.b_conv = Buf("conv", multi=True)
    scr.Sb = dr("s_Sb", [L // 128, 8, 128, 256], F32)
    scr.ypart = dr("s_ypart", [L, 2048], F32)
    scr.b_ssd = Buf("ssd", multi=True)
    scr.yssd = dr("s_yssd", [L, 2048], BF16)
    scr.b_mix = Buf("mix", multi=True)
    scr.hs5T = dr("s_hs5T", [2304, L], BF16)
    scr.b_s5 = Buf("s5", multi=True)
    scr.ys5T = dr("s_ys5T", [1152, L], BF16)
    scr.b_glu = Buf("glu", multi=True)
    scr.x1 = dr("s_x1", [L, 1024], F32)
    scr.b_x1 = Buf("x1", multi=True)
    scr.xcur = dr("s_xcur", [L, 1024], F32)
    scr.b_xcur = Buf("xcur", multi=True)
    scr.wgu16 = dr("s_wgu16", [cfg.E, 1024, 2048], BF16)
    scr.wdn16 = dr("s_wdn16", [cfg.E, 1024, 1024], BF16)
    scr.b_w16 = Buf("w16", multi=True)
    scr.x1T = dr("s_x1T", [1024, L], BF16)
    scr.rgate = dr("s_rgate", [L, cfg.E], F32)
    scr.b_x1T = Buf("x1T", multi=True)
    scr.attO = dr("s_attO", [3, L, 390], F32)
    scr.b_att = Buf("att", multi=True)
    scr.yatt = dr("s_yatt", [L, 384], BF16)
    return scr


def phase_conv(S, nc, cx, cfg, conv_w_l, conv_b_l, scr):
    L = cfg.L
    TS = min(2048, L)
    with ExitStack() as es:
        sb = lambda n, s, d: es.enter_context(nc.sbuf_tensor(uniq(n), s, d))
        ps = lambda n, s, d: es.enter_context(nc.psum_tensor(uniq(n), s, d))
        cw = sb("cv_w", [128, 32, 5], F32)
        cb = sb("cv_b", [128, 32], F32)
        b_cw = Buf("cv_w")
        with nc.allow_non_contiguous_dma(reason="tiny param load"):
            for k in range(5):
                S.dma("sp", cw[:, :, k], conv_w_l[k].rearrange("(t p) -> p t", p=128), writes=[b_cw], owner=b_cw)
            S.dma("sp", cb[:], conv_b_l.rearrange("(t p) -> p t", p=128), writes=[b_cw], owner=b_cw)
        xin = [sb(f"cv_x{i}", [128, TS + 4], F32) for i in range(2)]
        b_xin = bufs("cv_x", 2)
        acc = [sb(f"cv_a{i}", [128, TS], F32) for i in range(2)]
        b_acc = bufs("cv_a", 2)
        yb = [sb(f"cv_y{i}", [128, TS], BF16) for i in range(8)]
        b_yb = bufs("cv_y", 8)
        pt = [ps(f"cv_pt{i}", [128, 4, 128], BF16) for i in range(2)]
        b_pt = bufs("cv_pt", 2)
        ot = [sb(f"cv_o{i}", [128, 512], BF16) for i in range(3)]
        b_ot = bufs("cv_o", 3)
        nx = 0
        ny = 0
        npt = 0
        no = 0
        for cg in range(8):
            for t0 in range(0, L, TS):
                ys = []
                for j in range(4):
                    ct = cg * 4 + j
                    i = nx % 2
                    nx += 1
                    eng = "dve"
                    lo = max(0, t0 - 2)
                    hi = min(L, t0 + TS + 2)
                    d0 = lo - (t0 - 2)
                    if d0 > 0:
                        S.op("pool", lambda e: e.memset(xin[i][:, 0:d0], 0.0), writes=[b_xin[i]])
                    if d0 + (hi - lo) < TS + 4:
                        S.op("pool", lambda e: e.memset(xin[i][:, d0 + hi - lo:TS + 4], 0.0), writes=[b_xin[i]])
                    S.dma("sp", xin[i][:, d0:d0 + hi - lo], scr.xbcT[ct * 128:(ct + 1) * 128, lo:hi],
                          reads=[scr.b_proj], writes=[b_xin[i]], owner=b_xin[i])
                    a = acc[i]
                    S.op("act", lambda e: e.activation(out=a[:], in_=xin[i][:, 0:TS], func=AF.Copy, scale=cw[:, ct, 0:1]),
                         reads=[b_xin[i], b_cw], writes=[b_acc[i]])
                    for k in range(1, 5):
                        S.op(eng, lambda e: e.scalar_tensor_tensor(out=a[:], in0=xin[i][:, k:k + TS], scalar=cw[:, ct, k:k + 1],
                                                                   in1=a[:], op0=ALU.mult, op1=ALU.add),
                             reads=[b_xin[i], b_cw, b_acc[i]], writes=[b_acc[i]])
                    yi = ny % 8
                    ny += 1
                    S.op("act", lambda e: e.activation(out=yb[yi][:], in_=a[:], func=AF.Silu, bias=cb[:, ct:ct + 1], scale=1.0),
                         reads=[b_acc[i], b_cw], writes=[b_yb[yi]])
                    ys.append(yi)
                    if ct >= 16:
                        dst = scr.BT if ct < 24 else scr.CT
                        r0 = (ct - 16) * 128 if ct < 24 else (ct - 24) * 128
                        S.dma("sp", dst[r0:r0 + 128, t0:t0 + TS], yb[yi][:], reads=[b_yb[yi]], writes=[scr.b_conv],
                              owner=b_yb[yi])
                if cg < 6:
                    for tt in range(TS // 128):
                        pj = npt % 2
                        npt += 1
                        for j in range(4):
                            S.op("pe", lambda e: e.transpose(out=pt[pj][:, j, :], in_=yb[ys[j]][:, tt * 128:(tt + 1) * 128],
                                                             identity=cx.identb[:]),
                                 reads=[b_yb[ys[j]], cx.b_const], writes=[b_pt[pj]], inc=(j == 3))
                        oi = no % 3
                        no += 1
                        if no % 2:
                            S.op("act", lambda e: e.copy(out=ot[oi][:], in_=pt[pj][:].rearrange("p a b -> p (a b)")),
                                 reads=[b_pt[pj]], writes=[b_ot[oi]])
                        else:
                            S.op("dve", lambda e: e.tensor_copy(out=ot[oi][:], in_=pt[pj][:].rearrange("p a b -> p (a b)")),
                                 reads=[b_pt[pj]], writes=[b_ot[oi]])
                        S.dma("sp", scr.xsB[t0 + tt * 128:t0 + (tt + 1) * 128, cg * 512:(cg + 1) * 512], ot[oi][:],
                              reads=[b_ot[oi]], writes=[scr.b_conv], owner=b_ot[oi])
        S.barrier()


def make_masks(S, nc, cx):
    cx.maskLE = nc.alloc_sbuf_tensor("maskLE", [128, 128], F32)
    cx.maskGE = nc.alloc_sbuf_tensor("maskGE", [128, 128], F32)
    cx.U0 = nc.alloc_sbuf_tensor("U0", [128, 128], F32)
    cx.U1 = nc.alloc_sbuf_tensor("U1", [128, 128], F32)
    cx.ones = nc.alloc_sbuf_tensor("ones", [128, 128], F32)
    b = cx.b_const
    for t, pat, cm, op in ((cx.maskLE, 1, -1, ALU.is_ge), (cx.maskGE, -1, 1, ALU.is_ge),
                           (cx.U0, -1, 1, ALU.is_gt), (cx.U1, 1, -1, ALU.is_gt)):
        S.op("pool", lambda e: e.memset(t[:], 1.0), writes=[b])
        S.op("pool", lambda e: e.affine_select(out=t[:], in_=t[:], pattern=[[pat, 128]], base=0, channel_multiplier=cm,
                                               compare_op=op, fill=0.0), reads=[b], writes=[b])
    S.op("pool", lambda e: e.memset(cx.ones[:], 1.0), writes=[b])


def bc_r(ap, n):
    return ap.unsqueeze(1).to_broadcast([ap.shape[0], n, ap.shape[1]])


def bc_l(ap, n):
    return ap.unsqueeze(2).to_broadcast([ap.shape[0], ap.shape[1], n])


def phase_ssd(S, nc, cx, cfg, prm, l, scr):
    L = cfg.L
    NC = L // 128
    with ExitStack() as es:
        sb = lambda n, s, d: es.enter_context(nc.sbuf_tensor(uniq(n), s, d))
        ps = lambda n, s, d: es.enter_context(nc.psum_tensor(uniq(n), s, d))
        b_prm = Buf("sd_prm")
        biasb = sb("sd_bias", [128, 64], F32)
        negA = sb("sd_negA", [128, 64], F32)
        dsk = sb("sd_dsk", [128, 32], F32)
        nw = sb("sd_nw", [128, 2048], F32)
        S.dma("sp", biasb[:], prm["ssd_dt_bias"][l].rearrange("a b -> (a b)").partition_broadcast(128), writes=[b_prm], owner=b_prm)
        S.dma("sp", negA[:], prm["ssd_a_log"][l].rearrange("a b -> (a b)").partition_broadcast(128), writes=[b_prm], owner=b_prm)
        S.dma("sp", dsk[:], prm["ssd_d"][l].partition_broadcast(128), writes=[b_prm], owner=b_prm)
        S.dma("sp", nw[:], prm["ssd_norm_w"][l].partition_broadcast(128), writes=[b_prm], owner=b_prm)
        S.op("act", lambda e: e.activation(out=negA[:], in_=negA[:], func=AF.Exp), reads=[b_prm], writes=[b_prm])
        S.op("dve", lambda e: e.tensor_scalar(out=negA[:], in0=negA[:], scalar1=-1.0, scalar2=None, op0=ALU.mult),
             reads=[b_prm], writes=[b_prm])
        Hf = sb("sd_H", [128, 8, 256], F32)
        Hb = sb("sd_Hb", [128, 8, 256], BF16)
        b_H = bufs("sd_Hg", 8)
        tmpH = sb("sd_tH", [128, 256], F32)
        b_tmpH = Buf("sd_tH")
        dtt = [sb(f"sd_dt{i}", [128, 64], F32) for i in range(2)]
        b_dtt = bufs("sd_dt", 2)
        xB = [sb(f"sd_xB{i}", [128, 3072], BF16) for i in range(2)]
        b_xB = bufs("sd_xB", 2)
        BTc = [sb(f"sd_BT{i}", [128, 8, 128], BF16) for i in range(2)]
        b_BTc = bufs("sd_BT", 2)
        CTc = [sb(f"sd_CT{i}", [128, 8, 128], BF16) for i in range(2)]
        b_CTc = bufs("sd_CT", 2)
        names = ["ex", "delta", "a", "I", "Q", "G1", "G2", "G1d", "G2d", "dec", "tq"]
        sm = {n: sb("sd_" + n, [128, 64], F32) for n in names}
        b_sm = {n: Buf("sd_" + n) for n in names}
        ps_IT = ps("sd_psIT", [128, 2, 64], F32)
        ps_I = ps_IT[:, 0, :]
        ps_T = ps_IT[:, 1, :]
        b_psI = Buf("sd_psI")
        b_psT = Buf("sd_psT")
        ps_CBs = [ps(f"sd_psCB{i}", [128, 128], F32) for i in range(2)]
        b_psCBs = bufs("sd_psCB", 2)
        ps_seg = [ps(f"sd_psS{i}", [128, 512], F32) for i in range(2)]
        b_psseg = bufs("sd_psS", 2)
        ps_yd = ps("sd_psyd", [128, 4, 64], F32)
        b_psyd = Buf("sd_psyd")
        ps_st = ps("sd_psst", [128, 2, 256], F32)
        b_psst = bufs("sd_psst", 2)
        ps_yo = ps("sd_psyo", [128, 256], F32)
        b_psyo = Buf("sd_psyo")
        CBm4 = [sb(f"sd_CBm{i}", [128, 128], F32) for i in range(4)]
        b_CBm4 = bufs("sd_CBm", 4)
        Rt4 = [sb(f"sd_R{i}", [128, 4, 128], F32) for i in range(4)]
        b_Rt4 = bufs("sd_R", 4)
        Dx4 = [sb(f"sd_Dx{i}", [128, 4, 128], F32) for i in range(4)]
        b_Dx4 = bufs("sd_Dx", 4)
        MT4 = [sb(f"sd_MT{i}", [128, 4, 128], BF16) for i in range(4)]
        b_MT4 = bufs("sd_MT", 4)
        xw4 = [sb(f"sd_xw{i}", [128, 4, 64], BF16) for i in range(4)]
        b_xw4 = bufs("sd_xw", 4)
        yp = [sb(f"sd_yp{i}", [128, 2048], F32) for i in range(2)]
        b_yp = bufs("sd_yp", 2)
        ytmp = sb("sd_ytmp", [128, 256], F32)
        b_ytmp = Buf("sd_ytmp")
        sbst = [sb(f"sd_sb{i}", [128, 256], F32) for i in range(2)]
        b_sbst = bufs("sd_sb", 2)
        zt = [sb(f"sd_z{i}", [128, 2048], F32) for i in range(2)]
        b_zt = bufs("sd_z", 2)
        yo16 = [sb(f"sd_y16{i}", [128, 2048], BF16) for i in range(2)]
        b_yo16 = bufs("sd_y16", 2)
        ssq = sb("sd_ssq", [128, 2], F32)
        b_ssq = Buf("sd_ssq")

        for g in range(8):
            S.op("pool", lambda e: e.memset(Hf[:, g, :], 0.0), writes=[b_H[g]])
            S.op("pool", lambda e: e.memset(Hb[:, g, :], 0.0), writes=[b_H[g]])

        def chunk_scalars(c, i):
            S.dma("sp", dtt[i][:], scr.dt[c * 128:(c + 1) * 128, :], reads=[scr.b_proj], writes=[b_dtt[i]], owner=b_dtt[i])
            S.op("dve", lambda e: e.tensor_tensor(out=sm["ex"][:], in0=dtt[i][:], in1=biasb[:], op=ALU.add),
                 reads=[b_dtt[i], b_prm], writes=[b_sm["ex"]])
            S.op("act", lambda e: e.activation(out=sm["ex"][:], in_=sm["ex"][:], func=AF.Exp), reads=[b_sm["ex"]], writes=[b_sm["ex"]])
            S.op("act", lambda e: e.activation(out=sm["delta"][:], in_=sm["ex"][:], func=AF.Ln, bias=1.0, scale=1.0),
                 reads=[b_sm["ex"]], writes=[b_sm["delta"]])
            S.op("dve", lambda e: e.tensor_tensor(out=sm["a"][:], in0=sm["delta"][:], in1=negA[:], op=ALU.mult),
                 reads=[b_sm["delta"], b_prm], writes=[b_sm["a"]])
            S.op("pe", lambda e: e.matmul(ps_I, lhsT=cx.maskLE[:], rhs=sm["a"][:], start=True, stop=True),
                 reads=[b_sm["a"], cx.b_const], writes=[b_psI])
            S.op("pe", lambda e: e.matmul(ps_T, lhsT=cx.ones[:], rhs=sm["a"][:], start=True, stop=True),
                 reads=[b_sm["a"], cx.b_const], writes=[b_psT])
            S.op("dve", lambda e: e.tensor_copy(out=sm["Q"][:, 0:32], in_=ps_I[:, 0:32]), reads=[b_psI], writes=[b_sm["Q"]])
            S.op("dve", lambda e: e.tensor_tensor(out=sm["Q"][:, 32:64], in0=ps_I[:, 32:64], in1=sm["a"][:, 32:64], op=ALU.subtract),
                 reads=[b_psI, b_sm["a"], b_sm["Q"]], writes=[b_sm["Q"]])
            S.op("dve", lambda e: e.tensor_tensor(out=sm["tq"][:], in0=ps_T, in1=sm["Q"][:], op=ALU.subtract),
                 reads=[b_psT, b_sm["Q"]], writes=[b_sm["tq"]])
            S.op("act", lambda e: e.activation(out=sm["G1"][:], in_=sm["Q"][:], func=AF.Exp), reads=[b_sm["Q"]], writes=[b_sm["G1"]])
            S.op("act", lambda e: e.activation(out=sm["G2"][:], in_=sm["tq"][:], func=AF.Exp), reads=[b_sm["tq"]], writes=[b_sm["G2"]])
            S.op("act", lambda e: e.activation(out=sm["dec"][:], in_=ps_T, func=AF.Exp), reads=[b_psT], writes=[b_sm["dec"]])
            S.op("dve", lambda e: e.tensor_tensor(out=sm["G1d"][:], in0=sm["G1"][:], in1=sm["delta"][:], op=ALU.mult),
                 reads=[b_sm["G1"], b_sm["delta"]], writes=[b_sm["G1d"]])
            S.op("dve", lambda e: e.tensor_tensor(out=sm["G2d"][:], in0=sm["G2"][:], in1=sm["delta"][:], op=ALU.mult),
                 reads=[b_sm["G2"], b_sm["delta"]], writes=[b_sm["G2d"]])

        nseg = 0
        nst = 0
        for c in range(NC):
            i = c % 2
            chunk_scalars(c, i)
            S.dma("sp", xB[i][:], scr.xsB[c * 128:(c + 1) * 128, :], reads=[scr.b_conv], writes=[b_xB[i]], owner=b_xB[i])
            S.dma("sp", BTc[i][:], scr.BT[:, c * 128:(c + 1) * 128].rearrange("(g n) l -> n g l", n=128),
                  reads=[scr.b_conv], writes=[b_BTc[i]], owner=b_BTc[i])
            S.dma("sp", CTc[i][:], scr.CT[:, c * 128:(c + 1) * 128].rearrange("(g n) l -> n g l", n=128),
                  reads=[scr.b_conv], writes=[b_CTc[i]], owner=b_CTc[i])
            xv = xB[i][:, 0:2048].rearrange("p (g r q) -> p g r q", g=8, r=4)
            Btok = xB[i][:, 2048:3072].rearrange("p (g n) -> p g n", g=8)
            for g in range(8):
                gp = (g % 2) * 2
                CBm, b_CBm = CBm4[gp:gp + 2], b_CBm4[gp:gp + 2]
                Rt, b_Rt = Rt4[gp:gp + 2], b_Rt4[gp:gp + 2]
                Dx, b_Dx = Dx4[gp:gp + 2], b_Dx4[gp:gp + 2]
                MT, b_MT = MT4[gp:gp + 2], b_MT4[gp:gp + 2]
                xw, b_xw = xw4[gp:gp + 2], b_xw4[gp:gp + 2]
                ps_CB, b_psCB = ps_CBs[g % 2], b_psCBs[g % 2]
                S.op("pe", lambda e: e.matmul(ps_CB[:], lhsT=BTc[i][:, g, :], rhs=CTc[i][:, g, :], start=True, stop=True),
                     reads=[b_BTc[i], b_CTc[i]], writes=[b_psCB])
                S.op("dve", lambda e: e.tensor_tensor(out=CBm[0][:], in0=ps_CB[:], in1=cx.maskLE[:], op=ALU.mult),
                     reads=[b_psCB, cx.b_const], writes=[b_CBm[0]])
                S.op("dve", lambda e: e.tensor_tensor(out=CBm[1][:], in0=ps_CB[:], in1=cx.maskGE[:], op=ALU.mult),
                     reads=[b_psCB, cx.b_const], writes=[b_CBm[1]])
                for d in range(2):
                    cols = slice(d * 32 + g * 4, d * 32 + g * 4 + 4)
                    msk = cx.maskLE if d == 0 else cx.maskGE
                    U = cx.U0 if d == 0 else cx.U1
                    wd = sm["G2d"] if d == 0 else sm["G1d"]
                    b_wd = b_sm["G2d"] if d == 0 else b_sm["G1d"]
                    sj = nseg % 2
                    nseg += 1
                    S.op("pool", lambda e: e.tensor_tensor(out=Rt[d][:], in0=bc_r(msk[:], 4), in1=bc_l(sm["a"][:, cols], 128), op=ALU.mult),
                         reads=[cx.b_const, b_sm["a"]], writes=[b_Rt[d]])
                    S.op("pe", lambda e: e.matmul(ps_seg[sj][:], lhsT=U[:], rhs=Rt[d][:].rearrange("p r l -> p (r l)"), start=True, stop=True),
                         reads=[cx.b_const, b_Rt[d]], writes=[b_psseg[sj]])
                    S.op("act", lambda e: e.activation(out=Dx[d][:].rearrange("p r l -> p (r l)"), in_=ps_seg[sj][:], func=AF.Exp),
                         reads=[b_psseg[sj]], writes=[b_Dx[d]])
                    S.op("dve", lambda e: e.tensor_tensor(out=Dx[d][:], in0=Dx[d][:], in1=bc_l(sm["delta"][:, cols], 128), op=ALU.mult),
                         reads=[b_Dx[d], b_sm["delta"]], writes=[b_Dx[d]])
                    S.op("pool", lambda e: e.tensor_tensor(out=MT[d][:], in0=Dx[d][:], in1=bc_r(CBm[d][:], 4), op=ALU.mult),
                         reads=[b_Dx[d], b_CBm[d]], writes=[b_MT[d]])
                    S.op("dve", lambda e: e.tensor_tensor(out=xw[d][:], in0=xv[:, g, :, :], in1=bc_l(wd[:, cols], 64), op=ALU.mult),
                         reads=[b_xB[i], b_wd], writes=[b_xw[d]])
                    S.op("pe", lambda e: e.matmul(ps_st[:, d, :], lhsT=Btok[:, g, :], rhs=xw[d][:].rearrange("p r q -> p (r q)"),
                                                  start=True, stop=True),
                         reads=[b_xB[i], b_xw[d]], writes=[b_psst[d]])
                for r in range(4):
                    for d in range(2):
                        S.op("pe", lambda e: e.matmul(ps_yd[:, r, :], lhsT=MT[d][:, r, :], rhs=xv[:, g, r, :], start=(d == 0), stop=(d == 1)),
                             reads=[b_MT[d], b_xB[i]], writes=[b_psyd], inc=(r == 3 and d == 1))
                colsf = slice(g * 4, g * 4 + 4)
                S.op("pe", lambda e: e.matmul(ps_yo[:], lhsT=CTc[i][:, g, :], rhs=Hb[:, g, :], start=True, stop=True),
                     reads=[b_CTc[i], b_H[g]], writes=[b_psyo])
                S.op("dve", lambda e: e.tensor_tensor(out=ytmp[:].rearrange("p (r q) -> p r q", r=4), in0=ps_yo[:].rearrange("p (r q) -> p r q", r=4),
                                                      in1=bc_l(sm["G1"][:, colsf], 64), op=ALU.mult),
                     reads=[b_psyo, b_sm["G1"]], writes=[b_ytmp])
                S.op("dve", lambda e: e.tensor_tensor(out=yp[i][:, g * 256:(g + 1) * 256], in0=ps_yd[:].rearrange("p r q -> p (r q)"),
                                                      in1=ytmp[:], op=ALU.add),
                     reads=[b_psyd, b_ytmp], writes=[b_yp[i]])
                S.op("pool", lambda e: e.tensor_tensor(out=tmpH[:].rearrange("p (r q) -> p r q", r=4), in0=Hf[:, g, :].rearrange("p (r q) -> p r q", r=4),
                                                       in1=bc_l(sm["dec"][:, colsf], 64), op=ALU.mult),
                     reads=[b_H[g], b_sm["dec"]], writes=[b_tmpH])
                S.op("dve", lambda e: e.tensor_tensor(out=Hf[:, g, :], in0=tmpH[:], in1=ps_st[:, 0, :], op=ALU.add),
                     reads=[b_tmpH, b_psst[0]], writes=[b_H[g]])
                S.op("act", lambda e: e.copy(out=Hb[:, g, :], in_=Hf[:, g, :]), reads=[b_H[g]], writes=[b_H[g]])
                si = nst % 2
                nst += 1
                S.op("act", lambda e: e.copy(out=sbst[si][:], in_=ps_st[:, 1, :]), reads=[b_psst[1]], writes=[b_sbst[si]])
                S.dma("sp", scr.Sb[c, g], sbst[si][:], reads=[b_sbst[si]], writes=[scr.b_ssd], owner=b_sbst[si])
            S.dma("sp", scr.ypart[c * 128:(c + 1) * 128, :], yp[i][:], reads=[b_yp[i]], writes=[scr.b_ssd], owner=b_yp[i])
        S.barrier()
        for g in range(8):
            S.op("pool", lambda e: e.memset(Hf[:, g, :], 0.0), writes=[b_H[g]])
            S.op("pool", lambda e: e.memset(Hb[:, g, :], 0.0), writes=[b_H[g]])
        for ci, c in enumerate(range(NC - 1, -1, -1)):
            i = ci % 2
            chunk_scalars(c, i)
            S.dma("sp", CTc[i][:], scr.CT[:, c * 128:(c + 1) * 128].rearrange("(g n) l -> n g l", n=128),
                  reads=[scr.b_conv], writes=[b_CTc[i]], owner=b_CTc[i])
            S.dma("sp", xB[i][:, 0:2048], scr.xsB[c * 128:(c + 1) * 128, 0:2048], reads=[scr.b_conv], writes=[b_xB[i]], owner=b_xB[i])
            S.dma("sp", yp[i][:], scr.ypart[c * 128:(c + 1) * 128, :], reads=[scr.b_ssd], writes=[b_yp[i]], owner=b_yp[i])
            S.dma("sp", zt[i][:], scr.z[c * 128:(c + 1) * 128, :], reads=[scr.b_proj], writes=[b_zt[i]], owner=b_zt[i])
            for g in range(8):
                colsb = slice(32 + g * 4, 32 + g * 4 + 4)
                si = nst % 2
                nst += 1
                S.dma("sp", sbst[si][:], scr.Sb[c, g], reads=[scr.b_ssd], writes=[b_sbst[si]], owner=b_sbst[si])
                S.op("pe", lambda e: e.matmul(ps_yo[:], lhsT=CTc[i][:, g, :], rhs=Hb[:, g, :], start=True, stop=True),
                     reads=[b_CTc[i], b_H[g]], writes=[b_psyo])
                S.op("dve", lambda e: e.tensor_tensor(out=ytmp[:].rearrange("p (r q) -> p r q", r=4), in0=ps_yo[:].rearrange("p (r q) -> p r q", r=4),
                                                      in1=bc_l(sm["G2"][:, colsb], 64), op=ALU.mult),
                     reads=[b_psyo, b_sm["G2"]], writes=[b_ytmp])
                S.op("dve", lambda e: e.tensor_tensor(out=yp[i][:, g * 256:(g + 1) * 256], in0=yp[i][:, g * 256:(g + 1) * 256],
                                                      in1=ytmp[:], op=ALU.add),
                     reads=[b_yp[i], b_ytmp], writes=[b_yp[i]])
                S.op("pool", lambda e: e.tensor_tensor(out=tmpH[:].rearrange("p (r q) -> p r q", r=4), in0=Hf[:, g, :].rearrange("p (r q) -> p r q", r=4),
                                                       in1=bc_l(sm["dec"][:, colsb], 64), op=ALU.mult),
                     reads=[b_H[g], b_sm["dec"]], writes=[b_tmpH])
                S.op("pool", lambda e: e.tensor_tensor(out=Hf[:, g, :], in0=tmpH[:], in1=sbst[si][:], op=ALU.add),
                     reads=[b_tmpH, b_sbst[si]], writes=[b_H[g]])
                S.op("act", lambda e: e.copy(out=Hb[:, g, :], in_=Hf[:, g, :]), reads=[b_H[g]], writes=[b_H[g]])
            S.op("act", lambda e: e.activation(out=zt[i][:], in_=zt[i][:], func=AF.Silu), reads=[b_zt[i]], writes=[b_zt[i]])
            S.op("dve", lambda e: e.tensor_tensor(out=yo16[i][:].rearrange("p (h q) -> p h q", h=32), in0=xB[i][:, 0:2048].rearrange("p (h q) -> p h q", h=32),
                                                  in1=bc_l(dsk[:], 64), op=ALU.mult),
                 reads=[b_xB[i], b_prm], writes=[b_yo16[i]])
            S.op("dve", lambda e: e.tensor_tensor(out=yp[i][:], in0=yp[i][:], in1=yo16[i][:], op=ALU.add),
                 reads=[b_yp[i], b_yo16[i]], writes=[b_yp[i]])
            S.op("dve", lambda e: e.tensor_tensor(out=yp[i][:], in0=yp[i][:], in1=zt[i][:], op=ALU.mult),
                 reads=[b_yp[i], b_zt[i]], writes=[b_yp[i]])
            S.op("act", lambda e: e.activation(out=zt[i][:], in_=yp[i][:], func=AF.Square, accum_out=ssq[:, 0:1]),
                 reads=[b_yp[i]], writes=[b_zt[i], b_ssq])
            S.op("dve", lambda e: e.tensor_scalar(out=ssq[:, 1:2], in0=ssq[:, 0:1], scalar1=1.0 / 2048, scalar2=LN_EPS, op0=ALU.mult, op1=ALU.add),
                 reads=[b_ssq], writes=[b_ssq])
            S.op("act", lambda e: e.activation(out=ssq[:, 1:2], in_=ssq[:, 1:2], func=AF.Sqrt), reads=[b_ssq], writes=[b_ssq])
            S.op("dve", lambda e: e.reciprocal(out=ssq[:, 1:2], in_=ssq[:, 1:2]), reads=[b_ssq], writes=[b_ssq])
            S.op("dve", lambda e: e.scalar_tensor_tensor(out=yo16[i][:], in0=yp[i][:], scalar=ssq[:, 1:2], in1=nw[:], op0=ALU.mult, op1=ALU.mult),
                 reads=[b_yp[i], b_ssq, b_prm, b_yo16[i]], writes=[b_yo16[i]])
            S.dma("sp", scr.yssd[c * 128:(c + 1) * 128, :], yo16[i][:], reads=[b_yo16[i]], writes=[scr.b_mix], owner=b_yo16[i])
        S.barrier()


PARAM_SHAPES = lambda cfg: {
    "w_in": [cfg.depth, D, N_IN], "b_gate": [cfg.depth, 3, D],
    "ssd_conv_w": [cfg.depth, 5, 4096], "ssd_conv_b": [cfg.depth, 4096], "ssd_a_log": [cfg.depth, 2, 32],
    "ssd_dt_bias": [cfg.depth, 2, 32], "ssd_d": [cfg.depth, 32], "ssd_norm_w": [cfg.depth, 2048],
    "s5_a_re": [cfg.depth, 2, 72, 64], "s5_a_im": [cfg.depth, 2, 72, 64], "s5_log_step": [cfg.depth, 2, 72],
    "s5_b_re": [cfg.depth, 72, 64, 16], "s5_b_im": [cfg.depth, 72, 64, 16],
    "s5_c_re": [cfg.depth, 2, 72, 16, 64], "s5_c_im": [cfg.depth, 2, 72, 16, 64], "s5_d": [cfg.depth, 1152],
    "s5_glu_w1": [cfg.depth, 1152, 1152], "s5_glu_w2": [cfg.depth, 1152, 1152],
    "w_br_ssd": [cfg.depth, 2048, D], "w_br_attn": [cfg.depth, 384, D], "w_br_s5": [cfg.depth, 1152, D],
    "w_out": [cfg.depth, D, D], "ln1_g": [cfg.depth, D], "ln1_b": [cfg.depth, D],
    "router_w": [cfg.depth, D, cfg.E], "router_b": [cfg.depth, cfg.E],
    "exp_w_gate_up": [cfg.depth, cfg.E, D, 2048], "exp_b_gate_up": [cfg.depth, cfg.E, 2048],
    "exp_w_down": [cfg.depth, cfg.E, D, D], "exp_b_down": [cfg.depth, cfg.E, D],
    "ln2_g": [cfg.depth, D], "ln2_b": [cfg.depth, D],
}


def build(cfg, debug=(), phases=("inproj", "conv", "ssd", "att", "s5", "mix", "moe"), inject=()):
    nc = bass.Bass("TRN2", target_bir_lowering=False)
    L = cfg.L
    prm = {}
    x = nc.dram_tensor("x", [L, D], F32, kind="ExternalInput").ap()
    for name, shape in PARAM_SHAPES(cfg).items():
        prm[name] = nc.dram_tensor(name, list(shape), F32, kind="ExternalInput").ap()
    out = nc.dram_tensor("out", [L, D], F32, kind="ExternalOutput").ap()
    S = Sched(nc)
    cx = Ctx()
    make_consts(S, nc, cx)
    make_masks(S, nc, cx)
    make_att_bias(S, nc, cx)
    scr = alloc_scratch(nc, cfg)
    dbg = {}
    for name in debug:
        src = getattr(scr, name)
        dbg[name] = nc.dram_tensor("dbg_" + name, list(src.shape), src.dtype, kind="ExternalOutput").ap()
    cur = x
    b_inj = Buf("inj", multi=True)
    for name in inject:
        dst = getattr(scr, name)
        src = nc.dram_tensor("inj_" + name, list(dst.shape), dst.dtype, kind="ExternalInput").ap()
        dram_copy(S, dst, src, b_inj)
    if inject:
        S.barrier()
    for l in range(cfg.depth):
        if "inproj" in phases:
            phase_inproj(S, nc, cx, cfg, prm["w_in"][l], cur, scr)
        if "conv" in phases:
            phase_conv(S, nc, cx, cfg, prm["ssd_conv_w"][l], prm["ssd_conv_b"][l], scr)
        if "ssd" in phases:
            phase_ssd(S, nc, cx, cfg, prm, l, scr)
        if "att" in phases:
            phase_att(S, nc, cx, cfg, scr)
        if "s5" in phases:
            phase_s5(S, nc, cx, cfg, prm, l, scr)
        if "mix" in phases:
            phase_mix(S, nc, cx, cfg, prm, l, cur, scr)
            cur = scr.x1
        if "moe" in phases:
            last = (l == cfg.depth - 1) and not debug
            phase_moe(S, nc, cx, cfg, prm, l, scr, out if last else scr.xcur)
            cur = None if last else scr.xcur
    b_dbg = Buf("dbg", multi=True)
    for name in debug:
        dram_copy(S, dbg[name], getattr(scr, name), b_dbg)
    if cur is not None:
        dram_copy(S, out, cur, b_dbg)
    S.barrier()
    return nc, S


def kernel(**inputs):
    cfg = Cfg(L=8192, depth=4, n_exp=32)
    nc, _ = build(cfg)
    x = np.asarray(inputs["x"], dtype=np.float32)
    names = list(PARAM_SHAPES(cfg).keys())
    params = {k: np.ascontiguousarray(np.asarray(inputs[k], dtype=np.float32)) for k in names}
    in_maps = []
    for b in range(x.shape[0]):
        m = {"x": np.ascontiguousarray(x[b])}
        m.update(params)
        in_maps.append(m)
    res = run_bass_kernel_spmd(nc, in_maps, core_ids=list(range(x.shape[0])))
    return np.stack([np.asarray(r["out"], dtype=np.float32) for r in res.results], axis=0)


ATT_PAT = ((128, 1), (512, 4), (2048, 16))
NEG_BIG = -30000.0


def make_att_bias(S, nc, cx):
    cx.abias = nc.alloc_sbuf_tensor("abias", [128, 18, 256], F32)
    cx.b_abias = Buf("abias")
    b = cx.b_abias
    with ExitStack() as es:
        di = es.enter_context(nc.sbuf_tensor("ab_di", [128, 256], I32))
        df = es.enter_context(nc.sbuf_tensor("ab_df", [128, 256], F32))
        mk = es.enter_context(nc.sbuf_tensor("ab_mk", [128, 256], F32))
        bt = Buf("ab_tmp")
        S.op("pool", lambda e: e.iota(di[:], pattern=[[-128, 2], [1, 128]], base=64, channel_multiplier=-1), writes=[bt])
        S.op("dve", lambda e: e.tensor_copy(out=df[:], in_=di[:]), reads=[bt], writes=[bt])
        S.op("act", lambda e: e.activation(out=df[:], in_=df[:], func=AF.Abs), reads=[bt], writes=[bt])
        S.op("dve", lambda e: e.tensor_scalar(out=mk[:], in0=df[:], scalar1=64.0, scalar2=NEG_BIG, op0=ALU.is_gt, op1=ALU.mult),
             reads=[bt], writes=[bt])
        for g, (window, dil) in enumerate(ATT_PAT):
            for h in range(6):
                slope = 2.0 ** (-8.0 * (h * 3 + g + 1) / 18.0)
                S.op("dve", lambda e: e.scalar_tensor_tensor(out=cx.abias[:, g * 6 + h, :], in0=df[:], scalar=-slope * dil, in1=mk[:],
                                                             op0=ALU.mult, op1=ALU.add), reads=[bt], writes=[b])
        S.barrier()


def phase_att(S, nc, cx, cfg, scr):
    L = cfg.L
    with ExitStack() as es:
        sb = lambda n, s, d: es.enter_context(nc.sbuf_tensor(uniq(n), s, d))
        ps = lambda n, s, d: es.enter_context(nc.psum_tensor(uniq(n), s, d))
        PADM = 64 * 16
        qT2 = [sb(f"at_q{i}", [128, L], BF16) for i in range(3)]
        kT2 = [sb(f"at_k{i}", [128, L + 2 * PADM], BF16) for i in range(3)]
        b_qk = bufs("at_qk", 3)
        raw = [sb(f"at_raw{i}", [128, L], BF16) for i in range(2)]
        b_raw = bufs("at_raw", 2)
        Vn = [sb(f"at_v{i}", [128, 6, 65], BF16) for i in range(4)]
        b_Vn = bufs("at_v", 4)
        Ve = [sb(f"at_ve{i}", [128, 6, 65], BF16) for i in range(2)]
        b_Ve = bufs("at_ve", 2)
        for i in range(4):
            S.op("pool", lambda e: e.memset(Vn[i][:, :, 64:65], 1.0), writes=[b_Vn[i]])
        S.op("pool", lambda e: e.memset(Ve[0][:], 0.0), writes=[b_Ve[0]])
        S.op("pool", lambda e: e.memset(Ve[0][64:128, :, 64:65], 1.0), writes=[b_Ve[0]])
        S.op("pool", lambda e: e.memset(Ve[1][:], 0.0), writes=[b_Ve[1]])
        S.op("pool", lambda e: e.memset(Ve[1][0:64, :, 64:65], 1.0), writes=[b_Ve[1]])
        S_ps = [ps(f"at_ps{i}", [128, 2, 128], F32) for i in range(2)]
        b_Sps = bufs("at_ps", 2)
        O_ps = [ps(f"at_po{i}", [128, 6, 65], F32) for i in range(2)]
        b_Ops = bufs("at_po", 2)
        sbt = [sb(f"at_sb{i}", [128, 256], F32) for i in range(2)]
        b_sbt = bufs("at_sb", 2)
        PT = [sb(f"at_pt{i}", [128, 2, 128], BF16) for i in range(2)]
        b_PT = bufs("at_pt", 2)
        Ot = [sb(f"at_o{i}", [128, 390], F32) for i in range(2)]
        b_Ot = bufs("at_o", 2)
        nv = 0
        nsp = 0
        nop = 0
        for g, (window, dil) in enumerate(ATT_PAT):
            ls = L // dil
            PAD = 64 * dil
            nt = ls // 128
            for hp in range(3):
                r0 = g * 384 + hp * 128
                S.dma("sp", raw[0][:], scr.qT[r0:r0 + 128, :], reads=[scr.b_proj], writes=[b_raw[0]], owner=b_raw[0])
                S.dma("sp", raw[1][:], scr.kT[r0:r0 + 128, :], reads=[scr.b_proj], writes=[b_raw[1]], owner=b_raw[1])
                qv = qT2[hp][:, 0:L].rearrange("p (r i) -> p r i", r=dil)
                kv = kT2[hp][:, 0:dil * (ls + 128)].rearrange("p (r i) -> p r i", r=dil)
                S.op("pool", lambda e: e.tensor_copy(out=qv, in_=raw[0][:].rearrange("p (i r) -> p r i", r=dil)),
                     reads=[b_raw[0]], writes=[b_qk[hp]])
                S.op("pool", lambda e: e.memset(kv[:, :, 0:64], 0.0), writes=[b_qk[hp]])
                S.op("pool", lambda e: e.memset(kv[:, :, 64 + ls:128 + ls], 0.0), writes=[b_qk[hp]])
                S.op("dve", lambda e: e.tensor_copy(out=kv[:, :, 64:64 + ls], in_=raw[1][:].rearrange("p (i r) -> p r i", r=dil)),
                     reads=[b_raw[1]], writes=[b_qk[hp]])
            for r in range(dil):
                for n in range(nt):
                    vch = []
                    for c in range(2):
                        kp0 = 128 * n - 64 + 128 * c
                        lo_bad = kp0 < 0
                        hi_bad = kp0 + 128 > ls
                        if lo_bad:
                            vt, bv, p0, p1 = Ve[0], b_Ve[0], 64, 128
                        elif hi_bad:
                            vt, bv, p0, p1 = Ve[1], b_Ve[1], 0, 64
                        else:
                            vi = nv % 4
                            nv += 1
                            vt, bv, p0, p1 = Vn[vi], b_Vn[vi], 0, 128
                        tok0 = (kp0 + p0) * dil + r
                        npart = p1 - p0
                        src = scr.v[tok0:tok0 + (npart - 1) * dil + 1:dil, g * 384:(g + 1) * 384].rearrange("p (h e) -> p h e", h=6)
                        S.dma("sp", vt[p0:p1, :, 0:64], src, reads=[scr.b_proj], writes=[bv], owner=bv)
                        vch.append((vt, bv))
                    oj = nop % 2
                    nop += 1
                    for h in range(6):
                        hp, hh = h // 2, h % 2
                        sj = nsp % 2
                        nsp += 1
                        q0 = r * ls + 128 * n
                        for c in range(2):
                            k0 = r * (ls + 128) + 128 * n + 128 * c
                            S.op("pe", lambda e: e.matmul(S_ps[sj][:, c, :],
                                                          lhsT=kT2[hp][hh * 64:(hh + 1) * 64, k0:k0 + 128],
                                                          rhs=qT2[hp][hh * 64:(hh + 1) * 64, q0:q0 + 128],
                                                          start=True, stop=True),
                                 reads=[b_qk[hp]], writes=[b_Sps[sj]], inc=(c == 1))
                        S.op("dve", lambda e: e.scalar_tensor_tensor(out=sbt[sj][:], in0=S_ps[sj][:].rearrange("p c q -> p (c q)"), scalar=0.125,
                                                                     in1=cx.abias[:, g * 6 + h, :], op0=ALU.mult, op1=ALU.add),
                             reads=[b_Sps[sj], cx.b_abias], writes=[b_sbt[sj]])
                        S.op("act", lambda e: e.activation(out=PT[sj][:].rearrange("p c q -> p (c q)"), in_=sbt[sj][:], func=AF.Exp),
                             reads=[b_sbt[sj]], writes=[b_PT[sj]])
                        for c in range(2):
                            vt, bv = vch[c]
                            S.op("pe", lambda e: e.matmul(O_ps[oj][:, h, :], lhsT=PT[sj][:, c, :], rhs=vt[:, h, :], start=(c == 0), stop=(c == 1)),
                                 reads=[b_PT[sj], bv], writes=[b_Ops[oj]], inc=(c == 1))
                    S.op("act", lambda e: e.copy(out=Ot[oj][:], in_=O_ps[oj][:].rearrange("p h e -> p (h e)")), reads=[b_Ops[oj]], writes=[b_Ot[oj]])
                    t0 = (128 * n) * dil + r
                    S.dma("sp", scr.attO[g, t0:t0 + 127 * dil + 1:dil, :], Ot[oj][:], reads=[b_Ot[oj]], writes=[scr.b_att], owner=b_Ot[oj])
        S.barrier()
        At = [sb(f"at_m{i}", [128, 3, 390], F32) for i in range(2)]
        b_At = bufs("at_m", 2)
        rc = sb("at_rc", [128, 6], F32)
        b_rc = Buf("at_rc")
        yo = [sb(f"at_y{i}", [128, 384], BF16) for i in range(2)]
        b_yo = bufs("at_y", 2)
        for t in range(L // 128):
            i = t % 2
            for g in range(3):
                S.dma("sp", At[i][:, g, :], scr.attO[g, t * 128:(t + 1) * 128, :], reads=[scr.b_att], writes=[b_At[i]], owner=b_At[i])
            S.op("dve", lambda e: e.tensor_tensor(out=At[i][:, 0, :], in0=At[i][:, 0, :], in1=At[i][:, 1, :], op=ALU.add), reads=[b_At[i]], writes=[b_At[i]])
            S.op("dve", lambda e: e.tensor_tensor(out=At[i][:, 0, :], in0=At[i][:, 0, :], in1=At[i][:, 2, :], op=ALU.add), reads=[b_At[i]], writes=[b_At[i]])
            a3 = At[i][:, 0, :].rearrange("p (h e) -> p h e", h=6)
            S.op("dve", lambda e: e.reciprocal(out=rc[:], in_=a3[:, :, 64]), reads=[b_At[i]], writes=[b_rc])
            S.op("dve", lambda e: e.tensor_tensor(out=yo[i][:].rearrange("p (h e) -> p h e", h=6), in0=a3[:, :, 0:64], in1=bc_l(rc[:], 64), op=ALU.mult),
                 reads=[b_At[i], b_rc], writes=[b_yo[i]])
            S.dma("sp", scr.yatt[t * 128:(t + 1) * 128, :], yo[i][:], reads=[b_yo[i]], writes=[scr.b_mix], owner=b_yo[i])
        S.barrier()


def layer_norm_tile(S, h, b_h, out, b_out, gam, bet, b_par, st, b_st):
    for j in range(2):
        S.op("dve", lambda e: e.bn_stats(out=st[:, j * 6:(j + 1) * 6], in_=h[:, j * 512:(j + 1) * 512]), reads=[b_h], writes=[b_st])
    S.op("dve", lambda e: e.bn_aggr(out=st[:, 12:14], in_=st[:, 0:12]), reads=[b_st], writes=[b_st])
    S.op("dve", lambda e: e.tensor_scalar(out=st[:, 14:15], in0=st[:, 13:14], scalar1=LN_EPS, scalar2=None, op0=ALU.add), reads=[b_st], writes=[b_st])
    S.op("act", lambda e: e.activation(out=st[:, 14:15], in_=st[:, 14:15], func=AF.Sqrt), reads=[b_st], writes=[b_st])
    S.op("dve", lambda e: e.reciprocal(out=st[:, 14:15], in_=st[:, 14:15]), reads=[b_st], writes=[b_st])
    S.op("dve", lambda e: e.tensor_scalar(out=out[:], in0=h[:], scalar1=st[:, 12:13], scalar2=st[:, 14:15], op0=ALU.subtract, op1=ALU.mult),
         reads=[b_h, b_st], writes=[b_out])
    S.op("dve", lambda e: e.tensor_tensor(out=out[:], in0=out[:], in1=gam[:], op=ALU.mult), reads=[b_out, b_par], writes=[b_out])
    S.op("dve", lambda e: e.tensor_tensor(out=out[:], in0=out[:], in1=bet[:], op=ALU.add), reads=[b_out, b_par], writes=[b_out])


def load_w_bf16(S, w_ap, dst, b_dst, stg, b_stg, ctr, ncols_max=512):
    K, N = w_ap.shape
    for k in range(K // 128):
        for c0 in range(0, N, ncols_max):
            c1 = min(N, c0 + ncols_max)
            i = ctr[0] % 2
            ctr[0] += 1
            S.dma("sp", stg[i][:, 0:c1 - c0], w_ap[k * 128:(k + 1) * 128, c0:c1], writes=[b_stg[i]], owner=b_stg[i])
            eng = "pool" if ctr[0] % 2 else "act"
            if eng == "pool":
                S.op("pool", lambda e: e.tensor_copy(out=dst[:, k, c0:c1], in_=stg[i][:, 0:c1 - c0]), reads=[b_stg[i]], writes=[b_dst])
            else:
                S.op("act", lambda e: e.copy(out=dst[:, k, c0:c1], in_=stg[i][:, 0:c1 - c0]), reads=[b_stg[i]], writes=[b_dst])


def phase_mix(S, nc, cx, cfg, prm, l, x_ap, scr):
    L = cfg.L
    with ExitStack() as es:
        sb = lambda n, s, d: es.enter_context(nc.sbuf_tensor(uniq(n), s, d))
        ps = lambda n, s, d: es.enter_context(nc.psum_tensor(uniq(n), s, d))
        W1 = sb("gl_w1", [128, 18, 1152], BF16)
        W2 = sb("gl_w2", [128, 18, 1152], BF16)
        b_W = Buf("gl_w")
        stg = [sb(f"gl_st{i}", [128, 1152], F32) for i in range(2)]
        b_stg = bufs("gl_st", 2)
        for i in range(2):
            S.op("pool", lambda e: e.memset(stg[i][:], 0.0), writes=[b_stg[i]])
        n = 0
        for W, wname in ((W1, "s5_glu_w1"), (W2, "s5_glu_w2")):
            for kt in range(18):
                i = n % 2
                n += 1
                for gl in range(4):
                    g = kt * 4 + gl
                    S.dma("sp", stg[i][gl * 32:gl * 32 + 16, :], prm[wname][l][g * 16:(g + 1) * 16, :], writes=[b_stg[i]], owner=b_stg[i])
                S.op("pool", lambda e: e.tensor_copy(out=W[:, kt, :], in_=stg[i][:]), reads=[b_stg[i]], writes=[b_W])
        hT = [sb(f"gl_h{i}", [128, 18, 512], BF16) for i in range(2)]
        b_hT = bufs("gl_h", 2)
        g_ps = [ps(f"gl_pg{i}", [128, 512], F32) for i in range(2)]
        b_gps = bufs("gl_pg", 2)
        l_ps = [ps(f"gl_pl{i}", [128, 512], F32) for i in range(2)]
        b_lps = bufs("gl_pl", 2)
        sg = [sb(f"gl_sg{i}", [128, 512], F32) for i in range(2)]
        b_sg = bufs("gl_sg", 2)
        yo = [sb(f"gl_y{i}", [128, 512], BF16) for i in range(2)]
        b_yo = bufs("gl_y", 2)
        np_ = 0
        for tb in range(L // 512):
            i = tb % 2
            S.dma("sp", hT[i][:], scr.hs5T[:, tb * 512:(tb + 1) * 512].rearrange("(k p) t -> p k t", p=128),
                  reads=[scr.b_s5], writes=[b_hT[i]], owner=b_hT[i])
            for co in range(9):
                j = np_ % 2
                np_ += 1
                for kt in range(18):
                    S.op("pe", lambda e: e.matmul(g_ps[j][:], lhsT=W1[:, kt, co * 128:(co + 1) * 128], rhs=hT[i][:, kt, :], start=(kt == 0), stop=(kt == 17)),
                         reads=[b_W, b_hT[i]], writes=[b_gps[j]], inc=(kt == 17))
                for kt in range(18):
                    S.op("pe", lambda e: e.matmul(l_ps[j][:], lhsT=W2[:, kt, co * 128:(co + 1) * 128], rhs=hT[i][:, kt, :], start=(kt == 0), stop=(kt == 17)),
                         reads=[b_W, b_hT[i]], writes=[b_lps[j]], inc=(kt == 17))
                S.op("act", lambda e: e.activation(out=sg[j][:], in_=l_ps[j][:], func=AF.Sigmoid), reads=[b_lps[j]], writes=[b_sg[j]])
                S.op("dve", lambda e: e.tensor_tensor(out=yo[j][:], in0=g_ps[j][:], in1=sg[j][:], op=ALU.mult), reads=[b_gps[j], b_sg[j]], writes=[b_yo[j]])
                S.dma("sp", scr.ys5T[co * 128:(co + 1) * 128, tb * 512:(tb + 1) * 512], yo[j][:], reads=[b_yo[j]], writes=[scr.b_glu], owner=b_yo[j])
        S.barrier()
    with ExitStack() as es:
        sb = lambda n, s, d: es.enter_context(nc.sbuf_tensor(uniq(n), s, d))
        ps = lambda n, s, d: es.enter_context(nc.psum_tensor(uniq(n), s, d))
        Wssd = sb("mx_wssd", [128, 16, 1024], BF16)
        Watt = sb("mx_watt", [128, 3, 1024], BF16)
        Ws5 = sb("mx_ws5", [128, 9, 1024], BF16)
        Wout = sb("mx_wout", [128, 8, 1024], BF16)
        b_W = Buf("mx_w")
        stg = [sb(f"mx_st{i}", [128, 512], F32) for i in range(2)]
        b_stg = bufs("mx_st", 2)
        ctr = [0]
        load_w_bf16(S, prm["w_br_ssd"][l], Wssd, b_W, stg, b_stg, ctr)
        load_w_bf16(S, prm["w_br_attn"][l], Watt, b_W, stg, b_stg, ctr)
        load_w_bf16(S, prm["w_br_s5"][l], Ws5, b_W, stg, b_stg, ctr)
        load_w_bf16(S, prm["w_out"][l], Wout, b_W, stg, b_stg, ctr)
        bg = sb("mx_bg", [128, 3072], F32)
        lng = sb("mx_lng", [128, 1024], F32)
        lnb = sb("mx_lnb", [128, 1024], F32)
        b_par = Buf("mx_par")
        S.dma("sp", bg[:], prm["b_gate"][l].rearrange("a b -> (a b)").partition_broadcast(128), writes=[b_par], owner=b_par)
        S.dma("sp", lng[:], prm["ln1_g"][l].partition_broadcast(128), writes=[b_par], owner=b_par)
        S.dma("sp", lnb[:], prm["ln1_b"][l].partition_broadcast(128), writes=[b_par], owner=b_par)
        ys = [sb(f"mx_ys{i}", [128, 2048], BF16) for i in range(2)]
        b_ys = bufs("mx_ys", 2)
        ya = [sb(f"mx_ya{i}", [128, 384], BF16) for i in range(2)]
        b_ya = bufs("mx_ya", 2)
        y5T = [sb(f"mx_y5{i}", [128, 9, 128], BF16) for i in range(2)]
        b_y5T = bufs("mx_y5", 2)
        gt = [sb(f"mx_g{i}", [128, 3072], F32) for i in range(2)]
        b_gt = bufs("mx_g", 2)
        xt = [sb(f"mx_x{i}", [128, 1024], F32) for i in range(2)]
        b_xt = bufs("mx_x", 2)
        ysT = sb("mx_ysT", [128, 16, 128], BF16)
        b_ysT = Buf("mx_ysT")
        yaT = sb("mx_yaT", [128, 3, 128], BF16)
        b_yaT = Buf("mx_yaT")
        mT = sb("mx_mT", [128, 8, 128], BF16)
        b_mT = Buf("mx_mT")
        pt = [ps(f"mx_pt{i}", [128, 4, 128], BF16) for i in range(2)]
        b_pt = bufs("mx_pt", 2)
        Pb = [ps(f"mx_pb{i}", [128, 1024], F32) for i in range(2)]
        b_Pb = bufs("mx_pb", 2)
        acc = sb("mx_acc", [128, 1024], F32)
        b_acc = Buf("mx_acc")
        tmp = sb("mx_tmp", [128, 1024], F32)
        b_tmp = Buf("mx_tmp")
        mb = sb("mx_mb", [128, 1024], BF16)
        b_mb = Buf("mx_mb")
        hh = sb("mx_h", [128, 1024], F32)
        b_hh = Buf("mx_h")
        xo = [sb(f"mx_xo{i}", [128, 1024], F32) for i in range(2)]
        b_xo = bufs("mx_xo", 2)
        st = sb("mx_stt", [128, 16], F32)
        b_st = Buf("mx_stt")
        npt = 0
        npb = 0

        def transposes(src, nk, dst, b_src, b_dst):
            nonlocal npt
            for k0 in range(0, nk, 4):
                kn = min(4, nk - k0)
                j = npt % 2
                npt += 1
                for kk in range(kn):
                    S.op("pe", lambda e: e.transpose(out=pt[j][:, kk, :], in_=src[:, (k0 + kk) * 128:(k0 + kk + 1) * 128], identity=cx.identb[:]),
                         reads=[b_src, cx.b_const], writes=[b_pt[j]], inc=(kk == kn - 1))
                S.op("act", lambda e: e.copy(out=dst[:, k0:k0 + kn, :], in_=pt[j][:, 0:kn, :]), reads=[b_pt[j]], writes=[b_dst])

        for t in range(L // 128):
            i = t % 2
            rows = slice(t * 128, (t + 1) * 128)
            S.dma("sp", ys[i][:], scr.yssd[rows, :], reads=[scr.b_mix], writes=[b_ys[i]], owner=b_ys[i])
            S.dma("sp", ya[i][:], scr.yatt[rows, :], reads=[scr.b_mix], writes=[b_ya[i]], owner=b_ya[i])
            S.dma("sp", y5T[i][:], scr.ys5T[:, rows].rearrange("(k p) t -> p k t", p=128), reads=[scr.b_glu], writes=[b_y5T[i]], owner=b_y5T[i])
            S.dma("sp", gt[i][:], scr.gates[rows, :], reads=[scr.b_proj], writes=[b_gt[i]], owner=b_gt[i])
            S.dma("sp", xt[i][:], x_ap[rows, :], writes=[b_xt[i]], owner=b_xt[i])
            S.op("pool", lambda e: e.tensor_tensor(out=gt[i][:], in0=gt[i][:], in1=bg[:], op=ALU.add), reads=[b_gt[i], b_par], writes=[b_gt[i]])
            S.op("act", lambda e: e.activation(out=gt[i][:], in_=gt[i][:], func=AF.Sigmoid), reads=[b_gt[i]], writes=[b_gt[i]])
            transposes(ys[i], 16, ysT, b_ys[i], b_ysT)
            transposes(ya[i], 3, yaT, b_ya[i], b_yaT)
            for bi, (srcT, b_srcT, nk, W) in enumerate(((ysT, b_ysT, 16, Wssd), (yaT, b_yaT, 3, Watt), (y5T[i], b_y5T[i], 9, Ws5))):
                j = npb % 2
                npb += 1
                for nh in range(2):
                    for k in range(nk):
                        S.op("pe", lambda e: e.matmul(Pb[j][:, nh * 512:(nh + 1) * 512], lhsT=srcT[:, k, :], rhs=W[:, k, nh * 512:(nh + 1) * 512],
                                                      start=(k == 0), stop=(k == nk - 1)),
                             reads=[b_srcT, b_W], writes=[b_Pb[j]], inc=(k == nk - 1 and nh == 1))
                if bi == 0:
                    S.op("dve", lambda e: e.tensor_tensor(out=acc[:], in0=Pb[j][:], in1=gt[i][:, 0:1024], op=ALU.mult), reads=[b_Pb[j], b_gt[i]], writes=[b_acc])
                else:
                    S.op("dve", lambda e: e.tensor_tensor(out=tmp[:], in0=Pb[j][:], in1=gt[i][:, bi * 1024:(bi + 1) * 1024], op=ALU.mult),
                         reads=[b_Pb[j], b_gt[i]], writes=[b_tmp])
                    if bi == 1:
                        S.op("pool", lambda e: e.tensor_tensor(out=acc[:], in0=acc[:], in1=tmp[:], op=ALU.add), reads=[b_acc, b_tmp], writes=[b_acc])
                    else:
                        S.op("pool", lambda e: e.tensor_tensor(out=mb[:], in0=acc[:], in1=tmp[:], op=ALU.add), reads=[b_acc, b_tmp], writes=[b_mb])
            transposes(mb, 8, mT, b_mb, b_mT)
            j = npb % 2
            npb += 1
            for nh in range(2):
                for k in range(8):
                    S.op("pe", lambda e: e.matmul(Pb[j][:, nh * 512:(nh + 1) * 512], lhsT=mT[:, k, :], rhs=Wout[:, k, nh * 512:(nh + 1) * 512],
                                                  start=(k == 0), stop=(k == 7)),
                         reads=[b_mT, b_W], writes=[b_Pb[j]], inc=(k == 7 and nh == 1))
            S.op("dve", lambda e: e.scalar_tensor_tensor(out=hh[:], in0=xt[i][:], scalar=cfg.alpha, in1=Pb[j][:], op0=ALU.mult, op1=ALU.add),
                 reads=[b_xt[i], b_Pb[j]], writes=[b_hh])
            layer_norm_tile(S, hh, b_hh, xo[i], b_xo[i], lng, lnb, b_par, st, b_st)
            S.dma("sp", scr.x1[rows, :], xo[i][:], reads=[b_xo[i]], writes=[scr.b_x1], owner=b_xo[i])
        S.barrier()


def cast_dram_bf16(S, nc, src, dst, b_dst, stg, stb, b_stg, b_stb, ctr):
    R, N = src.shape
    for r in range(0, R, 128):
        i = ctr[0] % 2
        ctr[0] += 1
        S.dma("sp", stg[i][:, 0:N], src[r:r + 128, :], writes=[b_stg[i]], owner=b_stg[i])
        if ctr[0] % 2:
            S.op("pool", lambda e: e.tensor_copy(out=stb[i][:, 0:N], in_=stg[i][:, 0:N]), reads=[b_stg[i]], writes=[b_stb[i]])
        else:
            S.op("act", lambda e: e.copy(out=stb[i][:, 0:N], in_=stg[i][:, 0:N]), reads=[b_stg[i]], writes=[b_stb[i]])
        S.dma("sp", dst[r:r + 128, :], stb[i][:, 0:N], reads=[b_stb[i]], writes=[b_dst], owner=b_stb[i])


def phase_moe(S, nc, cx, cfg, prm, l, scr, xdst):
    L = cfg.L
    E = cfg.E
    with ExitStack() as es:
        sb = lambda n, s, d: es.enter_context(nc.sbuf_tensor(uniq(n), s, d))
        ps = lambda n, s, d: es.enter_context(nc.psum_tensor(uniq(n), s, d))
        stg = [sb(f"mo_cs{i}", [128, 2048], F32) for i in range(2)]
        stb = [sb(f"mo_cb{i}", [128, 2048], BF16) for i in range(2)]
        b_stg = bufs("mo_cs", 2)
        b_stb = bufs("mo_cb", 2)
        ctr = [0]
        for e_ in range(E):
            cast_dram_bf16(S, nc, prm["exp_w_gate_up"][l][e_], scr.wgu16[e_], scr.b_w16, stg, stb, b_stg, b_stb, ctr)
            cast_dram_bf16(S, nc, prm["exp_w_down"][l][e_], scr.wdn16[e_], scr.b_w16, stg, stb, b_stg, b_stb, ctr)
        Wr = sb("mo_wr", [128, 8, E], F32)
        Wrh = sb("mo_wrh", [128, 8, E], BF16)
        Wrl = sb("mo_wrl", [128, 8, E], BF16)
        rb = sb("mo_rb", [128, E], F32)
        b_wr = Buf("mo_wr")
        S.dma("sp", Wr[:], prm["router_w"][l].rearrange("(k p) e -> p k e", p=128), writes=[b_wr], owner=b_wr)
        S.dma("sp", rb[:], prm["router_b"][l].partition_broadcast(128), writes=[b_wr], owner=b_wr)
        S.op("dve", lambda e: e.tensor_copy(out=Wrh[:], in_=Wr[:]), reads=[b_wr], writes=[b_wr])
        S.op("dve", lambda e: e.tensor_tensor(out=Wr[:], in0=Wr[:], in1=Wrh[:], op=ALU.subtract), reads=[b_wr], writes=[b_wr])
        S.op("dve", lambda e: e.tensor_copy(out=Wrl[:], in_=Wr[:]), reads=[b_wr], writes=[b_wr])
        xt = [sb(f"mo_x{i}", [128, 1024], F32) for i in range(2)]
        b_xt = bufs("mo_x", 2)
        xh = sb("mo_xh", [128, 1024], BF16)
        xl = sb("mo_xl", [128, 1024], BF16)
        b_xhl = Buf("mo_xhl")
        xTl = sb("mo_xTl", [128, 8, 128], BF16)
        b_xTl = Buf("mo_xTl")
        xTb = [sb(f"mo_xTb{i}", [128, 8, 128], BF16) for i in range(2)]
        b_xTb = bufs("mo_xTb", 2)
        ptf = [ps(f"mo_ptf{i}", [128, 4, 128], BF16) for i in range(2)]
        b_ptf = bufs("mo_ptf", 2)
        lg_ps = ps("mo_lg", [128, E], F32)
        b_lgps = Buf("mo_lg")
        lg = sb("mo_lgs", [128, E], F32)
        b_lg = Buf("mo_lgs")
        v8 = sb("mo_v8", [128, 8], F32)
        b_v8 = Buf("mo_v8")
        mk = sb("mo_mk", [128, E], F32)
        b_mk = Buf("mo_mk")
        sm = sb("mo_sm", [128, 4], F32)
        b_sm = Buf("mo_sm")
        gts = [sb(f"mo_gt{i}", [128, E], F32) for i in range(2)]
        b_gts = bufs("mo_gt", 2)
        npt = 0
        for t in range(L // 128):
            i = t % 2
            rows = slice(t * 128, (t + 1) * 128)
            S.dma("sp", xt[i][:], scr.x1[rows, :], reads=[scr.b_x1], writes=[b_xt[i]], owner=b_xt[i])
            S.op("dve", lambda e: e.tensor_copy(out=xh[:], in_=xt[i][:]), reads=[b_xt[i]], writes=[b_xhl])
            S.op("dve", lambda e: e.tensor_tensor(out=xl[:], in0=xt[i][:], in1=xh[:], op=ALU.subtract), reads=[b_xt[i], b_xhl], writes=[b_xhl])
            for src, dstT, b_dstT in ((xh, xTb[i], b_xTb[i]), (xl, xTl, b_xTl)):
                for k4 in range(2):
                    j = npt % 2
                    npt += 1
                    for kk in range(4):
                        k = k4 * 4 + kk
                        S.op("pe", lambda e: e.transpose(out=ptf[j][:, kk, :], in_=src[:, k * 128:(k + 1) * 128], identity=cx.identb[:]),
                             reads=[b_xhl, cx.b_const], writes=[b_ptf[j]], inc=(kk == 3))
                    S.op("act", lambda e: e.copy(out=dstT[:, k4 * 4:(k4 + 1) * 4, :], in_=ptf[j][:]), reads=[b_ptf[j]], writes=[b_dstT])
            S.dma("sp", scr.x1T[:, rows].rearrange("(k p) t -> p k t", p=128), xTb[i][:], reads=[b_xTb[i]], writes=[scr.b_x1T], owner=b_xTb[i])
            n_mm = 0
            for k in range(8):
                for (xa, b_xa, wa) in ((xTb[i], b_xTb[i], Wrh), (xTl, b_xTl, Wrh), (xTb[i], b_xTb[i], Wrl)):
                    S.op("pe", lambda e: e.matmul(lg_ps[:], lhsT=xa[:, k, :], rhs=wa[:, k, :], start=(n_mm == 0), stop=(n_mm == 23)),
                         reads=[b_xa, b_wr], writes=[b_lgps], inc=(n_mm == 23))
                    n_mm += 1
            S.op("dve", lambda e: e.tensor_tensor(out=lg[:], in0=lg_ps[:], in1=rb[:], op=ALU.add), reads=[b_lgps, b_wr], writes=[b_lg])
            S.op("dve", lambda e: e.max(out=v8[:], in_=lg[:]), reads=[b_lg], writes=[b_v8])
            S.op("dve", lambda e: e.tensor_scalar(out=mk[:], in0=lg[:], scalar1=v8[:, 3:4], scalar2=None, op0=ALU.is_ge), reads=[b_lg, b_v8], writes=[b_mk])
            S.op("dve", lambda e: e.tensor_scalar(out=sm[:, 0:1], in0=v8[:, 0:1], scalar1=-1.0, scalar2=None, op0=ALU.mult), reads=[b_v8], writes=[b_sm])
            S.op("act", lambda e: e.activation(out=lg[:], in_=lg[:], func=AF.Exp, bias=sm[:, 0:1], scale=1.0), reads=[b_lg, b_sm], writes=[b_lg])
            S.op("dve", lambda e: e.tensor_tensor(out=lg[:], in0=lg[:], in1=mk[:], op=ALU.mult), reads=[b_lg, b_mk], writes=[b_lg])
            S.op("dve", lambda e: e.reduce_sum(out=sm[:, 1:2], in_=lg[:], axis=AX.X), reads=[b_lg, b_sm], writes=[b_sm])
            S.op("dve", lambda e: e.reciprocal(out=sm[:, 2:3], in_=sm[:, 1:2]), reads=[b_sm], writes=[b_sm])
            S.op("dve", lambda e: e.tensor_scalar(out=gts[i][:], in0=lg[:], scalar1=sm[:, 2:3], scalar2=None, op0=ALU.mult), reads=[b_lg, b_sm], writes=[b_gts[i]])
            S.dma("sp", scr.rgate[rows, :], gts[i][:], reads=[b_gts[i]], writes=[scr.b_x1T], owner=b_gts[i])
        S.barrier()
    TBm = 512
    with ExitStack() as es:
        sb = lambda n, s, d: es.enter_context(nc.sbuf_tensor(uniq(n), s, d))
        ps = lambda n, s, d: es.enter_context(nc.psum_tensor(uniq(n), s, d))
        Wgu = [sb(f"me_wgu{i}", [128, 8, 2048], BF16) for i in range(2)]
        Wdn = [sb(f"me_wdn{i}", [128, 8, 1024], BF16) for i in range(2)]
        b_Wgu = bufs("me_wgu", 2)
        b_Wdn = bufs("me_wdn", 2)
        bgu = sb("me_bgu", [128, E, 16], F32)
        bdn = sb("me_bdn", [E, 1024], F32)
        bdn16 = sb("me_bdn16", [E, 1024], BF16)
        lng = sb("me_lng", [128, 1024], F32)
        lnb = sb("me_lnb", [128, 1024], F32)
        b_par = Buf("me_par")
        with nc.allow_non_contiguous_dma(reason="tiny param load"):
            for e_ in range(E):
                S.dma("sp", bgu[:, e_, :], prm["exp_b_gate_up"][l][e_].rearrange("(c p) -> p c", p=128), writes=[b_par], owner=b_par)
        S.op("dve", lambda e: e.tensor_scalar(out=bgu[:, :, 8:16], in0=bgu[:, :, 8:16], scalar1=1.0, scalar2=None, op0=ALU.add), reads=[b_par], writes=[b_par])
        S.dma("sp", bdn[:], prm["exp_b_down"][l], writes=[b_par], owner=b_par)
        S.op("dve", lambda e: e.tensor_copy(out=bdn16[:], in_=bdn[:]), reads=[b_par], writes=[b_par])
        S.dma("sp", lng[:], prm["ln2_g"][l].partition_broadcast(128), writes=[b_par], owner=b_par)
        S.dma("sp", lnb[:], prm["ln2_b"][l].partition_broadcast(128), writes=[b_par], owner=b_par)
        xT = [sb(f"me_xT{i}", [128, 8, TBm], BF16) for i in range(2)]
        b_xT = bufs("me_xT", 2)
        gt = [sb(f"me_gt{i}", [128, 4, E], F32) for i in range(2)]
        b_gt = bufs("me_gt", 2)
        gT = sb("me_gT", [E, 4, 128], BF16)
        gtb = sb("me_gtb", [128, 4, E], BF16)
        b_gtb = Buf("me_gtb")
        b_gT = Buf("me_gT")
        acc = sb("me_acc", [128, 4, 1024], F32)
        b_acc = bufs("me_acc", 4)
        act = [sb(f"me_act{i}", [128, 8, TBm], BF16) for i in range(2)]
        b_act = bufs("me_act", 2)
        t1 = [sb(f"me_t1{i}", [128, TBm], F32) for i in range(2)]
        b_t1 = bufs("me_t1", 2)
        t2 = [sb(f"me_t2{i}", [128, TBm], F32) for i in range(2)]
        b_t2 = bufs("me_t2", 2)
        sg = [sb(f"me_sg{i}", [128, TBm], F32) for i in range(2)]
        b_sg = bufs("me_sg", 2)
        g_ps = [ps(f"me_pg{i}", [128, 512], F32) for i in range(2)]
        b_gps = bufs("me_pg", 2)
        l_ps = [ps(f"me_pl{i}", [128, 512], F32) for i in range(2)]
        b_lps = bufs("me_pl", 2)
        y_ps = [ps(f"me_py{i}", [128, 512], F32) for i in range(2)]
        b_yps = bufs("me_py", 2)
        ptg = ps("me_ptg", [E, 4, 128], BF16)
        b_ptg = Buf("me_ptg")
        x1t = [sb(f"me_x1{i}", [128, 1024], F32) for i in range(1)] * 2
        b_x1t = bufs("me_x1", 1) * 2
        xo = [sb(f"me_xo{i}", [128, 1024], F32) for i in range(1)] * 2
        b_xo = bufs("me_xo", 1) * 2
        st = sb("me_stt", [128, 16], F32)
        b_st = Buf("me_stt")
        nw = 0
        nfp = 0
        nyp = 0
        nx1 = 0
        for tb in range(L // TBm):
            bi = tb % 2
            cols = slice(tb * TBm, (tb + 1) * TBm)
            S.dma("sp", xT[bi][:], scr.x1T[:, cols].rearrange("(k p) t -> p k t", p=128), reads=[scr.b_x1T], writes=[b_xT[bi]], owner=b_xT[bi])
            S.dma("sp", gt[bi][:], scr.rgate[cols, :].rearrange("(a p) e -> p a e", p=128), reads=[scr.b_x1T], writes=[b_gt[bi]], owner=b_gt[bi])
            S.op("dve", lambda e: e.tensor_copy(out=gtb[:], in_=gt[bi][:]), reads=[b_gt[bi]], writes=[b_gtb])
            for tt in range(4):
                S.op("pe", lambda e: e.transpose(out=ptg[:, tt, :], in_=gtb[:, tt, :], identity=cx.identb[:]),
                     reads=[b_gtb, cx.b_const], writes=[b_ptg], inc=(tt == 3))
            S.op("act", lambda e: e.copy(out=gT[:], in_=ptg[:]), reads=[b_ptg], writes=[b_gT])
            for tt in range(4):
                for nh in range(2):
                    j = nyp % 2
                    nyp += 1
                    S.op("pe", lambda e: e.matmul(y_ps[j][:], lhsT=gT[:, tt, :], rhs=bdn16[:, nh * 512:(nh + 1) * 512], start=True, stop=True),
                         reads=[b_gT, b_par], writes=[b_yps[j]])
                    S.op("act", lambda e: e.copy(out=acc[:, tt, nh * 512:(nh + 1) * 512], in_=y_ps[j][:]), reads=[b_yps[j]], writes=[b_acc[tt]])
            for e_ in range(E):
                wi = nw % 2
                nw += 1
                S.dma("sp", Wgu[wi][:], scr.wgu16[e_].rearrange("(k p) f -> p k f", p=128), reads=[scr.b_w16], writes=[b_Wgu[wi]], owner=b_Wgu[wi])
                S.dma("sp", Wdn[wi][:], scr.wdn16[e_].rearrange("(k p) f -> p k f", p=128), reads=[scr.b_w16], writes=[b_Wdn[wi]], owner=b_Wdn[wi])
                ai = nw % 2
                for fj in range(8):
                    j = nfp % 2
                    nfp += 1
                    for k in range(8):
                        S.op("pe", lambda e: e.matmul(g_ps[j][:], lhsT=Wgu[wi][:, k, fj * 128:(fj + 1) * 128], rhs=xT[bi][:, k, :], start=(k == 0), stop=(k == 7)),
                             reads=[b_Wgu[wi], b_xT[bi]], writes=[b_gps[j]], inc=(k == 7))
                    for k in range(8):
                        S.op("pe", lambda e: e.matmul(l_ps[j][:], lhsT=Wgu[wi][:, k, 1024 + fj * 128:1024 + (fj + 1) * 128], rhs=xT[bi][:, k, :], start=(k == 0), stop=(k == 7)),
                             reads=[b_Wgu[wi], b_xT[bi]], writes=[b_lps[j]], inc=(k == 7))
                    S.op("dve", lambda e: e.tensor_scalar(out=t1[j][:], in0=g_ps[j][:], scalar1=bgu[:, e_, fj:fj + 1], scalar2=7.0, op0=ALU.add, op1=ALU.min),
                         reads=[b_gps[j], b_par], writes=[b_t1[j]])
                    S.op("act", lambda e: e.activation(out=sg[j][:], in_=t1[j][:], func=AF.Sigmoid, scale=1.702), reads=[b_t1[j]], writes=[b_sg[j]])
                    S.op("dve", lambda e: e.tensor_scalar(out=t2[j][:], in0=l_ps[j][:], scalar1=bgu[:, e_, 8 + fj:9 + fj], scalar2=8.0, op0=ALU.add, op1=ALU.min),
                         reads=[b_lps[j], b_par], writes=[b_t2[j]])
                    S.op("pool", lambda e: e.tensor_tensor(out=t1[j][:], in0=t1[j][:], in1=sg[j][:], op=ALU.mult), reads=[b_t1[j], b_sg[j]], writes=[b_t1[j]])
                    S.op("dve", lambda e: e.scalar_tensor_tensor(out=act[ai][:, fj, :], in0=t2[j][:], scalar=-6.0, in1=t1[j][:], op0=ALU.max, op1=ALU.mult),
                         reads=[b_t1[j], b_t2[j]], writes=[b_act[ai]])
                for tt in range(4):
                    for nh in range(2):
                        j = nyp % 2
                        nyp += 1
                        for fk in range(8):
                            S.op("pe", lambda e: e.matmul(y_ps[j][:], lhsT=act[ai][:, fk, tt * 128:(tt + 1) * 128], rhs=Wdn[wi][:, fk, nh * 512:(nh + 1) * 512],
                                                          start=(fk == 0), stop=(fk == 7)),
                                 reads=[b_act[ai], b_Wdn[wi]], writes=[b_yps[j]], inc=(fk == 7))
                        S.op("dve", lambda e: e.scalar_tensor_tensor(out=acc[:, tt, nh * 512:(nh + 1) * 512], in0=y_ps[j][:], scalar=gt[bi][:, tt, e_:e_ + 1],
                                                                     in1=acc[:, tt, nh * 512:(nh + 1) * 512], op0=ALU.mult, op1=ALU.add),
                             reads=[b_yps[j], b_gt[bi], b_acc[tt]], writes=[b_acc[tt]])
            for tt in range(4):
                xi = nx1 % 2
                nx1 += 1
                rows = slice(tb * TBm + tt * 128, tb * TBm + (tt + 1) * 128)
                S.dma("sp", x1t[xi][:], scr.x1[rows, :], reads=[scr.b_x1], writes=[b_x1t[xi]], owner=b_x1t[xi])
                S.op("dve", lambda e: e.scalar_tensor_tensor(out=x1t[xi][:], in0=x1t[xi][:], scalar=cfg.alpha, in1=acc[:, tt, :], op0=ALU.mult, op1=ALU.add),
                     reads=[b_x1t[xi], b_acc[tt]], writes=[b_x1t[xi]])
                layer_norm_tile(S, x1t[xi], b_x1t[xi], xo[xi], b_xo[xi], lng, lnb, b_par, st, b_st)
                S.dma("sp", xdst[rows, :], xo[xi][:], reads=[b_xo[xi]], writes=[scr.b_xcur], owner=b_xo[xi])
        S.barrier()


TWO_PI = 2.0 * math.pi


def phase_s5(S, nc, cx, cfg, prm, l, scr):
    L = cfg.L
    NCk = L // 8
    J = int(math.log2(NCk))
    CB = min(512, NCk)
    with ExitStack() as es:
        sb = lambda n, s, d: es.enter_context(nc.sbuf_tensor(uniq(n), s, d))
        ps = lambda n, s, d: es.enter_context(nc.psum_tensor(uniq(n), s, d))
        bP = Buf("s5_par")
        NP = 72

        def T(name, cols, dt=F32):
            return sb("s5_" + name, [128, cols], dt)

        ar, ai, ls = T("ar", NP), T("ai", NP), T("ls", NP)
        with nc.allow_non_contiguous_dma(reason="tiny param load"):
            for d in range(2):
                S.dma("sp", ar[:, d * 36:(d + 1) * 36], prm["s5_a_re"][l][d].rearrange("(p g) n -> (g n) p", g=2), writes=[bP], owner=bP)
                S.dma("sp", ai[:, d * 36:(d + 1) * 36], prm["s5_a_im"][l][d].rearrange("(p g) n -> (g n) p", g=2), writes=[bP], owner=bP)
                for gl in range(2):
                    S.dma("sp", ls[64 * gl:64 * gl + 64, d * 36:(d + 1) * 36],
                          prm["s5_log_step"][l][d].rearrange("(p g) -> g p", g=2)[gl].partition_broadcast(64), writes=[bP], owner=bP)
        BR = sb("s5_BR", [128, 36, 16], F32)
        BI = sb("s5_BI", [128, 36, 16], F32)
        S.dma("sp", BR[:], prm["s5_b_re"][l].rearrange("(p g) n c -> (g n) p c", g=2), writes=[bP], owner=bP)
        S.dma("sp", BI[:], prm["s5_b_im"][l].rearrange("(p g) n c -> (g n) p c", g=2), writes=[bP], owner=bP)
        dpad = sb("s5_dpad", [128, 18], F32)
        S.op("pool", lambda e: e.memset(dpad[:], 0.0), writes=[bP])
        with nc.allow_non_contiguous_dma(reason="tiny param load"):
            for g4 in range(4):
                S.dma("sp", dpad[32 * g4:32 * g4 + 16, :], prm["s5_d"][l].rearrange("(t g c) -> g c t", g=4, c=16)[g4], writes=[bP], owner=bP)

        def dv(fn, eng="dve"):
            S.op(eng, fn, reads=[bP], writes=[bP])

        def tt(out, a, b, op, eng="dve"):
            dv(lambda e: e.tensor_tensor(out=out, in0=a, in1=b, op=op), eng)

        def ts(out, a, s1, op0, s2=None, op1=None):
            if op1 is None:
                dv(lambda e: e.tensor_scalar(out=out, in0=a, scalar1=s1, scalar2=None, op0=op0))
            else:
                dv(lambda e: e.tensor_scalar(out=out, in0=a, scalar1=s1, scalar2=s2, op0=op0, op1=op1))

        def act(out, in_, func, scale=1.0):
            S.op("act", lambda e: e.activation(out=out, in_=in_, func=func, scale=scale), reads=[bP], writes=[bP])

        step, sr, th, mag = T("step", NP), T("sr", NP), T("th", NP), T("mag", NP)
        t0, t1_, t2_, ki = T("t0", NP), T("t1", NP), T("t2", NP), sb("s5_ki", [128, NP], I32)
        sn, cs = T("sn", NP), T("cs", NP)
        act(step[:], ls[:], AF.Exp)
        tt(sr[:], step[:], ar[:], ALU.mult)
        tt(th[:], step[:], ai[:], ALU.mult)
        act(mag[:], sr[:], AF.Exp)

        def sin_of(out, ang):
            ts(t0[:], ang, 1.0 / TWO_PI, ALU.mult, 0.5, ALU.add)
            dv(lambda e: e.tensor_copy(out=ki[:], in_=t0[:]))
            dv(lambda e: e.tensor_copy(out=t1_[:], in_=ki[:]))
            dv(lambda e: e.scalar_tensor_tensor(out=t2_[:], in0=t1_[:], scalar=-TWO_PI, in1=ang, op0=ALU.mult, op1=ALU.add))
            ts(t0[:], t2_[:], -math.pi, ALU.is_lt, TWO_PI, ALU.mult)
            tt(t2_[:], t2_[:], t0[:], ALU.add)
            ts(t0[:], t2_[:], math.pi, ALU.is_gt, -TWO_PI, ALU.mult)
            tt(t2_[:], t2_[:], t0[:], ALU.add)
            act(out, t2_[:], AF.Sin)

        thc = T("thc", NP)
        sin_of(sn[:], th[:])
        ts(thc[:], th[:], math.pi / 2, ALU.add)
        sin_of(cs[:], thc[:])
        abr, abi = T("abr", NP), T("abi", NP)
        tt(abr[:], mag[:], cs[:], ALU.mult)
        tt(abi[:], mag[:], sn[:], ALU.mult)
        den, m1, fr, fi = T("den", NP), T("m1", NP), T("fr", NP), T("fi", NP)
        tt(den[:], ar[:], ar[:], ALU.mult)
        tt(t0[:], ai[:], ai[:], ALU.mult)
        tt(den[:], den[:], t0[:], ALU.add)
        dv(lambda e: e.reciprocal(out=den[:], in_=den[:]))
        ts(m1[:], abr[:], -1.0, ALU.add)
        tt(fr[:], m1[:], ar[:], ALU.mult)
        tt(t0[:], abi[:], ai[:], ALU.mult)
        tt(fr[:], fr[:], t0[:], ALU.add)
        tt(fr[:], fr[:], den[:], ALU.mult)
        tt(fi[:], abi[:], ar[:], ALU.mult)
        tt(t0[:], m1[:], ai[:], ALU.mult)
        tt(fi[:], fi[:], t0[:], ALU.subtract)
        tt(fi[:], fi[:], den[:], ALU.mult)
        ivr, ivi = T("ivr", NP), T("ivi", NP)
        act(t0[:], sr[:], AF.Exp, scale=-2.0)
        tt(ivr[:], abr[:], t0[:], ALU.mult)
        tt(ivi[:], abi[:], t0[:], ALU.mult)
        ts(ivi[:], ivi[:], -1.0, ALU.mult)
        PWr = sb("s5_PWr", [128, 9, NP], F32)
        PWi = sb("s5_PWi", [128, 9, NP], F32)
        NGr = sb("s5_NGr", [128, 9, NP], F32)
        NGi = sb("s5_NGi", [128, 9, NP], F32)
        PWrR = sb("s5_PWrR", [128, 9, NP], F32)
        PWiR = sb("s5_PWiR", [128, 9, NP], F32)
        NGrR = sb("s5_NGrR", [128, 9, NP], F32)
        NGiR = sb("s5_NGiR", [128, 9, NP], F32)

        def cmul(or_, oi_, ar_, ai_, br_, bi_):
            tt(t0[:], ar_, br_, ALU.mult)
            tt(t1_[:], ai_, bi_, ALU.mult)
            tt(t2_[:], ar_, bi_, ALU.mult)
            tt(thc[:], ai_, br_, ALU.mult)
            tt(or_, t0[:], t1_[:], ALU.subtract)
            tt(oi_, t2_[:], thc[:], ALU.add)

        for (Pr, Pi, br_, bi_) in ((PWr, PWi, abr, abi), (NGr, NGi, ivr, ivi)):
            dv(lambda e: e.memset(Pr[:, 0, :], 1.0), "pool")
            dv(lambda e: e.memset(Pi[:, 0, :], 0.0), "pool")
            for e_ in range(1, 9):
                cmul(Pr[:, e_, :], Pi[:, e_, :], Pr[:, e_ - 1, :], Pi[:, e_ - 1, :], br_[:], bi_[:])
        for (Pr, PrR) in ((PWr, PWrR), (PWi, PWiR), (NGr, NGrR), (NGi, NGiR)):
            for e_ in range(9):
                dv(lambda e: e.tensor_copy(out=PrR[:, e_, :], in_=Pr[:, 8 - e_, :]), "pool")
        SPr = sb("s5_SPr", [128, J + 1, NP], F32)
        SPi = sb("s5_SPi", [128, J + 1, NP], F32)
        SPn = sb("s5_SPn", [128, J + 1, NP], F32)
        dv(lambda e: e.tensor_copy(out=SPr[:, 0, :], in_=PWr[:, 8, :]))
        dv(lambda e: e.tensor_copy(out=SPi[:, 0, :], in_=PWi[:, 8, :]))
        for j in range(1, J + 1):
            cmul(SPr[:, j, :], SPi[:, j, :], SPr[:, j - 1, :], SPi[:, j - 1, :], SPr[:, j - 1, :], SPi[:, j - 1, :])
        ts(SPn[:], SPi[:], -1.0, ALU.mult)
        bbr = sb("s5_bbr", [128, 2, 36, 16], F32)
        bbi = sb("s5_bbi", [128, 2, 36, 16], F32)
        tb1 = sb("s5_tb1", [128, 2, 36, 16], F32)
        frv = fr[:].rearrange("p (d q) -> p d q", d=2).unsqueeze(3).to_broadcast([128, 2, 36, 16])
        fiv = fi[:].rearrange("p (d q) -> p d q", d=2).unsqueeze(3).to_broadcast([128, 2, 36, 16])
        BRv = BR[:].unsqueeze(1).to_broadcast([128, 2, 36, 16])
        BIv = BI[:].unsqueeze(1).to_broadcast([128, 2, 36, 16])
        tt(bbr[:], frv, BRv, ALU.mult)
        tt(tb1[:], fiv, BIv, ALU.mult)
        tt(bbr[:], bbr[:], tb1[:], ALU.subtract)
        tt(bbi[:], frv, BIv, ALU.mult)
        tt(tb1[:], fiv, BRv, ALU.mult)
        tt(bbi[:], bbi[:], tb1[:], ALU.add)
        CRT = sb("s5_CRT", [128, 2, 36, 16], F32)
        CIT = sb("s5_CIT", [128, 2, 36, 16], F32)
        cin = sb("s5_cin", [128, 128], F32)
        cinb = sb("s5_cinb", [128, 128], BF16)
        pc = ps("s5_pc", [128, 128], BF16)
        b_pc = Buf("s5_pc")
        for (cname, CT_) in (("s5_c_re", CRT), ("s5_c_im", CIT)):
            for d in range(2):
                for pb in range(0, 36, 8):
                    npair = min(8, 36 - pb)
                    for q in range(npair):
                        pr_ = pb + q
                        S.dma("sp", cin[16 * q:16 * q + 16, :].rearrange("c (g n) -> c g n", g=2),
                              prm[cname][l][d][2 * pr_:2 * pr_ + 2].rearrange("g c n -> c g n"), writes=[bP], owner=bP)
                    dv(lambda e: e.tensor_copy(out=cinb[:], in_=cin[:]))
                    S.op("pe", lambda e: e.transpose(out=pc[:], in_=cinb[:], identity=cx.identb[:]), reads=[bP, cx.b_const], writes=[b_pc])
                    S.op("act", lambda e: e.copy(out=CT_[:, d, pb:pb + npair, :], in_=pc[:, 0:npair * 16].rearrange("p (q c) -> p q c", c=16)),
                         reads=[b_pc], writes=[bP])

        Uraw = sb("s5_Uraw", [128, L], BF16)
        b_Ur = Buf("s5_Ur")
        Us = sb("s5_Us", [128, 8, NCk], BF16)
        b_U = Buf("s5_U")
        Wi = sb("s5_Wi", [128, 8, 8, 128], BF16)
        b_Wi = Buf("s5_Wi")
        WOre = [sb(f"s5_WOre{i}", [128, 8, 128], BF16) for i in range(4)]
        WOim = [sb(f"s5_WOim{i}", [128, 8, 128], BF16) for i in range(4)]
        b_WO = bufs("s5_WO", 4)
        Lre = sb("s5_Lre", [128, 8, 128], BF16)
        Lim = sb("s5_Lim", [128, 8, 128], BF16)
        b_L = Buf("s5_L")
        LTre = sb("s5_LTre", [128, 8, 128], BF16)
        LTim = sb("s5_LTim", [128, 8, 128], BF16)
        b_LT = Buf("s5_LT")
        Rr = sb("s5_Rr", [128, 8, 16], F32)
        Ri = sb("s5_Ri", [128, 8, 16], F32)
        Rt = sb("s5_Rt", [128, 8, 16], F32)
        b_R = Buf("s5_R")
        ZR = [sb(f"s5_ZR{i}", [128, NCk], F32) for i in range(2)]
        ZI = [sb(f"s5_ZI{i}", [128, NCk], F32) for i in range(2)]
        b_Z = bufs("s5_Z", 2)
        ztmp = sb("s5_ztmp", [128, NCk], F32)
        b_ztmp = Buf("s5_ztmp")
        HR = [sb(f"s5_HR{i}", [128, NCk], BF16) for i in range(4)]
        HI = [sb(f"s5_HI{i}", [128, NCk], BF16) for i in range(4)]
        b_H = bufs("s5_H", 4)
        pt4 = ps("s5_pt4", [128, 4, 128], BF16)
        b_pt4 = Buf("s5_pt4")
        pK = [ps(f"s5_pK{i}", [128, 4, 128], F32) for i in range(2)]
        b_pK = bufs("s5_pK", 2)
        pS = [ps(f"s5_pS{i}", [128, CB], F32) for i in range(2)]
        b_pS = bufs("s5_pS", 2)
        pY = [ps(f"s5_pY{i}", [128, CB], F32) for i in range(2)]
        b_pY = bufs("s5_pY", 2)
        Dd = sb("s5_Dd", [128, 128], F32)
        b_Dd = Buf("s5_Dd")
        g1 = [sb(f"s5_g1{i}", [128, CB], F32) for i in range(2)]
        g2 = [sb(f"s5_g2{i}", [128, CB], F32) for i in range(2)]
        b_g = bufs("s5_g", 2)
        Yo = Uraw[:].rearrange("p (c t) -> p c t", t=8)
        b_Yo = b_Ur
        npk = 0
        npy = 0
        for tl in range(18):
            S.dma("sp", Uraw[:], scr.uT[tl * 128:(tl + 1) * 128, :], reads=[scr.b_proj], writes=[b_Ur], owner=b_Ur)
            S.op("pool", lambda e: e.tensor_copy(out=Us[:], in_=Uraw[:].rearrange("p (c s) -> p s c", s=8)), reads=[b_Ur], writes=[b_U])
            S.op("pool", lambda e: e.memset(Wi[:].rearrange("p s t c -> p (s t c)"), 0.0), writes=[b_Wi])
            S.op("dve", lambda e: e.tensor_scalar(out=Dd[:], in0=cx.identf[:], scalar1=dpad[:, tl:tl + 1], scalar2=None, op0=ALU.mult),
                 reads=[cx.b_const, bP], writes=[b_Dd])
            for pp in range(2):
                pr_ = 2 * tl + pp
                c0 = 64 * pp
                for d in range(2):
                    k = pp * 2 + d
                    dp = d * 36 + pr_
                    PrT, PiT = (PWr, PWi) if d == 0 else (PWrR, PWiR)
                    NrT, NiT = (NGr, NGi) if d == 0 else (NGrR, NGiR)
                    esl = slice(1, 9) if d == 0 else slice(0, 8)
                    pwr = PrT[:, esl, dp].unsqueeze(2).to_broadcast([128, 8, 16])
                    pwi = PiT[:, esl, dp].unsqueeze(2).to_broadcast([128, 8, 16])
                    ngr = NrT[:, esl, dp].unsqueeze(2).to_broadcast([128, 8, 16])
                    ngi = NiT[:, esl, dp].unsqueeze(2).to_broadcast([128, 8, 16])
                    crt = CRT[:, d, pr_, :].unsqueeze(1).to_broadcast([128, 8, 16])
                    cit = CIT[:, d, pr_, :].unsqueeze(1).to_broadcast([128, 8, 16])
                    bbrv = bbr[:, d, pr_, :].unsqueeze(1).to_broadcast([128, 8, 16])
                    bbiv = bbi[:, d, pr_, :].unsqueeze(1).to_broadcast([128, 8, 16])

                    def cplx(ar_, ai_, br_, bi_):
                        S.op("dve", lambda e: e.tensor_tensor(out=Rr[:], in0=ar_, in1=br_, op=ALU.mult), reads=[bP], writes=[b_R])
                        S.op("dve", lambda e: e.tensor_tensor(out=Rt[:], in0=ai_, in1=bi_, op=ALU.mult), reads=[bP, b_R], writes=[b_R])
                        S.op("dve", lambda e: e.tensor_tensor(out=Rr[:], in0=Rr[:], in1=Rt[:], op=ALU.subtract), reads=[b_R], writes=[b_R])
                        S.op("dve", lambda e: e.tensor_tensor(out=Ri[:], in0=ar_, in1=bi_, op=ALU.mult), reads=[bP, b_R], writes=[b_R])
                        S.op("dve", lambda e: e.tensor_tensor(out=Rt[:], in0=ai_, in1=br_, op=ALU.mult), reads=[bP, b_R], writes=[b_R])
                        S.op("dve", lambda e: e.tensor_tensor(out=Ri[:], in0=Ri[:], in1=Rt[:], op=ALU.add), reads=[b_R], writes=[b_R])

                    cplx(crt, cit, pwr, pwi)
                    S.op("pool", lambda e: e.memset(WOre[k][:].rearrange("p t c -> p (t c)"), 0.0), writes=[b_WO[k]])
                    S.op("pool", lambda e: e.memset(WOim[k][:].rearrange("p t c -> p (t c)"), 0.0), writes=[b_WO[k]])
                    for gl in range(2):
                        prt = slice(64 * gl, 64 * gl + 64)
                        cl = slice(c0 + 32 * gl, c0 + 32 * gl + 16)
                        S.op("act", lambda e: e.copy(out=WOre[k][prt, :, cl], in_=Rr[prt, :, :]), reads=[b_R], writes=[b_WO[k]])
                        S.op("act", lambda e: e.activation(out=WOim[k][prt, :, cl], in_=Ri[prt, :, :], func=AF.Copy, scale=-1.0), reads=[b_R], writes=[b_WO[k]])
                    cplx(ngr, ngi, bbrv, bbiv)
                    S.op("pool", lambda e: e.memset(Lre[:].rearrange("p t c -> p (t c)"), 0.0), writes=[b_L])
                    S.op("pool", lambda e: e.memset(Lim[:].rearrange("p t c -> p (t c)"), 0.0), writes=[b_L])
                    for gl in range(2):
                        prt = slice(64 * gl, 64 * gl + 64)
                        cl = slice(c0 + 32 * gl, c0 + 32 * gl + 16)
                        S.op("act", lambda e: e.copy(out=Lre[prt, :, cl], in_=Rr[prt, :, :]), reads=[b_R], writes=[b_L])
                        S.op("act", lambda e: e.copy(out=Lim[prt, :, cl], in_=Ri[prt, :, :]), reads=[b_R], writes=[b_L])
                    for (Lx, LTx) in ((Lre, LTre), (Lim, LTim)):
                        for s4 in range(2):
                            for ss in range(4):
                                S.op("pe", lambda e: e.transpose(out=pt4[:, ss, :], in_=Lx[:, s4 * 4 + ss, :], identity=cx.identb[:]),
                                     reads=[b_L, cx.b_const], writes=[b_pt4], inc=(ss == 3))
                            S.op("act", lambda e: e.copy(out=LTx[:, s4 * 4:(s4 + 1) * 4, :], in_=pt4[:]), reads=[b_pt4], writes=[b_LT])
                    for s_ in range(8):
                        for th_ in range(2):
                            j = npk % 2
                            npk += 1
                            tsl = slice(th_ * 4, th_ * 4 + 4)
                            S.op("pe", lambda e: e.matmul(pK[j][:].rearrange("p t c -> p (t c)"), lhsT=Lre[:, s_, :],
                                                          rhs=WOre[k][:, tsl, :].rearrange("p t c -> p (t c)"), start=True, stop=False),
                                 reads=[b_L, b_WO[k]], writes=[b_pK[j]], inc=False)
                            S.op("pe", lambda e: e.matmul(pK[j][:].rearrange("p t c -> p (t c)"), lhsT=Lim[:, s_, :],
                                                          rhs=WOim[k][:, tsl, :].rearrange("p t c -> p (t c)"), start=False, stop=True),
                                 reads=[b_L, b_WO[k]], writes=[b_pK[j]])
                            for tq in range(4):
                                t_ = th_ * 4 + tq
                                use = (t_ >= s_) if d == 0 else (t_ <= s_)
                                if not use:
                                    continue
                                S.op("dve", lambda e: e.tensor_tensor(out=Wi[:, s_, t_, :], in0=pK[j][:, tq, :], in1=Wi[:, s_, t_, :], op=ALU.add),
                                     reads=[b_pK[j], b_Wi], writes=[b_Wi])
                    zi = 0
                    for cb in range(NCk // CB):
                        csl = slice(cb * CB, (cb + 1) * CB)
                        for s_ in range(8):
                            S.op("pe", lambda e: e.matmul(pS[0][:], lhsT=LTre[:, s_, :], rhs=Us[:, s_, csl], start=(s_ == 0), stop=(s_ == 7)),
                                 reads=[b_LT, b_U], writes=[b_pS[0]], inc=(s_ == 7))
                        for s_ in range(8):
                            S.op("pe", lambda e: e.matmul(pS[1][:], lhsT=LTim[:, s_, :], rhs=Us[:, s_, csl], start=(s_ == 0), stop=(s_ == 7)),
                                 reads=[b_LT, b_U], writes=[b_pS[1]], inc=(s_ == 7))
                        p8r, p8i, p8n = SPr[:, 0, dp:dp + 1], SPi[:, 0, dp:dp + 1], SPn[:, 0, dp:dp + 1]
                        S.op("dve", lambda e: e.tensor_scalar(out=ztmp[:, csl], in0=pS[1][:], scalar1=p8n, scalar2=None, op0=ALU.mult),
                             reads=[b_pS[1], bP], writes=[b_ztmp])
                        S.op("dve", lambda e: e.scalar_tensor_tensor(out=ZR[0][:, csl], in0=pS[0][:], scalar=p8r, in1=ztmp[:, csl], op0=ALU.mult, op1=ALU.add),
                             reads=[b_pS[0], b_ztmp, bP], writes=[b_Z[0]])
                        S.op("dve", lambda e: e.tensor_scalar(out=ztmp[:, csl], in0=pS[1][:], scalar1=p8r, scalar2=None, op0=ALU.mult),
                             reads=[b_pS[1], bP, b_Z[0]], writes=[b_ztmp])
                        S.op("dve", lambda e: e.scalar_tensor_tensor(out=ZI[0][:, csl], in0=pS[0][:], scalar=p8i, in1=ztmp[:, csl], op0=ALU.mult, op1=ALU.add),
                             reads=[b_pS[0], b_ztmp, bP], writes=[b_Z[0]])
                    cur = 0
                    for j in range(J):
                        sh = 1 << j
                        nx = 1 - cur
                        qr, qi, qn = SPr[:, j, dp:dp + 1], SPi[:, j, dp:dp + 1], SPn[:, j, dp:dp + 1]
                        if d == 0:
                            dst, src, keep = slice(sh, NCk), slice(0, NCk - sh), slice(0, sh)
                        else:
                            dst, src, keep = slice(0, NCk - sh), slice(sh, NCk), slice(NCk - sh, NCk)
                        S.op("act", lambda e: e.copy(out=ZR[nx][:, keep], in_=ZR[cur][:, keep]), reads=[b_Z[cur]], writes=[b_Z[nx]])
                        S.op("act", lambda e: e.copy(out=ZI[nx][:, keep], in_=ZI[cur][:, keep]), reads=[b_Z[cur]], writes=[b_Z[nx]])
                        S.op("dve", lambda e: e.scalar_tensor_tensor(out=ztmp[:, dst], in0=ZR[cur][:, src], scalar=qr, in1=ZR[cur][:, dst], op0=ALU.mult, op1=ALU.add),
                             reads=[b_Z[cur], bP], writes=[b_ztmp])
                        S.op("dve", lambda e: e.scalar_tensor_tensor(out=ZR[nx][:, dst], in0=ZI[cur][:, src], scalar=qn, in1=ztmp[:, dst], op0=ALU.mult, op1=ALU.add),
                             reads=[b_Z[cur], b_ztmp, bP], writes=[b_Z[nx]])
                        S.op("dve", lambda e: e.scalar_tensor_tensor(out=ztmp[:, dst], in0=ZI[cur][:, src], scalar=qr, in1=ZI[cur][:, dst], op0=ALU.mult, op1=ALU.add),
                             reads=[b_Z[cur], bP, b_Z[nx]], writes=[b_ztmp])
                        S.op("dve", lambda e: e.scalar_tensor_tensor(out=ZI[nx][:, dst], in0=ZR[cur][:, src], scalar=qi, in1=ztmp[:, dst], op0=ALU.mult, op1=ALU.add),
                             reads=[b_Z[cur], b_ztmp, bP], writes=[b_Z[nx]])
                        cur = nx
                    if d == 0:
                        S.op("pool", lambda e: e.memset(HR[k][:, 0:1], 0.0), writes=[b_H[k]])
                        S.op("pool", lambda e: e.memset(HI[k][:, 0:1], 0.0), writes=[b_H[k]])
                        S.op("act", lambda e: e.copy(out=HR[k][:, 1:NCk], in_=ZR[cur][:, 0:NCk - 1]), reads=[b_Z[cur]], writes=[b_H[k]])
                        S.op("act", lambda e: e.copy(out=HI[k][:, 1:NCk], in_=ZI[cur][:, 0:NCk - 1]), reads=[b_Z[cur]], writes=[b_H[k]])
                    else:
                        S.op("pool", lambda e: e.memset(HR[k][:, NCk - 1:NCk], 0.0), writes=[b_H[k]])
                        S.op("pool", lambda e: e.memset(HI[k][:, NCk - 1:NCk], 0.0), writes=[b_H[k]])
                        S.op("act", lambda e: e.copy(out=HR[k][:, 0:NCk - 1], in_=ZR[cur][:, 1:NCk]), reads=[b_Z[cur]], writes=[b_H[k]])
                        S.op("act", lambda e: e.copy(out=HI[k][:, 0:NCk - 1], in_=ZI[cur][:, 1:NCk]), reads=[b_Z[cur]], writes=[b_H[k]])
            for s_ in range(8):
                S.op("dve", lambda e: e.tensor_tensor(out=Wi[:, s_, s_, :], in0=Wi[:, s_, s_, :], in1=Dd[:], op=ALU.add), reads=[b_Wi, b_Dd], writes=[b_Wi])
            for cb in range(NCk // CB):
                csl = slice(cb * CB, (cb + 1) * CB)
                for t_ in range(8):
                    j = npy % 2
                    npy += 1
                    for s_ in range(8):
                        S.op("pe", lambda e: e.matmul(pY[j][:], lhsT=Wi[:, s_, t_, :], rhs=Us[:, s_, csl], start=(s_ == 0), stop=False),
                             reads=[b_Wi, b_U], writes=[b_pY[j]], inc=False)
                    for k in range(4):
                        S.op("pe", lambda e: e.matmul(pY[j][:], lhsT=WOre[k][:, t_, :], rhs=HR[k][:, csl], start=False, stop=False),
                             reads=[b_WO[k], b_H[k]], writes=[b_pY[j]], inc=False)
                        S.op("pe", lambda e: e.matmul(pY[j][:], lhsT=WOim[k][:, t_, :], rhs=HI[k][:, csl], start=False, stop=(k == 3)),
                             reads=[b_WO[k], b_H[k]], writes=[b_pY[j]], inc=(k == 3))
                    S.op("act", lambda e: e.activation(out=g1[j][:], in_=pY[j][:], func=AF.Square), reads=[b_pY[j]], writes=[b_g[j]])
                    S.op("dve", lambda e: e.tensor_scalar(out=g1[j][:], in0=g1[j][:], scalar1=0.044715, scalar2=1.0, op0=ALU.mult, op1=ALU.add),
                         reads=[b_g[j]], writes=[b_g[j]])
                    S.op("dve", lambda e: e.tensor_tensor(out=g1[j][:], in0=pY[j][:], in1=g1[j][:], op=ALU.mult), reads=[b_pY[j], b_g[j]], writes=[b_g[j]])
                    S.op("act", lambda e: e.activation(out=g2[j][:], in_=g1[j][:], func=AF.Sigmoid, scale=1.5957691216057308), reads=[b_g[j]], writes=[b_g[j]])
                    S.op("dve", lambda e: e.tensor_tensor(out=Yo[:, csl, t_], in0=pY[j][:], in1=g2[j][:], op=ALU.mult), reads=[b_pY[j], b_g[j], b_Yo], writes=[b_Yo])
            S.dma("sp", scr.hs5T[tl * 128:(tl + 1) * 128, :], Uraw[:], reads=[b_Yo], writes=[scr.b_s5], owner=b_Yo)
        S.barrier()
```

```python
import math
from contextlib import ExitStack
import numpy as np
import concourse.bass as bass
import concourse.mybir as mybir
from concourse.bass_utils import run_bass_kernel_spmd

F32 = mybir.dt.float32
BF16 = mybir.dt.bfloat16
I32 = mybir.dt.int32
AF = mybir.ActivationFunctionType
ALU = mybir.AluOpType
AX = mybir.AxisListType


class Cfg:
    def __init__(self, L=8192, depth=4, n_exp=32):
        self.L = L
        self.depth = depth
        self.E = n_exp
        self.D = 1024
        self.alpha = (2 * 4) ** 0.25


class Buf:
    __slots__ = ("name", "writers", "readers", "dsem", "multi")

    def __init__(self, name, multi=False):
        self.name = name
        self.writers = {}
        self.readers = {}
        self.dsem = None
        self.multi = multi


class Sched:
    def __init__(self, nc):
        self.nc = nc
        self.eng = {"pe": nc.tensor, "dve": nc.vector, "act": nc.scalar,
                    "pool": nc.gpsimd, "sp": nc.sync}
        self.sems = []
        self.esem = {}
        self.cnt = {}
        self.latest = {}
        for k in self.eng:
            self.esem[k] = self._newsem("e_" + k)
            self.cnt[k] = 0
        self.known = {k: {} for k in self.eng}
        self.n_inst = 0
        self.n_wait = 0
        self.dsem_pool = {}
        self.free_dsems = []
        self.epoch_owners = []

    def _newsem(self, name):
        h = self.nc.alloc_semaphore(name=name)
        self.sems.append(h)
        return len(self.sems) - 1

    def _wait(self, e, deps):
        kn = self.known[e]
        for s, c in deps.items():
            if s == self.esem[e] and c > self.cnt[e]:
                continue
            if kn.get(s, 0) < c:
                self.eng[e].wait_ge(self.sems[s], c)
                kn[s] = c
                self.n_wait += 1

    @staticmethod
    def _merge(d, src):
        for s, c in src.items():
            if d.get(s, 0) < c:
                d[s] = c

    def _deps(self, reads, writes):
        deps = {}
        for b in reads:
            self._merge(deps, b.writers)
        for b in writes:
            if b.multi:
                continue
            self._merge(deps, b.writers)
            self._merge(deps, b.readers)
        return deps

    def _track(self, s, c, reads, writes):
        for b in writes:
            if b.multi:
                if b.writers.get(s, 0) < c:
                    b.writers[s] = c
                continue
            b.writers = {s: c}
            b.readers = {}
        for b in reads:
            if b.readers.get(s, 0) < c:
                b.readers[s] = c

    def op(self, e, fn, reads=(), writes=(), inc=True):
        self._wait(e, self._deps(reads, writes))
        ins = fn(self.eng[e])
        self.n_inst += 1
        s = self.esem[e]
        if inc:
            self.cnt[e] += 1
            ins.then_inc(self.sems[s], 1)
            c = self.cnt[e]
            self.latest[s] = c
        else:
            c = self.cnt[e] + 1
        self._track(s, c, reads, writes)
        return ins

    def dma(self, q, out, in_, reads=(), writes=(), owner=None, **kw):
        self._wait(q, self._deps(reads, writes))
        if owner.dsem is None:
            if owner.name not in self.dsem_pool:
                if self.free_dsems:
                    self.dsem_pool[owner.name] = self.free_dsems.pop()
                else:
                    self.dsem_pool[owner.name] = [self._newsem("d%d" % len(self.sems)), 0]
            owner.dsem = self.dsem_pool[owner.name]
            self.epoch_owners.append(owner)
        ins = self.eng[q].dma_start(out=out, in_=in_, **kw)
        owner.dsem[1] += 16
        s, c = owner.dsem[0], owner.dsem[1]
        ins.then_inc(self.sems[s], 16)
        self.latest[s] = c
        self.n_inst += 1
        self._track(s, c, reads, writes)
        return ins

    def barrier(self):
        for e in self.eng:
            self._wait(e, dict(self.latest))
        keep = {k: v for k, v in self.dsem_pool.items() if k in ("inj", "dbg")}
        self.free_dsems.extend(v for k, v in self.dsem_pool.items() if k not in keep)
        self.dsem_pool = keep
        for b in self.epoch_owners:
            b.dsem = None
        self.epoch_owners = []


def dram_copy(S, dst, src, b):
    n = dst.shape[0]
    step = 128 if n >= 128 else n
    for r in range(0, n, step):
        S.dma("sp", dst[r:r + step, :], src[r:r + step, :], writes=[b], owner=b)


_UNIQ = [0]


def uniq(name):
    _UNIQ[0] += 1
    return "%s_%d" % (name, _UNIQ[0])


def bufs(prefix, n):
    return [Buf(f"{prefix}{i}") for i in range(n)]


D = 1024
SSD_INNER = 2048
SSD_HEADS = 32
SSD_GROUPS = 8
SSD_STATE = 128
CONV_CH = 4096
ATT_W = 1152
S5_W = 1152
S5_G = 72
N_IN = 13888
C_GATE, C_Z, C_XBC, C_DT, C_Q, C_K, C_V, C_U = 0, 3072, 5120, 9216, 9280, 10432, 11584, 12736
LN_EPS = 1e-5


class Ctx:
    pass


def make_consts(S, nc, cx):
    cx.identf = nc.alloc_sbuf_tensor("identf", [128, 128], F32)
    cx.identb = nc.alloc_sbuf_tensor("identb", [128, 128], BF16)
    cx.b_const = Buf("const")
    b = cx.b_const
    S.op("pool", lambda e: e.memset(cx.identf[:], 0.0), writes=[b])
    S.op("pool", lambda e: e.affine_select(out=cx.identf[:], in_=cx.identf[:], pattern=[[-1, 128]], base=0,
                                           channel_multiplier=1, compare_op=ALU.not_equal, fill=1.0),
         reads=[b], writes=[b])
    S.op("dve", lambda e: e.tensor_copy(out=cx.identb[:], in_=cx.identf[:]), reads=[b], writes=[b])


def load_xT(S, nc, cx, x_ap, t0, ntok, xT, b_xT, xs, b_xs, xb, b_xb, pt, b_pt, ctr):
    for t in range(ntok // 128):
        i = ctr[0] % 2
        ctr[0] += 1
        S.dma("sp", xs[i][:], x_ap[t0 + t * 128:t0 + (t + 1) * 128, :], writes=[b_xs[i]], owner=b_xs[i])
        S.op("dve", lambda e: e.tensor_copy(out=xb[i][:], in_=xs[i][:]), reads=[b_xs[i]], writes=[b_xb[i]])
        for k4 in range(2):
            j = ctr[1] % 2
            ctr[1] += 1
            for kk in range(4):
                k = k4 * 4 + kk
                S.op("pe", lambda e: e.transpose(out=pt[j][:, kk, :], in_=xb[i][:, k * 128:(k + 1) * 128],
                                                 identity=cx.identb[:]),
                     reads=[b_xb[i], cx.b_const], writes=[b_pt[j]], inc=(kk == 3))
            S.op("act", lambda e: e.copy(out=xT[:, k4 * 4:(k4 + 1) * 4, t * 128:(t + 1) * 128], in_=pt[j][:]),
                 reads=[b_pt[j]], writes=[b_xT])


def phase_inproj(S, nc, cx, cfg, w_in_l, x_ap, scr):
    L = cfg.L
    TB = min(2048, L)
    secs = [
        (C_GATE, 3072, "tok", scr.gates, F32, 512),
        (C_Z, 2048, "tok", scr.z, F32, 512),
        (C_XBC, 4096, "feat", scr.xbcT, F32, 512),
        (C_DT, 64, "tok", scr.dt, F32, 64),
        (C_Q, 1152, "feat", scr.qT, BF16, 384),
        (C_K, 1152, "feat", scr.kT, BF16, 384),
        (C_V, 1152, "tok", scr.v, BF16, 384),
        (C_U, 1152, "featpad", scr.uT, BF16, 128),
    ]
    with ExitStack() as es:
        sb = lambda n, s, d: es.enter_context(nc.sbuf_tensor(uniq(n), s, d))
        ps = lambda n, s, d: es.enter_context(nc.psum_tensor(uniq(n), s, d))
        xT = sb("ip_xT", [128, 8, TB], BF16)
        b_xT = Buf("ip_xT")
        xs = [sb(f"ip_xs{i}", [128, D], F32) for i in range(2)]
        b_xs = bufs("ip_xs", 2)
        xb = [sb(f"ip_xb{i}", [128, D], BF16) for i in range(2)]
        b_xb = bufs("ip_xb", 2)
        pt = [ps(f"ip_pt{i}", [128, 4, 128], BF16) for i in range(2)]
        b_pt = bufs("ip_pt", 2)
        wst = [sb(f"ip_wst{i}", [128, 8, 512], F32) for i in range(2)]
        b_wst = bufs("ip_wst", 2)
        wb = [sb(f"ip_wb{i}", [128, 8, 512], BF16) for i in range(2)]
        b_wb = bufs("ip_wb", 2)
        po = [ps(f"ip_po{i}", [128, 512], F32) for i in range(4)]
        b_po = bufs("ip_po", 4)
        ot = [sb(f"ip_ot{i}", [128, 512], F32) for i in range(3)]
        otb = [sb(f"ip_otb{i}", [128, 512], BF16) for i in range(3)]
        b_ot = bufs("ip_ot", 3)
        ctr = [0, 0]
        nw = 0
        npo = 0
        no = 0
        for t0 in range(0, L, TB):
            load_xT(S, nc, cx, x_ap, t0, TB, xT, b_xT, xs, b_xs, xb, b_xb, pt, b_pt, ctr)
            for (c0, ncols, kind, dest, dt, cb) in secs:
                for cc in range(0, ncols, cb):
                    wi = nw % 2
                    nw += 1
                    wsrc = w_in_l[:, c0 + cc:c0 + cc + cb].rearrange("(k p) c -> p k c", p=128)
                    S.dma("sp", wst[wi][:, :, 0:cb], wsrc, writes=[b_wst[wi]], owner=b_wst[wi])
                    if kind == "featpad":
                        ng = cb // 16
                        S.op("pool", lambda e: e.memset(wb[wi][:], 0.0), writes=[b_wb[wi]])
                        S.op("pool", lambda e: e.tensor_copy(
                            out=wb[wi][:, :, 0:ng * 32].rearrange("p k (g c) -> p k g c", c=32)[:, :, :, 0:16],
                            in_=wst[wi][:, :, 0:cb].rearrange("p k (g c) -> p k g c", c=16)),
                            reads=[b_wst[wi]], writes=[b_wb[wi]])
                        wcols = ng * 32
                    else:
                        S.op("pool", lambda e: e.tensor_copy(out=wb[wi][:, :, 0:cb], in_=wst[wi][:, :, 0:cb]),
                             reads=[b_wst[wi]], writes=[b_wb[wi]])
                        wcols = cb
                    if kind == "tok":
                        for t in range(TB // 128):
                            pj = npo % 4
                            npo += 1
                            for k in range(8):
                                S.op("pe", lambda e: e.matmul(po[pj][:, 0:cb], lhsT=xT[:, k, t * 128:(t + 1) * 128],
                                                              rhs=wb[wi][:, k, 0:cb], start=(k == 0), stop=(k == 7)),
                                     reads=[b_xT, b_wb[wi]], writes=[b_po[pj]], inc=(k == 7))
                            oi = no % 3
                            no += 1
                            o_t = ot[oi] if dt == F32 else otb[oi]
                            ev = "act" if no % 2 else "dve"
                            if ev == "act":
                                S.op("act", lambda e: e.copy(out=o_t[:, 0:cb], in_=po[pj][:, 0:cb]),
                                     reads=[b_po[pj]], writes=[b_ot[oi]])
                            else:
                                S.op("dve", lambda e: e.tensor_copy(out=o_t[:, 0:cb], in_=po[pj][:, 0:cb]),
                                     reads=[b_po[pj]], writes=[b_ot[oi]])
                            S.dma("act", dest[t0 + t * 128:t0 + (t + 1) * 128, cc:cc + cb], o_t[:, 0:cb],
                                  reads=[b_ot[oi]], writes=[scr.b_proj], owner=b_ot[oi])
                    else:
                        if kind == "featpad":
                            r0 = (cc // 16) * 32
                        else:
                            r0 = cc
                        for ts in range(TB // 512):
                            for ch in range(wcols // 128):
                                pj = npo % 4
                                npo += 1
                                for k in range(8):
                                    S.op("pe", lambda e: e.matmul(po[pj][:], lhsT=wb[wi][:, k, ch * 128:(ch + 1) * 128],
                                                                  rhs=xT[:, k, ts * 512:(ts + 1) * 512],
                                                                  start=(k == 0), stop=(k == 7)),
                                         reads=[b_xT, b_wb[wi]], writes=[b_po[pj]], inc=(k == 7))
                                oi = no % 3
                                no += 1
                                o_t = ot[oi] if dt == F32 else otb[oi]
                                if no % 2:
                                    S.op("act", lambda e: e.copy(out=o_t[:], in_=po[pj][:]),
                                         reads=[b_po[pj]], writes=[b_ot[oi]])
                                else:
                                    S.op("dve", lambda e: e.tensor_copy(out=o_t[:], in_=po[pj][:]),
                                         reads=[b_po[pj]], writes=[b_ot[oi]])
                                S.dma("act", dest[r0 + ch * 128:r0 + (ch + 1) * 128, t0 + ts * 512:t0 + (ts + 1) * 512],
                                      o_t[:], reads=[b_ot[oi]], writes=[scr.b_proj], owner=b_ot[oi])
        S.barrier()


def alloc_scratch(nc, cfg):
    L = cfg.L
    scr = Ctx()
    dr = lambda n, s, d: nc.dram_tensor(n, s, d, kind="Internal").ap()
    scr.gates = dr("s_gates", [L, 3072], F32)
    scr.z = dr("s_z", [L, 2048], F32)
    scr.xbcT = dr("s_xbcT", [4096, L], F32)
    scr.dt = dr("s_dt", [L, 64], F32)
    scr.qT = dr("s_qT", [1152, L], BF16)
    scr.kT = dr("s_kT", [1152, L], BF16)
    scr.v = dr("s_v", [L, 1152], BF16)
    scr.uT = dr("s_uT", [2304, L], BF16)
    scr.b_proj = Buf("proj", multi=True)
    scr.BT = dr("s_BT", [1024, L], BF16)
    scr.CT = dr("s_CT", [1024, L], BF16)
    scr.xsB = dr("s_xsB", [L, 3072], BF16)
    scr.b_conv = Buf("conv", multi=True)
    scr.Sb = dr("s_Sb", [L // 128, 8, 128, 256], F32)
    scr.ypart = dr("s_ypart", [L, 2048], F32)
    scr.b_ssd = Buf("ssd", multi=True)
    scr.yssd = dr("s_yssd", [L, 2048], BF16)
    scr.b_mix = Buf("mix", multi=True)
    scr.hs5T = dr("s_hs5T", [2304, L], BF16)
    scr.b_s5 = Buf("s5", multi=True)
    scr.ys5T = dr("s_ys5T", [1152, L], BF16)
    scr.b_glu = Buf("glu", multi=True)
    scr.x1 = dr("s_x1", [L, 1024], F32)
    scr.b_x1 = Buf("x1", multi=True)
    scr.xcur = dr("s_xcur", [L, 1024], F32)
    scr.b_xcur = Buf("xcur", multi=True)
    scr.wgu16 = dr("s_wgu16", [cfg.E, 1024, 2048], BF16)
    scr.wdn16 = dr("s_wdn16", [cfg.E, 1024, 1024], BF16)
    scr.b_w16 = Buf("w16", multi=True)
    scr.x1T = dr("s_x1T", [1024, L], BF16)
    scr.rgate = dr("s_rgate", [L, cfg.E], F32)
    scr.b_x1T = Buf("x1T", multi=True)
    scr.attO = dr("s_attO", [3, L, 390], F32)
    scr.b_att = Buf("att", multi=True)
    scr.yatt = dr("s_yatt", [L, 384], BF16)
    return scr


def phase_conv(S, nc, cx, cfg, conv_w_l, conv_b_l, scr):
    L = cfg.L
    TS = min(2048, L)
    with ExitStack() as es:
        sb = lambda n, s, d: es.enter_context(nc.sbuf_tensor(uniq(n), s, d))
        ps = lambda n, s, d: es.enter_context(nc.psum_tensor(uniq(n), s, d))
        cw = sb("cv_w", [128, 32, 5], F32)
        cb = sb("cv_b", [128, 32], F32)
        b_cw = Buf("cv_w")
        with nc.allow_non_contiguous_dma(reason="tiny param load"):
            for k in range(5):
                S.dma("sp", cw[:, :, k], conv_w_l[k].rearrange("(t p) -> p t", p=128), writes=[b_cw], owner=b_cw)
            S.dma("sp", cb[:], conv_b_l.rearrange("(t p) -> p t", p=128), writes=[b_cw], owner=b_cw)
        xin = [sb(f"cv_x{i}", [128, TS + 4], F32) for i in range(2)]
        b_xin = bufs("cv_x", 2)
        acc = [sb(f"cv_a{i}", [128, TS], F32) for i in range(2)]
        b_acc = bufs("cv_a", 2)
        yb = [sb(f"cv_y{i}", [128, TS], BF16) for i in range(8)]
        b_yb = bufs("cv_y", 8)
        pt = [ps(f"cv_pt{i}", [128, 4, 128], BF16) for i in range(2)]
        b_pt = bufs("cv_pt", 2)
        ot = [sb(f"cv_o{i}", [128, 512], BF16) for i in range(3)]
        b_ot = bufs("cv_o", 3)
        nx = 0
        ny = 0
        npt = 0
        no = 0
        for cg in range(8):
            for t0 in range(0, L, TS):
                ys = []
                for j in range(4):
                    ct = cg * 4 + j
                    i = nx % 2
                    nx += 1
                    eng = "dve"
                    lo = max(0, t0 - 2)
                    hi = min(L, t0 + TS + 2)
                    d0 = lo - (t0 - 2)
                    if d0 > 0:
                        S.op("pool", lambda e: e.memset(xin[i][:, 0:d0], 0.0), writes=[b_xin[i]])
                    if d0 + (hi - lo) < TS + 4:
                        S.op("pool", lambda e: e.memset(xin[i][:, d0 + hi - lo:TS + 4], 0.0), writes=[b_xin[i]])
                    S.dma("sp", xin[i][:, d0:d0 + hi - lo], scr.xbcT[ct * 128:(ct + 1) * 128, lo:hi],
                          reads=[scr.b_proj], writes=[b_xin[i]], owner=b_xin[i])
                    a = acc[i]
                    S.op("act", lambda e: e.activation(out=a[:], in_=xin[i][:, 0:TS], func=AF.Copy, scale=cw[:, ct, 0:1]),
                         reads=[b_xin[i], b_cw], writes=[b_acc[i]])
                    for k in range(1, 5):
                        S.op(eng, lambda e: e.scalar_tensor_tensor(out=a[:], in0=xin[i][:, k:k + TS], scalar=cw[:, ct, k:k + 1],
                                                                   in1=a[:], op0=ALU.mult, op1=ALU.add),
                             reads=[b_xin[i], b_cw, b_acc[i]], writes=[b_acc[i]])
                    yi = ny % 8
                    ny += 1
                    S.op("act", lambda e: e.activation(out=yb[yi][:], in_=a[:], func=AF.Silu, bias=cb[:, ct:ct + 1], scale=1.0),
                         reads=[b_acc[i], b_cw], writes=[b_yb[yi]])
                    ys.append(yi)
                    if ct >= 16:
                        dst = scr.BT if ct < 24 else scr.CT
                        r0 = (ct - 16) * 128 if ct < 24 else (ct - 24) * 128
                        S.dma("sp", dst[r0:r0 + 128, t0:t0 + TS], yb[yi][:], reads=[b_yb[yi]], writes=[scr.b_conv],
                              owner=b_yb[yi])
                if cg < 6:
                    for tt in range(TS // 128):
                        pj = npt % 2
                        npt += 1
                        for j in range(4):
                            S.op("pe", lambda e: e.transpose(out=pt[pj][:, j, :], in_=yb[ys[j]][:, tt * 128:(tt + 1) * 128],
                                                             identity=cx.identb[:]),
                                 reads=[b_yb[ys[j]], cx.b_const], writes=[b_pt[pj]], inc=(j == 3))
                        oi = no % 3
                        no += 1
                        if no % 2:
                            S.op("act", lambda e: e.copy(out=ot[oi][:], in_=pt[pj][:].rearrange("p a b -> p (a b)")),
                                 reads=[b_pt[pj]], writes=[b_ot[oi]])
                        else:
                            S.op("dve", lambda e: e.tensor_copy(out=ot[oi][:], in_=pt[pj][:].rearrange("p a b -> p (a b)")),
                                 reads=[b_pt[pj]], writes=[b_ot[oi]])
                        S.dma("sp", scr.xsB[t0 + tt * 128:t0 + (tt + 1) * 128, cg * 512:(cg + 1) * 512], ot[oi][:],
                              reads=[b_ot[oi]], writes=[scr.b_conv], owner=b_ot[oi])
        S.barrier()


def make_masks(S, nc, cx):
    cx.maskLE = nc.alloc_sbuf_tensor("maskLE", [128, 128], F32)
    cx.maskGE = nc.alloc_sbuf_tensor("maskGE", [128, 128], F32)
    cx.U0 = nc.alloc_sbuf_tensor("U0", [128, 128], F32)
    cx.U1 = nc.alloc_sbuf_tensor("U1", [128, 128], F32)
    cx.ones = nc.alloc_sbuf_tensor("ones", [128, 128], F32)
    b = cx.b_const
    for t, pat, cm, op in ((cx.maskLE, 1, -1, ALU.is_ge), (cx.maskGE, -1, 1, ALU.is_ge),
                           (cx.U0, -1, 1, ALU.is_gt), (cx.U1, 1, -1, ALU.is_gt)):
        S.op("pool", lambda e: e.memset(t[:], 1.0), writes=[b])
        S.op("pool", lambda e: e.affine_select(out=t[:], in_=t[:], pattern=[[pat, 128]], base=0, channel_multiplier=cm,
                                               compare_op=op, fill=0.0), reads=[b], writes=[b])
    S.op("pool", lambda e: e.memset(cx.ones[:], 1.0), writes=[b])


def bc_r(ap, n):
    return ap.unsqueeze(1).to_broadcast([ap.shape[0], n, ap.shape[1]])


def bc_l(ap, n):
    return ap.unsqueeze(2).to_broadcast([ap.shape[0], ap.shape[1], n])


def phase_ssd(S, nc, cx, cfg, prm, l, scr):
    L = cfg.L
    NC = L // 128
    with ExitStack() as es:
        sb = lambda n, s, d: es.enter_context(nc.sbuf_tensor(uniq(n), s, d))
        ps = lambda n, s, d: es.enter_context(nc.psum_tensor(uniq(n), s, d))
        b_prm = Buf("sd_prm")
        biasb = sb("sd_bias", [128, 64], F32)
        negA = sb("sd_negA", [128, 64], F32)
        dsk = sb("sd_dsk", [128, 32], F32)
        nw = sb("sd_nw", [128, 2048], F32)
        S.dma("sp", biasb[:], prm["ssd_dt_bias"][l].rearrange("a b -> (a b)").partition_broadcast(128), writes=[b_prm], owner=b_prm)
        S.dma("sp", negA[:], prm["ssd_a_log"][l].rearrange("a b -> (a b)").partition_broadcast(128), writes=[b_prm], owner=b_prm)
        S.dma("sp", dsk[:], prm["ssd_d"][l].partition_broadcast(128), writes=[b_prm], owner=b_prm)
        S.dma("sp", nw[:], prm["ssd_norm_w"][l].partition_broadcast(128), writes=[b_prm], owner=b_prm)
        S.op("act", lambda e: e.activation(out=negA[:], in_=negA[:], func=AF.Exp), reads=[b_prm], writes=[b_prm])
        S.op("dve", lambda e: e.tensor_scalar(out=negA[:], in0=negA[:], scalar1=-1.0, scalar2=None, op0=ALU.mult),
             reads=[b_prm], writes=[b_prm])
        Hf = sb("sd_H", [128, 8, 256], F32)
        Hb = sb("sd_Hb", [128, 8, 256], BF16)
        b_H = bufs("sd_Hg", 8)
        tmpH = sb("sd_tH", [128, 256], F32)
        b_tmpH = Buf("sd_tH")
        dtt = [sb(f"sd_dt{i}", [128, 64], F32) for i in range(2)]
        b_dtt = bufs("sd_dt", 2)
        xB = [sb(f"sd_xB{i}", [128, 3072], BF16) for i in range(2)]
        b_xB = bufs("sd_xB", 2)
        BTc = [sb(f"sd_BT{i}", [128, 8, 128], BF16) for i in range(2)]
        b_BTc = bufs("sd_BT", 2)
        CTc = [sb(f"sd_CT{i}", [128, 8, 128], BF16) for i in range(2)]
        b_CTc = bufs("sd_CT", 2)
        names = ["ex", "delta", "a", "I", "Q", "G1", "G2", "G1d", "G2d", "dec", "tq"]
        sm = {n: sb("sd_" + n, [128, 64], F32) for n in names}
        b_sm = {n: Buf("sd_" + n) for n in names}
        ps_IT = ps("sd_psIT", [128, 2, 64], F32)
        ps_I = ps_IT[:, 0, :]
        ps_T = ps_IT[:, 1, :]
        b_psI = Buf("sd_psI")
        b_psT = Buf("sd_psT")
        ps_CBs = [ps(f"sd_psCB{i}", [128, 128], F32) for i in range(2)]
        b_psCBs = bufs("sd_psCB", 2)
        ps_seg = [ps(f"sd_psS{i}", [128, 512], F32) for i in range(2)]
        b_psseg = bufs("sd_psS", 2)
        ps_yd = ps("sd_psyd", [128, 4, 64], F32)
        b_psyd = Buf("sd_psyd")
        ps_st = ps("sd_psst", [128, 2, 256], F32)
        b_psst = bufs("sd_psst", 2)
        ps_yo = ps("sd_psyo", [128, 256], F32)
        b_psyo = Buf("sd_psyo")
        CBm4 = [sb(f"sd_CBm{i}", [128, 128], F32) for i in range(4)]
        b_CBm4 = bufs("sd_CBm", 4)
        Rt4 = [sb(f"sd_R{i}", [128, 4, 128], F32) for i in range(4)]
        b_Rt4 = bufs("sd_R", 4)
        Dx4 = [sb(f"sd_Dx{i}", [128, 4, 128], F32) for i in range(4)]
        b_Dx4 = bufs("sd_Dx", 4)
        MT4 = [sb(f"sd_MT{i}", [128, 4, 128], BF16) for i in range(4)]
        b_MT4 = bufs("sd_MT", 4)
        xw4 = [sb(f"sd_xw{i}", [128, 4, 64], BF16) for i in range(4)]
        b_xw4 = bufs("sd_xw", 4)
        yp = [sb(f"sd_yp{i}", [128, 2048], F32) for i in range(2)]
        b_yp = bufs("sd_yp", 2)
        ytmp = sb("sd_ytmp", [128, 256], F32)
        b_ytmp = Buf("sd_ytmp")
        sbst = [sb(f"sd_sb{i}", [128, 256], F32) for i in range(2)]
        b_sbst = bufs("sd_sb", 2)
        zt = [sb(f"sd_z{i}", [128, 2048], F32) for i in range(2)]
        b_zt = bufs("sd_z", 2)
        yo16 = [sb(f"sd_y16{i}", [128, 2048], BF16) for i in range(2)]
        b_yo16 = bufs("sd_y16", 2)
        ssq = sb("sd_ssq", [128, 2], F32)
        b_ssq = Buf("sd_ssq")

        for g in range(8):
            S.op("pool", lambda e: e.memset(Hf[:, g, :], 0.0), writes=[b_H[g]])
            S.op("pool", lambda e: e.memset(Hb[:, g, :], 0.0), writes=[b_H[g]])

        def chunk_scalars(c, i):
            S.dma("sp", dtt[i][:], scr.dt[c * 128:(c + 1) * 128, :], reads=[scr.b_proj], writes=[b_dtt[i]], owner=b_dtt[i])
            S.op("dve", lambda e: e.tensor_tensor(out=sm["ex"][:], in0=dtt[i][:], in1=biasb[:], op=ALU.add),
                 reads=[b_dtt[i], b_prm], writes=[b_sm["ex"]])
            S.op("act", lambda e: e.activation(out=sm["ex"][:], in_=sm["ex"][:], func=AF.Exp), reads=[b_sm["ex"]], writes=[b_sm["ex"]])
            S.op("act", lambda e: e.activation(out=sm["delta"][:], in_=sm["ex"][:], func=AF.Ln, bias=1.0, scale=1.0),
                 reads=[b_sm["ex"]], writes=[b_sm["delta"]])
            S.op("dve", lambda e: e.tensor_tensor(out=sm["a"][:], in0=sm["delta"][:], in1=negA[:], op=ALU.mult),
                 reads=[b_sm["delta"], b_prm], writes=[b_sm["a"]])
            S.op("pe", lambda e: e.matmul(ps_I, lhsT=cx.maskLE[:], rhs=sm["a"][:], start=True, stop=True),
                 reads=[b_sm["a"], cx.b_const], writes=[b_psI])
            S.op("pe", lambda e: e.matmul(ps_T, lhsT=cx.ones[:], rhs=sm["a"][:], start=True, stop=True),
                 reads=[b_sm["a"], cx.b_const], writes=[b_psT])
            S.op("dve", lambda e: e.tensor_copy(out=sm["Q"][:, 0:32], in_=ps_I[:, 0:32]), reads=[b_psI], writes=[b_sm["Q"]])
            S.op("dve", lambda e: e.tensor_tensor(out=sm["Q"][:, 32:64], in0=ps_I[:, 32:64], in1=sm["a"][:, 32:64], op=ALU.subtract),
                 reads=[b_psI, b_sm["a"], b_sm["Q"]], writes=[b_sm["Q"]])
            S.op("dve", lambda e: e.tensor_tensor(out=sm["tq"][:], in0=ps_T, in1=sm["Q"][:], op=ALU.subtract),
                 reads=[b_psT, b_sm["Q"]], writes=[b_sm["tq"]])
            S.op("act", lambda e: e.activation(out=sm["G1"][:], in_=sm["Q"][:], func=AF.Exp), reads=[b_sm["Q"]], writes=[b_sm["G1"]])
            S.op("act", lambda e: e.activation(out=sm["G2"][:], in_=sm["tq"][:], func=AF.Exp), reads=[b_sm["tq"]], writes=[b_sm["G2"]])
            S.op("act", lambda e: e.activation(out=sm["dec"][:], in_=ps_T, func=AF.Exp), reads=[b_psT], writes=[b_sm["dec"]])
            S.op("dve", lambda e: e.tensor_tensor(out=sm["G1d"][:], in0=sm["G1"][:], in1=sm["delta"][:], op=ALU.mult),
                 reads=[b_sm["G1"], b_sm["delta"]], writes=[b_sm["G1d"]])
            S.op("dve", lambda e: e.tensor_tensor(out=sm["G2d"][:], in0=sm["G2"][:], in1=sm["delta"][:], op=ALU.mult),
                 reads=[b_sm["G2"], b_sm["delta"]], writes=[b_sm["G2d"]])

        nseg = 0
        nst = 0
        for c in range(NC):
            i = c % 2
            chunk_scalars(c, i)
            S.dma("sp", xB[i][:], scr.xsB[c * 128:(c + 1) * 128, :], reads=[scr.b_conv], writes=[b_xB[i]], owner=b_xB[i])
            S.dma("sp", BTc[i][:], scr.BT[:, c * 128:(c + 1) * 128].rearrange("(g n) l -> n g l", n=128),
                  reads=[scr.b_conv], writes=[b_BTc[i]], owner=b_BTc[i])
            S.dma("sp", CTc[i][:], scr.CT[:, c * 128:(c + 1) * 128].rearrange("(g n) l -> n g l", n=128),
                  reads=[scr.b_conv], writes=[b_CTc[i]], owner=b_CTc[i])
            xv = xB[i][:, 0:2048].rearrange("p (g r q) -> p g r q", g=8, r=4)
            Btok = xB[i][:, 2048:3072].rearrange("p (g n) -> p g n", g=8)
            for g in range(8):
                gp = (g % 2) * 2
                CBm, b_CBm = CBm4[gp:gp + 2], b_CBm4[gp:gp + 2]
                Rt, b_Rt = Rt4[gp:gp + 2], b_Rt4[gp:gp + 2]
                Dx, b_Dx = Dx4[gp:gp + 2], b_Dx4[gp:gp + 2]
                MT, b_MT = MT4[gp:gp + 2], b_MT4[gp:gp + 2]
                xw, b_xw = xw4[gp:gp + 2], b_xw4[gp:gp + 2]
                ps_CB, b_psCB = ps_CBs[g % 2], b_psCBs[g % 2]
                S.op("pe", lambda e: e.matmul(ps_CB[:], lhsT=BTc[i][:, g, :], rhs=CTc[i][:, g, :], start=True, stop=True),
                     reads=[b_BTc[i], b_CTc[i]], writes=[b_psCB])
                S.op("dve", lambda e: e.tensor_tensor(out=CBm[0][:], in0=ps_CB[:], in1=cx.maskLE[:], op=ALU.mult),
                     reads=[b_psCB, cx.b_const], writes=[b_CBm[0]])
                S.op("dve", lambda e: e.tensor_tensor(out=CBm[1][:], in0=ps_CB[:], in1=cx.maskGE[:], op=ALU.mult),
                     reads=[b_psCB, cx.b_const], writes=[b_CBm[1]])
                for d in range(2):
                    cols = slice(d * 32 + g * 4, d * 32 + g * 4 + 4)
                    msk = cx.maskLE if d == 0 else cx.maskGE
                    U = cx.U0 if d == 0 else cx.U1
                    wd = sm["G2d"] if d == 0 else sm["G1d"]
                    b_wd = b_sm["G2d"] if d == 0 else b_sm["G1d"]
                    sj = nseg % 2
                    nseg += 1
                    S.op("pool", lambda e: e.tensor_tensor(out=Rt[d][:], in0=bc_r(msk[:], 4), in1=bc_l(sm["a"][:, cols], 128), op=ALU.mult),
                         reads=[cx.b_const, b_sm["a"]], writes=[b_Rt[d]])
                    S.op("pe", lambda e: e.matmul(ps_seg[sj][:], lhsT=U[:], rhs=Rt[d][:].rearrange("p r l -> p (r l)"), start=True, stop=True),
                         reads=[cx.b_const, b_Rt[d]], writes=[b_psseg[sj]])
                    S.op("act", lambda e: e.activation(out=Dx[d][:].rearrange("p r l -> p (r l)"), in_=ps_seg[sj][:], func=AF.Exp),
                         reads=[b_psseg[sj]], writes=[b_Dx[d]])
                    S.op("dve", lambda e: e.tensor_tensor(out=Dx[d][:], in0=Dx[d][:], in1=bc_l(sm["delta"][:, cols], 128), op=ALU.mult),
                         reads=[b_Dx[d], b_sm["delta"]], writes=[b_Dx[d]])
                    S.op("pool", lambda e: e.tensor_tensor(out=MT[d][:], in0=Dx[d][:], in1=bc_r(CBm[d][:], 4), op=ALU.mult),
                         reads=[b_Dx[d], b_CBm[d]], writes=[b_MT[d]])
                    S.op("dve", lambda e: e.tensor_tensor(out=xw[d][:], in0=xv[:, g, :, :], in1=bc_l(wd[:, cols], 64), op=ALU.mult),
                         reads=[b_xB[i], b_wd], writes=[b_xw[d]])
                    S.op("pe", lambda e: e.matmul(ps_st[:, d, :], lhsT=Btok[:, g, :], rhs=xw[d][:].rearrange("p r q -> p (r q)"),
                                                  start=True, stop=True),
                         reads=[b_xB[i], b_xw[d]], writes=[b_psst[d]])
                for r in range(4):
                    for d in range(2):
                        S.op("pe", lambda e: e.matmul(ps_yd[:, r, :], lhsT=MT[d][:, r, :], rhs=xv[:, g, r, :], start=(d == 0), stop=(d == 1)),
                             reads=[b_MT[d], b_xB[i]], writes=[b_psyd], inc=(r == 3 and d == 1))
                colsf = slice(g * 4, g * 4 + 4)
                S.op("pe", lambda e: e.matmul(ps_yo[:], lhsT=CTc[i][:, g, :], rhs=Hb[:, g, :], start=True, stop=True),
                     reads=[b_CTc[i], b_H[g]], writes=[b_psyo])
                S.op("dve", lambda e: e.tensor_tensor(out=ytmp[:].rearrange("p (r q) -> p r q", r=4), in0=ps_yo[:].rearrange("p (r q) -> p r q", r=4),
                                                      in1=bc_l(sm["G1"][:, colsf], 64), op=ALU.mult),
                     reads=[b_psyo, b_sm["G1"]], writes=[b_ytmp])
                S.op("dve", lambda e: e.tensor_tensor(out=yp[i][:, g * 256:(g + 1) * 256], in0=ps_yd[:].rearrange("p r q -> p (r q)"),
                                                      in1=ytmp[:], op=ALU.add),
                     reads=[b_psyd, b_ytmp], writes=[b_yp[i]])
                S.op("pool", lambda e: e.tensor_tensor(out=tmpH[:].rearrange("p (r q) -> p r q", r=4), in0=Hf[:, g, :].rearrange("p (r q) -> p r q", r=4),
                                                       in1=bc_l(sm["dec"][:, colsf], 64), op=ALU.mult),
                     reads=[b_H[g], b_sm["dec"]], writes=[b_tmpH])
                S.op("dve", lambda e: e.tensor_tensor(out=Hf[:, g, :], in0=tmpH[:], in1=ps_st[:, 0, :], op=ALU.add),
                     reads=[b_tmpH, b_psst[0]], writes=[b_H[g]])
                S.op("act", lambda e: e.copy(out=Hb[:, g, :], in_=Hf[:, g, :]), reads=[b_H[g]], writes=[b_H[g]])
                si = nst % 2
                nst += 1
                S.op("act", lambda e: e.copy(out=sbst[si][:], in_=ps_st[:, 1, :]), reads=[b_psst[1]], writes=[b_sbst[si]])
                S.dma("sp", scr.Sb[c, g], sbst[si][:], reads=[b_sbst[si]], writes=[scr.b_ssd], owner=b_sbst[si])
            S.dma("sp", scr.ypart[c * 128:(c + 1) * 128, :], yp[i][:], reads=[b_yp[i]], writes=[scr.b_ssd], owner=b_yp[i])
        S.barrier()
        for g in range(8):
            S.op("pool", lambda e: e.memset(Hf[:, g, :], 0.0), writes=[b_H[g]])
            S.op("pool", lambda e: e.memset(Hb[:, g, :], 0.0), writes=[b_H[g]])
        for ci, c in enumerate(range(NC - 1, -1, -1)):
            i = ci % 2
            chunk_scalars(c, i)
            S.dma("sp", CTc[i][:], scr.CT[:, c * 128:(c + 1) * 128].rearrange("(g n) l -> n g l", n=128),
                  reads=[scr.b_conv], writes=[b_CTc[i]], owner=b_CTc[i])
            S.dma("sp", xB[i][:, 0:2048], scr.xsB[c * 128:(c + 1) * 128, 0:2048], reads=[scr.b_conv], writes=[b_xB[i]], owner=b_xB[i])
            S.dma("sp", yp[i][:], scr.ypart[c * 128:(c + 1) * 128, :], reads=[scr.b_ssd], writes=[b_yp[i]], owner=b_yp[i])
            S.dma("sp", zt[i][:], scr.z[c * 128:(c + 1) * 128, :], reads=[scr.b_proj], writes=[b_zt[i]], owner=b_zt[i])
            for g in range(8):
                colsb = slice(32 + g * 4, 32 + g * 4 + 4)
                si = nst % 2
                nst += 1
                S.dma("sp", sbst[si][:], scr.Sb[c, g], reads=[scr.b_ssd], writes=[b_sbst[si]], owner=b_sbst[si])
                S.op("pe", lambda e: e.matmul(ps_yo[:], lhsT=CTc[i][:, g, :], rhs=Hb[:, g, :], start=True, stop=True),
                     reads=[b_CTc[i], b_H[g]], writes=[b_psyo])
                S.op("dve", lambda e: e.tensor_tensor(out=ytmp[:].rearrange("p (r q) -> p r q", r=4), in0=ps_yo[:].rearrange("p (r q) -> p r q", r=4),
                                                      in1=bc_l(sm["G2"][:, colsb], 64), op=ALU.mult),
                     reads=[b_psyo, b_sm["G2"]], writes=[b_ytmp])
                S.op("dve", lambda e: e.tensor_tensor(out=yp[i][:, g * 256:(g + 1) * 256], in0=yp[i][:, g * 256:(g + 1) * 256],
                                                      in1=ytmp[:], op=ALU.add),
                     reads=[b_yp[i], b_ytmp], writes=[b_yp[i]])
                S.op("pool", lambda e: e.tensor_tensor(out=tmpH[:].rearrange("p (r q) -> p r q", r=4), in0=Hf[:, g, :].rearrange("p (r q) -> p r q", r=4),
                                                       in1=bc_l(sm["dec"][:, colsb], 64), op=ALU.mult),
                     reads=[b_H[g], b_sm["dec"]], writes=[b_tmpH])
                S.op("pool", lambda e: e.tensor_tensor(out=Hf[:, g, :], in0=tmpH[:], in1=sbst[si][:], op=ALU.add),
                     reads=[b_tmpH, b_sbst[si]], writes=[b_H[g]])
                S.op("act", lambda e: e.copy(out=Hb[:, g, :], in_=Hf[:, g, :]), reads=[b_H[g]], writes=[b_H[g]])
            S.op("act", lambda e: e.activation(out=zt[i][:], in_=zt[i][:], func=AF.Silu), reads=[b_zt[i]], writes=[b_zt[i]])
            S.op("dve", lambda e: e.tensor_tensor(out=yo16[i][:].rearrange("p (h q) -> p h q", h=32), in0=xB[i][:, 0:2048].rearrange("p (h q) -> p h q", h=32),
                                                  in1=bc_l(dsk[:], 64), op=ALU.mult),
                 reads=[b_xB[i], b_prm], writes=[b_yo16[i]])
            S.op("dve", lambda e: e.tensor_tensor(out=yp[i][:], in0=yp[i][:], in1=yo16[i][:], op=ALU.add),
                 reads=[b_yp[i], b_yo16[i]], writes=[b_yp[i]])
            S.op("dve", lambda e: e.tensor_tensor(out=yp[i][:], in0=yp[i][:], in1=zt[i][:], op=ALU.mult),
                 reads=[b_yp[i], b_zt[i]], writes=[b_yp[i]])
            S.op("act", lambda e: e.activation(out=zt[i][:], in_=yp[i][:], func=AF.Square, accum_out=ssq[:, 0:1]),
                 reads=[b_yp[i]], writes=[b_zt[i], b_ssq])
            S.op("dve", lambda e: e.tensor_scalar(out=ssq[:, 1:2], in0=ssq[:, 0:1], scalar1=1.0 / 2048, scalar2=LN_EPS, op0=ALU.mult, op1=ALU.add),
                 reads=[b_ssq], writes=[b_ssq])
            S.op("act", lambda e: e.activation(out=ssq[:, 1:2], in_=ssq[:, 1:2], func=AF.Sqrt), reads=[b_ssq], writes=[b_ssq])
            S.op("dve", lambda e: e.reciprocal(out=ssq[:, 1:2], in_=ssq[:, 1:2]), reads=[b_ssq], writes=[b_ssq])
            S.op("dve", lambda e: e.scalar_tensor_tensor(out=yo16[i][:], in0=yp[i][:], scalar=ssq[:, 1:2], in1=nw[:], op0=ALU.mult, op1=ALU.mult),
                 reads=[b_yp[i], b_ssq, b_prm, b_yo16[i]], writes=[b_yo16[i]])
            S.dma("sp", scr.yssd[c * 128:(c + 1) * 128, :], yo16[i][:], reads=[b_yo16[i]], writes=[scr.b_mix], owner=b_yo16[i])
        S.barrier()


PARAM_SHAPES = lambda cfg: {
    "w_in": [cfg.depth, D, N_IN], "b_gate": [cfg.depth, 3, D],
    "ssd_conv_w": [cfg.depth, 5, 4096], "ssd_conv_b": [cfg.depth, 4096], "ssd_a_log": [cfg.depth, 2, 32],
    "ssd_dt_bias": [cfg.depth, 2, 32], "ssd_d": [cfg.depth, 32], "ssd_norm_w": [cfg.depth, 2048],
    "s5_a_re": [cfg.depth, 2, 72, 64], "s5_a_im": [cfg.depth, 2, 72, 64], "s5_log_step": [cfg.depth, 2, 72],
    "s5_b_re": [cfg.depth, 72, 64, 16], "s5_b_im": [cfg.depth, 72, 64, 16],
    "s5_c_re": [cfg.depth, 2, 72, 16, 64], "s5_c_im": [cfg.depth, 2, 72, 16, 64], "s5_d": [cfg.depth, 1152],
    "s5_glu_w1": [cfg.depth, 1152, 1152], "s5_glu_w2": [cfg.depth, 1152, 1152],
    "w_br_ssd": [cfg.depth, 2048, D], "w_br_attn": [cfg.depth, 384, D], "w_br_s5": [cfg.depth, 1152, D],
    "w_out": [cfg.depth, D, D], "ln1_g": [cfg.depth, D], "ln1_b": [cfg.depth, D],
    "router_w": [cfg.depth, D, cfg.E], "router_b": [cfg.depth, cfg.E],
    "exp_w_gate_up": [cfg.depth, cfg.E, D, 2048], "exp_b_gate_up": [cfg.depth, cfg.E, 2048],
    "exp_w_down": [cfg.depth, cfg.E, D, D], "exp_b_down": [cfg.depth, cfg.E, D],
    "ln2_g": [cfg.depth, D], "ln2_b": [cfg.depth, D],
}


def build(cfg, debug=(), phases=("inproj", "conv", "ssd", "att", "s5", "mix", "moe"), inject=()):
    nc = bass.Bass("TRN2", target_bir_lowering=False)
    L = cfg.L
    prm = {}
    x = nc.dram_tensor("x", [L, D], F32, kind="ExternalInput").ap()
    for name, shape in PARAM_SHAPES(cfg).items():
        prm[name] = nc.dram_tensor(name, list(shape), F32, kind="ExternalInput").ap()
    out = nc.dram_tensor("out", [L, D], F32, kind="ExternalOutput").ap()
    S = Sched(nc)
    cx = Ctx()
    make_consts(S, nc, cx)
    make_masks(S, nc, cx)
    make_att_bias(S, nc, cx)
    scr = alloc_scratch(nc, cfg)
    dbg = {}
    for name in debug:
        src = getattr(scr, name)
        dbg[name] = nc.dram_tensor("dbg_" + name, list(src.shape), src.dtype, kind="ExternalOutput").ap()
    cur = x
    b_inj = Buf("inj", multi=True)
    for name in inject:
        dst = getattr(scr, name)
        src = nc.dram_tensor("inj_" + name, list(dst.shape), dst.dtype, kind="ExternalInput").ap()
        dram_copy(S, dst, src, b_inj)
    if inject:
        S.barrier()
    for l in range(cfg.depth):
        if "inproj" in phases:
            phase_inproj(S, nc, cx, cfg, prm["w_in"][l], cur, scr)
        if "conv" in phases:
            phase_conv(S, nc, cx, cfg, prm["ssd_conv_w"][l], prm["ssd_conv_b"][l], scr)
        if "ssd" in phases:
            phase_ssd(S, nc, cx, cfg, prm, l, scr)
        if "att" in phases:
            phase_att(S, nc, cx, cfg, scr)
        if "s5" in phases:
            phase_s5(S, nc, cx, cfg, prm, l, scr)
        if "mix" in phases:
            phase_mix(S, nc, cx, cfg, prm, l, cur, scr)
            cur = scr.x1
        if "moe" in phases:
            last = (l == cfg.depth - 1) and not debug
            phase_moe(S, nc, cx, cfg, prm, l, scr, out if last else scr.xcur)
            cur = None if last else scr.xcur
    b_dbg = Buf("dbg", multi=True)
    for name in debug:
        dram_copy(S, dbg[name], getattr(scr, name), b_dbg)
    if cur is not None:
        dram_copy(S, out, cur, b_dbg)
    S.barrier()
    return nc, S


def kernel(**inputs):
    cfg = Cfg(L=8192, depth=4, n_exp=32)
    nc, _ = build(cfg)
    x = np.asarray(inputs["x"], dtype=np.float32)
    names = list(PARAM_SHAPES(cfg).keys())
    params = {k: np.ascontiguousarray(np.asarray(inputs[k], dtype=np.float32)) for k in names}
    in_maps = []
    for b in range(x.shape[0]):
        m = {"x": np.ascontiguousarray(x[b])}
        m.update(params)
        in_maps.append(m)
    res = run_bass_kernel_spmd(nc, in_maps, core_ids=list(range(x.shape[0])))
    return np.stack([np.asarray(r["out"], dtype=np.float32) for r in res.results], axis=0)


ATT_PAT = ((128, 1), (512, 4), (2048, 16))
NEG_BIG = -30000.0


def make_att_bias(S, nc, cx):
    cx.abias = nc.alloc_sbuf_tensor("abias", [128, 18, 256], F32)
    cx.b_abias = Buf("abias")
    b = cx.b_abias
    with ExitStack() as es:
        di = es.enter_context(nc.sbuf_tensor("ab_di", [128, 256], I32))
        df = es.enter_context(nc.sbuf_tensor("ab_df", [128, 256], F32))
        mk = es.enter_context(nc.sbuf_tensor("ab_mk", [128, 256], F32))
        bt = Buf("ab_tmp")
        S.op("pool", lambda e: e.iota(di[:], pattern=[[-128, 2], [1, 128]], base=64, channel_multiplier=-1), writes=[bt])
        S.op("dve", lambda e: e.tensor_copy(out=df[:], in_=di[:]), reads=[bt], writes=[bt])
        S.op("act", lambda e: e.activation(out=df[:], in_=df[:], func=AF.Abs), reads=[bt], writes=[bt])
        S.op("dve", lambda e: e.tensor_scalar(out=mk[:], in0=df[:], scalar1=64.0, scalar2=NEG_BIG, op0=ALU.is_gt, op1=ALU.mult),
             reads=[bt], writes=[bt])
        for g, (window, dil) in enumerate(ATT_PAT):
            for h in range(6):
                slope = 2.0 ** (-8.0 * (h * 3 + g + 1) / 18.0)
                S.op("dve", lambda e: e.scalar_tensor_tensor(out=cx.abias[:, g * 6 + h, :], in0=df[:], scalar=-slope * dil, in1=mk[:],
                                                             op0=ALU.mult, op1=ALU.add), reads=[bt], writes=[b])
        S.barrier()


def phase_att(S, nc, cx, cfg, scr):
    L = cfg.L
    with ExitStack() as es:
        sb = lambda n, s, d: es.enter_context(nc.sbuf_tensor(uniq(n), s, d))
        ps = lambda n, s, d: es.enter_context(nc.psum_tensor(uniq(n), s, d))
        PADM = 64 * 16
        qT2 = [sb(f"at_q{i}", [128, L], BF16) for i in range(3)]
        kT2 = [sb(f"at_k{i}", [128, L + 2 * PADM], BF16) for i in range(3)]
        b_qk = bufs("at_qk", 3)
        raw = [sb(f"at_raw{i}", [128, L], BF16) for i in range(2)]
        b_raw = bufs("at_raw", 2)
        Vn = [sb(f"at_v{i}", [128, 6, 65], BF16) for i in range(4)]
        b_Vn = bufs("at_v", 4)
        Ve = [sb(f"at_ve{i}", [128, 6, 65], BF16) for i in range(2)]
        b_Ve = bufs("at_ve", 2)
        for i in range(4):
            S.op("pool", lambda e: e.memset(Vn[i][:, :, 64:65], 1.0), writes=[b_Vn[i]])
        S.op("pool", lambda e: e.memset(Ve[0][:], 0.0), writes=[b_Ve[0]])
        S.op("pool", lambda e: e.memset(Ve[0][64:128, :, 64:65], 1.0), writes=[b_Ve[0]])
        S.op("pool", lambda e: e.memset(Ve[1][:], 0.0), writes=[b_Ve[1]])
        S.op("pool", lambda e: e.memset(Ve[1][0:64, :, 64:65], 1.0), writes=[b_Ve[1]])
        S_ps = [ps(f"at_ps{i}", [128, 2, 128], F32) for i in range(2)]
        b_Sps = bufs("at_ps", 2)
        O_ps = [ps(f"at_po{i}", [128, 6, 65], F32) for i in range(2)]
        b_Ops = bufs("at_po", 2)
        sbt = [sb(f"at_sb{i}", [128, 256], F32) for i in range(2)]
        b_sbt = bufs("at_sb", 2)
        PT = [sb(f"at_pt{i}", [128, 2, 128], BF16) for i in range(2)]
        b_PT = bufs("at_pt", 2)
        Ot = [sb(f"at_o{i}", [128, 390], F32) for i in range(2)]
        b_Ot = bufs("at_o", 2)
        nv = 0
        nsp = 0
        nop = 0
        for g, (window, dil) in enumerate(ATT_PAT):
            ls = L // dil
            PAD = 64 * dil
            nt = ls // 128
            for hp in range(3):
                r0 = g * 384 + hp * 128
                S.dma("sp", raw[0][:], scr.qT[r0:r0 + 128, :], reads=[scr.b_proj], writes=[b_raw[0]], owner=b_raw[0])
                S.dma("sp", raw[1][:], scr.kT[r0:r0 + 128, :], reads=[scr.b_proj], writes=[b_raw[1]], owner=b_raw[1])
                qv = qT2[hp][:, 0:L].rearrange("p (r i) -> p r i", r=dil)
                kv = kT2[hp][:, 0:dil * (ls + 128)].rearrange("p (r i) -> p r i", r=dil)
                S.op("pool", lambda e: e.tensor_copy(out=qv, in_=raw[0][:].rearrange("p (i r) -> p r i", r=dil)),
                     reads=[b_raw[0]], writes=[b_qk[hp]])
                S.op("pool", lambda e: e.memset(kv[:, :, 0:64], 0.0), writes=[b_qk[hp]])
                S.op("pool", lambda e: e.memset(kv[:, :, 64 + ls:128 + ls], 0.0), writes=[b_qk[hp]])
                S.op("dve", lambda e: e.tensor_copy(out=kv[:, :, 64:64 + ls], in_=raw[1][:].rearrange("p (i r) -> p r i", r=dil)),
                     reads=[b_raw[1]], writes=[b_qk[hp]])
            for r in range(dil):
                for n in range(nt):
                    vch = []
                    for c in range(2):
                        kp0 = 128 * n - 64 + 128 * c
                        lo_bad = kp0 < 0
                        hi_bad = kp0 + 128 > ls
                        if lo_bad:
                            vt, bv, p0, p1 = Ve[0], b_Ve[0], 64, 128
                        elif hi_bad:
                            vt, bv, p0, p1 = Ve[1], b_Ve[1], 0, 64
                        else:
                            vi = nv % 4
                            nv += 1
                            vt, bv, p0, p1 = Vn[vi], b_Vn[vi], 0, 128
                        tok0 = (kp0 + p0) * dil + r
                        npart = p1 - p0
                        src = scr.v[tok0:tok0 + (npart - 1) * dil + 1:dil, g * 384:(g + 1) * 384].rearrange("p (h e) -> p h e", h=6)
                        S.dma("sp", vt[p0:p1, :, 0:64], src, reads=[scr.b_proj], writes=[bv], owner=bv)
                        vch.append((vt, bv))
                    oj = nop % 2
                    nop += 1
                    for h in range(6):
                        hp, hh = h // 2, h % 2
                        sj = nsp % 2
                        nsp += 1
                        q0 = r * ls + 128 * n
                        for c in range(2):
                            k0 = r * (ls + 128) + 128 * n + 128 * c
                            S.op("pe", lambda e: e.matmul(S_ps[sj][:, c, :],
                                                          lhsT=kT2[hp][hh * 64:(hh + 1) * 64, k0:k0 + 128],
                                                          rhs=qT2[hp][hh * 64:(hh + 1) * 64, q0:q0 + 128],
                                                          start=True, stop=True),
                                 reads=[b_qk[hp]], writes=[b_Sps[sj]], inc=(c == 1))
                        S.op("dve", lambda e: e.scalar_tensor_tensor(out=sbt[sj][:], in0=S_ps[sj][:].rearrange("p c q -> p (c q)"), scalar=0.125,
                                                                     in1=cx.abias[:, g * 6 + h, :], op0=ALU.mult, op1=ALU.add),
                             reads=[b_Sps[sj], cx.b_abias], writes=[b_sbt[sj]])
                        S.op("act", lambda e: e.activation(out=PT[sj][:].rearrange("p c q -> p (c q)"), in_=sbt[sj][:], func=AF.Exp),
                             reads=[b_sbt[sj]], writes=[b_PT[sj]])
                        for c in range(2):
                            vt, bv = vch[c]
                            S.op("pe", lambda e: e.matmul(O_ps[oj][:, h, :], lhsT=PT[sj][:, c, :], rhs=vt[:, h, :], start=(c == 0), stop=(c == 1)),
                                 reads=[b_PT[sj], bv], writes=[b_Ops[oj]], inc=(c == 1))
                    S.op("act", lambda e: e.copy(out=Ot[oj][:], in_=O_ps[oj][:].rearrange("p h e -> p (h e)")), reads=[b_Ops[oj]], writes=[b_Ot[oj]])
                    t0 = (128 * n) * dil + r
                    S.dma("sp", scr.attO[g, t0:t0 + 127 * dil + 1:dil, :], Ot[oj][:], reads=[b_Ot[oj]], writes=[scr.b_att], owner=b_Ot[oj])
        S.barrier()
        At = [sb(f"at_m{i}", [128, 3, 390], F32) for i in range(2)]
        b_At = bufs("at_m", 2)
        rc = sb("at_rc", [128, 6], F32)
        b_rc = Buf("at_rc")
        yo = [sb(f"at_y{i}", [128, 384], BF16) for i in range(2)]
        b_yo = bufs("at_y", 2)
        for t in range(L // 128):
            i = t % 2
            for g in range(3):
                S.dma("sp", At[i][:, g, :], scr.attO[g, t * 128:(t + 1) * 128, :], reads=[scr.b_att], writes=[b_At[i]], owner=b_At[i])
            S.op("dve", lambda e: e.tensor_tensor(out=At[i][:, 0, :], in0=At[i][:, 0, :], in1=At[i][:, 1, :], op=ALU.add), reads=[b_At[i]], writes=[b_At[i]])
            S.op("dve", lambda e: e.tensor_tensor(out=At[i][:, 0, :], in0=At[i][:, 0, :], in1=At[i][:, 2, :], op=ALU.add), reads=[b_At[i]], writes=[b_At[i]])
            a3 = At[i][:, 0, :].rearrange("p (h e) -> p h e", h=6)
            S.op("dve", lambda e: e.reciprocal(out=rc[:], in_=a3[:, :, 64]), reads=[b_At[i]], writes=[b_rc])
            S.op("dve", lambda e: e.tensor_tensor(out=yo[i][:].rearrange("p (h e) -> p h e", h=6), in0=a3[:, :, 0:64], in1=bc_l(rc[:], 64), op=ALU.mult),
                 reads=[b_At[i], b_rc], writes=[b_yo[i]])
            S.dma("sp", scr.yatt[t * 128:(t + 1) * 128, :], yo[i][:], reads=[b_yo[i]], writes=[scr.b_mix], owner=b_yo[i])
        S.barrier()


def layer_norm_tile(S, h, b_h, out, b_out, gam, bet, b_par, st, b_st):
    for j in range(2):
        S.op("dve", lambda e: e.bn_stats(out=st[:, j * 6:(j + 1) * 6], in_=h[:, j * 512:(j + 1) * 512]), reads=[b_h], writes=[b_st])
    S.op("dve", lambda e: e.bn_aggr(out=st[:, 12:14], in_=st[:, 0:12]), reads=[b_st], writes=[b_st])
    S.op("dve", lambda e: e.tensor_scalar(out=st[:, 14:15], in0=st[:, 13:14], scalar1=LN_EPS, scalar2=None, op0=ALU.add), reads=[b_st], writes=[b_st])
    S.op("act", lambda e: e.activation(out=st[:, 14:15], in_=st[:, 14:15], func=AF.Sqrt), reads=[b_st], writes=[b_st])
    S.op("dve", lambda e: e.reciprocal(out=st[:, 14:15], in_=st[:, 14:15]), reads=[b_st], writes=[b_st])
    S.op("dve", lambda e: e.tensor_scalar(out=out[:], in0=h[:], scalar1=st[:, 12:13], scalar2=st[:, 14:15], op0=ALU.subtract, op1=ALU.mult),
         reads=[b_h, b_st], writes=[b_out])
    S.op("dve", lambda e: e.tensor_tensor(out=out[:], in0=out[:], in1=gam[:], op=ALU.mult), reads=[b_out, b_par], writes=[b_out])
    S.op("dve", lambda e: e.tensor_tensor(out=out[:], in0=out[:], in1=bet[:], op=ALU.add), reads=[b_out, b_par], writes=[b_out])


def load_w_bf16(S, w_ap, dst, b_dst, stg, b_stg, ctr, ncols_max=512):
    K, N = w_ap.shape
    for k in range(K // 128):
        for c0 in range(0, N, ncols_max):
            c1 = min(N, c0 + ncols_max)
            i = ctr[0] % 2
            ctr[0] += 1
            S.dma("sp", stg[i][:, 0:c1 - c0], w_ap[k * 128:(k + 1) * 128, c0:c1], writes=[b_stg[i]], owner=b_stg[i])
            eng = "pool" if ctr[0] % 2 else "act"
            if eng == "pool":
                S.op("pool", lambda e: e.tensor_copy(out=dst[:, k, c0:c1], in_=stg[i][:, 0:c1 - c0]), reads=[b_stg[i]], writes=[b_dst])
            else:
                S.op("act", lambda e: e.copy(out=dst[:, k, c0:c1], in_=stg[i][:, 0:c1 - c0]), reads=[b_stg[i]], writes=[b_dst])


def phase_mix(S, nc, cx, cfg, prm, l, x_ap, scr):
    L = cfg.L
    with ExitStack() as es:
        sb = lambda n, s, d: es.enter_context(nc.sbuf_tensor(uniq(n), s, d))
        ps = lambda n, s, d: es.enter_context(nc.psum_tensor(uniq(n), s, d))
        W1 = sb("gl_w1", [128, 18, 1152], BF16)
        W2 = sb("gl_w2", [128, 18, 1152], BF16)
        b_W = Buf("gl_w")
        stg = [sb(f"gl_st{i}", [128, 1152], F32) for i in range(2)]
        b_stg = bufs("gl_st", 2)
        for i in range(2):
            S.op("pool", lambda e: e.memset(stg[i][:], 0.0), writes=[b_stg[i]])
        n = 0
        for W, wname in ((W1, "s5_glu_w1"), (W2, "s5_glu_w2")):
            for kt in range(18):
                i = n % 2
                n += 1
                for gl in range(4):
                    g = kt * 4 + gl
                    S.dma("sp", stg[i][gl * 32:gl * 32 + 16, :], prm[wname][l][g * 16:(g + 1) * 16, :], writes=[b_stg[i]], owner=b_stg[i])
                S.op("pool", lambda e: e.tensor_copy(out=W[:, kt, :], in_=stg[i][:]), reads=[b_stg[i]], writes=[b_W])
        hT = [sb(f"gl_h{i}", [128, 18, 512], BF16) for i in range(2)]
        b_hT = bufs("gl_h", 2)
        g_ps = [ps(f"gl_pg{i}", [128, 512], F32) for i in range(2)]
        b_gps = bufs("gl_pg", 2)
        l_ps = [ps(f"gl_pl{i}", [128, 512], F32) for i in range(2)]
        b_lps = bufs("gl_pl", 2)
        sg = [sb(f"gl_sg{i}", [128, 512], F32) for i in range(2)]
        b_sg = bufs("gl_sg", 2)
        yo = [sb(f"gl_y{i}", [128, 512], BF16) for i in range(2)]
        b_yo = bufs("gl_y", 2)
        np_ = 0
        for tb in range(L // 512):
            i = tb % 2
            S.dma("sp", hT[i][:], scr.hs5T[:, tb * 512:(tb + 1) * 512].rearrange("(k p) t -> p k t", p=128),
                  reads=[scr.b_s5], writes=[b_hT[i]], owner=b_hT[i])
            for co in range(9):
                j = np_ % 2
                np_ += 1
                for kt in range(18):
                    S.op("pe", lambda e: e.matmul(g_ps[j][:], lhsT=W1[:, kt, co * 128:(co + 1) * 128], rhs=hT[i][:, kt, :], start=(kt == 0), stop=(kt == 17)),
                         reads=[b_W, b_hT[i]], writes=[b_gps[j]], inc=(kt == 17))
                for kt in range(18):
                    S.op("pe", lambda e: e.matmul(l_ps[j][:], lhsT=W2[:, kt, co * 128:(co + 1) * 128], rhs=hT[i][:, kt, :], start=(kt == 0), stop=(kt == 17)),
                         reads=[b_W, b_hT[i]], writes=[b_lps[j]], inc=(kt == 17))
                S.op("act", lambda e: e.activation(out=sg[j][:], in_=l_ps[j][:], func=AF.Sigmoid), reads=[b_lps[j]], writes=[b_sg[j]])
                S.op("dve", lambda e: e.tensor_tensor(out=yo[j][:], in0=g_ps[j][:], in1=sg[j][:], op=ALU.mult), reads=[b_gps[j], b_sg[j]], writes=[b_yo[j]])
                S.dma("sp", scr.ys5T[co * 128:(co + 1) * 128, tb * 512:(tb + 1) * 512], yo[j][:], reads=[b_yo[j]], writes=[scr.b_glu], owner=b_yo[j])
        S.barrier()
    with ExitStack() as es:
        sb = lambda n, s, d: es.enter_context(nc.sbuf_tensor(uniq(n), s, d))
        ps = lambda n, s, d: es.enter_context(nc.psum_tensor(uniq(n), s, d))
        Wssd = sb("mx_wssd", [128, 16, 1024], BF16)
        Watt = sb("mx_watt", [128, 3, 1024], BF16)
        Ws5 = sb("mx_ws5", [128, 9, 1024], BF16)
        Wout = sb("mx_wout", [128, 8, 1024], BF16)
        b_W = Buf("mx_w")
        stg = [sb(f"mx_st{i}", [128, 512], F32) for i in range(2)]
        b_stg = bufs("mx_st", 2)
        ctr = [0]
        load_w_bf16(S, prm["w_br_ssd"][l], Wssd, b_W, stg, b_stg, ctr)
        load_w_bf16(S, prm["w_br_attn"][l], Watt, b_W, stg, b_stg, ctr)
        load_w_bf16(S, prm["w_br_s5"][l], Ws5, b_W, stg, b_stg, ctr)
        load_w_bf16(S, prm["w_out"][l], Wout, b_W, stg, b_stg, ctr)
        bg = sb("mx_bg", [128, 3072], F32)
        lng = sb("mx_lng", [128, 1024], F32)
        lnb = sb("mx_lnb", [128, 1024], F32)
        b_par = Buf("mx_par")
        S.dma("sp", bg[:], prm["b_gate"][l].rearrange("a b -> (a b)").partition_broadcast(128), writes=[b_par], owner=b_par)
        S.dma("sp", lng[:], prm["ln1_g"][l].partition_broadcast(128), writes=[b_par], owner=b_par)
        S.dma("sp", lnb[:], prm["ln1_b"][l].partition_broadcast(128), writes=[b_par], owner=b_par)
        ys = [sb(f"mx_ys{i}", [128, 2048], BF16) for i in range(2)]
        b_ys = bufs("mx_ys", 2)
        ya = [sb(f"mx_ya{i}", [128, 384], BF16) for i in range(2)]
        b_ya = bufs("mx_ya", 2)
        y5T = [sb(f"mx_y5{i}", [128, 9, 128], BF16) for i in range(2)]
        b_y5T = bufs("mx_y5", 2)
        gt = [sb(f"mx_g{i}", [128, 3072], F32) for i in range(2)]
        b_gt = bufs("mx_g", 2)
        xt = [sb(f"mx_x{i}", [128, 1024], F32) for i in range(2)]
        b_xt = bufs("mx_x", 2)
        ysT = sb("mx_ysT", [128, 16, 128], BF16)
        b_ysT = Buf("mx_ysT")
        yaT = sb("mx_yaT", [128, 3, 128], BF16)
        b_yaT = Buf("mx_yaT")
        mT = sb("mx_mT", [128, 8, 128], BF16)
        b_mT = Buf("mx_mT")
        pt = [ps(f"mx_pt{i}", [128, 4, 128], BF16) for i in range(2)]
        b_pt = bufs("mx_pt", 2)
        Pb = [ps(f"mx_pb{i}", [128, 1024], F32) for i in range(2)]
        b_Pb = bufs("mx_pb", 2)
        acc = sb("mx_acc", [128, 1024], F32)
        b_acc = Buf("mx_acc")
        tmp = sb("mx_tmp", [128, 1024], F32)
        b_tmp = Buf("mx_tmp")
        mb = sb("mx_mb", [128, 1024], BF16)
        b_mb = Buf("mx_mb")
        hh = sb("mx_h", [128, 1024], F32)
        b_hh = Buf("mx_h")
        xo = [sb(f"mx_xo{i}", [128, 1024], F32) for i in range(2)]
        b_xo = bufs("mx_xo", 2)
        st = sb("mx_stt", [128, 16], F32)
        b_st = Buf("mx_stt")
        npt = 0
        npb = 0

        def transposes(src, nk, dst, b_src, b_dst):
            nonlocal npt
            for k0 in range(0, nk, 4):
                kn = min(4, nk - k0)
                j = npt % 2
                npt += 1
                for kk in range(kn):
                    S.op("pe", lambda e: e.transpose(out=pt[j][:, kk, :], in_=src[:, (k0 + kk) * 128:(k0 + kk + 1) * 128], identity=cx.identb[:]),
                         reads=[b_src, cx.b_const], writes=[b_pt[j]], inc=(kk == kn - 1))
                S.op("act", lambda e: e.copy(out=dst[:, k0:k0 + kn, :], in_=pt[j][:, 0:kn, :]), reads=[b_pt[j]], writes=[b_dst])

        for t in range(L // 128):
            i = t % 2
            rows = slice(t * 128, (t + 1) * 128)
            S.dma("sp", ys[i][:], scr.yssd[rows, :], reads=[scr.b_mix], writes=[b_ys[i]], owner=b_ys[i])
            S.dma("sp", ya[i][:], scr.yatt[rows, :], reads=[scr.b_mix], writes=[b_ya[i]], owner=b_ya[i])
            S.dma("sp", y5T[i][:], scr.ys5T[:, rows].rearrange("(k p) t -> p k t", p=128), reads=[scr.b_glu], writes=[b_y5T[i]], owner=b_y5T[i])
            S.dma("sp", gt[i][:], scr.gates[rows, :], reads=[scr.b_proj], writes=[b_gt[i]], owner=b_gt[i])
            S.dma("sp", xt[i][:], x_ap[rows, :], writes=[b_xt[i]], owner=b_xt[i])
            S.op("pool", lambda e: e.tensor_tensor(out=gt[i][:], in0=gt[i][:], in1=bg[:], op=ALU.add), reads=[b_gt[i], b_par], writes=[b_gt[i]])
            S.op("act", lambda e: e.activation(out=gt[i][:], in_=gt[i][:], func=AF.Sigmoid), reads=[b_gt[i]], writes=[b_gt[i]])
            transposes(ys[i], 16, ysT, b_ys[i], b_ysT)
            transposes(ya[i], 3, yaT, b_ya[i], b_yaT)
            for bi, (srcT, b_srcT, nk, W) in enumerate(((ysT, b_ysT, 16, Wssd), (yaT, b_yaT, 3, Watt), (y5T[i], b_y5T[i], 9, Ws5))):
                j = npb % 2
                npb += 1
                for nh in range(2):
                    for k in range(nk):
                        S.op("pe", lambda e: e.matmul(Pb[j][:, nh * 512:(nh + 1) * 512], lhsT=srcT[:, k, :], rhs=W[:, k, nh * 512:(nh + 1) * 512],
                                                      start=(k == 0), stop=(k == nk - 1)),
                             reads=[b_srcT, b_W], writes=[b_Pb[j]], inc=(k == nk - 1 and nh == 1))
                if bi == 0:
                    S.op("dve", lambda e: e.tensor_tensor(out=acc[:], in0=Pb[j][:], in1=gt[i][:, 0:1024], op=ALU.mult), reads=[b_Pb[j], b_gt[i]], writes=[b_acc])
                else:
                    S.op("dve", lambda e: e.tensor_tensor(out=tmp[:], in0=Pb[j][:], in1=gt[i][:, bi * 1024:(bi + 1) * 1024], op=ALU.mult),
                         reads=[b_Pb[j], b_gt[i]], writes=[b_tmp])
                    if bi == 1:
                        S.op("pool", lambda e: e.tensor_tensor(out=acc[:], in0=acc[:], in1=tmp[:], op=ALU.add), reads=[b_acc, b_tmp], writes=[b_acc])
                    else:
                        S.op("pool", lambda e: e.tensor_tensor(out=mb[:], in0=acc[:], in1=tmp[:], op=ALU.add), reads=[b_acc, b_tmp], writes=[b_mb])
            transposes(mb, 8, mT, b_mb, b_mT)
            j = npb % 2
            npb += 1
            for nh in range(2):
                for k in range(8):
                    S.op("pe", lambda e: e.matmul(Pb[j][:, nh * 512:(nh + 1) * 512], lhsT=mT[:, k, :], rhs=Wout[:, k, nh * 512:(nh + 1) * 512],
                                                  start=(k == 0), stop=(k == 7)),
                         reads=[b_mT, b_W], writes=[b_Pb[j]], inc=(k == 7 and nh == 1))
            S.op("dve", lambda e: e.scalar_tensor_tensor(out=hh[:], in0=xt[i][:], scalar=cfg.alpha, in1=Pb[j][:], op0=ALU.mult, op1=ALU.add),
                 reads=[b_xt[i], b_Pb[j]], writes=[b_hh])
            layer_norm_tile(S, hh, b_hh, xo[i], b_xo[i], lng, lnb, b_par, st, b_st)
            S.dma("sp", scr.x1[rows, :], xo[i][:], reads=[b_xo[i]], writes=[scr.b_x1], owner=b_xo[i])
        S.barrier()


def cast_dram_bf16_all(S, pairs, b_dst, stg, stb, b_stg, b_stb):
    tiles = []
    for src, dst in pairs:
        R, N = src.shape
        for r in range(0, R, 128):
            tiles.append((src[r:r + 128, :], dst[r:r + 128, :], N))
    ns = len(stg)

    def load(t):
        i = t % ns
        S.dma("sp", stg[i][:, 0:tiles[t][2]], tiles[t][0], writes=[b_stg[i]], owner=b_stg[i])

    load(0)
    for t in range(len(tiles)):
        if t + 1 < len(tiles):
            load(t + 1)
        i = t % ns
        N = tiles[t][2]
        if t % 2:
            S.op("pool", lambda e: e.tensor_copy(out=stb[i][:, 0:N], in_=stg[i][:, 0:N]), reads=[b_stg[i]], writes=[b_stb[i]])
        else:
            S.op("act", lambda e: e.copy(out=stb[i][:, 0:N], in_=stg[i][:, 0:N]), reads=[b_stg[i]], writes=[b_stb[i]])
        S.dma("sp", tiles[t][1], stb[i][:, 0:N], reads=[b_stb[i]], writes=[b_dst], owner=b_stb[i])


def phase_moe(S, nc, cx, cfg, prm, l, scr, xdst):
    L = cfg.L
    E = cfg.E
    with ExitStack() as es:
        sb = lambda n, s, d: es.enter_context(nc.sbuf_tensor(uniq(n), s, d))
        ps = lambda n, s, d: es.enter_context(nc.psum_tensor(uniq(n), s, d))
        stg = [sb(f"mo_cs{i}", [128, 2048], F32) for i in range(3)]
        stb = [sb(f"mo_cb{i}", [128, 2048], BF16) for i in range(3)]
        b_stg = bufs("mo_cs", 3)
        b_stb = bufs("mo_cb", 3)
        pairs = []
        for e_ in range(E):
            pairs.append((prm["exp_w_gate_up"][l][e_], scr.wgu16[e_]))
            pairs.append((prm["exp_w_down"][l][e_], scr.wdn16[e_]))
        cast_dram_bf16_all(S, pairs, scr.b_w16, stg, stb, b_stg, b_stb)
        Wr = sb("mo_wr", [128, 8, E], F32)
        Wrh = sb("mo_wrh", [128, 8, E], BF16)
        Wrl = sb("mo_wrl", [128, 8, E], BF16)
        rb = sb("mo_rb", [128, E], F32)
        b_wr = Buf("mo_wr")
        S.dma("sp", Wr[:], prm["router_w"][l].rearrange("(k p) e -> p k e", p=128), writes=[b_wr], owner=b_wr)
        S.dma("sp", rb[:], prm["router_b"][l].partition_broadcast(128), writes=[b_wr], owner=b_wr)
        S.op("dve", lambda e: e.tensor_copy(out=Wrh[:], in_=Wr[:]), reads=[b_wr], writes=[b_wr])
        S.op("dve", lambda e: e.tensor_tensor(out=Wr[:], in0=Wr[:], in1=Wrh[:], op=ALU.subtract), reads=[b_wr], writes=[b_wr])
        S.op("dve", lambda e: e.tensor_copy(out=Wrl[:], in_=Wr[:]), reads=[b_wr], writes=[b_wr])
        xt = [sb(f"mo_x{i}", [128, 1024], F32) for i in range(2)]
        b_xt = bufs("mo_x", 2)
        xh = sb("mo_xh", [128, 1024], BF16)
        xl = sb("mo_xl", [128, 1024], BF16)
        b_xhl = Buf("mo_xhl")
        xTl = sb("mo_xTl", [128, 8, 128], BF16)
        b_xTl = Buf("mo_xTl")
        xTb = [sb(f"mo_xTb{i}", [128, 8, 128], BF16) for i in range(2)]
        b_xTb = bufs("mo_xTb", 2)
        ptf = [ps(f"mo_ptf{i}", [128, 4, 128], BF16) for i in range(2)]
        b_ptf = bufs("mo_ptf", 2)
        lg_ps = ps("mo_lg", [128, E], F32)
        b_lgps = Buf("mo_lg")
        lg = sb("mo_lgs", [128, E], F32)
        b_lg = Buf("mo_lgs")
        v8 = sb("mo_v8", [128, 8], F32)
        b_v8 = Buf("mo_v8")
        mk = sb("mo_mk", [128, E], F32)
        b_mk = Buf("mo_mk")
        sm = sb("mo_sm", [128, 4], F32)
        b_sm = Buf("mo_sm")
        gts = [sb(f"mo_gt{i}", [128, E], F32) for i in range(2)]
        b_gts = bufs("mo_gt", 2)
        npt = 0
        for t in range(L // 128):
            i = t % 2
            rows = slice(t * 128, (t + 1) * 128)
            S.dma("sp", xt[i][:], scr.x1[rows, :], reads=[scr.b_x1], writes=[b_xt[i]], owner=b_xt[i])
            S.op("dve", lambda e: e.tensor_copy(out=xh[:], in_=xt[i][:]), reads=[b_xt[i]], writes=[b_xhl])
            S.op("dve", lambda e: e.tensor_tensor(out=xl[:], in0=xt[i][:], in1=xh[:], op=ALU.subtract), reads=[b_xt[i], b_xhl], writes=[b_xhl])
            for src, dstT, b_dstT in ((xh, xTb[i], b_xTb[i]), (xl, xTl, b_xTl)):
                for k4 in range(2):
                    j = npt % 2
                    npt += 1
                    for kk in range(4):
                        k = k4 * 4 + kk
                        S.op("pe", lambda e: e.transpose(out=ptf[j][:, kk, :], in_=src[:, k * 128:(k + 1) * 128], identity=cx.identb[:]),
                             reads=[b_xhl, cx.b_const], writes=[b_ptf[j]], inc=(kk == 3))
                    S.op("act", lambda e: e.copy(out=dstT[:, k4 * 4:(k4 + 1) * 4, :], in_=ptf[j][:]), reads=[b_ptf[j]], writes=[b_dstT])
            S.dma("sp", scr.x1T[:, rows].rearrange("(k p) t -> p k t", p=128), xTb[i][:], reads=[b_xTb[i]], writes=[scr.b_x1T], owner=b_xTb[i])
            n_mm = 0
            for k in range(8):
                for (xa, b_xa, wa) in ((xTb[i], b_xTb[i], Wrh), (xTl, b_xTl, Wrh), (xTb[i], b_xTb[i], Wrl)):
                    S.op("pe", lambda e: e.matmul(lg_ps[:], lhsT=xa[:, k, :], rhs=wa[:, k, :], start=(n_mm == 0), stop=(n_mm == 23)),
                         reads=[b_xa, b_wr], writes=[b_lgps], inc=(n_mm == 23))
                    n_mm += 1
            S.op("dve", lambda e: e.tensor_tensor(out=lg[:], in0=lg_ps[:], in1=rb[:], op=ALU.add), reads=[b_lgps, b_wr], writes=[b_lg])
            S.op("dve", lambda e: e.max(out=v8[:], in_=lg[:]), reads=[b_lg], writes=[b_v8])
            S.op("dve", lambda e: e.tensor_scalar(out=mk[:], in0=lg[:], scalar1=v8[:, 3:4], scalar2=None, op0=ALU.is_ge), reads=[b_lg, b_v8], writes=[b_mk])
            S.op("dve", lambda e: e.tensor_scalar(out=sm[:, 0:1], in0=v8[:, 0:1], scalar1=-1.0, scalar2=None, op0=ALU.mult), reads=[b_v8], writes=[b_sm])
            S.op("act", lambda e: e.activation(out=lg[:], in_=lg[:], func=AF.Exp, bias=sm[:, 0:1], scale=1.0), reads=[b_lg, b_sm], writes=[b_lg])
            S.op("dve", lambda e: e.tensor_tensor(out=lg[:], in0=lg[:], in1=mk[:], op=ALU.mult), reads=[b_lg, b_mk], writes=[b_lg])
            S.op("dve", lambda e: e.reduce_sum(out=sm[:, 1:2], in_=lg[:], axis=AX.X), reads=[b_lg, b_sm], writes=[b_sm])
            S.op("dve", lambda e: e.reciprocal(out=sm[:, 2:3], in_=sm[:, 1:2]), reads=[b_sm], writes=[b_sm])
            S.op("dve", lambda e: e.tensor_scalar(out=gts[i][:], in0=lg[:], scalar1=sm[:, 2:3], scalar2=None, op0=ALU.mult), reads=[b_lg, b_sm], writes=[b_gts[i]])
            S.dma("sp", scr.rgate[rows, :], gts[i][:], reads=[b_gts[i]], writes=[scr.b_x1T], owner=b_gts[i])
        S.barrier()
    TBm = 512
    with ExitStack() as es:
        sb = lambda n, s, d: es.enter_context(nc.sbuf_tensor(uniq(n), s, d))
        ps = lambda n, s, d: es.enter_context(nc.psum_tensor(uniq(n), s, d))
        Wgu = [sb(f"me_wgu{i}", [128, 8, 2048], BF16) for i in range(2)]
        Wdn = [sb(f"me_wdn{i}", [128, 8, 1024], BF16) for i in range(2)]
        b_Wgu = bufs("me_wgu", 2)
        b_Wdn = bufs("me_wdn", 2)
        bgu = sb("me_bgu", [128, E, 16], F32)
        bdn = sb("me_bdn", [E, 1024], F32)
        bdn16 = sb("me_bdn16", [E, 1024], BF16)
        lng = sb("me_lng", [128, 1024], F32)
        lnb = sb("me_lnb", [128, 1024], F32)
        b_par = Buf("me_par")
        with nc.allow_non_contiguous_dma(reason="tiny param load"):
            for e_ in range(E):
                S.dma("sp", bgu[:, e_, :], prm["exp_b_gate_up"][l][e_].rearrange("(c p) -> p c", p=128), writes=[b_par], owner=b_par)
        S.op("dve", lambda e: e.tensor_scalar(out=bgu[:, :, 8:16], in0=bgu[:, :, 8:16], scalar1=1.0, scalar2=None, op0=ALU.add), reads=[b_par], writes=[b_par])
        S.dma("sp", bdn[:], prm["exp_b_down"][l], writes=[b_par], owner=b_par)
        S.op("dve", lambda e: e.tensor_copy(out=bdn16[:], in_=bdn[:]), reads=[b_par], writes=[b_par])
        S.dma("sp", lng[:], prm["ln2_g"][l].partition_broadcast(128), writes=[b_par], owner=b_par)
        S.dma("sp", lnb[:], prm["ln2_b"][l].partition_broadcast(128), writes=[b_par], owner=b_par)
        xT = [sb(f"me_xT{i}", [128, 8, TBm], BF16) for i in range(2)]
        b_xT = bufs("me_xT", 2)
        gt = [sb(f"me_gt{i}", [128, 4, E], F32) for i in range(2)]
        b_gt = bufs("me_gt", 2)
        gT = sb("me_gT", [E, 4, 128], BF16)
        gtb = sb("me_gtb", [128, 4, E], BF16)
        b_gtb = Buf("me_gtb")
        b_gT = Buf("me_gT")
        acc = sb("me_acc", [128, 4, 1024], F32)
        b_acc = bufs("me_acc", 4)
        act = [sb(f"me_act{i}", [128, 8, TBm], BF16) for i in range(2)]
        b_act = bufs("me_act", 2)
        t1 = [sb(f"me_t1{i}", [128, TBm], F32) for i in range(2)]
        b_t1 = bufs("me_t1", 2)
        t2 = [sb(f"me_t2{i}", [128, TBm], F32) for i in range(2)]
        b_t2 = bufs("me_t2", 2)
        sg = [sb(f"me_sg{i}", [128, TBm], F32) for i in range(2)]
        b_sg = bufs("me_sg", 2)
        g_ps = [ps(f"me_pg{i}", [128, 512], F32) for i in range(2)]
        b_gps = bufs("me_pg", 2)
        l_ps = [ps(f"me_pl{i}", [128, 512], F32) for i in range(2)]
        b_lps = bufs("me_pl", 2)
        y_ps = [ps(f"me_py{i}", [128, 512], F32) for i in range(2)]
        b_yps = bufs("me_py", 2)
        ptg = ps("me_ptg", [E, 4, 128], BF16)
        b_ptg = Buf("me_ptg")
        x1t = [sb(f"me_x1{i}", [128, 1024], F32) for i in range(1)] * 2
        b_x1t = bufs("me_x1", 1) * 2
        xo = [sb(f"me_xo{i}", [128, 1024], F32) for i in range(1)] * 2
        b_xo = bufs("me_xo", 1) * 2
        st = sb("me_stt", [128, 16], F32)
        b_st = Buf("me_stt")
        nw = 0
        nfp = 0
        nyp = 0
        nx1 = 0
        for tb in range(L // TBm):
            bi = tb % 2
            cols = slice(tb * TBm, (tb + 1) * TBm)
            S.dma("sp", xT[bi][:], scr.x1T[:, cols].rearrange("(k p) t -> p k t", p=128), reads=[scr.b_x1T], writes=[b_xT[bi]], owner=b_xT[bi])
            S.dma("sp", gt[bi][:], scr.rgate[cols, :].rearrange("(a p) e -> p a e", p=128), reads=[scr.b_x1T], writes=[b_gt[bi]], owner=b_gt[bi])
            S.op("dve", lambda e: e.tensor_copy(out=gtb[:], in_=gt[bi][:]), reads=[b_gt[bi]], writes=[b_gtb])
            for tt in range(4):
                S.op("pe", lambda e: e.transpose(out=ptg[:, tt, :], in_=gtb[:, tt, :], identity=cx.identb[:]),
                     reads=[b_gtb, cx.b_const], writes=[b_ptg], inc=(tt == 3))
            S.op("act", lambda e: e.copy(out=gT[:], in_=ptg[:]), reads=[b_ptg], writes=[b_gT])
            for tt in range(4):
                for nh in range(2):
                    j = nyp % 2
                    nyp += 1
                    S.op("pe", lambda e: e.matmul(y_ps[j][:], lhsT=gT[:, tt, :], rhs=bdn16[:, nh * 512:(nh + 1) * 512], start=True, stop=True),
                         reads=[b_gT, b_par], writes=[b_yps[j]])
                    S.op("act", lambda e: e.copy(out=acc[:, tt, nh * 512:(nh + 1) * 512], in_=y_ps[j][:]), reads=[b_yps[j]], writes=[b_acc[tt]])
            for e_ in range(E):
                wi = nw % 2
                nw += 1
                S.dma("sp", Wgu[wi][:], scr.wgu16[e_].rearrange("(k p) f -> p k f", p=128), reads=[scr.b_w16], writes=[b_Wgu[wi]], owner=b_Wgu[wi])
                S.dma("sp", Wdn[wi][:], scr.wdn16[e_].rearrange("(k p) f -> p k f", p=128), reads=[scr.b_w16], writes=[b_Wdn[wi]], owner=b_Wdn[wi])
                ai = nw % 2
                for fj in range(8):
                    j = nfp % 2
                    nfp += 1
                    for k in range(8):
                        S.op("pe", lambda e: e.matmul(g_ps[j][:], lhsT=Wgu[wi][:, k, fj * 128:(fj + 1) * 128], rhs=xT[bi][:, k, :], start=(k == 0), stop=(k == 7)),
                             reads=[b_Wgu[wi], b_xT[bi]], writes=[b_gps[j]], inc=(k == 7))
                    for k in range(8):
                        S.op("pe", lambda e: e.matmul(l_ps[j][:], lhsT=Wgu[wi][:, k, 1024 + fj * 128:1024 + (fj + 1) * 128], rhs=xT[bi][:, k, :], start=(k == 0), stop=(k == 7)),
                             reads=[b_Wgu[wi], b_xT[bi]], writes=[b_lps[j]], inc=(k == 7))
                    S.op("dve", lambda e: e.tensor_scalar(out=t1[j][:], in0=g_ps[j][:], scalar1=bgu[:, e_, fj:fj + 1], scalar2=7.0, op0=ALU.add, op1=ALU.min),
                         reads=[b_gps[j], b_par], writes=[b_t1[j]])
                    S.op("act", lambda e: e.activation(out=sg[j][:], in_=t1[j][:], func=AF.Sigmoid, scale=1.702), reads=[b_t1[j]], writes=[b_sg[j]])
                    S.op("dve", lambda e: e.tensor_scalar(out=t2[j][:], in0=l_ps[j][:], scalar1=bgu[:, e_, 8 + fj:9 + fj], scalar2=8.0, op0=ALU.add, op1=ALU.min),
                         reads=[b_lps[j], b_par], writes=[b_t2[j]])
                    S.op("pool", lambda e: e.tensor_tensor(out=t1[j][:], in0=t1[j][:], in1=sg[j][:], op=ALU.mult), reads=[b_t1[j], b_sg[j]], writes=[b_t1[j]])
                    S.op("dve", lambda e: e.scalar_tensor_tensor(out=act[ai][:, fj, :], in0=t2[j][:], scalar=-6.0, in1=t1[j][:], op0=ALU.max, op1=ALU.mult),
                         reads=[b_t1[j], b_t2[j]], writes=[b_act[ai]])
                for tt in range(4):
                    for nh in range(2):
                        j = nyp % 2
                        nyp += 1
                        for fk in range(8):
                            S.op("pe", lambda e: e.matmul(y_ps[j][:], lhsT=act[ai][:, fk, tt * 128:(tt + 1) * 128], rhs=Wdn[wi][:, fk, nh * 512:(nh + 1) * 512],
                                                          start=(fk == 0), stop=(fk == 7)),
                                 reads=[b_act[ai], b_Wdn[wi]], writes=[b_yps[j]], inc=(fk == 7))
                        S.op("dve", lambda e: e.scalar_tensor_tensor(out=acc[:, tt, nh * 512:(nh + 1) * 512], in0=y_ps[j][:], scalar=gt[bi][:, tt, e_:e_ + 1],
                                                                     in1=acc[:, tt, nh * 512:(nh + 1) * 512], op0=ALU.mult, op1=ALU.add),
                             reads=[b_yps[j], b_gt[bi], b_acc[tt]], writes=[b_acc[tt]])
            for tt in range(4):
                xi = nx1 % 2
                nx1 += 1
                rows = slice(tb * TBm + tt * 128, tb * TBm + (tt + 1) * 128)
                S.dma("sp", x1t[xi][:], scr.x1[rows, :], reads=[scr.b_x1], writes=[b_x1t[xi]], owner=b_x1t[xi])
                S.op("dve", lambda e: e.scalar_tensor_tensor(out=x1t[xi][:], in0=x1t[xi][:], scalar=cfg.alpha, in1=acc[:, tt, :], op0=ALU.mult, op1=ALU.add),
                     reads=[b_x1t[xi], b_acc[tt]], writes=[b_x1t[xi]])
                layer_norm_tile(S, x1t[xi], b_x1t[xi], xo[xi], b_xo[xi], lng, lnb, b_par, st, b_st)
                S.dma("sp", xdst[rows, :], xo[xi][:], reads=[b_xo[xi]], writes=[scr.b_xcur], owner=b_xo[xi])
        S.barrier()


TWO_PI = 2.0 * math.pi


def phase_s5(S, nc, cx, cfg, prm, l, scr):
    L = cfg.L
    NCk = L // 8
    J = int(math.log2(NCk))
    CB = min(512, NCk)
    with ExitStack() as es:
        sb = lambda n, s, d: es.enter_context(nc.sbuf_tensor(uniq(n), s, d))
        ps = lambda n, s, d: es.enter_context(nc.psum_tensor(uniq(n), s, d))
        bP = Buf("s5_par")
        NP = 72

        def T(name, cols, dt=F32):
            return sb("s5_" + name, [128, cols], dt)

        ar, ai, ls = T("ar", NP), T("ai", NP), T("ls", NP)
        with nc.allow_non_contiguous_dma(reason="tiny param load"):
            for d in range(2):
                S.dma("sp", ar[:, d * 36:(d + 1) * 36], prm["s5_a_re"][l][d].rearrange("(p g) n -> (g n) p", g=2), writes=[bP], owner=bP)
                S.dma("sp", ai[:, d * 36:(d + 1) * 36], prm["s5_a_im"][l][d].rearrange("(p g) n -> (g n) p", g=2), writes=[bP], owner=bP)
                for gl in range(2):
                    S.dma("sp", ls[64 * gl:64 * gl + 64, d * 36:(d + 1) * 36],
                          prm["s5_log_step"][l][d].rearrange("(p g) -> g p", g=2)[gl].partition_broadcast(64), writes=[bP], owner=bP)
        BR = sb("s5_BR", [128, 36, 16], F32)
        BI = sb("s5_BI", [128, 36, 16], F32)
        S.dma("sp", BR[:], prm["s5_b_re"][l].rearrange("(p g) n c -> (g n) p c", g=2), writes=[bP], owner=bP)
        S.dma("sp", BI[:], prm["s5_b_im"][l].rearrange("(p g) n c -> (g n) p c", g=2), writes=[bP], owner=bP)
        dpad = sb("s5_dpad", [128, 18], F32)
        S.op("pool", lambda e: e.memset(dpad[:], 0.0), writes=[bP])
        with nc.allow_non_contiguous_dma(reason="tiny param load"):
            for g4 in range(4):
                S.dma("sp", dpad[32 * g4:32 * g4 + 16, :], prm["s5_d"][l].rearrange("(t g c) -> g c t", g=4, c=16)[g4], writes=[bP], owner=bP)

        def dv(fn, eng="dve"):
            S.op(eng, fn, reads=[bP], writes=[bP])

        def tt(out, a, b, op, eng="dve"):
            dv(lambda e: e.tensor_tensor(out=out, in0=a, in1=b, op=op), eng)

        def ts(out, a, s1, op0, s2=None, op1=None):
            if op1 is None:
                dv(lambda e: e.tensor_scalar(out=out, in0=a, scalar1=s1, scalar2=None, op0=op0))
            else:
                dv(lambda e: e.tensor_scalar(out=out, in0=a, scalar1=s1, scalar2=s2, op0=op0, op1=op1))

        def act(out, in_, func, scale=1.0):
            S.op("act", lambda e: e.activation(out=out, in_=in_, func=func, scale=scale), reads=[bP], writes=[bP])

        step, sr, th, mag = T("step", NP), T("sr", NP), T("th", NP), T("mag", NP)
        t0, t1_, t2_, ki = T("t0", NP), T("t1", NP), T("t2", NP), sb("s5_ki", [128, NP], I32)
        sn, cs = T("sn", NP), T("cs", NP)
        act(step[:], ls[:], AF.Exp)
        tt(sr[:], step[:], ar[:], ALU.mult)
        tt(th[:], step[:], ai[:], ALU.mult)
        act(mag[:], sr[:], AF.Exp)

        def sin_of(out, ang):
            ts(t0[:], ang, 1.0 / TWO_PI, ALU.mult, 0.5, ALU.add)
            dv(lambda e: e.tensor_copy(out=ki[:], in_=t0[:]))
            dv(lambda e: e.tensor_copy(out=t1_[:], in_=ki[:]))
            dv(lambda e: e.scalar_tensor_tensor(out=t2_[:], in0=t1_[:], scalar=-TWO_PI, in1=ang, op0=ALU.mult, op1=ALU.add))
            ts(t0[:], t2_[:], -math.pi, ALU.is_lt, TWO_PI, ALU.mult)
            tt(t2_[:], t2_[:], t0[:], ALU.add)
            ts(t0[:], t2_[:], math.pi, ALU.is_gt, -TWO_PI, ALU.mult)
            tt(t2_[:], t2_[:], t0[:], ALU.add)
            act(out, t2_[:], AF.Sin)

        thc = T("thc", NP)
        sin_of(sn[:], th[:])
        ts(thc[:], th[:], math.pi / 2, ALU.add)
        sin_of(cs[:], thc[:])
        abr, abi = T("abr", NP), T("abi", NP)
        tt(abr[:], mag[:], cs[:], ALU.mult)
        tt(abi[:], mag[:], sn[:], ALU.mult)
        den, m1, fr, fi = T("den", NP), T("m1", NP), T("fr", NP), T("fi", NP)
        tt(den[:], ar[:], ar[:], ALU.mult)
        tt(t0[:], ai[:], ai[:], ALU.mult)
        tt(den[:], den[:], t0[:], ALU.add)
        dv(lambda e: e.reciprocal(out=den[:], in_=den[:]))
        ts(m1[:], abr[:], -1.0, ALU.add)
        tt(fr[:], m1[:], ar[:], ALU.mult)
        tt(t0[:], abi[:], ai[:], ALU.mult)
        tt(fr[:], fr[:], t0[:], ALU.add)
        tt(fr[:], fr[:], den[:], ALU.mult)
        tt(fi[:], abi[:], ar[:], ALU.mult)
        tt(t0[:], m1[:], ai[:], ALU.mult)
        tt(fi[:], fi[:], t0[:], ALU.subtract)
        tt(fi[:], fi[:], den[:], ALU.mult)
        ivr, ivi = T("ivr", NP), T("ivi", NP)
        act(t0[:], sr[:], AF.Exp, scale=-2.0)
        tt(ivr[:], abr[:], t0[:], ALU.mult)
        tt(ivi[:], abi[:], t0[:], ALU.mult)
        ts(ivi[:], ivi[:], -1.0, ALU.mult)
        PWr = sb("s5_PWr", [128, 9, NP], F32)
        PWi = sb("s5_PWi", [128, 9, NP], F32)
        NGr = sb("s5_NGr", [128, 9, NP], F32)
        NGi = sb("s5_NGi", [128, 9, NP], F32)
        PWrR = sb("s5_PWrR", [128, 9, NP], F32)
        PWiR = sb("s5_PWiR", [128, 9, NP], F32)
        NGrR = sb("s5_NGrR", [128, 9, NP], F32)
        NGiR = sb("s5_NGiR", [128, 9, NP], F32)

        def cmul(or_, oi_, ar_, ai_, br_, bi_):
            tt(t0[:], ar_, br_, ALU.mult)
            tt(t1_[:], ai_, bi_, ALU.mult)
            tt(t2_[:], ar_, bi_, ALU.mult)
            tt(thc[:], ai_, br_, ALU.mult)
            tt(or_, t0[:], t1_[:], ALU.subtract)
            tt(oi_, t2_[:], thc[:], ALU.add)

        for (Pr, Pi, br_, bi_) in ((PWr, PWi, abr, abi), (NGr, NGi, ivr, ivi)):
            dv(lambda e: e.memset(Pr[:, 0, :], 1.0), "pool")
            dv(lambda e: e.memset(Pi[:, 0, :], 0.0), "pool")
            for e_ in range(1, 9):
                cmul(Pr[:, e_, :], Pi[:, e_, :], Pr[:, e_ - 1, :], Pi[:, e_ - 1, :], br_[:], bi_[:])
        for (Pr, PrR) in ((PWr, PWrR), (PWi, PWiR), (NGr, NGrR), (NGi, NGiR)):
            for e_ in range(9):
                dv(lambda e: e.tensor_copy(out=PrR[:, e_, :], in_=Pr[:, 8 - e_, :]), "pool")
        SPr = sb("s5_SPr", [128, J + 1, NP], F32)
        SPi = sb("s5_SPi", [128, J + 1, NP], F32)
        SPn = sb("s5_SPn", [128, J + 1, NP], F32)
        dv(lambda e: e.tensor_copy(out=SPr[:, 0, :], in_=PWr[:, 8, :]))
        dv(lambda e: e.tensor_copy(out=SPi[:, 0, :], in_=PWi[:, 8, :]))
        for j in range(1, J + 1):
            cmul(SPr[:, j, :], SPi[:, j, :], SPr[:, j - 1, :], SPi[:, j - 1, :], SPr[:, j - 1, :], SPi[:, j - 1, :])
        ts(SPn[:], SPi[:], -1.0, ALU.mult)
        bbr = sb("s5_bbr", [128, 2, 36, 16], F32)
        bbi = sb("s5_bbi", [128, 2, 36, 16], F32)
        tb1 = sb("s5_tb1", [128, 2, 36, 16], F32)
        frv = fr[:].rearrange("p (d q) -> p d q", d=2).unsqueeze(3).to_broadcast([128, 2, 36, 16])
        fiv = fi[:].rearrange("p (d q) -> p d q", d=2).unsqueeze(3).to_broadcast([128, 2, 36, 16])
        BRv = BR[:].unsqueeze(1).to_broadcast([128, 2, 36, 16])
        BIv = BI[:].unsqueeze(1).to_broadcast([128, 2, 36, 16])
        tt(bbr[:], frv, BRv, ALU.mult)
        tt(tb1[:], fiv, BIv, ALU.mult)
        tt(bbr[:], bbr[:], tb1[:], ALU.subtract)
        tt(bbi[:], frv, BIv, ALU.mult)
        tt(tb1[:], fiv, BRv, ALU.mult)
        tt(bbi[:], bbi[:], tb1[:], ALU.add)
        CRT = sb("s5_CRT", [128, 2, 36, 16], F32)
        CIT = sb("s5_CIT", [128, 2, 36, 16], F32)
        cin = sb("s5_cin", [128, 128], F32)
        cinb = sb("s5_cinb", [128, 128], BF16)
        pc = ps("s5_pc", [128, 128], BF16)
        b_pc = Buf("s5_pc")
        for (cname, CT_) in (("s5_c_re", CRT), ("s5_c_im", CIT)):
            for d in range(2):
                for pb in range(0, 36, 8):
                    npair = min(8, 36 - pb)
                    for q in range(npair):
                        pr_ = pb + q
                        S.dma("sp", cin[16 * q:16 * q + 16, :].rearrange("c (g n) -> c g n", g=2),
                              prm[cname][l][d][2 * pr_:2 * pr_ + 2].rearrange("g c n -> c g n"), writes=[bP], owner=bP)
                    dv(lambda e: e.tensor_copy(out=cinb[:], in_=cin[:]))
                    S.op("pe", lambda e: e.transpose(out=pc[:], in_=cinb[:], identity=cx.identb[:]), reads=[bP, cx.b_const], writes=[b_pc])
                    S.op("act", lambda e: e.copy(out=CT_[:, d, pb:pb + npair, :], in_=pc[:, 0:npair * 16].rearrange("p (q c) -> p q c", c=16)),
                         reads=[b_pc], writes=[bP])

        Uraw = sb("s5_Uraw", [128, L], BF16)
        b_Ur = Buf("s5_Ur")
        Us = sb("s5_Us", [128, 8, NCk], BF16)
        b_U = Buf("s5_U")
        Wi = sb("s5_Wi", [128, 8, 8, 128], BF16)
        b_Wi = Buf("s5_Wi")
        WOre = [sb(f"s5_WOre{i}", [128, 8, 128], BF16) for i in range(4)]
        WOim = [sb(f"s5_WOim{i}", [128, 8, 128], BF16) for i in range(4)]
        b_WO = bufs("s5_WO", 4)
        Lre = sb("s5_Lre", [128, 8, 128], BF16)
        Lim = sb("s5_Lim", [128, 8, 128], BF16)
        b_L = Buf("s5_L")
        LTre = sb("s5_LTre", [128, 8, 128], BF16)
        LTim = sb("s5_LTim", [128, 8, 128], BF16)
        b_LT = Buf("s5_LT")
        Rr = sb("s5_Rr", [128, 8, 16], F32)
        Ri = sb("s5_Ri", [128, 8, 16], F32)
        Rt = sb("s5_Rt", [128, 8, 16], F32)
        b_R = Buf("s5_R")
        ZR = [sb(f"s5_ZR{i}", [128, NCk], F32) for i in range(2)]
        ZI = [sb(f"s5_ZI{i}", [128, NCk], F32) for i in range(2)]
        b_Z = bufs("s5_Z", 2)
        ztmp = sb("s5_ztmp", [128, NCk], F32)
        b_ztmp = Buf("s5_ztmp")
        HR = [sb(f"s5_HR{i}", [128, NCk], BF16) for i in range(4)]
        HI = [sb(f"s5_HI{i}", [128, NCk], BF16) for i in range(4)]
        b_H = bufs("s5_H", 4)
        pt4 = ps("s5_pt4", [128, 4, 128], BF16)
        b_pt4 = Buf("s5_pt4")
        pK = [ps(f"s5_pK{i}", [128, 4, 128], F32) for i in range(2)]
        b_pK = bufs("s5_pK", 2)
        pS = [ps(f"s5_pS{i}", [128, CB], F32) for i in range(2)]
        b_pS = bufs("s5_pS", 2)
        pY = [ps(f"s5_pY{i}", [128, CB], F32) for i in range(2)]
        b_pY = bufs("s5_pY", 2)
        Dd = sb("s5_Dd", [128, 128], F32)
        b_Dd = Buf("s5_Dd")
        g1 = [sb(f"s5_g1{i}", [128, CB], F32) for i in range(2)]
        g2 = [sb(f"s5_g2{i}", [128, CB], F32) for i in range(2)]
        b_g = bufs("s5_g", 2)
        Yo = Uraw[:].rearrange("p (c t) -> p c t", t=8)
        b_Yo = b_Ur
        npk = 0
        npy = 0
        for tl in range(18):
            S.dma("sp", Uraw[:], scr.uT[tl * 128:(tl + 1) * 128, :], reads=[scr.b_proj], writes=[b_Ur], owner=b_Ur)
            S.op("pool", lambda e: e.tensor_copy(out=Us[:], in_=Uraw[:].rearrange("p (c s) -> p s c", s=8)), reads=[b_Ur], writes=[b_U])
            S.op("pool", lambda e: e.memset(Wi[:].rearrange("p s t c -> p (s t c)"), 0.0), writes=[b_Wi])
            S.op("dve", lambda e: e.tensor_scalar(out=Dd[:], in0=cx.identf[:], scalar1=dpad[:, tl:tl + 1], scalar2=None, op0=ALU.mult),
                 reads=[cx.b_const, bP], writes=[b_Dd])
            for pp in range(2):
                pr_ = 2 * tl + pp
                c0 = 64 * pp
                for d in range(2):
                    k = pp * 2 + d
                    dp = d * 36 + pr_
                    PrT, PiT = (PWr, PWi) if d == 0 else (PWrR, PWiR)
                    NrT, NiT = (NGr, NGi) if d == 0 else (NGrR, NGiR)
                    esl = slice(1, 9) if d == 0 else slice(0, 8)
                    pwr = PrT[:, esl, dp].unsqueeze(2).to_broadcast([128, 8, 16])
                    pwi = PiT[:, esl, dp].unsqueeze(2).to_broadcast([128, 8, 16])
                    ngr = NrT[:, esl, dp].unsqueeze(2).to_broadcast([128, 8, 16])
                    ngi = NiT[:, esl, dp].unsqueeze(2).to_broadcast([128, 8, 16])
                    crt = CRT[:, d, pr_, :].unsqueeze(1).to_broadcast([128, 8, 16])
                    cit = CIT[:, d, pr_, :].unsqueeze(1).to_broadcast([128, 8, 16])
                    bbrv = bbr[:, d, pr_, :].unsqueeze(1).to_broadcast([128, 8, 16])
                    bbiv = bbi[:, d, pr_, :].unsqueeze(1).to_broadcast([128, 8, 16])

                    def cplx(ar_, ai_, br_, bi_):
                        S.op("dve", lambda e: e.tensor_tensor(out=Rr[:], in0=ar_, in1=br_, op=ALU.mult), reads=[bP], writes=[b_R])
                        S.op("dve", lambda e: e.tensor_tensor(out=Rt[:], in0=ai_, in1=bi_, op=ALU.mult), reads=[bP, b_R], writes=[b_R])
                        S.op("dve", lambda e: e.tensor_tensor(out=Rr[:], in0=Rr[:], in1=Rt[:], op=ALU.subtract), reads=[b_R], writes=[b_R])
                        S.op("dve", lambda e: e.tensor_tensor(out=Ri[:], in0=ar_, in1=bi_, op=ALU.mult), reads=[bP, b_R], writes=[b_R])
                        S.op("dve", lambda e: e.tensor_tensor(out=Rt[:], in0=ai_, in1=br_, op=ALU.mult), reads=[bP, b_R], writes=[b_R])
                        S.op("dve", lambda e: e.tensor_tensor(out=Ri[:], in0=Ri[:], in1=Rt[:], op=ALU.add), reads=[b_R], writes=[b_R])

                    cplx(crt, cit, pwr, pwi)
                    S.op("pool", lambda e: e.memset(WOre[k][:].rearrange("p t c -> p (t c)"), 0.0), writes=[b_WO[k]])
                    S.op("pool", lambda e: e.memset(WOim[k][:].rearrange("p t c -> p (t c)"), 0.0), writes=[b_WO[k]])
                    for gl in range(2):
                        prt = slice(64 * gl, 64 * gl + 64)
                        cl = slice(c0 + 32 * gl, c0 + 32 * gl + 16)
                        S.op("act", lambda e: e.copy(out=WOre[k][prt, :, cl], in_=Rr[prt, :, :]), reads=[b_R], writes=[b_WO[k]])
                        S.op("act", lambda e: e.activation(out=WOim[k][prt, :, cl], in_=Ri[prt, :, :], func=AF.Copy, scale=-1.0), reads=[b_R], writes=[b_WO[k]])
                    cplx(ngr, ngi, bbrv, bbiv)
                    S.op("pool", lambda e: e.memset(Lre[:].rearrange("p t c -> p (t c)"), 0.0), writes=[b_L])
                    S.op("pool", lambda e: e.memset(Lim[:].rearrange("p t c -> p (t c)"), 0.0), writes=[b_L])
                    for gl in range(2):
                        prt = slice(64 * gl, 64 * gl + 64)
                        cl = slice(c0 + 32 * gl, c0 + 32 * gl + 16)
                        S.op("act", lambda e: e.copy(out=Lre[prt, :, cl], in_=Rr[prt, :, :]), reads=[b_R], writes=[b_L])
                        S.op("act", lambda e: e.copy(out=Lim[prt, :, cl], in_=Ri[prt, :, :]), reads=[b_R], writes=[b_L])
                    for (Lx, LTx) in ((Lre, LTre), (Lim, LTim)):
                        for s4 in range(2):
                            for ss in range(4):
                                S.op("pe", lambda e: e.transpose(out=pt4[:, ss, :], in_=Lx[:, s4 * 4 + ss, :], identity=cx.identb[:]),
                                     reads=[b_L, cx.b_const], writes=[b_pt4], inc=(ss == 3))
                            S.op("act", lambda e: e.copy(out=LTx[:, s4 * 4:(s4 + 1) * 4, :], in_=pt4[:]), reads=[b_pt4], writes=[b_LT])
                    for s_ in range(8):
                        for th_ in range(2):
                            j = npk % 2
                            npk += 1
                            tsl = slice(th_ * 4, th_ * 4 + 4)
                            S.op("pe", lambda e: e.matmul(pK[j][:].rearrange("p t c -> p (t c)"), lhsT=Lre[:, s_, :],
                                                          rhs=WOre[k][:, tsl, :].rearrange("p t c -> p (t c)"), start=True, stop=False),
                                 reads=[b_L, b_WO[k]], writes=[b_pK[j]], inc=False)
                            S.op("pe", lambda e: e.matmul(pK[j][:].rearrange("p t c -> p (t c)"), lhsT=Lim[:, s_, :],
                                                          rhs=WOim[k][:, tsl, :].rearrange("p t c -> p (t c)"), start=False, stop=True),
                                 reads=[b_L, b_WO[k]], writes=[b_pK[j]])
                            for tq in range(4):
                                t_ = th_ * 4 + tq
                                use = (t_ >= s_) if d == 0 else (t_ <= s_)
                                if not use:
                                    continue
                                S.op("dve", lambda e: e.tensor_tensor(out=Wi[:, s_, t_, :], in0=pK[j][:, tq, :], in1=Wi[:, s_, t_, :], op=ALU.add),
                                     reads=[b_pK[j], b_Wi], writes=[b_Wi])
                    zi = 0
                    for cb in range(NCk // CB):
                        csl = slice(cb * CB, (cb + 1) * CB)
                        for s_ in range(8):
                            S.op("pe", lambda e: e.matmul(pS[0][:], lhsT=LTre[:, s_, :], rhs=Us[:, s_, csl], start=(s_ == 0), stop=(s_ == 7)),
                                 reads=[b_LT, b_U], writes=[b_pS[0]], inc=(s_ == 7))
                        for s_ in range(8):
                            S.op("pe", lambda e: e.matmul(pS[1][:], lhsT=LTim[:, s_, :], rhs=Us[:, s_, csl], start=(s_ == 0), stop=(s_ == 7)),
                                 reads=[b_LT, b_U], writes=[b_pS[1]], inc=(s_ == 7))
                        p8r, p8i, p8n = SPr[:, 0, dp:dp + 1], SPi[:, 0, dp:dp + 1], SPn[:, 0, dp:dp + 1]
                        S.op("dve", lambda e: e.tensor_scalar(out=ztmp[:, csl], in0=pS[1][:], scalar1=p8n, scalar2=None, op0=ALU.mult),
                             reads=[b_pS[1], bP], writes=[b_ztmp])
                        S.op("dve", lambda e: e.scalar_tensor_tensor(out=ZR[0][:, csl], in0=pS[0][:], scalar=p8r, in1=ztmp[:, csl], op0=ALU.mult, op1=ALU.add),
                             reads=[b_pS[0], b_ztmp, bP], writes=[b_Z[0]])
                        S.op("dve", lambda e: e.tensor_scalar(out=ztmp[:, csl], in0=pS[1][:], scalar1=p8r, scalar2=None, op0=ALU.mult),
                             reads=[b_pS[1], bP, b_Z[0]], writes=[b_ztmp])
                        S.op("dve", lambda e: e.scalar_tensor_tensor(out=ZI[0][:, csl], in0=pS[0][:], scalar=p8i, in1=ztmp[:, csl], op0=ALU.mult, op1=ALU.add),
                             reads=[b_pS[0], b_ztmp, bP], writes=[b_Z[0]])
                    cur = 0
                    for j in range(J):
                        sh = 1 << j
                        nx = 1 - cur
                        qr, qi, qn = SPr[:, j, dp:dp + 1], SPi[:, j, dp:dp + 1], SPn[:, j, dp:dp + 1]
                        if d == 0:
                            dst, src, keep = slice(sh, NCk), slice(0, NCk - sh), slice(0, sh)
                        else:
                            dst, src, keep = slice(0, NCk - sh), slice(sh, NCk), slice(NCk - sh, NCk)
                        S.op("act", lambda e: e.copy(out=ZR[nx][:, keep], in_=ZR[cur][:, keep]), reads=[b_Z[cur]], writes=[b_Z[nx]])
                        S.op("act", lambda e: e.copy(out=ZI[nx][:, keep], in_=ZI[cur][:, keep]), reads=[b_Z[cur]], writes=[b_Z[nx]])
                        S.op("dve", lambda e: e.scalar_tensor_tensor(out=ztmp[:, dst], in0=ZR[cur][:, src], scalar=qr, in1=ZR[cur][:, dst], op0=ALU.mult, op1=ALU.add),
                             reads=[b_Z[cur], bP], writes=[b_ztmp])
                        S.op("dve", lambda e: e.scalar_tensor_tensor(out=ZR[nx][:, dst], in0=ZI[cur][:, src], scalar=qn, in1=ztmp[:, dst], op0=ALU.mult, op1=ALU.add),
                             reads=[b_Z[cur], b_ztmp, bP], writes=[b_Z[nx]])
                        S.op("dve", lambda e: e.scalar_tensor_tensor(out=ztmp[:, dst], in0=ZI[cur][:, src], scalar=qr, in1=ZI[cur][:, dst], op0=ALU.mult, op1=ALU.add),
                             reads=[b_Z[cur], bP, b_Z[nx]], writes=[b_ztmp])
                        S.op("dve", lambda e: e.scalar_tensor_tensor(out=ZI[nx][:, dst], in0=ZR[cur][:, src], scalar=qi, in1=ztmp[:, dst], op0=ALU.mult, op1=ALU.add),
                             reads=[b_Z[cur], b_ztmp, bP], writes=[b_Z[nx]])
                        cur = nx
                    if d == 0:
                        S.op("pool", lambda e: e.memset(HR[k][:, 0:1], 0.0), writes=[b_H[k]])
                        S.op("pool", lambda e: e.memset(HI[k][:, 0:1], 0.0), writes=[b_H[k]])
                        S.op("act", lambda e: e.copy(out=HR[k][:, 1:NCk], in_=ZR[cur][:, 0:NCk - 1]), reads=[b_Z[cur]], writes=[b_H[k]])
                        S.op("act", lambda e: e.copy(out=HI[k][:, 1:NCk], in_=ZI[cur][:, 0:NCk - 1]), reads=[b_Z[cur]], writes=[b_H[k]])
                    else:
                        S.op("pool", lambda e: e.memset(HR[k][:, NCk - 1:NCk], 0.0), writes=[b_H[k]])
                        S.op("pool", lambda e: e.memset(HI[k][:, NCk - 1:NCk], 0.0), writes=[b_H[k]])
                        S.op("act", lambda e: e.copy(out=HR[k][:, 0:NCk - 1], in_=ZR[cur][:, 1:NCk]), reads=[b_Z[cur]], writes=[b_H[k]])
                        S.op("act", lambda e: e.copy(out=HI[k][:, 0:NCk - 1], in_=ZI[cur][:, 1:NCk]), reads=[b_Z[cur]], writes=[b_H[k]])
            for s_ in range(8):
                S.op("dve", lambda e: e.tensor_tensor(out=Wi[:, s_, s_, :], in0=Wi[:, s_, s_, :], in1=Dd[:], op=ALU.add), reads=[b_Wi, b_Dd], writes=[b_Wi])
            for cb in range(NCk // CB):
                csl = slice(cb * CB, (cb + 1) * CB)
                for t_ in range(8):
                    j = npy % 2
                    npy += 1
                    for s_ in range(8):
                        S.op("pe", lambda e: e.matmul(pY[j][:], lhsT=Wi[:, s_, t_, :], rhs=Us[:, s_, csl], start=(s_ == 0), stop=False),
                             reads=[b_Wi, b_U], writes=[b_pY[j]], inc=False)
                    for k in range(4):
                        S.op("pe", lambda e: e.matmul(pY[j][:], lhsT=WOre[k][:, t_, :], rhs=HR[k][:, csl], start=False, stop=False),
                             reads=[b_WO[k], b_H[k]], writes=[b_pY[j]], inc=False)
                        S.op("pe", lambda e: e.matmul(pY[j][:], lhsT=WOim[k][:, t_, :], rhs=HI[k][:, csl], start=False, stop=(k == 3)),
                             reads=[b_WO[k], b_H[k]], writes=[b_pY[j]], inc=(k == 3))
                    S.op("act", lambda e: e.activation(out=g1[j][:], in_=pY[j][:], func=AF.Square), reads=[b_pY[j]], writes=[b_g[j]])
                    S.op("dve", lambda e: e.tensor_scalar(out=g1[j][:], in0=g1[j][:], scalar1=0.044715, scalar2=1.0, op0=ALU.mult, op1=ALU.add),
                         reads=[b_g[j]], writes=[b_g[j]])
                    S.op("dve", lambda e: e.tensor_tensor(out=g1[j][:], in0=pY[j][:], in1=g1[j][:], op=ALU.mult), reads=[b_pY[j], b_g[j]], writes=[b_g[j]])
                    S.op("act", lambda e: e.activation(out=g2[j][:], in_=g1[j][:], func=AF.Sigmoid, scale=1.5957691216057308), reads=[b_g[j]], writes=[b_g[j]])
                    S.op("dve", lambda e: e.tensor_tensor(out=Yo[:, csl, t_], in0=pY[j][:], in1=g2[j][:], op=ALU.mult), reads=[b_pY[j], b_g[j], b_Yo], writes=[b_Yo])
            S.dma("sp", scr.hs5T[tl * 128:(tl + 1) * 128, :], Uraw[:], reads=[b_Yo], writes=[scr.b_s5], owner=b_Yo)
        S.barrier()
```
